# Optimizing a Trainium2 kernel written in Bass

```python
import math
import jax
import jax.numpy as jnp
from jax import lax
import numpy as np

D_MODEL = 1024
BATCH = 8
SEQ = 4096
DEPTH = 4

HEAD_DIM = 64
NSA_HEADS = 8
NSA_KV_GROUPS = 2
NSA_HPG = NSA_HEADS // NSA_KV_GROUPS
CMP_STRIDE = 16
CMP_BLOCK = 2 * CMP_STRIDE
CMP_HIDDEN = 256
SEL_BLOCK = 64
SEL_TOPK = 16
SEL_LOCAL = 2
WINDOW = 512
NSA_QBLOCK = 64
DN_HEADS = 8
DN_CONV = 4
DN_CHUNK = 64
REL_BUCKETS = 32
REL_MAX_DIST = 1024
N_EXPERTS = 16
N_GROUPS = 4
EXPERTS_PER_GROUP = N_EXPERTS // N_GROUPS
TOPK_GROUPS = 1
TOPK = 2
D_EXPERT = 512
MOE_BLOCK = 256

NORM_EPS = 1e-6
FORCE_SCORE = 1e9
NEG = -1e30

NSA_Q = NSA_HEADS * HEAD_DIM
NSA_KV = NSA_KV_GROUPS * HEAD_DIM
DN_W = DN_HEADS * HEAD_DIM
IN_SIZES = (NSA_Q, 6 * NSA_KV, 3 * NSA_HEADS, 3 * DN_W, DN_HEADS, DN_HEADS, DN_W, 2 * D_MODEL)
IN_SPLITS = tuple(sum(IN_SIZES[:i + 1]) for i in range(len(IN_SIZES) - 1))
D_IN = sum(IN_SIZES)

kernel_name = 'hybrid_nsa_deltanet_grouped_moe_adaln'


def _rms(x, g):
    xf = x.astype(jnp.float32)
    y = xf * lax.rsqrt(jnp.mean(xf * xf, axis=-1, keepdims=True) + NORM_EPS)
    return (y * g.astype(jnp.float32)).astype(x.dtype)


def _l2n(x):
    xf = x.astype(jnp.float32)
    return xf * lax.rsqrt(jnp.sum(xf * xf, axis=-1, keepdims=True) + NORM_EPS)


def _masked_softmax(logits, mask):
    p = jax.nn.softmax(jnp.where(mask, logits.astype(jnp.float32), NEG), axis=-1)
    return jnp.where(mask, p, 0.0)


def _rel_bucket(dist):
    exact = REL_BUCKETS // 2
    dist = jnp.maximum(dist, 0)
    far = jnp.maximum(dist, exact).astype(jnp.float32)
    large = exact + (jnp.log(far / exact) / math.log(REL_MAX_DIST / exact) * (REL_BUCKETS - exact)).astype(jnp.int32)
    return jnp.where(dist < exact, dist, jnp.minimum(large, REL_BUCKETS - 1))


def _compress(t, pos, w1, w2):
    b, s, g, d = t.shape
    ch = t.reshape(b, s // CMP_STRIDE, CMP_STRIDE, g, d)
    blk = jnp.concatenate([ch[:, :-1], ch[:, 1:]], axis=2) + pos[:, None, :]
    flat = jnp.swapaxes(blk, 2, 3).reshape(b, s // CMP_STRIDE - 1, g, CMP_BLOCK * d)
    return jax.nn.gelu(flat @ w1) @ w2


def _nsa(q, kv, gate, rel_bias, qk_g, cmp_pos, cmp_w1, cmp_w2):
    b, s, _ = q.shape
    G, HPG, dh, QB = NSA_KV_GROUPS, NSA_HPG, HEAD_DIM, NSA_QBLOCK
    n_cmp = s // CMP_STRIDE - 1
    n_blk = s // SEL_BLOCK
    n_sel = min(SEL_TOPK, n_blk)
    n_qb = s // QB
    q = _rms(q.reshape(b, s, G, HPG, dh), qk_g[0]) * (dh ** -0.5)
    kv = kv.reshape(b, s, 6, G, dh)
    k_cmp = _rms(_compress(kv[:, :, 0], cmp_pos[0], cmp_w1[0], cmp_w2[0]), qk_g[1])
    v_cmp = _compress(kv[:, :, 1], cmp_pos[1], cmp_w1[1], cmp_w2[1])
    k_slc = _rms(kv[:, :, 2], qk_g[2]).reshape(b, n_blk, SEL_BLOCK, G, dh).transpose(0, 3, 1, 2, 4)
    v_slc = kv[:, :, 3].reshape(b, n_blk, SEL_BLOCK, G, dh).transpose(0, 3, 1, 2, 4)
    pad = ((0, 0), (WINDOW, 0), (0, 0), (0, 0))
    k_win = jnp.pad(_rms(kv[:, :, 4], qk_g[3]), pad)
    v_win = jnp.pad(kv[:, :, 5], pad)
    gate = jax.nn.sigmoid(gate).reshape(b, s, G, HPG, 3)
    bias_g = rel_bias.reshape(REL_BUCKETS, G, HPG)
    ratio, nsub = SEL_BLOCK // CMP_STRIDE, CMP_BLOCK // CMP_STRIDE
    delta = jnp.arange(n_cmp)[:, None] - ratio * jnp.arange(n_blk)[None, :]
    m_idx = delta[..., None] + jnp.arange(nsub)
    overlap = jnp.sum((m_idx >= 0) & (m_idx < ratio), axis=-1).astype(jnp.float32)
    cmp_end = jnp.arange(n_cmp) * CMP_STRIDE + CMP_BLOCK - 1
    b_ix = jnp.arange(b)[:, None, None, None]
    g_ix = jnp.arange(G)[None, None, :, None]

    def head_bias(dist):
        return rel_bias[_rel_bucket(dist)].reshape(dist.shape[0], dist.shape[1], G, HPG).transpose(0, 2, 3, 1)

    def block(args):
        qb, gb, blk = args
        t = blk * QB + jnp.arange(QB)
        dist_c = t[:, None] - cmp_end[None, :]
        s_c = jnp.einsum('bqghd,bngd->bqghn', qb, k_cmp) + head_bias(dist_c)
        p_c = _masked_softmax(s_c, (dist_c >= 0)[:, None, None, :])
        o_c = jnp.einsum('bqghn,bngd->bqghd', p_c.astype(v_cmp.dtype), v_cmp)
        imp = jnp.einsum('bqghn,nj->bqgj', p_c, overlap)
        cur = t // SEL_BLOCK
        jb = jnp.arange(n_blk)[None, :]
        valid = (jb <= cur[:, None])[:, None, :]
        forced = valid & ((jb == 0) | (jb > cur[:, None] - SEL_LOCAL))[:, None, :]
        score = jnp.where(forced, FORCE_SCORE, jnp.where(valid, imp, -1.0))
        _, idx = lax.top_k(score, n_sel)
        kb = k_slc[b_ix, g_ix, idx]
        vb = v_slc[b_ix, g_ix, idx]
        pos = idx[..., None] * SEL_BLOCK + jnp.arange(SEL_BLOCK)
        dist_s = t[None, :, None, None, None] - pos
        bias_s = jnp.moveaxis(bias_g[_rel_bucket(dist_s), g_ix[..., None]], -1, 3)
        s_s = jnp.einsum('bqghd,bqgnkd->bqghnk', qb, kb) + bias_s
        mask_s = (dist_s >= 0).reshape(b, QB, G, 1, n_sel * SEL_BLOCK)
        p_s = _masked_softmax(s_s.reshape(b, QB, G, HPG, n_sel * SEL_BLOCK), mask_s)
        o_s = jnp.einsum('bqghm,bqgmd->bqghd', p_s.astype(vb.dtype), vb.reshape(b, QB, G, n_sel * SEL_BLOCK, dh))
        kw = lax.dynamic_slice_in_dim(k_win, blk * QB, WINDOW + QB, axis=1)
        vw = lax.dynamic_slice_in_dim(v_win, blk * QB, WINDOW + QB, axis=1)
        kpos = blk * QB - WINDOW + jnp.arange(WINDOW + QB)
        dist_w = t[:, None] - kpos[None, :]
        mask_w = (dist_w >= 0) & (dist_w < WINDOW) & (kpos[None, :] >= 0)
        s_w = jnp.einsum('bqghd,bkgd->bqghk', qb, kw) + head_bias(dist_w)
        p_w = _masked_softmax(s_w, mask_w[:, None, None, :])
        o_w = jnp.einsum('bqghk,bkgd->bqghd', p_w.astype(vw.dtype), vw)
        return gb[..., 0:1] * o_c + gb[..., 1:2] * o_s + gb[..., 2:3] * o_w

    q_blocks = jnp.moveaxis(q.reshape(b, n_qb, QB, G, HPG, dh), 1, 0)
    g_blocks = jnp.moveaxis(gate.reshape(b, n_qb, QB, G, HPG, 3), 1, 0)
    o = lax.map(block, (q_blocks, g_blocks, jnp.arange(n_qb)))
    return jnp.moveaxis(o, 0, 1).reshape(b, s, NSA_Q)


def _gated_deltanet(qkv, beta_raw, a_raw, z, conv_w, a_log, dt_bias, norm_g):
    b, s, ch = qkv.shape
    H, d, C = DN_HEADS, HEAD_DIM, DN_CHUNK
    n_ch = s // C
    qkv = jax.nn.silu(lax.conv_general_dilated(
        qkv, conv_w.reshape(DN_CONV, 1, ch), window_strides=(1,), padding=[(DN_CONV - 1, 0)],
        dimension_numbers=('NWC', 'WIO', 'NWC'), feature_group_count=ch))
    qkv = qkv.reshape(b, s, 3, H, d)
    q = _l2n(qkv[:, :, 0]) * (d ** -0.5)
    k = _l2n(qkv[:, :, 1])
    v = qkv[:, :, 2].astype(jnp.float32)
    beta = jax.nn.sigmoid(beta_raw.astype(jnp.float32))
    g = -jnp.exp(a_log.astype(jnp.float32)) * jax.nn.softplus(a_raw.astype(jnp.float32) + dt_bias.astype(jnp.float32))

    def chunks(t):
        return jnp.moveaxis(t.reshape(b, n_ch, C, H, *t.shape[3:]), 3, 1)

    q, k, v, beta, g = chunks(q), chunks(k), chunks(v), chunks(beta), chunks(g)
    g_cum = jnp.cumsum(g, axis=-1)
    causal = jnp.tril(jnp.ones((C, C), dtype=bool))
    strict = jnp.tril(jnp.ones((C, C), dtype=bool), -1)
    decay = jnp.where(causal, jnp.exp(jnp.where(causal, g_cum[..., :, None] - g_cum[..., None, :], 0.0)), 0.0)
    k_beta = k * beta[..., None]
    lower = jnp.where(strict, jnp.einsum('bhnid,bhnjd->bhnij', k_beta, k) * decay, 0.0)
    eye = jnp.eye(C, dtype=jnp.float32)
    t_inv = lax.linalg.triangular_solve(lower + eye, jnp.broadcast_to(eye, lower.shape),
                                        left_side=True, lower=True, unit_diagonal=True)
    u = t_inv @ (v * beta[..., None])
    w = t_inv @ (k_beta * jnp.exp(g_cum)[..., None])
    attn = jnp.where(causal, jnp.einsum('bhnid,bhnjd->bhnij', q, k) * decay, 0.0)
    g_last = g_cum[..., -1:]
    q_dec = q * jnp.exp(g_cum)[..., None]
    k_dec = k * jnp.exp(g_last - g_cum)[..., None]

    def step(state, xs):
        qd, kd, uc, wc, ac, gl = xs
        v_new = uc - wc @ state
        out = qd @ state + ac @ v_new
        state = state * jnp.exp(gl)[..., None] + jnp.swapaxes(kd, -1, -2) @ v_new
        return state, out

    xs = tuple(jnp.moveaxis(t, 2, 0) for t in (q_dec, k_dec, u, w, attn, g_last))
    _, o = lax.scan(step, jnp.zeros((b, H, d, d), jnp.float32), xs)
    o = jnp.moveaxis(o, 0, 2).reshape(b, H, s, d).transpose(0, 2, 1, 3)
    o = _rms(o, norm_g) * jax.nn.silu(z.astype(jnp.float32)).reshape(b, s, H, d)
    return o.reshape(b, s, DN_W).astype(z.dtype)


def _mixer(h, w_in, rel_bias, qk_g, cmp_pos, cmp_w1, cmp_w2, conv_w, a_log, dt_bias, dn_norm_g,
           w_br_a, w_br_b, w_out):
    b, s, _ = h.shape
    q_a, kv_a, gate_a, qkv_b, beta_b, a_b, z_b, merge = jnp.split(h @ w_in, IN_SPLITS, axis=-1)
    y_a = _nsa(q_a, kv_a, gate_a, rel_bias, qk_g, cmp_pos, cmp_w1, cmp_w2)
    y_b = _gated_deltanet(qkv_b, beta_b, a_b, z_b, conv_w, a_log, dt_bias, dn_norm_g)
    merge = jax.nn.sigmoid(merge).reshape(b, s, 2, D_MODEL)
    y = merge[:, :, 0] * (y_a @ w_br_a) + merge[:, :, 1] * (y_b @ w_br_b)
    return y @ w_out


def _moe(h, router_w, router_b, w_gate, w_up, w_down):
    b, s, D = h.shape
    T = b * s
    xt = h.reshape(T, D)
    scores = jax.nn.sigmoid((xt @ router_w).astype(jnp.float32))
    sel = scores + router_b.astype(jnp.float32)
    grp_score = lax.top_k(sel.reshape(T, N_GROUPS, EXPERTS_PER_GROUP), 2)[0].sum(-1)
    _, top_grp = lax.top_k(grp_score, TOPK_GROUPS)
    grp_mask = jnp.any(top_grp[..., None] == jnp.arange(N_GROUPS), axis=1)
    masked = jnp.where(jnp.repeat(grp_mask, EXPERTS_PER_GROUP, axis=1), sel, -jnp.inf)
    _, e_idx = lax.top_k(masked, TOPK)
    wts = jnp.take_along_axis(scores, e_idx, axis=1)
    wts = wts / jnp.sum(wts, axis=-1, keepdims=True)
    A = T * TOPK
    e_flat = e_idx.reshape(A)
    order = jnp.argsort(e_flat)
    e_s = e_flat[order]
    tok_s = jnp.repeat(jnp.arange(T, dtype=jnp.int32), TOPK)[order]
    w_s = wts.reshape(A)[order]
    counts = jnp.zeros((N_EXPERTS,), jnp.int32).at[e_flat].add(1)
    padded = (counts + MOE_BLOCK - 1) // MOE_BLOCK * MOE_BLOCK
    p_end = jnp.cumsum(padded)
    dest = (p_end - padded)[e_s] + jnp.arange(A) - (jnp.cumsum(counts) - counts)[e_s]
    P = A + N_EXPERTS * MOE_BLOCK
    NB = P // MOE_BLOCK
    buf_tok = jnp.full((P,), T, jnp.int32).at[dest].set(tok_s)
    buf_w = jnp.zeros((P,), h.dtype).at[dest].set(w_s.astype(h.dtype))
    blk_e = jnp.minimum(jnp.searchsorted(p_end, jnp.arange(NB) * MOE_BLOCK, side='right'), N_EXPERTS - 1)
    xb = jnp.concatenate([xt, jnp.zeros((1, D), h.dtype)], axis=0)[buf_tok].reshape(NB, MOE_BLOCK, D)

    def expert_block(args):
        xblk, e = args
        return (jax.nn.silu(xblk @ w_gate[e]) * (xblk @ w_up[e])) @ w_down[e]

    yb = lax.map(expert_block, (xb, blk_e)).reshape(P, D) * buf_w[:, None]
    out = jnp.zeros((T + 1, D), h.dtype).at[buf_tok].add(yb)[:T]
    return out.reshape(b, s, D)


def setup_inputs(seed: int = 0) -> dict:
    key = jax.random.key(seed)
    ks = jax.random.split(key, 24)
    L, D, E, F = DEPTH, D_MODEL, N_EXPERTS, D_EXPERT

    def nrm(k, shape, scale):
        return jax.random.normal(k, shape, jnp.float32) * scale

    dt = jnp.exp(jax.random.uniform(ks[16], (L, DN_HEADS), jnp.float32, math.log(1e-3), math.log(1e-1)))
    return {
        'x': nrm(ks[0], (BATCH, SEQ, D), 1.0),
        'c': nrm(ks[1], (BATCH, D), 1.0),
        'rel_bias': nrm(ks[2], (REL_BUCKETS, NSA_HEADS), 0.2),
        'router_w': nrm(ks[3], (D, E), D ** -0.5),
        'router_b': nrm(ks[4], (E,), 0.01),
        'ada_w': nrm(ks[5], (L, D, 6 * D), 0.2 * D ** -0.5),
        'ada_b': nrm(ks[6], (L, 6 * D), 0.02),
        'norm1_g': 1.0 + nrm(ks[7], (L, D), 0.02),
        'norm2_g': 1.0 + nrm(ks[8], (L, D), 0.02),
        'w_in': nrm(ks[9], (L, D, D_IN), D ** -0.5),
        'qk_norm_g': 1.0 + nrm(ks[10], (L, 4, HEAD_DIM), 0.02),
        'cmp_pos': nrm(ks[11], (L, 2, CMP_BLOCK, HEAD_DIM), 0.1),
        'cmp_w1': nrm(ks[12], (L, 2, CMP_BLOCK * HEAD_DIM, CMP_HIDDEN), (CMP_BLOCK * HEAD_DIM) ** -0.5),
        'cmp_w2': nrm(ks[13], (L, 2, CMP_HIDDEN, HEAD_DIM), CMP_HIDDEN ** -0.5),
        'dn_conv_w': nrm(ks[14], (L, DN_CONV, 3 * DN_W), DN_CONV ** -0.5),
        'dn_a_log': jnp.log(jax.random.uniform(ks[15], (L, DN_HEADS), jnp.float32, 1.0, 16.0)),
        'dn_dt_bias': dt + jnp.log(-jnp.expm1(-dt)),
        'dn_norm_g': 1.0 + nrm(ks[17], (L, HEAD_DIM), 0.02),
        'w_branch_a': nrm(ks[18], (L, NSA_Q, D), NSA_Q ** -0.5),
        'w_branch_b': nrm(ks[19], (L, DN_W, D), DN_W ** -0.5),
        'w_out': nrm(ks[20], (L, D, D), D ** -0.5),
        'moe_w_gate': nrm(ks[21], (L, E, D, F), D ** -0.5),
        'moe_w_up': nrm(ks[22], (L, E, D, F), D ** -0.5),
        'moe_w_down': nrm(ks[23], (L, E, F, D), F ** -0.5),
    }


def reference(x, c, rel_bias, router_w, router_b, ada_w, ada_b, norm1_g, norm2_g, w_in, qk_norm_g,
              cmp_pos, cmp_w1, cmp_w2, dn_conv_w, dn_a_log, dn_dt_bias, dn_norm_g, w_branch_a,
              w_branch_b, w_out, moe_w_gate, moe_w_up, moe_w_down):
    c_act = jax.nn.silu(c)
    for l in range(DEPTH):
        mod = (c_act @ ada_w[l] + ada_b[l])[:, None, :]
        sh1, sc1, g1, sh2, sc2, g2 = jnp.split(mod, 6, axis=-1)
        h = _rms(x, norm1_g[l]) * (1.0 + sc1) + sh1
        x = x + g1 * _mixer(h, w_in[l], rel_bias, qk_norm_g[l], cmp_pos[l], cmp_w1[l], cmp_w2[l],
                            dn_conv_w[l], dn_a_log[l], dn_dt_bias[l], dn_norm_g[l],
                            w_branch_a[l], w_branch_b[l], w_out[l])
        h = _rms(x, norm2_g[l]) * (1.0 + sc2) + sh2
        x = x + g2 * _moe(h, router_w, router_b, moe_w_gate[l], moe_w_up[l], moe_w_down[l])
    return x
```

```python
import math
from contextlib import ExitStack
import numpy as np
import concourse.bass as bass
import concourse.mybir as mybir
from concourse.bass_utils import run_bass_kernel_spmd

F32 = mybir.dt.float32
AF = mybir.ActivationFunctionType
ALU = mybir.AluOpType
AX = mybir.AxisListType

D = 1024
HD = 64
NH = 8
DIN = 5416
NEGM = -30000.0
EPS = 1e-6
U0 = 384
OFFMAX = 1024
WGEN = U0 + OFFMAX + 512
WWIN = U0 + 512 + 512
NEGPAD = 1024


class Sched:
    def __init__(self, nc, es, ndma=14):
        self.nc = nc
        self.eng = {'pe': nc.tensor, 'act': nc.scalar, 'dve': nc.vector, 'pool': nc.gpsimd, 'sp': nc.sync}
        self.sem = {k: es.enter_context(nc.semaphore('s_' + k)) for k in self.eng}
        self.cnt = {k: 0 for k in self.eng}
        self.dsem = [es.enter_context(nc.semaphore('d%d' % i)) for i in range(ndma)]
        self.dcnt = [0] * ndma
        self.dnext = 0
        self.seen = {k: {} for k in self.eng}
        self.res = {}
        self.nops = 0

    def _deps(self, r, w):
        deps = {}

        def add(t):
            if t is not None and deps.get(t[0], 0) < t[1]:
                deps[t[0]] = t[1]
        for k in r:
            st = self.res.get(k)
            if st:
                add(st[0])
        for k in w:
            st = self.res.get(k)
            if st:
                add(st[0])
                for s, v in st[1].items():
                    add((s, v))
        return deps

    def _wait(self, e, deps):
        for s, v in deps.items():
            if s == 'pe' and e == 'pe':
                continue
            if self.seen[e].get(s, 0) < v:
                sem = self.sem[s] if isinstance(s, str) else self.dsem[s]
                self.eng[e].wait_ge(sem, v)
                self.seen[e][s] = v

    def _mark(self, tag, r, w):
        for k in r:
            st = self.res.setdefault(k, [None, {}])
            if st[1].get(tag[0], 0) < tag[1]:
                st[1][tag[0]] = tag[1]
        for k in w:
            self.res[k] = [tag, {}]

    def op(self, e, emit, r=(), w=(), inc=True):
        self._wait(e, self._deps(r, w))
        inst = emit(self.eng[e])
        self.nops += 1
        if inc:
            self.cnt[e] += 1
            inst.then_inc(self.sem[e], 1)
            tag = (e, self.cnt[e])
        else:
            tag = (e, self.cnt[e] + 1)
        self._mark(tag, r, w)

    def dma(self, out, in_, r=(), w=(), q='sp'):
        i = self.dnext
        self.dnext = (i + 1) % len(self.dsem)
        deps = self._deps(r, w)
        if self.dcnt[i]:
            deps[i] = max(deps.get(i, 0), self.dcnt[i])
        self._wait(q, deps)
        self.dcnt[i] += 16
        self.eng[q].dma_start(out=out, in_=in_).then_inc(self.dsem[i], 16)
        self.nops += 1
        self._mark((i, self.dcnt[i]), r, w)

    def barrier(self):
        deps = {s: c for s, c in self.cnt.items() if c}
        for i, v in enumerate(self.dcnt):
            if v:
                deps[i] = v
        for e in self.eng:
            d = dict(deps)
            self._wait(e, d)
        self.res = {}


class Ring:
    def __init__(self, tiles, name):
        self.tiles = tiles
        self.name = name
        self.i = 0

    def next(self):
        k = self.i % len(self.tiles)
        self.i += 1
        return self.tiles[k], (self.name, k)


def rel_bucket_np(dist):
    exact = 16
    dist = np.maximum(dist, 0)
    far = np.maximum(dist, exact).astype(np.float32)
    large = exact + (np.log(far / np.float32(exact)) / np.float32(math.log(1024 / exact)) * np.float32(32 - exact)).astype(np.int32)
    return np.where(dist < exact, dist, np.minimum(large, 31))


def build_program(SEQ, DEPTH, dbg=()):
    NT = SEQ // 128
    QT = SEQ // 512
    NCH = SEQ // 64
    NCMP = SEQ // 16 - 1
    NBLK = SEQ // 64
    NCT = (NCMP + 127) // 128
    FDW = NEGPAD + SEQ
    JB = NBLK
    nc = bass.Bass("TRN2", target_bir_lowering=False)

    def din(name, shape):
        return nc.dram_tensor(name, list(shape), F32, kind="ExternalInput").ap()

    def dscr(name, shape, kind="Internal"):
        if name in dbg:
            kind = "ExternalOutput"
        return nc.dram_tensor(name, list(shape), F32, kind=kind).ap()

    L = DEPTH
    x_in = din("x", (SEQ, D))
    cT_in = din("cT", (128, 8))
    fdg_in = din("fdg", (NH, FDW))
    fdw_in = din("fdw", (NH, FDW))
    router_w = din("router_w", (D, 16))
    router_b = din("router_b", (16,))
    ada_w = din("ada_w", (L, D, 6 * D))
    ada_b = din("ada_b", (L, 6 * D))
    norm1_g = din("norm1_g", (L, D))
    norm2_g = din("norm2_g", (L, D))
    w_in = din("w_in", (L, D, DIN))
    qk_norm_g = din("qk_norm_g", (L, 4, HD))
    cmp_pos = din("cmp_pos", (L, 2, 32, HD))
    cmp_w1 = din("cmp_w1", (L, 2, 2048, 256))
    cmp_w2 = din("cmp_w2", (L, 2, 256, HD))
    dn_conv_w = din("dn_conv_w", (L, 4, 1536))
    dn_a_log = din("dn_a_log", (L, 8))
    dn_dt_bias = din("dn_dt_bias", (L, 8))
    dn_norm_g = din("dn_norm_g", (L, HD))
    w_br_a = din("w_branch_a", (L, 512, D))
    w_br_b = din("w_branch_b", (L, 512, D))
    w_out = din("w_out", (L, D, D))
    moe_wg = din("moe_w_gate", (L, 16, D, 512))
    moe_wu = din("moe_w_up", (L, 16, D, 512))
    moe_wd = din("moe_w_down", (L, 16, 512, D))
    out = nc.dram_tensor("out", [SEQ, D], F32, kind="ExternalOutput").ap()

    qT_d = dscr("qT_d", (4, 128, SEQ))
    kcT_d = dscr("kcT_d", (2, 128, SEQ))
    kslcT_d = dscr("kslcT_d", (128, SEQ))
    kwinT_d = dscr("kwinT_d", (128, SEQ))
    vslc_d = dscr("vslc_d", (SEQ, 128))
    vwin_d = dscr("vwin_d", (SEQ, 128))
    gate_d = dscr("gate_d", (SEQ, 24))
    dnraw_d = dscr("dnraw_d", (12, 128, SEQ))
    dnc_d = dscr("dnc_d", (12, 128, SEQ))
    bg_d = dscr("bg_d", (SEQ, 16))
    zs_d = dscr("zs_d", (SEQ, 512))
    mergeT_d = dscr("mergeT_d", (16, 128, SEQ))
    obr_d = dscr("obr_d", (3, SEQ, 512))
    ybT_d = dscr("ybT_d", (4, 128, SEQ))
    bct_d = dscr("bct_d", (NH, NCT * 128, SEQ))
    bgen_d = dscr("bgen_d", (128, NH, WGEN))
    bwin_d = dscr("bwin_d", (128, NH, WWIN))
    selbT_d = dscr("selbT_d", (128, SEQ))
    mod_d = dscr("mod_d", (L, 6 * D))

    es = ExitStack()
    with es:
        S = Sched(nc, es)

        def sb(name, shape):
            return es.enter_context(nc.sbuf_tensor(name, list(shape), F32))

        PS = [es.enter_context(nc.psum_tensor("ps%d" % i, [128, 512], F32)) for i in range(8)]
        psr = Ring(PS, "ps")

        def tt(e, o, a, b, op, r, w):
            S.op(e, lambda g: g.tensor_tensor(out=o, in0=a, in1=b, op=op), r, w)

        def ts(e, o, a, s1, op0, r, w, s2=None, op1=None):
            if op1 is None:
                S.op(e, lambda g: g.tensor_scalar(out=o, in0=a, scalar1=s1, scalar2=None, op0=op0), r, w)
            else:
                S.op(e, lambda g: g.tensor_scalar(out=o, in0=a, scalar1=s1, scalar2=s2, op0=op0, op1=op1), r, w)

        def stt(o, a, sc, b, op0, op1, r, w):
            S.op('dve', lambda g: g.scalar_tensor_tensor(out=o, in0=a, scalar=sc, in1=b, op0=op0, op1=op1), r, w)

        def act(o, a, f, r, w, bias=None, scale=1.0, accum=None):
            kw = {}
            if bias is not None:
                kw['bias'] = bias
            if accum is not None:
                kw['accum_out'] = accum
            S.op('act', lambda g: g.activation(out=o, in_=a, func=f, scale=scale, **kw), r, w)

        def mm(o, lT, rh, st, sp, r, w, inc=True):
            S.op('pe', lambda g: g.matmul(o, lT, rh, start=st, stop=sp), r, w, inc=inc)

        def tr(o, a, idn, r, w, inc=True):
            S.op('pe', lambda g: g.transpose(o, a, idn), r, w, inc=inc)

        def cp(e, o, a, r, w):
            S.op(e, lambda g: g.tensor_copy(o, a), r, w)

        def recip(o, a, r, w):
            S.op('dve', lambda g: g.reciprocal(o, a), r, w)

        def memset(e, o, v, w):
            S.op(e, lambda g: g.memset(o, v), (), w)

        def asel(o, a, pattern, base, cm, fill, r, w, op=ALU.is_ge):
            S.op('pool', lambda g: g.affine_select(out=o, in_=a, pattern=pattern, compare_op=op, fill=fill,
                                                   base=base, channel_multiplier=cm), r, w)

        ident = sb("ident", (128, 128))
        ones = sb("ones", (128, 128))
        bdones = sb("bdones", (128, 128))
        UT = sb("UT", (128, 64))
        maskU = sb("maskU", (64, 64))
        maskL = sb("maskL", (64, 64))
        nsU = sb("nsU", (64, 64))
        nsL = sb("nsL", (64, 64))
        ovl = sb("ovl", (128, NCT, JB))
        memset('pool', ident[:], 0.0, ['ident'])
        asel(ident[:], ident[:], [[-1, 128]], 0, 1, 1.0, ['ident'], ['ident'], op=ALU.not_equal)
        memset('pool', ones[:], 1.0, ['ones'])
        memset('pool', bdones[:], 0.0, ['bdones'])
        memset('pool', bdones[0:64, 0:64], 1.0, ['bdones'])
        memset('pool', bdones[64:128, 64:128], 1.0, ['bdones'])
        for h0 in (0, 64):
            memset('pool', UT[h0:h0 + 64, :], 1.0, ['UT'])
            asel(UT[h0:h0 + 64, :], UT[h0:h0 + 64, :], [[1, 64]], 0, -1, 0.0, ['UT'], ['UT'])
        memset('pool', maskU[:], 0.0, ['maskU'])
        asel(maskU[:], maskU[:], [[1, 64]], 0, -1, NEGM, ['maskU'], ['maskU'])
        memset('pool', maskL[:], 0.0, ['maskL'])
        asel(maskL[:], maskL[:], [[-1, 64]], 0, 1, NEGM, ['maskL'], ['maskL'])
        memset('pool', nsU[:], -1.0, ['nsU'])
        asel(nsU[:], nsU[:], [[1, 64]], -1, -1, 0.0, ['nsU'], ['nsU'])
        memset('pool', nsL[:], -1.0, ['nsL'])
        asel(nsL[:], nsL[:], [[-1, 64]], -1, 1, 0.0, ['nsL'], ['nsL'])
        ovt = sb("ovt", (128, NCT, JB))
        memset('pool', ovl[:], 0.0, ['ovl'])
        for m in (0, 1):
            memset('pool', ovt[:], 1.0, ['ovt'])
            asel(ovt[:], ovt[:], [[128, NCT], [-4, JB]], m, 1, 0.0, ['ovt'], ['ovt'])
            asel(ovt[:], ovt[:], [[-128, NCT], [4, JB]], 3 - m, -1, 0.0, ['ovt'], ['ovt'])
            tt('pool', ovl[:], ovl[:], ovt[:], ALU.add, ['ovl', 'ovt'], ['ovl'])

        with nc.allow_non_contiguous_dma(reason="table build"):
            for p in range(128):
                o0 = NEGPAD - U0 - p
                S.dma(bgen_d[p, :, :], fdg_in[:, o0:o0 + WGEN], (), [('bgen', p)], q='sp')
                S.dma(bwin_d[p, :, :], fdw_in[:, o0:o0 + WWIN], (), [('bwin', p)], q='pool')
            for n in range(NCT * 128):
                o0 = NEGPAD - (16 * n + 31)
                if n >= NCMP:
                    o0 = 0
                q = 'sp' if n % 2 == 0 else 'pool'
                if n >= NCMP:
                    S.dma(bct_d[:, n, 0:NEGPAD], fdg_in[:, 0:NEGPAD], (), [('bct', n)], q=q)
                    for c0 in range(NEGPAD, SEQ, NEGPAD):
                        S.dma(bct_d[:, n, c0:c0 + NEGPAD], fdg_in[:, 0:NEGPAD], (), [('bct', n, c0)], q=q)
                elif o0 >= 0:
                    S.dma(bct_d[:, n, :], fdg_in[:, o0:o0 + SEQ], (), [('bct', n)], q=q)
                else:
                    nn = -o0
                    for c0 in range(0, nn, NEGPAD):
                        cw = min(NEGPAD, nn - c0)
                        S.dma(bct_d[:, n, c0:c0 + cw], fdg_in[:, 0:cw], (), [('bct', n, c0)], q=q)
                    S.dma(bct_d[:, n, nn:SEQ], fdg_in[:, 0:SEQ - nn], (), [('bct', n)], q=q)
        S.barrier()

        cact = sb("cact", (128, 8))
        S.dma(cact[:], cT_in[:, :], (), ['cact'])
        act(cact[:], cact[:], AF.Silu, ['cact'], ['cact'])
        cbc = sb("cbc", (128, 8, 128))
        for kc in range(8):
            ts('dve', cbc[:, kc, :], ones[:], cact[:, kc:kc + 1], ALU.mult, ['ones', 'cact'], ['cbc'])
        rtrb = sb("rtrb", (128, 16))
        S.dma(rtrb[:], router_b.partition_broadcast(128), (), ['rtrb'])
        rtrw = sb("rtrw", (128, 8, 16))
        with nc.allow_non_contiguous_dma(reason="router w"):
            S.dma(rtrw[:], router_w.rearrange("(kc p) e -> p kc e", p=128), (), ['rtrw'])

        def load_w(ring, src2d, c0, ncols, kch=8):
            t, k = ring.next()
            with nc.allow_non_contiguous_dma(reason="weight slab"):
                S.dma(t[:, 0:kch, 0:ncols], src2d.rearrange("(kc p) n -> p kc n", p=128)[:, :, c0:c0 + ncols], (), [k])
            return t, k

        def norm_tile(src, t0, Arow, shrow, xr, hr, sm):
            xt, xk = xr.next()
            S.dma(xt[:], src[t0:t0 + 128, :], [('x', t0 // 128)], [xk])
            ht, hk = hr.next()
            s, sk = sm.next()
            act(ht[:], xt[:], AF.Square, [xk], [hk, sk], accum=s[:, 0:1])
            act(s[:, 1:2], s[:, 0:1], AF.Sqrt, [sk], [sk], bias=EPS, scale=1.0 / D)
            recip(s[:, 2:3], s[:, 1:2], [sk], [sk])
            stt(ht[:], xt[:], s[:, 2:3], Arow, ALU.mult, ALU.mult, [xk, sk, 'modrow'], [hk])
            tt('pool', ht[:], ht[:], shrow, ALU.add, [hk, 'modrow'], [hk])
            return ht, hk, xt, xk

        def transpose_to(ht, hk, hT, hTk, sub, nkc=8):
            for k0 in range(0, nkc, 4):
                ps, pk = psr.next()
                for kc in range(k0, k0 + 4):
                    tr(ps[:, (kc - k0) * 128:(kc - k0 + 1) * 128], ht[:, kc * 128:(kc + 1) * 128], ident[:],
                       [hk, 'ident'], [pk], inc=(kc == k0 + 3))
                e = 'act' if (k0 // 4) % 2 == 0 else 'dve'
                dst = hT[:, k0:k0 + 4, sub * 128:(sub + 1) * 128]
                srcv = ps[:].rearrange("p (a b) -> p a b", a=4)
                if e == 'act':
                    act(dst, srcv, AF.Copy, [pk], [hTk])
                else:
                    cp('dve', dst, srcv, [pk], [hTk])

        def phase_A(l, src_x, A1row, sh1row):
            with ExitStack() as ph:
                def psb(name, shape):
                    return ph.enter_context(nc.sbuf_tensor("%s_A%d" % (name, l), list(shape), F32))
                wr = Ring([psb("wsl%d" % i, (128, 8, 512)) for i in range(3)], "wsl")
                xr = Ring([psb("xa%d" % i, (128, D)) for i in range(2)], "xa")
                hr = Ring([psb("ha%d" % i, (128, D)) for i in range(2)], "ha")
                hTr = Ring([psb("hT%d" % i, (128, 8, 512)) for i in range(2)], "hT")
                st = Ring([psb("st%d" % i, (128, 512)) for i in range(4)], "st")
                sq = Ring([psb("sq%d" % i, (128, 512)) for i in range(2)], "sq")
                sm = Ring([psb("sm%d" % i, (128, 8)) for i in range(4)], "sm")
                gains = psb("gains", (128, 4))
                dtb = psb("dtb", (128, 8))
                nea = psb("nea", (128, 8))
                with nc.allow_non_contiguous_dma(reason="small"):
                    for h0 in (0, 64):
                        S.dma(gains[h0:h0 + 64, :], qk_norm_g[l].rearrange("i d -> d i"), (), ['gains'])
                ts('dve', gains[:, 0:1], gains[:, 0:1], HD ** -0.5, ALU.mult, ['gains'], ['gains'])
                S.dma(dtb[:], dn_dt_bias[l].partition_broadcast(128), (), ['dtb'])
                S.dma(nea[:], dn_a_log[l].partition_broadcast(128), (), ['nea'])
                act(nea[:], nea[:], AF.Exp, ['nea'], ['nea'])
                ts('dve', nea[:], nea[:], -1.0, ALU.mult, ['nea'], ['nea'])

                def rms64_store(ps, pk, gcol, dst, dkey):
                    q1, qk1 = sq.next()
                    act(q1[:], ps[:], AF.Square, [pk], [qk1])
                    p2, pk2 = psr.next()
                    mm(p2[:], bdones[:], q1[:], True, True, ['bdones', qk1], [pk2])
                    act(q1[:], p2[:], AF.Sqrt, [pk2], [qk1], bias=EPS, scale=1.0 / 64)
                    recip(q1[:], q1[:], [qk1], [qk1])
                    o, ok = st.next()
                    stt(o[:], ps[:], gcol, q1[:], ALU.mult, ALU.mult, [pk, qk1, 'gains'], [ok])
                    S.dma(dst, o[:], [ok], [dkey], q='pool')

                def fm_store(ps, pk, dst, dkey, func=AF.Copy):
                    o, ok = st.next()
                    act(o[:], ps[:], func, [pk], [ok])
                    S.dma(dst, o[:], [ok], [dkey], q='pool')

                for j in range(QT):
                    t0 = 512 * j
                    hT, hTk = hTr.next()
                    for sub in range(4):
                        ht, hk, _, _ = norm_tile(src_x, t0 + 128 * sub, A1row, sh1row, xr, hr, sm)
                        transpose_to(ht, hk, hT, hTk, sub)

                    def fm(wt, wk, lsel):
                        ps, pk = psr.next()
                        for kc in range(8):
                            mm(ps[:], lsel(kc), hT[:, kc, :], kc == 0, kc == 7, [wk, hTk], [pk], inc=(kc == 7))
                        return ps, pk

                    def tm(wt, wk, c0, ncols, sub):
                        ps, pk = psr.next()
                        for kc in range(8):
                            mm(ps[:, 0:ncols], hT[:, kc, sub * 128:(sub + 1) * 128], wt[:, kc, c0:c0 + ncols],
                               kc == 0, kc == 7, [wk, hTk], [pk], inc=(kc == 7))
                        return ps, pk
                    cs = slice(t0, t0 + 512)
                    wt, wk = wr.next()
                    with nc.allow_non_contiguous_dma(reason="q slab"):
                        for a in range(2):
                            for c in range(4):
                                S.dma(wt[:, :, c * 128 + a * 64:c * 128 + a * 64 + 64],
                                      w_in[l].rearrange("(kc p) n -> p kc n", p=128)[:, :, a * 256 + c * 64:a * 256 + c * 64 + 64], (), [wk])
                    for c in range(4):
                        ps, pk = fm(wt, wk, lambda kc, c=c: wt[:, kc, c * 128:(c + 1) * 128])
                        rms64_store(ps, pk, gains[:, 0:1], qT_d[c, :, cs], ('qT', c, j))
                    wt, wk = load_w(wr, w_in[l], 512, 512)
                    for c in range(3):
                        ps, pk = fm(wt, wk, lambda kc, c=c: wt[:, kc, c * 128:(c + 1) * 128])
                        if c < 2:
                            fm_store(ps, pk, kcT_d[c, :, cs], ('kcT', c, j))
                        else:
                            rms64_store(ps, pk, gains[:, 2:3], kslcT_d[:, cs], ('kslcT', j))
                    for sub in range(4):
                        ps, pk = tm(wt, wk, 384, 128, sub)
                        o, ok = st.next()
                        act(o[:, 0:128], ps[:, 0:128], AF.Copy, [pk], [ok])
                        S.dma(vslc_d[t0 + 128 * sub:t0 + 128 * sub + 128, :], o[:, 0:128], [ok], [('vslc', j, sub)], q='pool')
                    wt, wk = load_w(wr, w_in[l], 1024, 280)
                    ps, pk = fm(wt, wk, lambda kc: wt[:, kc, 0:128])
                    rms64_store(ps, pk, gains[:, 3:4], kwinT_d[:, cs], ('kwinT', j))
                    for sub in range(4):
                        r0 = t0 + 128 * sub
                        ps, pk = tm(wt, wk, 128, 152, sub)
                        o, ok = st.next()
                        act(o[:, 0:128], ps[:, 0:128], AF.Copy, [pk], [ok])
                        act(o[:, 128:152], ps[:, 128:152], AF.Sigmoid, [pk], [ok])
                        S.dma(vwin_d[r0:r0 + 128, :], o[:, 0:128], [ok], [('vwin', j, sub)], q='pool')
                        S.dma(gate_d[r0:r0 + 128, :], o[:, 128:152], [ok], [('gate', j, sub)], q='pool')
                    for i in range(3):
                        wt, wk = load_w(wr, w_in[l], 1304 + 512 * i, 512)
                        for c in range(4):
                            ps, pk = fm(wt, wk, lambda kc, c=c: wt[:, kc, c * 128:(c + 1) * 128])
                            fm_store(ps, pk, dnraw_d[4 * i + c, :, cs], ('dnraw', 4 * i + c, j))
                    wt, wk = load_w(wr, w_in[l], 2840, 16)
                    for sub in range(4):
                        r0 = t0 + 128 * sub
                        ps, pk = tm(wt, wk, 0, 16, sub)
                        o, ok = st.next()
                        act(o[:, 0:8], ps[:, 0:8], AF.Sigmoid, [pk], [ok])
                        tt('dve', o[:, 8:16], ps[:, 8:16], dtb[:], ALU.add, [pk, 'dtb'], [ok])
                        act(o[:, 8:16], o[:, 8:16], AF.Exp, [ok], [ok])
                        act(o[:, 8:16], o[:, 8:16], AF.Ln, [ok], [ok], bias=1.0)
                        tt('dve', o[:, 8:16], o[:, 8:16], nea[:], ALU.mult, [ok, 'nea'], [ok])
                        S.dma(bg_d[r0:r0 + 128, :], o[:, 0:16], [ok], [('bg', j, sub)], q='pool')
                    wt, wk = load_w(wr, w_in[l], 2856, 512)
                    for sub in range(4):
                        r0 = t0 + 128 * sub
                        ps, pk = tm(wt, wk, 0, 512, sub)
                        fm_store(ps, pk, zs_d[r0:r0 + 128, :], ('zs', j, sub), func=AF.Silu)
                    for i in range(4):
                        wt, wk = load_w(wr, w_in[l], 3368 + 512 * i, 512)
                        for c in range(4):
                            ps, pk = fm(wt, wk, lambda kc, c=c: wt[:, kc, c * 128:(c + 1) * 128])
                            fm_store(ps, pk, mergeT_d[4 * i + c, :, cs], ('mergeT', 4 * i + c, j), func=AF.Sigmoid)
                S.barrier()
        def phase_B(l):
            with ExitStack() as ph:
                def psb(name, shape):
                    return ph.enter_context(nc.sbuf_tensor("%s_B%d" % (name, l), list(shape), F32))
                kst = [psb("kst%d" % g, (128, NCT * 128)) for g in range(2)]
                for g in range(2):
                    memset('pool', kst[g][:], 0.0, ['kcmpT'])
                vcmp = psb("vcmp", (128, NCT, 2, 65 + JB))
                memset('pool', vcmp[:], 0.0, ['vcmp'])
                memset('pool', vcmp[:, :, :, 64:65], 1.0, ['vcmp'])
                for g in range(2):
                    cp('pool', vcmp[:, :, g, 65:65 + JB], ovl[:], ['ovl', 'vcmp'], ['vcmp'])
                gains = psb("gains", (128, 4))
                with nc.allow_non_contiguous_dma(reason="small"):
                    for h0 in (0, 64):
                        S.dma(gains[h0:h0 + 64, :], qk_norm_g[l].rearrange("i d -> d i"), (), ['gains'])
                with ExitStack() as ph2:
                    def psb2(name, shape):
                        return ph2.enter_context(nc.sbuf_tensor("%s_B2%d" % (name, l), list(shape), F32))
                    kcT = psb2("kcT", (128, SEQ))
                    w1 = psb2("w1", (128, 32, 256))
                    posT = psb2("posT", (128, 32))
                    w2 = psb2("w2", (128, 2, 64))
                    pbias = psb2("pbias", (128, 2))
                    gx = psb2("gx", (128, 2, 2, 256))
                    gt = psb2("gt", (128, 256))
                    sqc = psb2("sqc", (128, 256))
                    for kvi in range(2):
                        S.dma(kcT[:], kcT_d[kvi, :, :], [('kcT', kvi, j) for j in range(QT)], ['kcT'])
                        with nc.allow_non_contiguous_dma(reason="cmp weights"):
                            for h0 in (0, 64):
                                S.dma(w1[h0:h0 + 64, :, :], cmp_w1[l, kvi].rearrange("(j d) f -> d j f", d=64), (), ['w1'])
                                S.dma(posT[h0:h0 + 64, :], cmp_pos[l, kvi].rearrange("j d -> d j"), (), ['posT'])
                            S.dma(w2[:], cmp_w2[l, kvi].rearrange("(fc p) d -> p fc d", p=128), (), ['w2'])
                        for fc in range(2):
                            ps, pk = psr.next()
                            for j in range(32):
                                mm(ps[:, 0:1], w1[0:64, j, fc * 128:(fc + 1) * 128], posT[0:64, j:j + 1], j == 0, j == 31,
                                   ['w1', 'posT'], [pk], inc=(j == 31))
                            cp('dve', pbias[:, fc:fc + 1], ps[:, 0:1], [pk], ['pbias'])
                        for g in range(2):
                            hs = slice(64 * g, 64 * g + 64)
                            for fc in range(2):
                                ps, pk = psr.next()
                                for j in range(32):
                                    mm(ps[:, 0:NCMP], w1[hs, j, fc * 128:(fc + 1) * 128], kcT[hs, j:j + 16 * (NCMP - 1) + 1:16],
                                       j == 0, j == 31, ['w1', 'kcT'], [pk], inc=(j == 31))
                                xs = gx[:, g, fc, 0:NCMP]
                                ts('dve', xs, ps[:, 0:NCMP], pbias[:, fc:fc + 1], ALU.add, [pk, 'pbias'], ['gx'])
                                tt('dve', gt[:, 0:NCMP], xs, xs, ALU.mult, ['gx'], ['gt'])
                                ts('dve', gt[:, 0:NCMP], gt[:, 0:NCMP], 0.044715, ALU.mult, ['gt'], ['gt'], s2=1.0, op1=ALU.add)
                                tt('dve', gt[:, 0:NCMP], gt[:, 0:NCMP], xs, ALU.mult, ['gt', 'gx'], ['gt'])
                                act(gt[:, 0:NCMP], gt[:, 0:NCMP], AF.Sigmoid, ['gt'], ['gt'], scale=1.5957691216057308)
                                tt('dve', xs, xs, gt[:, 0:NCMP], ALU.mult, ['gx', 'gt'], ['gx'])
                            if kvi == 0:
                                ps, pk = psr.next()
                                for fc in range(2):
                                    mm(ps[hs, 0:NCMP], w2[:, fc, :], gx[:, g, fc, 0:NCMP], fc == 0, fc == 1, ['w2', 'gx'], [pk], inc=(fc == 1))
                                act(sqc[hs, 0:NCMP], ps[hs, 0:NCMP], AF.Square, [pk], ['sqc'])
                                p2, pk2 = psr.next()
                                mm(p2[hs, 0:NCMP], ones[hs, 0:64], sqc[hs, 0:NCMP], True, True, ['ones', 'sqc'], [pk2])
                                act(sqc[hs, 0:NCMP], p2[hs, 0:NCMP], AF.Sqrt, [pk2], ['sqc'], bias=EPS, scale=1.0 / 64)
                                recip(sqc[hs, 0:NCMP], sqc[hs, 0:NCMP], ['sqc'], ['sqc'])
                                stt(kst[g][hs, 0:NCMP], ps[hs, 0:NCMP], gains[hs, 1:2], sqc[hs, 0:NCMP], ALU.mult, ALU.mult,
                                    [pk, 'sqc', 'gains'], ['kcmpT'])
                            else:
                                for nt in range(NCT):
                                    nn = min(128, NCMP - nt * 128)
                                    ps, pk = psr.next()
                                    for fc in range(2):
                                        mm(ps[0:nn, 0:64], gx[:, g, fc, nt * 128:nt * 128 + nn], w2[:, fc, :], fc == 0, fc == 1,
                                           ['w2', 'gx'], [pk], inc=(fc == 1))
                                    cp('dve', vcmp[0:nn, nt, g, 0:64], ps[0:nn, 0:64], [pk], ['vcmp'])
                    S.barrier()
                qr = Ring([psb("qTb%d" % i, (128, SEQ)) for i in range(2)], "qTb")
                btr = Ring([psb("bt%d" % i, (128, NCT, 512)) for i in range(2)], "bt")
                pcr = Ring([psb("pc%d" % i, (128, NCT, 512)) for i in range(2)], "pc")
                osr = Ring([psb("os%d" % i, (128, 64)) for i in range(4)], "os")
                rdr = Ring([psb("rd%d" % i, (128, 2)) for i in range(4)], "rd")
                gate_sb = psb("gate_sb", (128, NT, 24))
                impacc = psb("impacc", (128, NT, 2, JB))
                with nc.allow_non_contiguous_dma(reason="gate"):
                    S.dma(gate_sb[:], gate_d.rearrange("(t p) c -> p t c", p=128),
                          [('gate', j, s) for j in range(QT) for s in range(4)], ['gate_sb'])
                for c in range(4):
                    qT, qk = qr.next()
                    S.dma(qT[:], qT_d[c, :, :], [('qT', c, j) for j in range(QT)], [qk])
                    for half in range(2):
                        h = c + 4 * half
                        g = half
                        hs = slice(64 * half, 64 * half + 64)
                        for jq in range(QT):
                            tq0 = 512 * jq
                            nts = [nt for nt in range(NCT) if 16 * 128 * nt + 31 <= tq0 + 511]
                            bt, bk = btr.next()
                            pc, pck = pcr.next()
                            for nt in nts:
                                S.dma(bt[:, nt, :], bct_d[h, nt * 128:(nt + 1) * 128, tq0:tq0 + 512], (), [bk])
                            for nt in nts:
                                ps, pk = psr.next()
                                mm(ps[:], kst[g][:, nt * 128:(nt + 1) * 128], qT[:, tq0:tq0 + 512], True, True, ['kcmpT', qk], [pk])
                                tt('dve', pc[:, nt, :], ps[:], bt[:, nt, :], ALU.add, [pk, bk], [pck])
                                act(pc[:, nt, :], pc[:, nt, :], AF.Exp, [pck], [pck])
                            for sub in range(4):
                                tsi = 4 * jq + sub
                                po, pok = psr.next()
                                W = 65 + JB
                                for nt in nts:
                                    mm(po[:, 0:W], pc[:, nt, sub * 128:(sub + 1) * 128], vcmp[:, nt, g, :], nt == nts[0], nt == nts[-1],
                                       [pck, 'vcmp'], [pok], inc=(nt == nts[-1]))
                                rd, rk = rdr.next()
                                ts('dve', rd[:, 0:1], po[:, 64:65], 1e-30, ALU.add, [pok], [rk])
                                recip(rd[:, 1:2], rd[:, 0:1], [rk], [rk])
                                o, ok = osr.next()
                                ts('dve', o[:], po[:, 0:64], rd[:, 1:2], ALU.mult, [pok, rk, 'gate_sb'], [ok],
                                   s2=gate_sb[:, tsi, 3 * h:3 * h + 1], op1=ALU.mult)
                                S.dma(obr_d[0, tsi * 128:(tsi + 1) * 128, 64 * h:64 * h + 64], o[:], [ok], [('obr', 0, h, tsi)], q='pool')
                                if c == 0:
                                    ts('dve', impacc[:, tsi, g, :], po[:, 65:W], rd[:, 1:2], ALU.mult, [pok, rk], [('imp', tsi, g)])
                                else:
                                    stt(impacc[:, tsi, g, :], po[:, 65:W], rd[:, 1:2], impacc[:, tsi, g, :], ALU.mult, ALU.add,
                                        [pok, rk, ('imp', tsi, g)], [('imp', tsi, g)])
                selM = psb("selM", (128, NT, JB))
                selA = psb("selA", (128, NT, JB))
                memset('pool', selM[:], 1.0, ['selM'])
                asel(selM[:], selM[:], [[128, NT], [-64, JB]], -128, 1, 0.0, ['selM'], ['selM'])
                memset('pool', selM[:, :, 0:1], 0.0, ['selM'])
                memset('pool', selA[:], 0.0, ['selA'])
                asel(selA[:], selA[:], [[128, NT], [-64, JB]], -128, 1, 1e9, ['selA'], ['selA'])
                asel(selA[:], selA[:], [[128, NT], [-64, JB]], 0, 1, -1.0, ['selA'], ['selA'])
                memset('pool', selA[:, :, 0:1], 1e9, ['selA'])
                scr = Ring([psb("sc%d" % i, (128, 2, JB)) for i in range(2)], "sc")
                sc2r = Ring([psb("sd%d" % i, (128, JB)) for i in range(2)], "sd")
                m8r = Ring([psb("m8%d" % i, (128, 16)) for i in range(4)], "m8")
                sbr = Ring([psb("sbi%d" % i, (128, 128)) for i in range(2)], "sbi")
                sto = Ring([psb("sto%d" % i, (128, 128)) for i in range(2)], "sto")
                for tsi in range(NT):
                    sc, sck = scr.next()
                    sbi, sbk = sbr.next()
                    if JB < 64:
                        memset('pool', sbi[:], 0.0, [sbk])
                    for g in range(2):
                        tt('dve', sc[:, g, :], impacc[:, tsi, g, :], selM[:, tsi, :], ALU.mult, [('imp', tsi, g), 'selM'], [sck])
                        tt('dve', sc[:, g, :], sc[:, g, :], selA[:, tsi, :], ALU.add, [sck, 'selA'], [sck])
                        m8, mk = m8r.next()
                        sd, sdk = sc2r.next()
                        S.op('dve', lambda e, a=m8, b=sc, g=g: e.max(out=a[:, 0:8], in_=b[:, g, :]), [sck], [mk])
                        S.op('dve', lambda e, a=m8, b=sc, d=sd, g=g: e.match_replace(out=d[:], in_to_replace=a[:, 0:8], in_values=b[:, g, :],
                                                                                   imm_value=-3e38), [sck, mk], [sdk])
                        S.op('dve', lambda e, a=m8, d=sd: e.max(out=a[:, 8:16], in_=d[:]), [sdk], [mk])
                        ts('dve', sbi[:, 64 * g:64 * g + JB], sc[:, g, :], m8[:, 15:16], ALU.is_ge, [sck, mk], [sbk], s2=-NEGM, op1=ALU.mult)
                        ts('dve', sbi[:, 64 * g:64 * g + JB], sbi[:, 64 * g:64 * g + JB], NEGM, ALU.add, [sbk], [sbk])
                    ps, pk = psr.next()
                    tr(ps[:, 0:128], sbi[:], ident[:], [sbk, 'ident'], [pk])
                    so, sok = sto.next()
                    act(so[:], ps[:, 0:128], AF.Copy, [pk], [sok])
                    S.dma(selbT_d[:, tsi * 128:(tsi + 1) * 128], so[:], [sok], [('selbT', tsi)], q='pool')
                S.barrier()

        def phase_C(l):
            for br in (1, 2):
                with ExitStack() as ph:
                    def psb(name, shape):
                        return ph.enter_context(nc.sbuf_tensor("%s_C%d_%d" % (name, l, br), list(shape), F32))
                    Wt = WGEN if br == 1 else WWIN
                    tab_d = bgen_d if br == 1 else bwin_d
                    KT_d = kslcT_d if br == 1 else kwinT_d
                    V_d = vslc_d if br == 1 else vwin_d
                    tab = psb("tab", (128, NH, Wt))
                    for h in range(NH):
                        S.dma(tab[:, h, :], tab_d[:, h, :], (), ['tab'])
                    LS = [psb("LS%d" % g, (128, SEQ)) for g in range(2)]
                    for g in range(2):
                        if br == 1:
                            S.dma(LS[g][0:64, :], KT_d[64 * g:64 * g + 64, :], (), [('LS', g)])
                            v = LS[g][64:128, :]
                            memset('pool', v, 1.0, [('LS', g)])
                            asel(v, v, [[1, SEQ]], 0, -64, 0.0, [('LS', g)], [('LS', g)])
                            asel(v, v, [[-1, SEQ]], 63, 64, 0.0, [('LS', g)], [('LS', g)])
                        else:
                            memset('pool', LS[g][64 * (1 - g):64 * (1 - g) + 64, :], 0.0, [('LS', g)])
                            S.dma(LS[g][64 * g:64 * g + 64, :], KT_d[64 * g:64 * g + 64, :], (), [('LS', g)])
                    V = psb("V", (128, NT, 2, 65))
                    memset('pool', V[:, :, :, 64:65], 1.0, ['V'])
                    with nc.allow_non_contiguous_dma(reason="V"):
                        for g in range(2):
                            S.dma(V[:, :, g, 0:64], V_d.rearrange("(t p) c -> p t c", p=128)[:, :, 64 * g:64 * g + 64], (), ['V'])
                    gate_sb = psb("gate_sb", (128, NT, 24))
                    with nc.allow_non_contiguous_dma(reason="gate"):
                        S.dma(gate_sb[:], gate_d.rearrange("(t p) c -> p t c", p=128), (), ['gate_sb'])
                    qr = Ring([psb("qTc%d" % i, (128, SEQ)) for i in range(2)], "qTc")
                    ptr = Ring([psb("pt%d" % i, (128, 512)) for i in range(4)], "pt")
                    osr = Ring([psb("os%d" % i, (128, 64)) for i in range(4)], "os")
                    rdr = Ring([psb("rd%d" % i, (128, 2)) for i in range(4)], "rd")
                    b31 = psb("b31", (128, NH))
                    with nc.allow_non_contiguous_dma(reason="b31"):
                        S.dma(b31[:], fdg_in[:, NEGPAD + OFFMAX - 1].partition_broadcast(128), (), ['b31'])
                    psr4 = Ring(PS[0:4], "ps")
                    LAG = 2
                    for c in range(4):
                        if br == 2:
                            qT, qk = qr.next()
                            S.dma(qT[:], qT_d[c, :, :], (), [qk])
                        for half in range(2):
                            h = c + 4 * half
                            g = half
                            if br == 1:
                                qT, qk = qr.next()
                                S.dma(qT[0:64, :], qT_d[c, 64 * half:64 * half + 64, :], (), [qk])
                                S.dma(qT[64:128, :], selbT_d[64 * g:64 * g + 64, :], (), [qk])
                            pend = []

                            def stage3(item):
                                jq_, tk0_, pt_, ptk_, tks_, last_ = item
                                tq0_ = 512 * jq_
                                for sub in range(4):
                                    if tk0_ > tq0_ + 128 * sub + 127:
                                        continue
                                    mm(PS[4 + sub][:, 0:65], pt_[:, sub * 128:(sub + 1) * 128], V[:, tk0_ // 128, g, :],
                                       tk0_ == tks_[0], tk0_ == last_[sub], [ptk_, 'V'], [('ps', 4 + sub)], inc=(tk0_ == last_[sub]))
                                if tk0_ == tks_[-1]:
                                    for sub in range(4):
                                        tsi = 4 * jq_ + sub
                                        po = PS[4 + sub]
                                        pok = ('ps', 4 + sub)
                                        rd, rk = rdr.next()
                                        ts('dve', rd[:, 0:1], po[:, 64:65], 1e-30, ALU.add, [pok], [rk])
                                        recip(rd[:, 1:2], rd[:, 0:1], [rk], [rk])
                                        o, ok = osr.next()
                                        ts('dve', o[:], po[:, 0:64], rd[:, 1:2], ALU.mult, [pok, rk, 'gate_sb'], [ok],
                                           s2=gate_sb[:, tsi, 3 * h + br:3 * h + br + 1], op1=ALU.mult)
                                        S.dma(obr_d[br, tsi * 128:(tsi + 1) * 128, 64 * h:64 * h + 64], o[:], [ok], [('obr', br, h, tsi)], q='pool')

                            for jq in range(QT):
                                tq0 = 512 * jq
                                lo = 0 if br == 1 else max(0, tq0 - 512)
                                tks = list(range(lo, tq0 + 512, 128))
                                last = {sub: max(tk for tk in tks if tk <= tq0 + 128 * sub + 127) for sub in range(4)}
                                for tk0 in tks:
                                    ps, pk = psr4.next()
                                    mm(ps[:], LS[g][:, tk0:tk0 + 128], qT[:, tq0:tq0 + 512], True, True, [('LS', g), qk], [pk])
                                    pt, ptk = ptr.next()
                                    if tq0 - tk0 >= OFFMAX + 128:
                                        act(pt[:], ps[:], AF.Exp, [pk, 'b31'], [ptk], bias=b31[:, h:h + 1])
                                    else:
                                        off = min(tq0 - tk0, OFFMAX) + U0
                                        tt('dve', pt[:], ps[:], tab[:, h, off:off + 512], ALU.add, [pk, 'tab'], [ptk])
                                        act(pt[:], pt[:], AF.Exp, [ptk], [ptk])
                                    pend.append((jq, tk0, pt, ptk, tks, last))
                                    if len(pend) > LAG:
                                        stage3(pend.pop(0))
                            while pend:
                                stage3(pend.pop(0))
                    S.barrier()
        def bc(ap2, n, axis):
            P, A = ap2.shape
            if axis == 2:
                return ap2.unsqueeze(2).to_broadcast([P, A, n])
            return ap2.unsqueeze(1).to_broadcast([P, n, A])

        def phase_D(l):
            with ExitStack() as ph:
                def psb(name, shape):
                    return ph.enter_context(nc.sbuf_tensor("%s_D1%d" % (name, l), list(shape), F32))
                xr = Ring([psb("xin%d" % i, (128, SEQ + 3)) for i in range(2)], "xin")
                ar = Ring([psb("acc%d" % i, (128, SEQ)) for i in range(2)], "acc")
                sq = Ring([psb("sq%d" % i, (128, 512)) for i in range(2)], "sq")
                cw = psb("cw", (128, 4, 12))
                with nc.allow_non_contiguous_dma(reason="conv w"):
                    for i in range(4):
                        S.dma(cw[:, i, :], dn_conv_w[l, i].rearrange("(c p) -> p c", p=128), (), ['cw'])
                for ch in range(12):
                    xin, xk = xr.next()
                    acc, ak = ar.next()
                    memset('pool', xin[:, 0:3], 0.0, [xk])
                    S.dma(xin[:, 3:SEQ + 3], dnraw_d[ch, :, :], (), [xk])
                    ts('dve', acc[:], xin[:, 0:SEQ], cw[:, 0, ch:ch + 1], ALU.mult, [xk, 'cw'], [ak])
                    for i in range(1, 4):
                        stt(acc[:], xin[:, i:SEQ + i], cw[:, i, ch:ch + 1], acc[:], ALU.mult, ALU.add, [xk, 'cw', ak], [ak])
                    act(acc[:], acc[:], AF.Silu, [ak], [ak])
                    if ch < 8:
                        for j in range(QT):
                            cs = slice(512 * j, 512 * j + 512)
                            q1, qk1 = sq.next()
                            act(q1[:], acc[:, cs], AF.Square, [ak], [qk1])
                            p2, pk2 = psr.next()
                            mm(p2[:], bdones[:], q1[:], True, True, ['bdones', qk1], [pk2])
                            act(q1[:], p2[:], AF.Sqrt, [pk2], [qk1], bias=EPS, scale=1.0)
                            recip(q1[:], q1[:], [qk1], [qk1])
                            if ch < 4:
                                stt(acc[:, cs], acc[:, cs], HD ** -0.5, q1[:], ALU.mult, ALU.mult, [ak, qk1], [ak])
                            else:
                                tt('dve', acc[:, cs], acc[:, cs], q1[:], ALU.mult, [ak, qk1], [ak])
                    S.dma(dnc_d[ch, :, :], acc[:], [ak], [('dnc', ch)], q='pool')
                S.barrier()
            if 'stopD1' in dbg:
                return
            with ExitStack() as ph:
                def psb(name, shape):
                    return ph.enter_context(nc.sbuf_tensor("%s_D2%d" % (name, l), list(shape), F32))
                NG = NCH * 8
                g_tm = psb("g_tm", (64, NCH, 8))
                b_tm = psb("b_tm", (64, NCH, 8))
                gc_tm = psb("gc_tm", (64, NCH, 8))
                eg_tm = psb("eg_tm", (64, NCH, 8))
                bg_tm = psb("bg_tm", (64, NCH, 8))
                kds_tm = psb("kds_tm", (64, NCH, 8))
                egl = psb("egl", (64, NCH, 8))
                sel63 = psb("sel63", (64, 64))
                ngrow = psb("ngrow", (64, 64))
                with nc.allow_non_contiguous_dma(reason="bg"):
                    S.dma(b_tm[:], bg_d.rearrange("(n i) c -> i n c", i=64)[:, :, 0:8], (), ['b_tm'])
                    S.dma(g_tm[:], bg_d.rearrange("(n i) c -> i n c", i=64)[:, :, 8:16], (), ['g_tm'])
                S.dma(ngrow[:], dn_norm_g[l].partition_broadcast(64), (), ['ngrow'])
                ts('dve', sel63[:], ones[0:64, 0:64], ident[0:64, 63:64], ALU.mult, ['ones', 'ident'], ['sel63'])
                gflat = g_tm[:].rearrange("p n h -> p (n h)")
                gcflat = gc_tm[:].rearrange("p n h -> p (n h)")
                for c0 in range(0, NG, 512):
                    cw_ = min(512, NG - c0)
                    ps, pk = psr.next()
                    mm(ps[0:64, 0:cw_], UT[0:64, :], gflat[:, c0:c0 + cw_], True, True, ['UT', 'g_tm'], [pk])
                    cp('dve', gcflat[:, c0:c0 + cw_], ps[0:64, 0:cw_], [pk], ['gc_tm'])
                    ps2, pk2 = psr.next()
                    mm(ps2[0:64, 0:cw_], sel63[:], gcflat[:, c0:c0 + cw_], True, True, ['sel63', 'gc_tm'], [pk2])
                    act(egl[:].rearrange("p n h -> p (n h)")[:, c0:c0 + cw_], ps2[0:64, 0:cw_], AF.Exp, [pk2], ['egl'])
                    tt('dve', kds_tm[:].rearrange("p n h -> p (n h)")[:, c0:c0 + cw_], ps2[0:64, 0:cw_], gcflat[:, c0:c0 + cw_],
                       ALU.subtract, [pk2, 'gc_tm'], ['kds_tm'])
                act(kds_tm[:], kds_tm[:], AF.Exp, ['kds_tm'], ['kds_tm'])
                act(eg_tm[:], gc_tm[:], AF.Exp, ['gc_tm'], ['eg_tm'])
                tt('dve', bg_tm[:], b_tm[:], eg_tm[:], ALU.mult, ['b_tm', 'eg_tm'], ['bg_tm'])

                Xr = Ring([psb("X%d" % i, (64, 24, 64)) for i in range(2)], "X")
                zr = Ring([psb("z%d" % i, (64, 512)) for i in range(2)], "z")

                def t3(name):
                    return psb(name, (64, 8, 64))
                ktm, vtm, qtm = t3("ktm"), t3("vtm"), t3("qtm")
                rhsg, rhsb = t3("rhsg"), t3("rhsb")
                DTr, decT, dec = t3("DTr"), t3("decT"), t3("dec")
                AT, t1 = t3("AT"), t3("t1")
                Xa, Xb, Ya, Yb, TT = t3("Xa"), t3("Xb"), t3("Ya"), t3("Yb"), t3("TT")
                vb, kbg, kd, qdT, wT, u_sb, vn = t3("vb"), t3("kbg"), t3("kd"), t3("qdT"), t3("wT"), t3("u_sb"), t3("vn")
                Sst, o_sb, osq = t3("Sst"), t3("o_sb"), t3("osq")
                ssum = psb("ssum", (64, 16))
                ytm = psb("ytm", (64, 512))
                ybo = Ring([psb("ybo%d" % i, (128, 4, 64)) for i in range(2)], "ybo")
                memset('pool', Sst[:], 0.0, ['Sst'])
                I64 = ident[0:64, 0:64]

                def hmm(pst, pk, lhs, rhs, r, first=True, last=True):
                    for h in range(8):
                        mm(pst[0:64, 64 * h:64 * h + 64], lhs(h), rhs(h), first, last, r, [pk], inc=(h == 7))

                def v3(ps):
                    return ps[0:64, :].rearrange("p (h c) -> p h c", h=8)

                if 'd2s0' in dbg:
                    S.barrier(); return
                for n in range(NCH):
                    X, Xk = Xr.next()
                    with nc.allow_non_contiguous_dma(reason="chunk load"):
                        S.dma(X[:], dnc_d.rearrange("c (a p) t -> p (c a) t", a=2)[:, :, 64 * n:64 * n + 64], (), [Xk])
                    z, zk = zr.next()
                    S.dma(z[:], zs_d[64 * n:64 * n + 64, :], (), [zk])
                    for (dst, dk_, c0) in ((qtm, 'qtm', 0), (ktm, 'ktm', 8), (vtm, 'vtm', 16)):
                        ps, pk = psr.next()
                        for kc in range(8):
                            tr(ps[0:64, 64 * kc:64 * kc + 64], X[:, c0 + kc, :], I64, [Xk, 'ident'], [pk], inc=(kc == 7))
                        if dk_ == 'ktm':
                            cp('dve', dst[:], v3(ps), [pk], [dk_])
                        else:
                            act(dst[:], v3(ps), AF.Copy, [pk], [dk_])
                    if 'd2s1' in dbg:
                        continue
                    tt('dve', rhsg[:], bc(g_tm[:, n, :], 64, 2), bc(UT[0:64, :], 8, 1), ALU.mult, ['g_tm', 'UT'], ['rhsg'])
                    tt('dve', rhsb[:], bc(b_tm[:, n, :], 64, 2), bc(I64, 8, 1), ALU.mult, ['b_tm', 'ident'], ['rhsb'])
                    pg, pgk = psr.next()
                    mm(pg[0:64, :], ones[0:64, 0:64], rhsg[:].rearrange("p h c -> p (h c)"), True, True, ['ones', 'rhsg'], [pgk])
                    pb, pbk = psr.next()
                    mm(pb[0:64, :], ones[0:64, 0:64], rhsb[:].rearrange("p h c -> p (h c)"), True, True, ['ones', 'rhsb'], [pbk])
                    tt('dve', DTr[:], v3(pg), bc(gc_tm[:, n, :], 64, 2), ALU.subtract, [pgk, 'gc_tm'], ['DTr'])
                    tt('dve', decT[:], DTr[:], bc(maskU[:], 8, 1), ALU.add, ['DTr', 'maskU'], ['decT'])
                    act(decT[:], decT[:], AF.Exp, ['decT'], ['decT'])
                    ts('dve', dec[:], DTr[:], -1.0, ALU.mult, ['DTr'], ['dec'])
                    tt('dve', dec[:], dec[:], bc(maskL[:], 8, 1), ALU.add, ['dec', 'maskL'], ['dec'])
                    act(dec[:], dec[:], AF.Exp, ['dec'], ['dec'])
                    if 'd2s2' in dbg:
                        continue
                    pkk, pkkk = psr.next()
                    hmm(pkk, pkkk, lambda h: X[:, 8 + h, :], lambda h: X[:, 8 + h, :], [Xk])
                    pqk, pqkk = psr.next()
                    hmm(pqk, pqkk, lambda h: X[:, 8 + h, :], lambda h: X[:, h, :], [Xk])
                    tt('dve', AT[:], v3(pqk), decT[:], ALU.mult, [pqkk, 'decT'], ['AT'])
                    tt('dve', t1[:], v3(pkk), decT[:], ALU.mult, [pkkk, 'decT'], ['t1'])
                    tt('dve', t1[:], v3(pb), t1[:], ALU.mult, [pbk, 't1'], ['t1'])
                    tt('dve', Ya[:], t1[:], bc(nsU[:], 8, 1), ALU.mult, ['t1', 'nsU'], ['Ya'])
                    tt('dve', t1[:], v3(pkk), dec[:], ALU.mult, [pkkk, 'dec'], ['t1'])
                    tt('dve', t1[:], t1[:], bc(b_tm[:, n, :], 64, 2), ALU.mult, ['t1', 'b_tm'], ['t1'])
                    tt('dve', Xa[:], t1[:], bc(nsL[:], 8, 1), ALU.mult, ['t1', 'nsL'], ['Xa'])
                    tt('dve', TT[:], Ya[:], bc(I64, 8, 1), ALU.add, ['Ya', 'ident'], ['TT'])
                    if 'd2s3' in dbg:
                        continue
                    Xc, Xn_, Yc, Yn_ = (Xa, 'Xa'), (Xb, 'Xb'), (Ya, 'Ya'), (Yb, 'Yb')
                    for lvl in range(1, 6):
                        p1, p1k = psr.next()
                        hmm(p1, p1k, lambda h: Yc[0][:, h, :], lambda h: Xc[0][:, h, :], [Yc[1], Xc[1]])
                        if lvl < 5:
                            p2, p2k = psr.next()
                            hmm(p2, p2k, lambda h: Xc[0][:, h, :], lambda h: Yc[0][:, h, :], [Yc[1], Xc[1]])
                        act(Xn_[0][:], v3(p1), AF.Copy, [p1k], [Xn_[1]])
                        if lvl < 5:
                            cp('dve', Yn_[0][:], v3(p2), [p2k], [Yn_[1]])
                        Xc, Xn_ = Xn_, Xc
                        if lvl < 5:
                            Yc, Yn_ = Yn_, Yc
                        p3, p3k = psr.next()
                        hmm(p3, p3k, lambda h: Xc[0][:, h, :], lambda h: TT[:, h, :], [Xc[1], 'TT'])
                        tt('dve', TT[:], TT[:], v3(p3), ALU.add, ['TT', p3k], ['TT'])
                    if 'd2s4' in dbg:
                        continue
                    tt('dve', vb[:], vtm[:], bc(b_tm[:, n, :], 64, 2), ALU.mult, ['vtm', 'b_tm'], ['vb'])
                    tt('dve', kbg[:], ktm[:], bc(bg_tm[:, n, :], 64, 2), ALU.mult, ['ktm', 'bg_tm'], ['kbg'])
                    tt('dve', kd[:], ktm[:], bc(kds_tm[:, n, :], 64, 2), ALU.mult, ['ktm', 'kds_tm'], ['kd'])
                    tt('dve', qtm[:], qtm[:], bc(eg_tm[:, n, :], 64, 2), ALU.mult, ['qtm', 'eg_tm'], ['qtm'])
                    pu, puk = psr.next()
                    hmm(pu, puk, lambda h: TT[:, h, :], lambda h: vb[:, h, :], ['TT', 'vb'])
                    act(u_sb[:], v3(pu), AF.Copy, [puk], ['u_sb'])
                    pw, pwk = psr.next()
                    hmm(pw, pwk, lambda h: kbg[:, h, :], lambda h: TT[:, h, :], ['TT', 'kbg'])
                    act(wT[:], v3(pw), AF.Copy, [pwk], ['wT'])
                    pq, pqk2 = psr.next()
                    for h in range(8):
                        tr(pq[0:64, 64 * h:64 * h + 64], qtm[:, h, :], I64, ['qtm', 'ident'], [pqk2], inc=(h == 7))
                    cp('dve', qdT[:], v3(pq), [pqk2], ['qdT'])
                    if 'd2s5' in dbg:
                        continue
                    pv, pvk = psr.next()
                    hmm(pv, pvk, lambda h: wT[:, h, :], lambda h: Sst[:, h, :], ['wT', 'Sst'])
                    tt('dve', vn[:], u_sb[:], v3(pv), ALU.subtract, ['u_sb', pvk], ['vn'])
                    po, pok = psr.next()
                    for h in range(8):
                        mm(po[0:64, 64 * h:64 * h + 64], qdT[:, h, :], Sst[:, h, :], True, False, ['qdT', 'Sst'], [pok], inc=False)
                        mm(po[0:64, 64 * h:64 * h + 64], AT[:, h, :], vn[:, h, :], False, True, ['AT', 'vn'], [pok], inc=(h == 7))
                    pS, pSk = psr.next()
                    hmm(pS, pSk, lambda h: kd[:, h, :], lambda h: vn[:, h, :], ['kd', 'vn'])
                    tt('dve', Sst[:], Sst[:], bc(egl[:, n, :], 64, 2), ALU.mult, ['Sst', 'egl'], ['Sst'])
                    tt('dve', Sst[:], Sst[:], v3(pS), ALU.add, ['Sst', pSk], ['Sst'])
                    if 'd2s6' in dbg:
                        continue
                    act(o_sb[:], v3(po), AF.Copy, [pok], ['o_sb'])
                    tt('dve', osq[:], o_sb[:], o_sb[:], ALU.mult, ['o_sb'], ['osq'])
                    S.op('dve', lambda e: e.tensor_reduce(out=ssum[:, 0:8], in_=osq[:], op=ALU.add, axis=AX.X), ['osq'], ['ssum'])
                    act(ssum[:, 8:16], ssum[:, 0:8], AF.Sqrt, ['ssum'], ['ssum'], bias=EPS, scale=1.0 / 64)
                    recip(ssum[:, 8:16], ssum[:, 8:16], ['ssum'], ['ssum'])
                    tt('dve', o_sb[:], o_sb[:], bc(ssum[:, 8:16], 64, 2), ALU.mult, ['o_sb', 'ssum'], ['o_sb'])
                    tt('dve', o_sb[:], o_sb[:], bc(ngrow[:], 8, 1), ALU.mult, ['o_sb', 'ngrow'], ['o_sb'])
                    tt('dve', ytm[:], o_sb[:].rearrange("p h c -> p (h c)"), z[:], ALU.mult, ['o_sb', zk], ['ytm'])
                    py, pyk = psr.next()
                    for kc in range(4):
                        tr(py[:, 64 * kc:64 * kc + 64], ytm[:, 128 * kc:128 * kc + 128], I64, ['ytm', 'ident'], [pyk], inc=(kc == 3))
                    yo, yok = ybo.next()
                    act(yo[:], py[:, 0:256].rearrange("p (a b) -> p a b", a=4), AF.Copy, [pyk], [yok])
                    with nc.allow_non_contiguous_dma(reason="yb store"):
                        S.dma(ybT_d.rearrange("c p t -> p c t")[:, :, 64 * n:64 * n + 64], yo[:], [yok], [('ybT', n)], q='pool')
                S.barrier()

        def phase_E(l, src_x):
            with ExitStack() as ph:
                def psb(name, shape):
                    return ph.enter_context(nc.sbuf_tensor("%s_E%d" % (name, l), list(shape), F32))
                wa = psb("wa", (128, 4, D))
                wb = psb("wb", (128, 4, D))
                wo = psb("wo", (128, 8, D))
                g1r = psb("g1r", (128, D))
                S.dma(g1r[:], mod_d[l, 2 * D:3 * D].partition_broadcast(128), (), ['g1r'])
                S.dma(wa[:], w_br_a[l].rearrange("(kc p) n -> p kc n", p=128), (), ['wa'])
                S.dma(wb[:], w_br_b[l].rearrange("(kc p) n -> p kc n", p=128), (), ['wb'])
                S.dma(wo[:], w_out[l].rearrange("(kc p) n -> p kc n", p=128), (), ['wo'])
                obr = Ring([psb("ob%d" % i, (128, 3, 512)) for i in range(2)], "ob")
                yaT = psb("yaT", (128, 4, 512))
                ybT = psb("ybT", (128, 4, 512))
                mg = Ring([psb("mg%d" % i, (128, 2, 512)) for i in range(2)], "mg")
                yT = psb("yT", (128, 8, 512))
                tmp = Ring([psb("tmp%d" % i, (128, 512)) for i in range(2)], "tmp")
                xr = Ring([psb("xe%d" % i, (128, D)) for i in range(2)], "xe")
                for j in range(QT):
                    t0 = 512 * j
                    cs = slice(t0, t0 + 512)
                    for sub in range(4):
                        r0 = t0 + 128 * sub
                        ob, obk = obr.next()
                        S.dma(ob[:], obr_d[:, r0:r0 + 128, :].rearrange("b p c -> p b c"), (), [obk])
                        tt('dve', ob[:, 0, :], ob[:, 0, :], ob[:, 1, :], ALU.add, [obk], [obk])
                        tt('pool', ob[:, 0, :], ob[:, 0, :], ob[:, 2, :], ALU.add, [obk], [obk])
                        transpose_to(ob[:, 0, :], obk, yaT, 'yaT', sub, nkc=4)
                    S.dma(ybT[:], ybT_d.rearrange("c p t -> p c t")[:, :, cs], (), ['ybT'])
                    for dc in range(8):
                        m, mk = mg.next()
                        S.dma(m[:, 0, :], mergeT_d[dc, :, cs], (), [mk])
                        S.dma(m[:, 1, :], mergeT_d[8 + dc, :, cs], (), [mk])
                        pa, pak = psr.next()
                        for kc in range(4):
                            mm(pa[:], wa[:, kc, 128 * dc:128 * dc + 128], yaT[:, kc, :], kc == 0, kc == 3, ['wa', 'yaT'], [pak], inc=(kc == 3))
                        pb, pbk = psr.next()
                        for kc in range(4):
                            mm(pb[:], wb[:, kc, 128 * dc:128 * dc + 128], ybT[:, kc, :], kc == 0, kc == 3, ['wb', 'ybT'], [pbk], inc=(kc == 3))
                        t, tk = tmp.next()
                        tt('dve', t[:], pa[:], m[:, 0, :], ALU.mult, [pak, mk], [tk])
                        tt('dve', m[:, 1, :], pb[:], m[:, 1, :], ALU.mult, [pbk, mk], [mk])
                        tt('pool', yT[:, dc, :], t[:], m[:, 1, :], ALU.add, [tk, mk], ['yT'])
                    for sub in range(4):
                        r0 = t0 + 128 * sub
                        xt, xk = xr.next()
                        S.dma(xt[:], src_x[r0:r0 + 128, :], [('x', r0 // 128)], [xk])
                        for nh in range(2):
                            po, pok = psr.next()
                            for kc in range(8):
                                mm(po[:], yT[:, kc, 128 * sub:128 * sub + 128], wo[:, kc, 512 * nh:512 * nh + 512], kc == 0, kc == 7,
                                   ['yT', 'wo'], [pok], inc=(kc == 7))
                            t, tk = tmp.next()
                            tt('dve', t[:], po[:], g1r[:, 512 * nh:512 * nh + 512], ALU.mult, [pok, 'g1r'], [tk])
                            tt('pool', xt[:, 512 * nh:512 * nh + 512], xt[:, 512 * nh:512 * nh + 512], t[:], ALU.add, [xk, tk], [xk])
                        S.dma(out[r0:r0 + 128, :], xt[:], [xk], [('x', r0 // 128)], q='pool')
                S.barrier()

        def phase_F(l):
            with ExitStack() as ph:
                def psb(name, shape):
                    return ph.enter_context(nc.sbuf_tensor("%s_F%d" % (name, l), list(shape), F32))
                A2r = psb("A2r", (128, D))
                sh2r = psb("sh2r", (128, D))
                g2r = psb("g2r", (128, D))
                S.dma(sh2r[:], mod_d[l, 3 * D:4 * D].partition_broadcast(128), (), ['modrow'])
                S.dma(A2r[:], mod_d[l, 4 * D:5 * D].partition_broadcast(128), (), ['modrow'])
                S.dma(g2r[:], mod_d[l, 5 * D:6 * D].partition_broadcast(128), (), ['g2r'])
                xs = [psb("xf%d" % i, (128, D)) for i in range(4)]
                hr = Ring([psb("hf%d" % i, (128, D)) for i in range(2)], "hf")
                sm = Ring([psb("smf%d" % i, (128, 8)) for i in range(4)], "smf")
                hT = psb("hTf", (128, 8, 512))
                wgr = Ring([psb("wg%d" % i, (128, 8, 512)) for i in range(2)], "wg")
                wur = Ring([psb("wu%d" % i, (128, 8, 512)) for i in range(2)], "wu")
                wdr = Ring([psb("wd%d" % i, (128, 4, D)) for i in range(2)], "wd")
                actT = psb("actT", (128, 4, 512))
                sg = Ring([psb("sg%d" % i, (128, 512)) for i in range(2)], "sg")
                accs = [psb("acc%d" % i, (128, D)) for i in range(4)]
                gw = psb("gw", (128, 4, 16))
                rt = psb("rt", (128, 8, 16))
                rs = psb("rs", (128, 16))
                for j in range(QT):
                    t0 = 512 * j
                    for sub in range(4):
                        xring = Ring([xs[sub]], "xf%d" % sub)
                        ht, hk, xt, xk = norm_tile(out, t0 + 128 * sub, A2r[:], sh2r[:], xring, hr, sm)
                        transpose_to(ht, hk, hT, 'hTf', sub)
                    for sub in range(4):
                        pr, prk = psr.next()
                        for kc in range(8):
                            mm(pr[:, 0:16], hT[:, kc, 128 * sub:128 * sub + 128], rtrw[:, kc, :], kc == 0, kc == 7, ['hTf', 'rtrw'], [prk], inc=(kc == 7))
                        sc = rt[:, 0, :]
                        sel = rt[:, 1, :]
                        w1_ = rt[:, 2, :]
                        w2_ = rt[:, 3, :]
                        act(sc, pr[:, 0:16], AF.Sigmoid, [prk], ['rt'])
                        tt('dve', sel, sc, rtrb[:], ALU.add, ['rt', 'rtrb'], ['rt'])
                        sel3 = sel.rearrange("p (g e) -> p g e", g=4)
                        S.op('dve', lambda e, s3=sel3: e.tensor_reduce(out=rs[:, 0:4], in_=s3, op=ALU.max, axis=AX.X), ['rt'], ['rs'])
                        tt('dve', w1_.rearrange("p (g e) -> p g e", g=4), sel3, bc(rs[:, 0:4], 4, 2), ALU.is_ge, ['rt', 'rs'], ['rt'])
                        stt(w2_, w1_, -1e9, sel, ALU.mult, ALU.add, ['rt'], ['rt'])
                        S.op('dve', lambda e, a=w2_: e.tensor_reduce(out=rs[:, 4:8], in_=a.rearrange("p (g e) -> p g e", g=4), op=ALU.max, axis=AX.X),
                             ['rt'], ['rs'])
                        tt('dve', rs[:, 8:12], rs[:, 0:4], rs[:, 4:8], ALU.add, ['rs'], ['rs'])
                        S.op('dve', lambda e: e.tensor_reduce(out=rs[:, 12:13], in_=rs[:, 8:12], op=ALU.max, axis=AX.X), ['rs'], ['rs'])
                        ts('dve', rs[:, 4:8], rs[:, 8:12], rs[:, 12:13], ALU.is_ge, ['rs'], ['rs'], s2=-1.0, op1=ALU.add)
                        ts('dve', rs[:, 4:8], rs[:, 4:8], 1e9, ALU.mult, ['rs'], ['rs'])
                        tt('dve', w1_.rearrange("p (g e) -> p g e", g=4), sel3, bc(rs[:, 4:8], 4, 2), ALU.add, ['rt', 'rs'], ['rt'])
                        S.op('dve', lambda e, a=w1_: e.max(out=rt[:, 4, 0:8], in_=a), ['rt'], ['rt'])
                        ts('dve', w2_, w1_, rt[:, 4, 1:2], ALU.is_ge, ['rt'], ['rt'])
                        tt('dve', w2_, w2_, sc, ALU.mult, ['rt'], ['rt'])
                        S.op('dve', lambda e, a=w2_: e.tensor_reduce(out=rs[:, 13:14], in_=a, op=ALU.add, axis=AX.X), ['rt'], ['rs'])
                        recip(rs[:, 14:15], rs[:, 13:14], ['rs'], ['rs'])
                        ts('dve', gw[:, sub, :], w2_, rs[:, 14:15], ALU.mult, ['rt', 'rs'], ['gw'])
                    for e_ in range(16):
                        wg, wgk = wgr.next()
                        wu, wuk = wur.next()
                        wd, wdk = wdr.next()
                        S.dma(wg[:], moe_wg[l, e_].rearrange("(kc p) n -> p kc n", p=128), (), [wgk])
                        S.dma(wu[:], moe_wu[l, e_].rearrange("(kc p) n -> p kc n", p=128), (), [wuk])
                        S.dma(wd[:], moe_wd[l, e_].rearrange("(kc p) n -> p kc n", p=128), (), [wdk])
                        for fc in range(4):
                            pg_, pgk_ = psr.next()
                            for kc in range(8):
                                mm(pg_[:], wg[:, kc, 128 * fc:128 * fc + 128], hT[:, kc, :], kc == 0, kc == 7, [wgk, 'hTf'], [pgk_], inc=(kc == 7))
                            pu_, puk_ = psr.next()
                            for kc in range(8):
                                mm(pu_[:], wu[:, kc, 128 * fc:128 * fc + 128], hT[:, kc, :], kc == 0, kc == 7, [wuk, 'hTf'], [puk_], inc=(kc == 7))
                            s_, sk_ = sg.next()
                            act(s_[:], pg_[:], AF.Silu, [pgk_], [sk_])
                            tt('dve', actT[:, fc, :], pu_[:], s_[:], ALU.mult, [puk_, sk_], [('actT', fc)])
                        for sub in range(4):
                            for nh in range(2):
                                pd, pdk = psr.next()
                                for fc in range(4):
                                    mm(pd[:], actT[:, fc, 128 * sub:128 * sub + 128], wd[:, fc, 512 * nh:512 * nh + 512], fc == 0, fc == 3,
                                       [('actT', fc), wdk], [pdk], inc=(fc == 3))
                                av = accs[sub][:, 512 * nh:512 * nh + 512]
                                ak = ('accf', sub, nh)
                                if e_ == 0:
                                    ts('dve', av, pd[:], gw[:, sub, e_:e_ + 1], ALU.mult, [pdk, 'gw'], [ak])
                                else:
                                    stt(av, pd[:], gw[:, sub, e_:e_ + 1], av, ALU.mult, ALU.add, [pdk, 'gw', ak], [ak])
                    for sub in range(4):
                        r0 = t0 + 128 * sub
                        xk = ("xf%d" % sub, 0)
                        tt('pool', accs[sub][:], accs[sub][:], g2r[:], ALU.mult, [('accf', sub, 0), ('accf', sub, 1), 'g2r'],
                           [('accf', sub, 0), ('accf', sub, 1)])
                        tt('pool', xs[sub][:], xs[sub][:], accs[sub][:], ALU.add, [xk, ('accf', sub, 0), ('accf', sub, 1)], [xk])
                        S.dma(out[r0:r0 + 128, :], xs[sub][:], [xk], [('x', r0 // 128)], q='pool')
                S.barrier()

        with ExitStack() as ph:
            modrow = ph.enter_context(nc.sbuf_tensor("modrow", [128, 6 * D], F32))
            rowtmp = ph.enter_context(nc.sbuf_tensor("rowtmp", [128, D], F32))
            wr0 = Ring([ph.enter_context(nc.sbuf_tensor("wsl0_%d" % i, [128, 8, 512], F32)) for i in range(2)], "wsl0")
            for l in range(L):
                S.dma(modrow[:], ada_b[l].partition_broadcast(128), ['modrow'], ['modrow'])
                for oc in range(12):
                    wt, wk = load_w(wr0, ada_w[l], oc * 512, 512)
                    ps, pk = psr.next()
                    for kc in range(8):
                        mm(ps[:], cbc[:, kc, :], wt[:, kc, :], kc == 0, kc == 7, ['cbc', wk], [pk], inc=(kc == 7))
                    tt('dve', modrow[:, oc * 512:(oc + 1) * 512], ps[:], modrow[:, oc * 512:(oc + 1) * 512], ALU.add,
                       [pk, 'modrow'], ['modrow'])
                for (o_, ng) in ((1, norm1_g), (4, norm2_g)):
                    S.dma(rowtmp[:], ng[l].partition_broadcast(128), ['rowtmp'], ['rowtmp'])
                    stt(modrow[:, o_ * D:(o_ + 1) * D], modrow[:, o_ * D:(o_ + 1) * D], 1.0, rowtmp[:], ALU.add, ALU.mult,
                        ['modrow', 'rowtmp'], ['modrow'])
                S.dma(mod_d[l:l + 1, :], modrow[0:1, :], ['modrow'], [('mod', l)], q='pool')
            S.barrier()

        for l in range(L if 'stop0' not in dbg else 0):
            src_x = x_in if l == 0 else out
            with ExitStack() as phm:
                A1t = phm.enter_context(nc.sbuf_tensor("A1t_%d" % l, [128, D], F32))
                sh1t = phm.enter_context(nc.sbuf_tensor("sh1t_%d" % l, [128, D], F32))
                S.dma(sh1t[:], mod_d[l, 0:D].partition_broadcast(128), (), ['modrow'])
                S.dma(A1t[:], mod_d[l, D:2 * D].partition_broadcast(128), (), ['modrow'])
                A1row, sh1row = A1t[:], sh1t[:]
                phase_A(l, src_x, A1row, sh1row)
            if 'stopA' in dbg:
                break
            phase_B(l)
            if 'stopB' in dbg:
                break
            phase_C(l)
            if 'stopC' in dbg:
                break
            phase_D(l)
            if 'stopD' in dbg:
                break
            phase_E(l, src_x)
            if 'stopE' in dbg:
                break
            phase_F(l)
        S.barrier()
    return nc


def _host_tables(rel_bias, SEQ):
    FDW = NEGPAD + SEQ
    d = np.arange(SEQ)
    bk = rel_bucket_np(d)
    g = np.asarray(rel_bias, np.float32)[bk].T
    fdg = np.full((NH, FDW), NEGM, np.float32)
    fdg[:, NEGPAD:] = g
    fdw = np.full((NH, FDW), NEGM, np.float32)
    fdw[:, NEGPAD:NEGPAD + 512] = g[:, :512]
    return fdg, fdw


_CACHE = {}


def kernel(**inputs):
    x = np.asarray(inputs["x"], np.float32)
    B, SEQ, _ = x.shape
    L = int(np.asarray(inputs["ada_w"]).shape[0])
    key = (SEQ, L)
    if key not in _CACHE:
        _CACHE[key] = build_program(SEQ, L)
    nc = _CACHE[key]
    fdg, fdw = _host_tables(inputs["rel_bias"], SEQ)
    names = ["router_w", "router_b", "ada_w", "ada_b", "norm1_g", "norm2_g", "w_in", "qk_norm_g", "cmp_pos", "cmp_w1",
             "cmp_w2", "dn_conv_w", "dn_a_log", "dn_dt_bias", "dn_norm_g", "w_branch_a", "w_branch_b", "w_out",
             "moe_w_gate", "moe_w_up", "moe_w_down"]
    shared = {n: np.ascontiguousarray(np.asarray(inputs[n], np.float32)) for n in names}
    shared["fdg"] = fdg
    shared["fdw"] = fdw
    c = np.asarray(inputs["c"], np.float32)
    in_maps = []
    for b in range(B):
        m = dict(shared)
        m["x"] = np.ascontiguousarray(x[b])
        m["cT"] = np.ascontiguousarray(c[b].reshape(8, 128).T)
        in_maps.append(m)
    res = run_bass_kernel_spmd(nc, in_maps, core_ids=list(range(B)))
    return np.stack([np.asarray(r["out"], np.float32) for r in res.results], axis=0)
```

```python
import math
from contextlib import ExitStack
import numpy as np
import concourse.bass as bass
import concourse.mybir as mybir
from concourse.bass_utils import run_bass_kernel_spmd

F32 = mybir.dt.float32
I32 = mybir.dt.int32
AF = mybir.ActivationFunctionType
ALU = mybir.AluOpType
AX = mybir.AxisListType

D = 1024
HD = 64
NH = 8
DIN = 5416
NEGM = -30000.0
EPS = 1e-6
U0 = 384
OFFMAX = 1024
WGEN = U0 + OFFMAX + 512
WWIN = U0 + 512 + 512
NEGPAD = 1024


class Sched:
    def __init__(self, nc, es, ndma=14):
        self.nc = nc
        self.eng = {'pe': nc.tensor, 'act': nc.scalar, 'dve': nc.vector, 'pool': nc.gpsimd, 'sp': nc.sync}
        self.sem = {k: es.enter_context(nc.semaphore('s_' + k)) for k in self.eng}
        self.cnt = {k: 0 for k in self.eng}
        self.dsem = [es.enter_context(nc.semaphore('d%d' % i)) for i in range(ndma)]
        self.dcnt = [0] * ndma
        self.dnext = 0
        self.seen = {k: {} for k in self.eng}
        self.res = {}
        self.nops = 0

    def _deps(self, r, w):
        deps = {}

        def add(t):
            if t is not None and deps.get(t[0], 0) < t[1]:
                deps[t[0]] = t[1]
        for k in r:
            st = self.res.get(k)
            if st:
                add(st[0])
        for k in w:
            st = self.res.get(k)
            if st:
                add(st[0])
                for s, v in st[1].items():
                    add((s, v))
        return deps

    def _wait(self, e, deps):
        for s, v in deps.items():
            if s == 'pe' and e == 'pe':
                continue
            if self.seen[e].get(s, 0) < v:
                sem = self.sem[s] if isinstance(s, str) else self.dsem[s]
                self.eng[e].wait_ge(sem, v)
                self.seen[e][s] = v

    def _mark(self, tag, r, w):
        for k in r:
            st = self.res.setdefault(k, [None, {}])
            if st[1].get(tag[0], 0) < tag[1]:
                st[1][tag[0]] = tag[1]
        for k in w:
            self.res[k] = [tag, {}]

    def op(self, e, emit, r=(), w=(), inc=True):
        self._wait(e, self._deps(r, w))
        inst = emit(self.eng[e])
        self.nops += 1
        if inc:
            self.cnt[e] += 1
            inst.then_inc(self.sem[e], 1)
            tag = (e, self.cnt[e])
        else:
            tag = (e, self.cnt[e] + 1)
        self._mark(tag, r, w)

    def dma(self, out, in_, r=(), w=(), q='sp'):
        i = self.dnext
        self.dnext = (i + 1) % len(self.dsem)
        deps = self._deps(r, w)
        if self.dcnt[i]:
            deps[i] = max(deps.get(i, 0), self.dcnt[i])
        self._wait(q, deps)
        self.dcnt[i] += 16
        self.eng[q].dma_start(out=out, in_=in_).then_inc(self.dsem[i], 16)
        self.nops += 1
        self._mark((i, self.dcnt[i]), r, w)

    def idma(self, out, in_, out_off, in_off, r=(), w=()):
        i = self.dnext
        self.dnext = (i + 1) % len(self.dsem)
        deps = self._deps(r, w)
        if self.dcnt[i]:
            deps[i] = max(deps.get(i, 0), self.dcnt[i])
        self._wait('pool', deps)
        self.dcnt[i] += 16
        self.eng['pool'].indirect_dma_start(out=out, out_offset=out_off, in_=in_, in_offset=in_off).then_inc(self.dsem[i], 16)
        self.nops += 1
        self._mark((i, self.dcnt[i]), r, w)

    def barrier(self):
        deps = {s: c for s, c in self.cnt.items() if c}
        for i, v in enumerate(self.dcnt):
            if v:
                deps[i] = v
        for e in self.eng:
            d = dict(deps)
            self._wait(e, d)
        self.res = {}


class Ring:
    def __init__(self, tiles, name):
        self.tiles = tiles
        self.name = name
        self.i = 0

    def next(self):
        k = self.i % len(self.tiles)
        self.i += 1
        return self.tiles[k], (self.name, k)


def rel_bucket_np(dist):
    exact = 16
    dist = np.maximum(dist, 0)
    far = np.maximum(dist, exact).astype(np.float32)
    large = exact + (np.log(far / np.float32(exact)) / np.float32(math.log(1024 / exact)) * np.float32(32 - exact)).astype(np.int32)
    return np.where(dist < exact, dist, np.minimum(large, 31))


def build_program(SEQ, DEPTH, dbg=()):
    NT = SEQ // 128
    QT = SEQ // 512
    NCH = SEQ // 64
    NCMP = SEQ // 16 - 1
    NBLK = SEQ // 64
    NCT = (NCMP + 127) // 128
    FDW = NEGPAD + SEQ
    JB = NBLK
    nc = bass.Bass("TRN2", target_bir_lowering=False)

    def din(name, shape):
        return nc.dram_tensor(name, list(shape), F32, kind="ExternalInput").ap()

    def dscr(name, shape, kind="Internal"):
        if name in dbg:
            kind = "ExternalOutput"
        return nc.dram_tensor(name, list(shape), F32, kind=kind).ap()

    L = DEPTH
    x_in = din("x", (SEQ, D))
    cT_in = din("cT", (128, 8))
    fdg_in = din("fdg", (NH, FDW))
    fdw_in = din("fdw", (NH, FDW))
    router_w = din("router_w", (D, 16))
    router_b = din("router_b", (16,))
    ada_w = din("ada_w", (L, D, 6 * D))
    ada_b = din("ada_b", (L, 6 * D))
    norm1_g = din("norm1_g", (L, D))
    norm2_g = din("norm2_g", (L, D))
    w_in = din("w_in", (L, D, DIN))
    qk_norm_g = din("qk_norm_g", (L, 4, HD))
    cmp_pos = din("cmp_pos", (L, 2, 32, HD))
    cmp_w1 = din("cmp_w1", (L, 2, 2048, 256))
    cmp_w2 = din("cmp_w2", (L, 2, 256, HD))
    dn_conv_w = din("dn_conv_w", (L, 4, 1536))
    dn_a_log = din("dn_a_log", (L, 8))
    dn_dt_bias = din("dn_dt_bias", (L, 8))
    dn_norm_g = din("dn_norm_g", (L, HD))
    w_br_a = din("w_branch_a", (L, 512, D))
    w_br_b = din("w_branch_b", (L, 512, D))
    w_out = din("w_out", (L, D, D))
    moe_wg = din("moe_w_gate", (L, 16, D, 512))
    moe_wu = din("moe_w_up", (L, 16, D, 512))
    moe_wd = din("moe_w_down", (L, 16, 512, D))
    out = nc.dram_tensor("out", [SEQ, D], F32, kind="ExternalOutput").ap()

    qT_d = dscr("qT_d", (4, 128, SEQ))
    kcT_d = dscr("kcT_d", (2, 128, SEQ))
    kslcT_d = dscr("kslcT_d", (128, SEQ))
    kwinT_d = dscr("kwinT_d", (128, SEQ))
    vslc_d = dscr("vslc_d", (SEQ, 128))
    vwin_d = dscr("vwin_d", (SEQ, 128))
    gate_d = dscr("gate_d", (SEQ, 24))
    dnraw_d = dscr("dnraw_d", (12, 128, SEQ))
    dnc_d = dscr("dnc_d", (12, 128, SEQ))
    bg_d = dscr("bg_d", (SEQ, 16))
    zs_d = dscr("zs_d", (SEQ, 512))
    mergeT_d = dscr("mergeT_d", (16, 128, SEQ))
    obr_d = dscr("obr_d", (3, SEQ, 512))
    ybT_d = dscr("ybT_d", (4, 128, SEQ))
    bct_d = dscr("bct_d", (NH, NCT * 128, SEQ))
    bgen_d = dscr("bgen_d", (128, NH, WGEN))
    bwin_d = dscr("bwin_d", (128, NH, WWIN))
    selbT_d = dscr("selbT_d", (128, SEQ))
    MBLK = 256
    NBK = (2 * SEQ + 16 * MBLK) // MBLK
    h2_d = dscr("h2_d", (SEQ, D))
    Xs_d = dscr("Xs_d", (NBK * MBLK, D))
    Ys_d = dscr("Ys_d", (NBK * MBLK, D))
    mod_d = dscr("mod_d", (L, 6 * D))

    es = ExitStack()
    with es:
        S = Sched(nc, es)

        def sb(name, shape):
            return es.enter_context(nc.sbuf_tensor(name, list(shape), F32))

        PS = [es.enter_context(nc.psum_tensor("ps%d" % i, [128, 512], F32)) for i in range(8)]
        psr = Ring(PS, "ps")

        def tt(e, o, a, b, op, r, w):
            S.op(e, lambda g: g.tensor_tensor(out=o, in0=a, in1=b, op=op), r, w)

        def ts(e, o, a, s1, op0, r, w, s2=None, op1=None):
            if op1 is None:
                S.op(e, lambda g: g.tensor_scalar(out=o, in0=a, scalar1=s1, scalar2=None, op0=op0), r, w)
            else:
                S.op(e, lambda g: g.tensor_scalar(out=o, in0=a, scalar1=s1, scalar2=s2, op0=op0, op1=op1), r, w)

        def stt(o, a, sc, b, op0, op1, r, w):
            S.op('dve', lambda g: g.scalar_tensor_tensor(out=o, in0=a, scalar=sc, in1=b, op0=op0, op1=op1), r, w)

        def act(o, a, f, r, w, bias=None, scale=1.0, accum=None):
            kw = {}
            if bias is not None:
                kw['bias'] = bias
            if accum is not None:
                kw['accum_out'] = accum
            S.op('act', lambda g: g.activation(out=o, in_=a, func=f, scale=scale, **kw), r, w)

        def mm(o, lT, rh, st, sp, r, w, inc=True):
            S.op('pe', lambda g: g.matmul(o, lT, rh, start=st, stop=sp), r, w, inc=inc)

        def tr(o, a, idn, r, w, inc=True):
            S.op('pe', lambda g: g.transpose(o, a, idn), r, w, inc=inc)

        def cp(e, o, a, r, w):
            S.op(e, lambda g: g.tensor_copy(o, a), r, w)

        def recip(o, a, r, w):
            S.op('dve', lambda g: g.reciprocal(o, a), r, w)

        def memset(e, o, v, w):
            S.op(e, lambda g: g.memset(o, v), (), w)

        def asel(o, a, pattern, base, cm, fill, r, w, op=ALU.is_ge):
            S.op('pool', lambda g: g.affine_select(out=o, in_=a, pattern=pattern, compare_op=op, fill=fill,
                                                   base=base, channel_multiplier=cm), r, w)

        ident = sb("ident", (128, 128))
        ones = sb("ones", (128, 128))
        bdones = sb("bdones", (128, 128))
        UT = sb("UT", (128, 64))
        maskU = sb("maskU", (64, 64))
        maskL = sb("maskL", (64, 64))
        nsU = sb("nsU", (64, 64))
        nsL = sb("nsL", (64, 64))
        ovl = sb("ovl", (128, NCT, JB))
        memset('pool', ident[:], 0.0, ['ident'])
        asel(ident[:], ident[:], [[-1, 128]], 0, 1, 1.0, ['ident'], ['ident'], op=ALU.not_equal)
        memset('pool', ones[:], 1.0, ['ones'])
        memset('pool', bdones[:], 0.0, ['bdones'])
        memset('pool', bdones[0:64, 0:64], 1.0, ['bdones'])
        memset('pool', bdones[64:128, 64:128], 1.0, ['bdones'])
        for h0 in (0, 64):
            memset('pool', UT[h0:h0 + 64, :], 1.0, ['UT'])
            asel(UT[h0:h0 + 64, :], UT[h0:h0 + 64, :], [[1, 64]], 0, -1, 0.0, ['UT'], ['UT'])
        memset('pool', maskU[:], 0.0, ['maskU'])
        asel(maskU[:], maskU[:], [[1, 64]], 0, -1, NEGM, ['maskU'], ['maskU'])
        memset('pool', maskL[:], 0.0, ['maskL'])
        asel(maskL[:], maskL[:], [[-1, 64]], 0, 1, NEGM, ['maskL'], ['maskL'])
        memset('pool', nsU[:], -1.0, ['nsU'])
        asel(nsU[:], nsU[:], [[1, 64]], -1, -1, 0.0, ['nsU'], ['nsU'])
        memset('pool', nsL[:], -1.0, ['nsL'])
        asel(nsL[:], nsL[:], [[-1, 64]], -1, 1, 0.0, ['nsL'], ['nsL'])
        ovt = sb("ovt", (128, NCT, JB))
        memset('pool', ovl[:], 0.0, ['ovl'])
        for m in (0, 1):
            memset('pool', ovt[:], 1.0, ['ovt'])
            asel(ovt[:], ovt[:], [[128, NCT], [-4, JB]], m, 1, 0.0, ['ovt'], ['ovt'])
            asel(ovt[:], ovt[:], [[-128, NCT], [4, JB]], 3 - m, -1, 0.0, ['ovt'], ['ovt'])
            tt('pool', ovl[:], ovl[:], ovt[:], ALU.add, ['ovl', 'ovt'], ['ovl'])

        with nc.allow_non_contiguous_dma(reason="table build"):
            for p in range(128):
                o0 = NEGPAD - U0 - p
                S.dma(bgen_d[p, :, :], fdg_in[:, o0:o0 + WGEN], (), [('bgen', p)], q='sp')
                S.dma(bwin_d[p, :, :], fdw_in[:, o0:o0 + WWIN], (), [('bwin', p)], q='pool')
            for n in range(NCT * 128):
                o0 = NEGPAD - (16 * n + 31)
                if n >= NCMP:
                    o0 = 0
                q = 'sp' if n % 2 == 0 else 'pool'
                if n >= NCMP:
                    S.dma(bct_d[:, n, 0:NEGPAD], fdg_in[:, 0:NEGPAD], (), [('bct', n)], q=q)
                    for c0 in range(NEGPAD, SEQ, NEGPAD):
                        S.dma(bct_d[:, n, c0:c0 + NEGPAD], fdg_in[:, 0:NEGPAD], (), [('bct', n, c0)], q=q)
                elif o0 >= 0:
                    S.dma(bct_d[:, n, :], fdg_in[:, o0:o0 + SEQ], (), [('bct', n)], q=q)
                else:
                    nn = -o0
                    for c0 in range(0, nn, NEGPAD):
                        cw = min(NEGPAD, nn - c0)
                        S.dma(bct_d[:, n, c0:c0 + cw], fdg_in[:, 0:cw], (), [('bct', n, c0)], q=q)
                    S.dma(bct_d[:, n, nn:SEQ], fdg_in[:, 0:SEQ - nn], (), [('bct', n)], q=q)
        S.barrier()

        cact = sb("cact", (128, 8))
        S.dma(cact[:], cT_in[:, :], (), ['cact'])
        act(cact[:], cact[:], AF.Silu, ['cact'], ['cact'])
        cbc = sb("cbc", (128, 8, 128))
        for kc in range(8):
            ts('dve', cbc[:, kc, :], ones[:], cact[:, kc:kc + 1], ALU.mult, ['ones', 'cact'], ['cbc'])
        rtrb = sb("rtrb", (128, 16))
        S.dma(rtrb[:], router_b.partition_broadcast(128), (), ['rtrb'])
        rtrw = sb("rtrw", (128, 8, 16))
        with nc.allow_non_contiguous_dma(reason="router w"):
            S.dma(rtrw[:], router_w.rearrange("(kc p) e -> p kc e", p=128), (), ['rtrw'])

        def load_w(ring, src2d, c0, ncols, kch=8):
            t, k = ring.next()
            with nc.allow_non_contiguous_dma(reason="weight slab"):
                S.dma(t[:, 0:kch, 0:ncols], src2d.rearrange("(kc p) n -> p kc n", p=128)[:, :, c0:c0 + ncols], (), [k])
            return t, k

        def norm_tile(src, t0, Arow, shrow, xr, hr, sm):
            xt, xk = xr.next()
            S.dma(xt[:], src[t0:t0 + 128, :], [('x', t0 // 128)], [xk])
            ht, hk = hr.next()
            s, sk = sm.next()
            act(ht[:], xt[:], AF.Square, [xk], [hk, sk], accum=s[:, 0:1])
            act(s[:, 1:2], s[:, 0:1], AF.Sqrt, [sk], [sk], bias=EPS, scale=1.0 / D)
            recip(s[:, 2:3], s[:, 1:2], [sk], [sk])
            stt(ht[:], xt[:], s[:, 2:3], Arow, ALU.mult, ALU.mult, [xk, sk, 'modrow'], [hk])
            tt('pool', ht[:], ht[:], shrow, ALU.add, [hk, 'modrow'], [hk])
            return ht, hk, xt, xk

        def transpose_to(ht, hk, hT, hTk, sub, nkc=8):
            for k0 in range(0, nkc, 4):
                ps, pk = psr.next()
                for kc in range(k0, k0 + 4):
                    tr(ps[:, (kc - k0) * 128:(kc - k0 + 1) * 128], ht[:, kc * 128:(kc + 1) * 128], ident[:],
                       [hk, 'ident'], [pk], inc=(kc == k0 + 3))
                e = 'act' if (k0 // 4) % 2 == 0 else 'dve'
                dst = hT[:, k0:k0 + 4, sub * 128:(sub + 1) * 128]
                srcv = ps[:].rearrange("p (a b) -> p a b", a=4)
                if e == 'act':
                    act(dst, srcv, AF.Copy, [pk], [hTk])
                else:
                    cp('dve', dst, srcv, [pk], [hTk])

        def phase_A(l, src_x, A1row, sh1row):
            with ExitStack() as ph:
                def psb(name, shape):
                    return ph.enter_context(nc.sbuf_tensor("%s_A%d" % (name, l), list(shape), F32))
                wr = Ring([psb("wsl%d" % i, (128, 8, 512)) for i in range(3)], "wsl")
                xr = Ring([psb("xa%d" % i, (128, D)) for i in range(2)], "xa")
                hr = Ring([psb("ha%d" % i, (128, D)) for i in range(2)], "ha")
                hTr = Ring([psb("hT%d" % i, (128, 8, 512)) for i in range(2)], "hT")
                st = Ring([psb("st%d" % i, (128, 512)) for i in range(4)], "st")
                sq = Ring([psb("sq%d" % i, (128, 512)) for i in range(2)], "sq")
                sm = Ring([psb("sm%d" % i, (128, 8)) for i in range(4)], "sm")
                gains = psb("gains", (128, 4))
                dtb = psb("dtb", (128, 8))
                nea = psb("nea", (128, 8))
                with nc.allow_non_contiguous_dma(reason="small"):
                    for h0 in (0, 64):
                        S.dma(gains[h0:h0 + 64, :], qk_norm_g[l].rearrange("i d -> d i"), (), ['gains'])
                ts('dve', gains[:, 0:1], gains[:, 0:1], HD ** -0.5, ALU.mult, ['gains'], ['gains'])
                S.dma(dtb[:], dn_dt_bias[l].partition_broadcast(128), (), ['dtb'])
                S.dma(nea[:], dn_a_log[l].partition_broadcast(128), (), ['nea'])
                act(nea[:], nea[:], AF.Exp, ['nea'], ['nea'])
                ts('dve', nea[:], nea[:], -1.0, ALU.mult, ['nea'], ['nea'])

                def rms64_store(ps, pk, gcol, dst, dkey):
                    q1, qk1 = sq.next()
                    act(q1[:], ps[:], AF.Square, [pk], [qk1])
                    p2, pk2 = psr.next()
                    mm(p2[:], bdones[:], q1[:], True, True, ['bdones', qk1], [pk2])
                    act(q1[:], p2[:], AF.Sqrt, [pk2], [qk1], bias=EPS, scale=1.0 / 64)
                    recip(q1[:], q1[:], [qk1], [qk1])
                    o, ok = st.next()
                    stt(o[:], ps[:], gcol, q1[:], ALU.mult, ALU.mult, [pk, qk1, 'gains'], [ok])
                    S.dma(dst, o[:], [ok], [dkey], q='pool')

                def fm_store(ps, pk, dst, dkey, func=AF.Copy):
                    o, ok = st.next()
                    act(o[:], ps[:], func, [pk], [ok])
                    S.dma(dst, o[:], [ok], [dkey], q='pool')

                for j in range(QT):
                    t0 = 512 * j
                    hT, hTk = hTr.next()
                    for sub in range(4):
                        ht, hk, _, _ = norm_tile(src_x, t0 + 128 * sub, A1row, sh1row, xr, hr, sm)
                        transpose_to(ht, hk, hT, hTk, sub)

                    def fm(wt, wk, lsel):
                        ps, pk = psr.next()
                        for kc in range(8):
                            mm(ps[:], lsel(kc), hT[:, kc, :], kc == 0, kc == 7, [wk, hTk], [pk], inc=(kc == 7))
                        return ps, pk

                    def tm(wt, wk, c0, ncols, sub):
                        ps, pk = psr.next()
                        for kc in range(8):
                            mm(ps[:, 0:ncols], hT[:, kc, sub * 128:(sub + 1) * 128], wt[:, kc, c0:c0 + ncols],
                               kc == 0, kc == 7, [wk, hTk], [pk], inc=(kc == 7))
                        return ps, pk
                    cs = slice(t0, t0 + 512)
                    wt, wk = wr.next()
                    with nc.allow_non_contiguous_dma(reason="q slab"):
                        for a in range(2):
                            for c in range(4):
                                S.dma(wt[:, :, c * 128 + a * 64:c * 128 + a * 64 + 64],
                                      w_in[l].rearrange("(kc p) n -> p kc n", p=128)[:, :, a * 256 + c * 64:a * 256 + c * 64 + 64], (), [wk])
                    for c in range(4):
                        ps, pk = fm(wt, wk, lambda kc, c=c: wt[:, kc, c * 128:(c + 1) * 128])
                        rms64_store(ps, pk, gains[:, 0:1], qT_d[c, :, cs], ('qT', c, j))
                    wt, wk = load_w(wr, w_in[l], 512, 512)
                    for c in range(3):
                        ps, pk = fm(wt, wk, lambda kc, c=c: wt[:, kc, c * 128:(c + 1) * 128])
                        if c < 2:
                            fm_store(ps, pk, kcT_d[c, :, cs], ('kcT', c, j))
                        else:
                            rms64_store(ps, pk, gains[:, 2:3], kslcT_d[:, cs], ('kslcT', j))
                    for sub in range(4):
                        ps, pk = tm(wt, wk, 384, 128, sub)
                        o, ok = st.next()
                        act(o[:, 0:128], ps[:, 0:128], AF.Copy, [pk], [ok])
                        S.dma(vslc_d[t0 + 128 * sub:t0 + 128 * sub + 128, :], o[:, 0:128], [ok], [('vslc', j, sub)], q='pool')
                    wt, wk = load_w(wr, w_in[l], 1024, 280)
                    ps, pk = fm(wt, wk, lambda kc: wt[:, kc, 0:128])
                    rms64_store(ps, pk, gains[:, 3:4], kwinT_d[:, cs], ('kwinT', j))
                    for sub in range(4):
                        r0 = t0 + 128 * sub
                        ps, pk = tm(wt, wk, 128, 152, sub)
                        o, ok = st.next()
                        act(o[:, 0:128], ps[:, 0:128], AF.Copy, [pk], [ok])
                        act(o[:, 128:152], ps[:, 128:152], AF.Sigmoid, [pk], [ok])
                        S.dma(vwin_d[r0:r0 + 128, :], o[:, 0:128], [ok], [('vwin', j, sub)], q='pool')
                        S.dma(gate_d[r0:r0 + 128, :], o[:, 128:152], [ok], [('gate', j, sub)], q='pool')
                    for i in range(3):
                        wt, wk = load_w(wr, w_in[l], 1304 + 512 * i, 512)
                        for c in range(4):
                            ps, pk = fm(wt, wk, lambda kc, c=c: wt[:, kc, c * 128:(c + 1) * 128])
                            fm_store(ps, pk, dnraw_d[4 * i + c, :, cs], ('dnraw', 4 * i + c, j))
                    wt, wk = load_w(wr, w_in[l], 2840, 16)
                    for sub in range(4):
                        r0 = t0 + 128 * sub
                        ps, pk = tm(wt, wk, 0, 16, sub)
                        o, ok = st.next()
                        act(o[:, 0:8], ps[:, 0:8], AF.Sigmoid, [pk], [ok])
                        tt('dve', o[:, 8:16], ps[:, 8:16], dtb[:], ALU.add, [pk, 'dtb'], [ok])
                        act(o[:, 8:16], o[:, 8:16], AF.Exp, [ok], [ok])
                        act(o[:, 8:16], o[:, 8:16], AF.Ln, [ok], [ok], bias=1.0)
                        tt('dve', o[:, 8:16], o[:, 8:16], nea[:], ALU.mult, [ok, 'nea'], [ok])
                        S.dma(bg_d[r0:r0 + 128, :], o[:, 0:16], [ok], [('bg', j, sub)], q='pool')
                    wt, wk = load_w(wr, w_in[l], 2856, 512)
                    for sub in range(4):
                        r0 = t0 + 128 * sub
                        ps, pk = tm(wt, wk, 0, 512, sub)
                        fm_store(ps, pk, zs_d[r0:r0 + 128, :], ('zs', j, sub), func=AF.Silu)
                    for i in range(4):
                        wt, wk = load_w(wr, w_in[l], 3368 + 512 * i, 512)
                        for c in range(4):
                            ps, pk = fm(wt, wk, lambda kc, c=c: wt[:, kc, c * 128:(c + 1) * 128])
                            fm_store(ps, pk, mergeT_d[4 * i + c, :, cs], ('mergeT', 4 * i + c, j), func=AF.Sigmoid)
                S.barrier()
        def phase_B(l):
            with ExitStack() as ph:
                def psb(name, shape):
                    return ph.enter_context(nc.sbuf_tensor("%s_B%d" % (name, l), list(shape), F32))
                kst = [psb("kst%d" % g, (128, NCT * 128)) for g in range(2)]
                for g in range(2):
                    memset('pool', kst[g][:], 0.0, ['kcmpT'])
                vcmp = psb("vcmp", (128, NCT, 2, 65 + JB))
                memset('pool', vcmp[:], 0.0, ['vcmp'])
                memset('pool', vcmp[:, :, :, 64:65], 1.0, ['vcmp'])
                for g in range(2):
                    cp('pool', vcmp[:, :, g, 65:65 + JB], ovl[:], ['ovl', 'vcmp'], ['vcmp'])
                gains = psb("gains", (128, 4))
                with nc.allow_non_contiguous_dma(reason="small"):
                    for h0 in (0, 64):
                        S.dma(gains[h0:h0 + 64, :], qk_norm_g[l].rearrange("i d -> d i"), (), ['gains'])
                with ExitStack() as ph2:
                    def psb2(name, shape):
                        return ph2.enter_context(nc.sbuf_tensor("%s_B2%d" % (name, l), list(shape), F32))
                    kcT = psb2("kcT", (128, SEQ))
                    w1 = psb2("w1", (128, 32, 256))
                    posT = psb2("posT", (128, 32))
                    w2 = psb2("w2", (128, 2, 64))
                    pbias = psb2("pbias", (128, 2))
                    gx = psb2("gx", (128, 2, 2, 256))
                    gt = psb2("gt", (128, 256))
                    sqc = psb2("sqc", (128, 256))
                    for kvi in range(2):
                        S.dma(kcT[:], kcT_d[kvi, :, :], [('kcT', kvi, j) for j in range(QT)], ['kcT'])
                        with nc.allow_non_contiguous_dma(reason="cmp weights"):
                            for h0 in (0, 64):
                                S.dma(w1[h0:h0 + 64, :, :], cmp_w1[l, kvi].rearrange("(j d) f -> d j f", d=64), (), ['w1'])
                                S.dma(posT[h0:h0 + 64, :], cmp_pos[l, kvi].rearrange("j d -> d j"), (), ['posT'])
                            S.dma(w2[:], cmp_w2[l, kvi].rearrange("(fc p) d -> p fc d", p=128), (), ['w2'])
                        for fc in range(2):
                            ps, pk = psr.next()
                            for j in range(32):
                                mm(ps[:, 0:1], w1[0:64, j, fc * 128:(fc + 1) * 128], posT[0:64, j:j + 1], j == 0, j == 31,
                                   ['w1', 'posT'], [pk], inc=(j == 31))
                            cp('dve', pbias[:, fc:fc + 1], ps[:, 0:1], [pk], ['pbias'])
                        for g in range(2):
                            hs = slice(64 * g, 64 * g + 64)
                            for fc in range(2):
                                ps, pk = psr.next()
                                for j in range(32):
                                    mm(ps[:, 0:NCMP], w1[hs, j, fc * 128:(fc + 1) * 128], kcT[hs, j:j + 16 * (NCMP - 1) + 1:16],
                                       j == 0, j == 31, ['w1', 'kcT'], [pk], inc=(j == 31))
                                xs = gx[:, g, fc, 0:NCMP]
                                ts('dve', xs, ps[:, 0:NCMP], pbias[:, fc:fc + 1], ALU.add, [pk, 'pbias'], ['gx'])
                                tt('dve', gt[:, 0:NCMP], xs, xs, ALU.mult, ['gx'], ['gt'])
                                ts('dve', gt[:, 0:NCMP], gt[:, 0:NCMP], 0.044715, ALU.mult, ['gt'], ['gt'], s2=1.0, op1=ALU.add)
                                tt('dve', gt[:, 0:NCMP], gt[:, 0:NCMP], xs, ALU.mult, ['gt', 'gx'], ['gt'])
                                act(gt[:, 0:NCMP], gt[:, 0:NCMP], AF.Sigmoid, ['gt'], ['gt'], scale=1.5957691216057308)
                                tt('dve', xs, xs, gt[:, 0:NCMP], ALU.mult, ['gx', 'gt'], ['gx'])
                            if kvi == 0:
                                ps, pk = psr.next()
                                for fc in range(2):
                                    mm(ps[hs, 0:NCMP], w2[:, fc, :], gx[:, g, fc, 0:NCMP], fc == 0, fc == 1, ['w2', 'gx'], [pk], inc=(fc == 1))
                                act(sqc[hs, 0:NCMP], ps[hs, 0:NCMP], AF.Square, [pk], ['sqc'])
                                p2, pk2 = psr.next()
                                mm(p2[hs, 0:NCMP], ones[hs, 0:64], sqc[hs, 0:NCMP], True, True, ['ones', 'sqc'], [pk2])
                                act(sqc[hs, 0:NCMP], p2[hs, 0:NCMP], AF.Sqrt, [pk2], ['sqc'], bias=EPS, scale=1.0 / 64)
                                recip(sqc[hs, 0:NCMP], sqc[hs, 0:NCMP], ['sqc'], ['sqc'])
                                stt(kst[g][hs, 0:NCMP], ps[hs, 0:NCMP], gains[hs, 1:2], sqc[hs, 0:NCMP], ALU.mult, ALU.mult,
                                    [pk, 'sqc', 'gains'], ['kcmpT'])
                            else:
                                for nt in range(NCT):
                                    nn = min(128, NCMP - nt * 128)
                                    ps, pk = psr.next()
                                    for fc in range(2):
                                        mm(ps[0:nn, 0:64], gx[:, g, fc, nt * 128:nt * 128 + nn], w2[:, fc, :], fc == 0, fc == 1,
                                           ['w2', 'gx'], [pk], inc=(fc == 1))
                                    cp('dve', vcmp[0:nn, nt, g, 0:64], ps[0:nn, 0:64], [pk], ['vcmp'])
                    S.barrier()
                qr = Ring([psb("qTb%d" % i, (128, SEQ)) for i in range(2)], "qTb")
                btr = Ring([psb("bt%d" % i, (128, NCT, 512)) for i in range(2)], "bt")
                pcr = Ring([psb("pc%d" % i, (128, NCT, 512)) for i in range(2)], "pc")
                osr = Ring([psb("os%d" % i, (128, 64)) for i in range(4)], "os")
                rdr = Ring([psb("rd%d" % i, (128, 2)) for i in range(4)], "rd")
                gate_sb = psb("gate_sb", (128, NT, 24))
                impacc = psb("impacc", (128, NT, 2, JB))
                with nc.allow_non_contiguous_dma(reason="gate"):
                    S.dma(gate_sb[:], gate_d.rearrange("(t p) c -> p t c", p=128),
                          [('gate', j, s) for j in range(QT) for s in range(4)], ['gate_sb'])
                for c in range(4):
                    qT, qk = qr.next()
                    S.dma(qT[:], qT_d[c, :, :], [('qT', c, j) for j in range(QT)], [qk])
                    for half in range(2):
                        h = c + 4 * half
                        g = half
                        hs = slice(64 * half, 64 * half + 64)
                        for jq in range(QT):
                            tq0 = 512 * jq
                            nts = [nt for nt in range(NCT) if 16 * 128 * nt + 31 <= tq0 + 511]
                            bt, bk = btr.next()
                            pc, pck = pcr.next()
                            for nt in nts:
                                S.dma(bt[:, nt, :], bct_d[h, nt * 128:(nt + 1) * 128, tq0:tq0 + 512], (), [bk])
                            for nt in nts:
                                ps, pk = psr.next()
                                mm(ps[:], kst[g][:, nt * 128:(nt + 1) * 128], qT[:, tq0:tq0 + 512], True, True, ['kcmpT', qk], [pk])
                                tt('dve', pc[:, nt, :], ps[:], bt[:, nt, :], ALU.add, [pk, bk], [pck])
                                act(pc[:, nt, :], pc[:, nt, :], AF.Exp, [pck], [pck])
                            for sub in range(4):
                                tsi = 4 * jq + sub
                                po, pok = psr.next()
                                W = 65 + JB
                                for nt in nts:
                                    mm(po[:, 0:W], pc[:, nt, sub * 128:(sub + 1) * 128], vcmp[:, nt, g, :], nt == nts[0], nt == nts[-1],
                                       [pck, 'vcmp'], [pok], inc=(nt == nts[-1]))
                                rd, rk = rdr.next()
                                ts('dve', rd[:, 0:1], po[:, 64:65], 1e-30, ALU.add, [pok], [rk])
                                recip(rd[:, 1:2], rd[:, 0:1], [rk], [rk])
                                o, ok = osr.next()
                                ts('dve', o[:], po[:, 0:64], rd[:, 1:2], ALU.mult, [pok, rk, 'gate_sb'], [ok],
                                   s2=gate_sb[:, tsi, 3 * h:3 * h + 1], op1=ALU.mult)
                                S.dma(obr_d[0, tsi * 128:(tsi + 1) * 128, 64 * h:64 * h + 64], o[:], [ok], [('obr', 0, h, tsi)], q='pool')
                                if c == 0:
                                    ts('dve', impacc[:, tsi, g, :], po[:, 65:W], rd[:, 1:2], ALU.mult, [pok, rk], [('imp', tsi, g)])
                                else:
                                    stt(impacc[:, tsi, g, :], po[:, 65:W], rd[:, 1:2], impacc[:, tsi, g, :], ALU.mult, ALU.add,
                                        [pok, rk, ('imp', tsi, g)], [('imp', tsi, g)])
                selM = psb("selM", (128, NT, JB))
                selA = psb("selA", (128, NT, JB))
                memset('pool', selM[:], 1.0, ['selM'])
                asel(selM[:], selM[:], [[128, NT], [-64, JB]], -128, 1, 0.0, ['selM'], ['selM'])
                memset('pool', selM[:, :, 0:1], 0.0, ['selM'])
                memset('pool', selA[:], 0.0, ['selA'])
                asel(selA[:], selA[:], [[128, NT], [-64, JB]], -128, 1, 1e9, ['selA'], ['selA'])
                asel(selA[:], selA[:], [[128, NT], [-64, JB]], 0, 1, -1.0, ['selA'], ['selA'])
                memset('pool', selA[:, :, 0:1], 1e9, ['selA'])
                scr = Ring([psb("sc%d" % i, (128, 2, JB)) for i in range(2)], "sc")
                sc2r = Ring([psb("sd%d" % i, (128, JB)) for i in range(2)], "sd")
                m8r = Ring([psb("m8%d" % i, (128, 16)) for i in range(4)], "m8")
                sbr = Ring([psb("sbi%d" % i, (128, 128)) for i in range(2)], "sbi")
                sto = Ring([psb("sto%d" % i, (128, 128)) for i in range(2)], "sto")
                for tsi in range(NT):
                    sc, sck = scr.next()
                    sbi, sbk = sbr.next()
                    if JB < 64:
                        memset('pool', sbi[:], 0.0, [sbk])
                    for g in range(2):
                        tt('dve', sc[:, g, :], impacc[:, tsi, g, :], selM[:, tsi, :], ALU.mult, [('imp', tsi, g), 'selM'], [sck])
                        tt('dve', sc[:, g, :], sc[:, g, :], selA[:, tsi, :], ALU.add, [sck, 'selA'], [sck])
                        m8, mk = m8r.next()
                        sd, sdk = sc2r.next()
                        S.op('dve', lambda e, a=m8, b=sc, g=g: e.max(out=a[:, 0:8], in_=b[:, g, :]), [sck], [mk])
                        S.op('dve', lambda e, a=m8, b=sc, d=sd, g=g: e.match_replace(out=d[:], in_to_replace=a[:, 0:8], in_values=b[:, g, :],
                                                                                   imm_value=-3e38), [sck, mk], [sdk])
                        S.op('dve', lambda e, a=m8, d=sd: e.max(out=a[:, 8:16], in_=d[:]), [sdk], [mk])
                        ts('dve', sbi[:, 64 * g:64 * g + JB], sc[:, g, :], m8[:, 15:16], ALU.is_ge, [sck, mk], [sbk], s2=-NEGM, op1=ALU.mult)
                        ts('dve', sbi[:, 64 * g:64 * g + JB], sbi[:, 64 * g:64 * g + JB], NEGM, ALU.add, [sbk], [sbk])
                    ps, pk = psr.next()
                    tr(ps[:, 0:128], sbi[:], ident[:], [sbk, 'ident'], [pk])
                    so, sok = sto.next()
                    act(so[:], ps[:, 0:128], AF.Copy, [pk], [sok])
                    S.dma(selbT_d[:, tsi * 128:(tsi + 1) * 128], so[:], [sok], [('selbT', tsi)], q='pool')
                S.barrier()

        def phase_C(l):
            for br in (1, 2):
                with ExitStack() as ph:
                    def psb(name, shape):
                        return ph.enter_context(nc.sbuf_tensor("%s_C%d_%d" % (name, l, br), list(shape), F32))
                    Wt = WGEN if br == 1 else WWIN
                    tab_d = bgen_d if br == 1 else bwin_d
                    KT_d = kslcT_d if br == 1 else kwinT_d
                    V_d = vslc_d if br == 1 else vwin_d
                    tab = psb("tab", (128, NH, Wt))
                    for h in range(NH):
                        S.dma(tab[:, h, :], tab_d[:, h, :], (), ['tab'])
                    LS = [psb("LS%d" % g, (128, SEQ)) for g in range(2)]
                    for g in range(2):
                        if br == 1:
                            S.dma(LS[g][0:64, :], KT_d[64 * g:64 * g + 64, :], (), [('LS', g)])
                            v = LS[g][64:128, :]
                            memset('pool', v, 1.0, [('LS', g)])
                            asel(v, v, [[1, SEQ]], 0, -64, 0.0, [('LS', g)], [('LS', g)])
                            asel(v, v, [[-1, SEQ]], 63, 64, 0.0, [('LS', g)], [('LS', g)])
                        else:
                            memset('pool', LS[g][64 * (1 - g):64 * (1 - g) + 64, :], 0.0, [('LS', g)])
                            S.dma(LS[g][64 * g:64 * g + 64, :], KT_d[64 * g:64 * g + 64, :], (), [('LS', g)])
                    V = psb("V", (128, NT, 2, 65))
                    memset('pool', V[:, :, :, 64:65], 1.0, ['V'])
                    with nc.allow_non_contiguous_dma(reason="V"):
                        for g in range(2):
                            S.dma(V[:, :, g, 0:64], V_d.rearrange("(t p) c -> p t c", p=128)[:, :, 64 * g:64 * g + 64], (), ['V'])
                    gate_sb = psb("gate_sb", (128, NT, 24))
                    with nc.allow_non_contiguous_dma(reason="gate"):
                        S.dma(gate_sb[:], gate_d.rearrange("(t p) c -> p t c", p=128), (), ['gate_sb'])
                    qr = Ring([psb("qTc%d" % i, (128, SEQ)) for i in range(2)], "qTc")
                    ptr = Ring([psb("pt%d" % i, (128, 512)) for i in range(4)], "pt")
                    osr = Ring([psb("os%d" % i, (128, 64)) for i in range(4)], "os")
                    rdr = Ring([psb("rd%d" % i, (128, 2)) for i in range(4)], "rd")
                    b31 = psb("b31", (128, NH))
                    with nc.allow_non_contiguous_dma(reason="b31"):
                        S.dma(b31[:], fdg_in[:, NEGPAD + OFFMAX - 1].partition_broadcast(128), (), ['b31'])
                    psr4 = Ring(PS[0:4], "ps")
                    LAG = 2
                    for c in range(4):
                        if br == 2:
                            qT, qk = qr.next()
                            S.dma(qT[:], qT_d[c, :, :], (), [qk])
                        for half in range(2):
                            h = c + 4 * half
                            g = half
                            if br == 1:
                                qT, qk = qr.next()
                                S.dma(qT[0:64, :], qT_d[c, 64 * half:64 * half + 64, :], (), [qk])
                                S.dma(qT[64:128, :], selbT_d[64 * g:64 * g + 64, :], (), [qk])
                            pend = []

                            def stage3(item):
                                jq_, tk0_, pt_, ptk_, tks_, last_ = item
                                tq0_ = 512 * jq_
                                for sub in range(4):
                                    if tk0_ > tq0_ + 128 * sub + 127:
                                        continue
                                    mm(PS[4 + sub][:, 0:65], pt_[:, sub * 128:(sub + 1) * 128], V[:, tk0_ // 128, g, :],
                                       tk0_ == tks_[0], tk0_ == last_[sub], [ptk_, 'V'], [('ps', 4 + sub)], inc=(tk0_ == last_[sub]))
                                if tk0_ == tks_[-1]:
                                    for sub in range(4):
                                        tsi = 4 * jq_ + sub
                                        po = PS[4 + sub]
                                        pok = ('ps', 4 + sub)
                                        rd, rk = rdr.next()
                                        ts('dve', rd[:, 0:1], po[:, 64:65], 1e-30, ALU.add, [pok], [rk])
                                        recip(rd[:, 1:2], rd[:, 0:1], [rk], [rk])
                                        o, ok = osr.next()
                                        ts('dve', o[:], po[:, 0:64], rd[:, 1:2], ALU.mult, [pok, rk, 'gate_sb'], [ok],
                                           s2=gate_sb[:, tsi, 3 * h + br:3 * h + br + 1], op1=ALU.mult)
                                        S.dma(obr_d[br, tsi * 128:(tsi + 1) * 128, 64 * h:64 * h + 64], o[:], [ok], [('obr', br, h, tsi)], q='pool')

                            for jq in range(QT):
                                tq0 = 512 * jq
                                lo = 0 if br == 1 else max(0, tq0 - 512)
                                tks = list(range(lo, tq0 + 512, 128))
                                last = {sub: max(tk for tk in tks if tk <= tq0 + 128 * sub + 127) for sub in range(4)}
                                for tk0 in tks:
                                    ps, pk = psr4.next()
                                    mm(ps[:], LS[g][:, tk0:tk0 + 128], qT[:, tq0:tq0 + 512], True, True, [('LS', g), qk], [pk])
                                    pt, ptk = ptr.next()
                                    if tq0 - tk0 >= OFFMAX + 128:
                                        act(pt[:], ps[:], AF.Exp, [pk, 'b31'], [ptk], bias=b31[:, h:h + 1])
                                    else:
                                        off = min(tq0 - tk0, OFFMAX) + U0
                                        tt('dve', pt[:], ps[:], tab[:, h, off:off + 512], ALU.add, [pk, 'tab'], [ptk])
                                        act(pt[:], pt[:], AF.Exp, [ptk], [ptk])
                                    pend.append((jq, tk0, pt, ptk, tks, last))
                                    if len(pend) > LAG:
                                        stage3(pend.pop(0))
                            while pend:
                                stage3(pend.pop(0))
                    S.barrier()
        def bc(ap2, n, axis):
            P, A = ap2.shape
            if axis == 2:
                return ap2.unsqueeze(2).to_broadcast([P, A, n])
            return ap2.unsqueeze(1).to_broadcast([P, n, A])

        def phase_D(l):
            with ExitStack() as ph:
                def psb(name, shape):
                    return ph.enter_context(nc.sbuf_tensor("%s_D1%d" % (name, l), list(shape), F32))
                xr = Ring([psb("xin%d" % i, (128, SEQ + 3)) for i in range(2)], "xin")
                ar = Ring([psb("acc%d" % i, (128, SEQ)) for i in range(2)], "acc")
                sq = Ring([psb("sq%d" % i, (128, 512)) for i in range(2)], "sq")
                cw = psb("cw", (128, 4, 12))
                with nc.allow_non_contiguous_dma(reason="conv w"):
                    for i in range(4):
                        S.dma(cw[:, i, :], dn_conv_w[l, i].rearrange("(c p) -> p c", p=128), (), ['cw'])
                for ch in range(12):
                    xin, xk = xr.next()
                    acc, ak = ar.next()
                    memset('pool', xin[:, 0:3], 0.0, [xk])
                    S.dma(xin[:, 3:SEQ + 3], dnraw_d[ch, :, :], (), [xk])
                    ts('dve', acc[:], xin[:, 0:SEQ], cw[:, 0, ch:ch + 1], ALU.mult, [xk, 'cw'], [ak])
                    for i in range(1, 4):
                        stt(acc[:], xin[:, i:SEQ + i], cw[:, i, ch:ch + 1], acc[:], ALU.mult, ALU.add, [xk, 'cw', ak], [ak])
                    act(acc[:], acc[:], AF.Silu, [ak], [ak])
                    if ch < 8:
                        for j in range(QT):
                            cs = slice(512 * j, 512 * j + 512)
                            q1, qk1 = sq.next()
                            act(q1[:], acc[:, cs], AF.Square, [ak], [qk1])
                            p2, pk2 = psr.next()
                            mm(p2[:], bdones[:], q1[:], True, True, ['bdones', qk1], [pk2])
                            act(q1[:], p2[:], AF.Sqrt, [pk2], [qk1], bias=EPS, scale=1.0)
                            recip(q1[:], q1[:], [qk1], [qk1])
                            if ch < 4:
                                stt(acc[:, cs], acc[:, cs], HD ** -0.5, q1[:], ALU.mult, ALU.mult, [ak, qk1], [ak])
                            else:
                                tt('dve', acc[:, cs], acc[:, cs], q1[:], ALU.mult, [ak, qk1], [ak])
                    S.dma(dnc_d[ch, :, :], acc[:], [ak], [('dnc', ch)], q='pool')
                S.barrier()
            if 'stopD1' in dbg:
                return
            with ExitStack() as ph:
                def psb(name, shape):
                    return ph.enter_context(nc.sbuf_tensor("%s_D2%d" % (name, l), list(shape), F32))
                NG = NCH * 8
                g_tm = psb("g_tm", (64, NCH, 8))
                b_tm = psb("b_tm", (64, NCH, 8))
                gc_tm = psb("gc_tm", (64, NCH, 8))
                eg_tm = psb("eg_tm", (64, NCH, 8))
                bg_tm = psb("bg_tm", (64, NCH, 8))
                kds_tm = psb("kds_tm", (64, NCH, 8))
                egl = psb("egl", (64, NCH, 8))
                sel63 = psb("sel63", (64, 64))
                ngrow = psb("ngrow", (64, 64))
                with nc.allow_non_contiguous_dma(reason="bg"):
                    S.dma(b_tm[:], bg_d.rearrange("(n i) c -> i n c", i=64)[:, :, 0:8], (), ['b_tm'])
                    S.dma(g_tm[:], bg_d.rearrange("(n i) c -> i n c", i=64)[:, :, 8:16], (), ['g_tm'])
                S.dma(ngrow[:], dn_norm_g[l].partition_broadcast(64), (), ['ngrow'])
                ts('dve', sel63[:], ones[0:64, 0:64], ident[0:64, 63:64], ALU.mult, ['ones', 'ident'], ['sel63'])
                gflat = g_tm[:].rearrange("p n h -> p (n h)")
                gcflat = gc_tm[:].rearrange("p n h -> p (n h)")
                for c0 in range(0, NG, 512):
                    cw_ = min(512, NG - c0)
                    ps, pk = psr.next()
                    mm(ps[0:64, 0:cw_], UT[0:64, :], gflat[:, c0:c0 + cw_], True, True, ['UT', 'g_tm'], [pk])
                    cp('dve', gcflat[:, c0:c0 + cw_], ps[0:64, 0:cw_], [pk], ['gc_tm'])
                    ps2, pk2 = psr.next()
                    mm(ps2[0:64, 0:cw_], sel63[:], gcflat[:, c0:c0 + cw_], True, True, ['sel63', 'gc_tm'], [pk2])
                    act(egl[:].rearrange("p n h -> p (n h)")[:, c0:c0 + cw_], ps2[0:64, 0:cw_], AF.Exp, [pk2], ['egl'])
                    tt('dve', kds_tm[:].rearrange("p n h -> p (n h)")[:, c0:c0 + cw_], ps2[0:64, 0:cw_], gcflat[:, c0:c0 + cw_],
                       ALU.subtract, [pk2, 'gc_tm'], ['kds_tm'])
                act(kds_tm[:], kds_tm[:], AF.Exp, ['kds_tm'], ['kds_tm'])
                act(eg_tm[:], gc_tm[:], AF.Exp, ['gc_tm'], ['eg_tm'])
                tt('dve', bg_tm[:], b_tm[:], eg_tm[:], ALU.mult, ['b_tm', 'eg_tm'], ['bg_tm'])

                Xr = Ring([psb("X%d" % i, (64, 24, 64)) for i in range(2)], "X")
                zr = Ring([psb("z%d" % i, (64, 512)) for i in range(2)], "z")

                def t3(name):
                    return psb(name, (64, 8, 64))
                ktm, vtm, qtm = t3("ktm"), t3("vtm"), t3("qtm")
                rhsg, rhsb = t3("rhsg"), t3("rhsb")
                DTr, decT, dec = t3("DTr"), t3("decT"), t3("dec")
                AT, t1 = t3("AT"), t3("t1")
                Xa, Xb, Ya, Yb, TT = t3("Xa"), t3("Xb"), t3("Ya"), t3("Yb"), t3("TT")
                vb, kbg, kd, qdT, wT, u_sb, vn = t3("vb"), t3("kbg"), t3("kd"), t3("qdT"), t3("wT"), t3("u_sb"), t3("vn")
                Sst, o_sb, osq = t3("Sst"), t3("o_sb"), t3("osq")
                ssum = psb("ssum", (64, 16))
                ytm = psb("ytm", (64, 512))
                ybo = Ring([psb("ybo%d" % i, (128, 4, 64)) for i in range(2)], "ybo")
                memset('pool', Sst[:], 0.0, ['Sst'])
                I64 = ident[0:64, 0:64]

                def hmm(pst, pk, lhs, rhs, r, first=True, last=True):
                    for h in range(8):
                        mm(pst[0:64, 64 * h:64 * h + 64], lhs(h), rhs(h), first, last, r, [pk], inc=(h == 7))

                def v3(ps):
                    return ps[0:64, :].rearrange("p (h c) -> p h c", h=8)

                if 'd2s0' in dbg:
                    S.barrier(); return
                for n in range(NCH):
                    X, Xk = Xr.next()
                    with nc.allow_non_contiguous_dma(reason="chunk load"):
                        S.dma(X[:], dnc_d.rearrange("c (a p) t -> p (c a) t", a=2)[:, :, 64 * n:64 * n + 64], (), [Xk])
                    z, zk = zr.next()
                    S.dma(z[:], zs_d[64 * n:64 * n + 64, :], (), [zk])
                    for (dst, dk_, c0) in ((qtm, 'qtm', 0), (ktm, 'ktm', 8), (vtm, 'vtm', 16)):
                        ps, pk = psr.next()
                        for kc in range(8):
                            tr(ps[0:64, 64 * kc:64 * kc + 64], X[:, c0 + kc, :], I64, [Xk, 'ident'], [pk], inc=(kc == 7))
                        if dk_ == 'ktm':
                            cp('dve', dst[:], v3(ps), [pk], [dk_])
                        else:
                            act(dst[:], v3(ps), AF.Copy, [pk], [dk_])
                    if 'd2s1' in dbg:
                        continue
                    tt('dve', rhsg[:], bc(g_tm[:, n, :], 64, 2), bc(UT[0:64, :], 8, 1), ALU.mult, ['g_tm', 'UT'], ['rhsg'])
                    tt('dve', rhsb[:], bc(b_tm[:, n, :], 64, 2), bc(I64, 8, 1), ALU.mult, ['b_tm', 'ident'], ['rhsb'])
                    pg, pgk = psr.next()
                    mm(pg[0:64, :], ones[0:64, 0:64], rhsg[:].rearrange("p h c -> p (h c)"), True, True, ['ones', 'rhsg'], [pgk])
                    pb, pbk = psr.next()
                    mm(pb[0:64, :], ones[0:64, 0:64], rhsb[:].rearrange("p h c -> p (h c)"), True, True, ['ones', 'rhsb'], [pbk])
                    tt('dve', DTr[:], v3(pg), bc(gc_tm[:, n, :], 64, 2), ALU.subtract, [pgk, 'gc_tm'], ['DTr'])
                    tt('dve', decT[:], DTr[:], bc(maskU[:], 8, 1), ALU.add, ['DTr', 'maskU'], ['decT'])
                    act(decT[:], decT[:], AF.Exp, ['decT'], ['decT'])
                    ts('dve', dec[:], DTr[:], -1.0, ALU.mult, ['DTr'], ['dec'])
                    tt('dve', dec[:], dec[:], bc(maskL[:], 8, 1), ALU.add, ['dec', 'maskL'], ['dec'])
                    act(dec[:], dec[:], AF.Exp, ['dec'], ['dec'])
                    if 'd2s2' in dbg:
                        continue
                    pkk, pkkk = psr.next()
                    hmm(pkk, pkkk, lambda h: X[:, 8 + h, :], lambda h: X[:, 8 + h, :], [Xk])
                    pqk, pqkk = psr.next()
                    hmm(pqk, pqkk, lambda h: X[:, 8 + h, :], lambda h: X[:, h, :], [Xk])
                    tt('dve', AT[:], v3(pqk), decT[:], ALU.mult, [pqkk, 'decT'], ['AT'])
                    tt('dve', t1[:], v3(pkk), decT[:], ALU.mult, [pkkk, 'decT'], ['t1'])
                    tt('dve', t1[:], v3(pb), t1[:], ALU.mult, [pbk, 't1'], ['t1'])
                    tt('dve', Ya[:], t1[:], bc(nsU[:], 8, 1), ALU.mult, ['t1', 'nsU'], ['Ya'])
                    tt('dve', t1[:], v3(pkk), dec[:], ALU.mult, [pkkk, 'dec'], ['t1'])
                    tt('dve', t1[:], t1[:], bc(b_tm[:, n, :], 64, 2), ALU.mult, ['t1', 'b_tm'], ['t1'])
                    tt('dve', Xa[:], t1[:], bc(nsL[:], 8, 1), ALU.mult, ['t1', 'nsL'], ['Xa'])
                    tt('dve', TT[:], Ya[:], bc(I64, 8, 1), ALU.add, ['Ya', 'ident'], ['TT'])
                    if 'd2s3' in dbg:
                        continue
                    Xc, Xn_, Yc, Yn_ = (Xa, 'Xa'), (Xb, 'Xb'), (Ya, 'Ya'), (Yb, 'Yb')
                    for lvl in range(1, 6):
                        p1, p1k = psr.next()
                        hmm(p1, p1k, lambda h: Yc[0][:, h, :], lambda h: Xc[0][:, h, :], [Yc[1], Xc[1]])
                        if lvl < 5:
                            p2, p2k = psr.next()
                            hmm(p2, p2k, lambda h: Xc[0][:, h, :], lambda h: Yc[0][:, h, :], [Yc[1], Xc[1]])
                        act(Xn_[0][:], v3(p1), AF.Copy, [p1k], [Xn_[1]])
                        if lvl < 5:
                            cp('dve', Yn_[0][:], v3(p2), [p2k], [Yn_[1]])
                        Xc, Xn_ = Xn_, Xc
                        if lvl < 5:
                            Yc, Yn_ = Yn_, Yc
                        p3, p3k = psr.next()
                        hmm(p3, p3k, lambda h: Xc[0][:, h, :], lambda h: TT[:, h, :], [Xc[1], 'TT'])
                        tt('dve', TT[:], TT[:], v3(p3), ALU.add, ['TT', p3k], ['TT'])
                    if 'd2s4' in dbg:
                        continue
                    tt('dve', vb[:], vtm[:], bc(b_tm[:, n, :], 64, 2), ALU.mult, ['vtm', 'b_tm'], ['vb'])
                    tt('dve', kbg[:], ktm[:], bc(bg_tm[:, n, :], 64, 2), ALU.mult, ['ktm', 'bg_tm'], ['kbg'])
                    tt('dve', kd[:], ktm[:], bc(kds_tm[:, n, :], 64, 2), ALU.mult, ['ktm', 'kds_tm'], ['kd'])
                    tt('dve', qtm[:], qtm[:], bc(eg_tm[:, n, :], 64, 2), ALU.mult, ['qtm', 'eg_tm'], ['qtm'])
                    pu, puk = psr.next()
                    hmm(pu, puk, lambda h: TT[:, h, :], lambda h: vb[:, h, :], ['TT', 'vb'])
                    act(u_sb[:], v3(pu), AF.Copy, [puk], ['u_sb'])
                    pw, pwk = psr.next()
                    hmm(pw, pwk, lambda h: kbg[:, h, :], lambda h: TT[:, h, :], ['TT', 'kbg'])
                    act(wT[:], v3(pw), AF.Copy, [pwk], ['wT'])
                    pq, pqk2 = psr.next()
                    for h in range(8):
                        tr(pq[0:64, 64 * h:64 * h + 64], qtm[:, h, :], I64, ['qtm', 'ident'], [pqk2], inc=(h == 7))
                    cp('dve', qdT[:], v3(pq), [pqk2], ['qdT'])
                    if 'd2s5' in dbg:
                        continue
                    pv, pvk = psr.next()
                    hmm(pv, pvk, lambda h: wT[:, h, :], lambda h: Sst[:, h, :], ['wT', 'Sst'])
                    tt('dve', vn[:], u_sb[:], v3(pv), ALU.subtract, ['u_sb', pvk], ['vn'])
                    po, pok = psr.next()
                    for h in range(8):
                        mm(po[0:64, 64 * h:64 * h + 64], qdT[:, h, :], Sst[:, h, :], True, False, ['qdT', 'Sst'], [pok], inc=False)
                        mm(po[0:64, 64 * h:64 * h + 64], AT[:, h, :], vn[:, h, :], False, True, ['AT', 'vn'], [pok], inc=(h == 7))
                    pS, pSk = psr.next()
                    hmm(pS, pSk, lambda h: kd[:, h, :], lambda h: vn[:, h, :], ['kd', 'vn'])
                    tt('dve', Sst[:], Sst[:], bc(egl[:, n, :], 64, 2), ALU.mult, ['Sst', 'egl'], ['Sst'])
                    tt('dve', Sst[:], Sst[:], v3(pS), ALU.add, ['Sst', pSk], ['Sst'])
                    if 'd2s6' in dbg:
                        continue
                    act(o_sb[:], v3(po), AF.Copy, [pok], ['o_sb'])
                    tt('dve', osq[:], o_sb[:], o_sb[:], ALU.mult, ['o_sb'], ['osq'])
                    S.op('dve', lambda e: e.tensor_reduce(out=ssum[:, 0:8], in_=osq[:], op=ALU.add, axis=AX.X), ['osq'], ['ssum'])
                    act(ssum[:, 8:16], ssum[:, 0:8], AF.Sqrt, ['ssum'], ['ssum'], bias=EPS, scale=1.0 / 64)
                    recip(ssum[:, 8:16], ssum[:, 8:16], ['ssum'], ['ssum'])
                    tt('dve', o_sb[:], o_sb[:], bc(ssum[:, 8:16], 64, 2), ALU.mult, ['o_sb', 'ssum'], ['o_sb'])
                    tt('dve', o_sb[:], o_sb[:], bc(ngrow[:], 8, 1), ALU.mult, ['o_sb', 'ngrow'], ['o_sb'])
                    tt('dve', ytm[:], o_sb[:].rearrange("p h c -> p (h c)"), z[:], ALU.mult, ['o_sb', zk], ['ytm'])
                    py, pyk = psr.next()
                    for kc in range(4):
                        tr(py[:, 64 * kc:64 * kc + 64], ytm[:, 128 * kc:128 * kc + 128], I64, ['ytm', 'ident'], [pyk], inc=(kc == 3))
                    yo, yok = ybo.next()
                    act(yo[:], py[:, 0:256].rearrange("p (a b) -> p a b", a=4), AF.Copy, [pyk], [yok])
                    with nc.allow_non_contiguous_dma(reason="yb store"):
                        S.dma(ybT_d.rearrange("c p t -> p c t")[:, :, 64 * n:64 * n + 64], yo[:], [yok], [('ybT', n)], q='pool')
                S.barrier()

        def phase_E(l, src_x):
            with ExitStack() as ph:
                def psb(name, shape):
                    return ph.enter_context(nc.sbuf_tensor("%s_E%d" % (name, l), list(shape), F32))
                wa = psb("wa", (128, 4, D))
                wb = psb("wb", (128, 4, D))
                wo = psb("wo", (128, 8, D))
                g1r = psb("g1r", (128, D))
                S.dma(g1r[:], mod_d[l, 2 * D:3 * D].partition_broadcast(128), (), ['g1r'])
                S.dma(wa[:], w_br_a[l].rearrange("(kc p) n -> p kc n", p=128), (), ['wa'])
                S.dma(wb[:], w_br_b[l].rearrange("(kc p) n -> p kc n", p=128), (), ['wb'])
                S.dma(wo[:], w_out[l].rearrange("(kc p) n -> p kc n", p=128), (), ['wo'])
                obr = Ring([psb("ob%d" % i, (128, 3, 512)) for i in range(2)], "ob")
                yaT = psb("yaT", (128, 4, 512))
                ybT = psb("ybT", (128, 4, 512))
                mg = Ring([psb("mg%d" % i, (128, 2, 512)) for i in range(2)], "mg")
                yT = psb("yT", (128, 8, 512))
                tmp = Ring([psb("tmp%d" % i, (128, 512)) for i in range(2)], "tmp")
                xr = Ring([psb("xe%d" % i, (128, D)) for i in range(2)], "xe")
                for j in range(QT):
                    t0 = 512 * j
                    cs = slice(t0, t0 + 512)
                    for sub in range(4):
                        r0 = t0 + 128 * sub
                        ob, obk = obr.next()
                        S.dma(ob[:], obr_d[:, r0:r0 + 128, :].rearrange("b p c -> p b c"), (), [obk])
                        tt('dve', ob[:, 0, :], ob[:, 0, :], ob[:, 1, :], ALU.add, [obk], [obk])
                        tt('pool', ob[:, 0, :], ob[:, 0, :], ob[:, 2, :], ALU.add, [obk], [obk])
                        transpose_to(ob[:, 0, :], obk, yaT, 'yaT', sub, nkc=4)
                    S.dma(ybT[:], ybT_d.rearrange("c p t -> p c t")[:, :, cs], (), ['ybT'])
                    for dc in range(8):
                        m, mk = mg.next()
                        S.dma(m[:, 0, :], mergeT_d[dc, :, cs], (), [mk])
                        S.dma(m[:, 1, :], mergeT_d[8 + dc, :, cs], (), [mk])
                        pa, pak = psr.next()
                        for kc in range(4):
                            mm(pa[:], wa[:, kc, 128 * dc:128 * dc + 128], yaT[:, kc, :], kc == 0, kc == 3, ['wa', 'yaT'], [pak], inc=(kc == 3))
                        pb, pbk = psr.next()
                        for kc in range(4):
                            mm(pb[:], wb[:, kc, 128 * dc:128 * dc + 128], ybT[:, kc, :], kc == 0, kc == 3, ['wb', 'ybT'], [pbk], inc=(kc == 3))
                        t, tk = tmp.next()
                        tt('dve', t[:], pa[:], m[:, 0, :], ALU.mult, [pak, mk], [tk])
                        tt('dve', m[:, 1, :], pb[:], m[:, 1, :], ALU.mult, [pbk, mk], [mk])
                        tt('pool', yT[:, dc, :], t[:], m[:, 1, :], ALU.add, [tk, mk], ['yT'])
                    for sub in range(4):
                        r0 = t0 + 128 * sub
                        xt, xk = xr.next()
                        S.dma(xt[:], src_x[r0:r0 + 128, :], [('x', r0 // 128)], [xk])
                        for nh in range(2):
                            po, pok = psr.next()
                            for kc in range(8):
                                mm(po[:], yT[:, kc, 128 * sub:128 * sub + 128], wo[:, kc, 512 * nh:512 * nh + 512], kc == 0, kc == 7,
                                   ['yT', 'wo'], [pok], inc=(kc == 7))
                            t, tk = tmp.next()
                            tt('dve', t[:], po[:], g1r[:, 512 * nh:512 * nh + 512], ALU.mult, [pok, 'g1r'], [tk])
                            tt('pool', xt[:, 512 * nh:512 * nh + 512], xt[:, 512 * nh:512 * nh + 512], t[:], ALU.add, [xk, tk], [xk])
                        S.dma(out[r0:r0 + 128, :], xt[:], [xk], [('x', r0 // 128)], q='pool')
                S.barrier()

        def phase_F(l):
            NE = 16
            with ExitStack() as ph:
                def psb(name, shape, dt=F32):
                    return ph.enter_context(nc.sbuf_tensor("%s_F%d" % (name, l), list(shape), dt))
                A2r = psb("A2r", (128, D))
                sh2r = psb("sh2r", (128, D))
                g2r = psb("g2r", (128, D))
                S.dma(sh2r[:], mod_d[l, 3 * D:4 * D].partition_broadcast(128), (), ['modrow'])
                S.dma(A2r[:], mod_d[l, 4 * D:5 * D].partition_broadcast(128), (), ['modrow'])
                S.dma(g2r[:], mod_d[l, 5 * D:6 * D].partition_broadcast(128), (), ['g2r'])
                xr = Ring([psb("xf%d" % i, (128, D)) for i in range(2)], "xf")
                hr = Ring([psb("hf%d" % i, (128, D)) for i in range(2)], "hf")
                sm = Ring([psb("smf%d" % i, (128, 8)) for i in range(4)], "smf")
                hTr = Ring([psb("hTf%d" % i, (128, 8, 256)) for i in range(2)], "hTf")
                rt = psb("rt", (128, 8, 16))
                rs = psb("rs", (128, 16))
                M1a = psb("M1a", (128, NT, NE))
                M2a = psb("M2a", (128, NT, NE))
                wn = psb("wn", (128, NT, 2))
                SU = psb("SU", (128, 128))
                memset('pool', SU[:], 1.0, ['SU'])
                asel(SU[:], SU[:], [[1, 128]], -1, -1, 0.0, ['SU'], ['SU'])
                for ti in range(NT):
                    hT, hTk = hTr.next()
                    ht, hk, xt, xk = norm_tile(out, 128 * ti, A2r[:], sh2r[:], xr, hr, sm)
                    S.dma(h2_d[128 * ti:128 * ti + 128, :], ht[:], [hk], [('h2', ti)], q='pool')
                    transpose_to(ht, hk, hT, hTk, 0)
                    pr, prk = psr.next()
                    for kc in range(8):
                        mm(pr[:, 0:16], hT[:, kc, 0:128], rtrw[:, kc, :], kc == 0, kc == 7, [hTk, 'rtrw'], [prk], inc=(kc == 7))
                    sc = rt[:, 0, :]
                    sel = rt[:, 1, :]
                    w1_ = rt[:, 2, :]
                    w2_ = rt[:, 3, :]
                    act(sc, pr[:, 0:16], AF.Sigmoid, [prk], ['rt'])
                    tt('dve', sel, sc, rtrb[:], ALU.add, ['rt', 'rtrb'], ['rt'])
                    sel3 = sel.rearrange("p (g e) -> p g e", g=4)
                    S.op('dve', lambda e, s3=sel3: e.tensor_reduce(out=rs[:, 0:4], in_=s3, op=ALU.max, axis=AX.X), ['rt'], ['rs'])
                    tt('dve', w1_.rearrange("p (g e) -> p g e", g=4), sel3, bc(rs[:, 0:4], 4, 2), ALU.is_ge, ['rt', 'rs'], ['rt'])
                    stt(w2_, w1_, -1e9, sel, ALU.mult, ALU.add, ['rt'], ['rt'])
                    S.op('dve', lambda e, a=w2_: e.tensor_reduce(out=rs[:, 4:8], in_=a.rearrange("p (g e) -> p g e", g=4), op=ALU.max, axis=AX.X),
                         ['rt'], ['rs'])
                    tt('dve', rs[:, 8:12], rs[:, 0:4], rs[:, 4:8], ALU.add, ['rs'], ['rs'])
                    S.op('dve', lambda e: e.tensor_reduce(out=rs[:, 12:13], in_=rs[:, 8:12], op=ALU.max, axis=AX.X), ['rs'], ['rs'])
                    ts('dve', rs[:, 4:8], rs[:, 8:12], rs[:, 12:13], ALU.is_ge, ['rs'], ['rs'], s2=-1.0, op1=ALU.add)
                    ts('dve', rs[:, 4:8], rs[:, 4:8], 1e9, ALU.mult, ['rs'], ['rs'])
                    tt('dve', w1_.rearrange("p (g e) -> p g e", g=4), sel3, bc(rs[:, 4:8], 4, 2), ALU.add, ['rt', 'rs'], ['rt'])
                    S.op('dve', lambda e, a=w1_: e.max(out=rt[:, 4, 0:8], in_=a), ['rt'], ['rt'])
                    ts('dve', M1a[:, ti, :], w1_, rt[:, 4, 0:1], ALU.is_ge, ['rt'], ['M1a'])
                    ts('dve', w2_, w1_, rt[:, 4, 1:2], ALU.is_ge, ['rt'], ['rt'])
                    tt('dve', M2a[:, ti, :], w2_, M1a[:, ti, :], ALU.subtract, ['rt', 'M1a'], ['M2a'])
                    tt('dve', w2_, M1a[:, ti, :], sc, ALU.mult, ['rt', 'M1a'], ['rt'])
                    S.op('dve', lambda e, a=w2_: e.tensor_reduce(out=rs[:, 13:14], in_=a, op=ALU.add, axis=AX.X), ['rt'], ['rs'])
                    tt('dve', w2_, M2a[:, ti, :], sc, ALU.mult, ['rt', 'M2a'], ['rt'])
                    S.op('dve', lambda e, a=w2_: e.tensor_reduce(out=rs[:, 14:15], in_=a, op=ALU.add, axis=AX.X), ['rt'], ['rs'])
                    tt('dve', rs[:, 15:16], rs[:, 13:14], rs[:, 14:15], ALU.add, ['rs'], ['rs'])
                    recip(rs[:, 15:16], rs[:, 15:16], ['rs'], ['rs'])
                    ts('dve', wn[:, ti, :], rs[:, 13:15], rs[:, 15:16], ALU.mult, ['rs'], ['wn'])
                NG = NT * NE
                Mall = psb("Mall", (128, NT, NE))
                cnt = psb("cnt", (128, NT, NE))
                base = psb("base", (128, NT, NE))
                dest = psb("dest", (128, NT, NE))
                dtmp = psb("dtmp", (128, NT, NE))
                d12 = psb("d12", (128, 2, NT))
                idx12 = psb("idx12", (128, 2, NT), I32)
                ev = psb("ev", (128, 8, NE))
                ebf = psb("ebf", (128, NBK))
                bidx_i = psb("bidx_i", (128, NBK), I32)
                bidx = psb("bidx", (128, NBK))
                pcol_i = psb("pcol_i", (128, 1), I32)
                pcol = psb("pcol", (128, 1))
                widx = psb("widx", (128, NBK), I32)
                tt('dve', Mall[:], M1a[:], M2a[:], ALU.add, ['M1a', 'M2a'], ['Mall'])
                Mf = Mall[:].rearrange("p t e -> p (t e)")
                prk_, prkk = psr.next()
                pcn, pcnk = psr.next()
                for c0 in range(0, NG, 512):
                    cw_ = min(512, NG - c0)
                    assert NG <= 512
                    mm(prk_[:, 0:cw_], SU[:], Mf[:, c0:c0 + cw_], True, True, ['SU', 'Mall'], [prkk])
                    mm(pcn[:, 0:cw_], ones[:], Mf[:, c0:c0 + cw_], True, True, ['ones', 'Mall'], [pcnk])
                cp('dve', cnt[:].rearrange("p t e -> p (t e)"), pcn[:, 0:NG], [pcnk], ['cnt'])
                memset('dve', base[:, 0, :], 0.0, ['base'])
                for ti in range(1, NT):
                    tt('dve', base[:, ti, :], base[:, ti - 1, :], cnt[:, ti - 1, :], ALU.add, ['base', 'cnt'], ['base'])
                tt('dve', ev[:, 0, :], base[:, NT - 1, :], cnt[:, NT - 1, :], ALU.add, ['base', 'cnt'], ['ev'])
                memset('dve', ev[:, 1, :], 0.0, ['ev'])
                for m in range(SEQ // MBLK):
                    stt(ev[:, 1, :], ev[:, 0, :], float(MBLK * m), ev[:, 1, :], ALU.is_gt, ALU.add, ['ev'], ['ev'])
                ts('dve', ev[:, 2, :], ev[:, 1, :], float(MBLK), ALU.mult, ['ev'], ['ev'])
                cp('dve', ev[:, 3, 0:1], ev[:, 2, 0:1], ['ev'], ['ev'])
                for e_ in range(1, NE):
                    tt('dve', ev[:, 3, e_:e_ + 1], ev[:, 3, e_ - 1:e_], ev[:, 2, e_:e_ + 1], ALU.add, ['ev'], ['ev'])
                tt('dve', ev[:, 4, :], ev[:, 3, :], ev[:, 2, :], ALU.subtract, ['ev'], ['ev'])
                tt('dve', dest[:].rearrange("p t e -> p (t e)"), prk_[:, 0:NG], base[:].rearrange("p t e -> p (t e)"), ALU.add, [prkk, 'base'], ['dest'])
                tt('dve', dest[:], dest[:], bc(ev[:, 4, :], NT, 1), ALU.add, ['dest', 'ev'], ['dest'])
                for k_, Mk in ((0, M1a), (1, M2a)):
                    tt('dve', dtmp[:], dest[:], Mk[:], ALU.mult, ['dest', 'M1a', 'M2a'], ['dtmp'])
                    S.op('dve', lambda e, k_=k_: e.tensor_reduce(out=d12[:, k_, :], in_=dtmp[:], op=ALU.add, axis=AX.X), ['dtmp'], ['d12'])
                cp('dve', idx12[:], d12[:], ['d12'], ['idx12'])
                S.op('pool', lambda e: e.iota(bidx_i[:], pattern=[[MBLK, NBK]], base=0, channel_multiplier=0), (), ['bidx_i'])
                S.op('pool', lambda e: e.iota(pcol_i[:], pattern=[[0, 1]], base=0, channel_multiplier=1), (), ['pcol_i'])
                cp('dve', bidx[:], bidx_i[:], ['bidx_i'], ['bidx'])
                cp('dve', pcol[:], pcol_i[:], ['pcol_i'], ['pcol'])
                memset('dve', ebf[:], 0.0, ['ebf'])
                for e_ in range(NE):
                    stt(ebf[:], bidx[:], ev[:, 3, e_:e_ + 1], ebf[:], ALU.is_ge, ALU.add, ['bidx', 'ev', 'ebf'], ['ebf'])
                ts('dve', ebf[:], ebf[:], float(NE - 1), ALU.min, ['ebf'], ['ebf'], s2=128.0, op1=ALU.mult)
                ts('dve', ebf[:], ebf[:], pcol[:, 0:1], ALU.add, ['ebf', 'pcol'], ['ebf'], s2=float(l * 16 * 128), op1=ALU.add)
                cp('dve', widx[:], ebf[:], ['ebf'], ['widx'])
                for ti in range(NT):
                    ht, hk = hr.next()
                    S.dma(ht[:], h2_d[128 * ti:128 * ti + 128, :], [('h2', ti)], [hk], q='sp')
                    for k_ in range(2):
                        S.idma(Xs_d[:, :], ht[:, :], bass.IndirectOffsetOnAxis(ap=idx12[:, k_, ti:ti + 1], axis=0), None,
                               [hk, 'idx12'], [('Xs', ti, k_)])
                S.barrier()
                with ExitStack() as ph3:
                    def psb3(name, shape, dt=F32):
                        return ph3.enter_context(nc.sbuf_tensor("%s_F3%d" % (name, l), list(shape), dt))
                    wgr = Ring([psb3("wg%d" % i, (128, 8 * 512)) for i in range(2)], "wg")
                    wur = Ring([psb3("wu%d" % i, (128, 8 * 512)) for i in range(2)], "wu")
                    wdr = Ring([psb3("wd%d" % i, (128, 4 * D)) for i in range(2)], "wd")
                    xgr = Ring([psb3("xg%d" % i, (128, D)) for i in range(4)], "xg")
                    actT = psb3("actT", (128, 4, 256))
                    sg = Ring([psb3("sg%d" % i, (128, 256)) for i in range(2)], "sg")
                    yor = Ring([psb3("yo%d" % i, (128, D)) for i in range(2)], "yo")
                    wg_v = moe_wg.rearrange("l e (p kc) n -> (l e p) (kc n)", kc=8)
                    wu_v = moe_wu.rearrange("l e (p kc) n -> (l e p) (kc n)", kc=8)
                    wd_v = moe_wd.rearrange("l e (p fc) n -> (l e p) (fc n)", fc=4)
                    deferred = []
                    for b_ in range(NBK):
                        off = bass.IndirectOffsetOnAxis(ap=widx[:, b_:b_ + 1], axis=0)
                        wg, wgk = wgr.next()
                        wu, wuk = wur.next()
                        wd, wdk = wdr.next()
                        S.idma(wg[:, :], wg_v, None, off, ['widx'], [wgk])
                        S.idma(wu[:, :], wu_v, None, off, ['widx'], [wuk])
                        S.idma(wd[:, :], wd_v, None, off, ['widx'], [wdk])
                        hT, hTk = hTr.next()
                        xgs = []
                        for sub in range(2):
                            xg, xgk = xgr.next()
                            r0 = b_ * MBLK + 128 * sub
                            S.dma(xg[:], Xs_d[r0:r0 + 128, :], (), [xgk], q='sp')
                            xgs.append((xg, xgk))
                        for d_ in deferred:
                            S.dma(*d_[0], **d_[1])
                        deferred = []
                        for sub in range(2):
                            xg, xgk = xgs[sub]
                            for k0 in (0, 4):
                                ps, pk = psr.next()
                                for kc in range(k0, k0 + 4):
                                    tr(ps[:, (kc - k0) * 128:(kc - k0 + 1) * 128], xg[:, kc:D:8], ident[:], [xgk, 'ident'], [pk], inc=(kc == k0 + 3))
                                dst = hT[:, k0:k0 + 4, sub * 128:(sub + 1) * 128]
                                srcv = ps[:].rearrange("p (a b) -> p a b", a=4)
                                if k0 == 0:
                                    act(dst, srcv, AF.Copy, [pk], [hTk])
                                else:
                                    cp('dve', dst, srcv, [pk], [hTk])
                        for fc in range(4):
                            pg_, pgk_ = psr.next()
                            for kc in range(8):
                                mm(pg_[:, 0:256], wg[:, kc * 512 + fc:(kc + 1) * 512:4], hT[:, kc, :], kc == 0, kc == 7, [wgk, hTk], [pgk_], inc=(kc == 7))
                            pu_, puk_ = psr.next()
                            for kc in range(8):
                                mm(pu_[:, 0:256], wu[:, kc * 512 + fc:(kc + 1) * 512:4], hT[:, kc, :], kc == 0, kc == 7, [wuk, hTk], [puk_], inc=(kc == 7))
                            s_, sk_ = sg.next()
                            act(s_[:], pg_[:, 0:256], AF.Silu, [pgk_], [sk_])
                            tt('dve', actT[:, fc, :], pu_[:, 0:256], s_[:], ALU.mult, [puk_, sk_], [('actT', fc)])
                        for sub in range(2):
                            yo, yok = yor.next()
                            for nh in range(2):
                                pd, pdk = psr.next()
                                for fc in range(4):
                                    mm(pd[:], actT[:, fc, 128 * sub:128 * sub + 128], wd[:, fc * D + 512 * nh:fc * D + 512 * nh + 512], fc == 0, fc == 3,
                                       [('actT', fc), wdk], [pdk], inc=(fc == 3))
                                if nh == 0:
                                    act(yo[:, 0:512], pd[:], AF.Copy, [pdk], [yok])
                                else:
                                    cp('dve', yo[:, 512:1024], pd[:], [pdk], [yok])
                            r0 = b_ * MBLK + 128 * sub
                            deferred.append(((Ys_d[r0:r0 + 128, :], yo[:], [yok], [('Ys', b_, sub)]), dict(q='sp')))
                    for d_ in deferred:
                        S.dma(*d_[0], **d_[1])
                    S.barrier()
                y1r = Ring([psb("y1_%d" % i, (128, D)) for i in range(2)], "y1")
                y2r = Ring([psb("y2_%d" % i, (128, D)) for i in range(2)], "y2")
                for ti in range(NT):
                    y1, y1k = y1r.next()
                    y2, y2k = y2r.next()
                    S.idma(y1[:, :], Ys_d[:, :], None, bass.IndirectOffsetOnAxis(ap=idx12[:, 0, ti:ti + 1], axis=0), ['idx12'], [y1k])
                    S.idma(y2[:, :], Ys_d[:, :], None, bass.IndirectOffsetOnAxis(ap=idx12[:, 1, ti:ti + 1], axis=0), ['idx12'], [y2k])
                    xt, xk = xr.next()
                    S.dma(xt[:], out[128 * ti:128 * ti + 128, :], [('x', ti)], [xk], q='sp')
                    ts('dve', y1[:], y1[:], wn[:, ti, 0:1], ALU.mult, [y1k, 'wn'], [y1k])
                    stt(y1[:], y2[:], wn[:, ti, 1:2], y1[:], ALU.mult, ALU.add, [y2k, 'wn', y1k], [y1k])
                    tt('pool', y1[:], y1[:], g2r[:], ALU.mult, [y1k, 'g2r'], [y1k])
                    tt('pool', xt[:], xt[:], y1[:], ALU.add, [xk, y1k], [xk])
                    S.dma(out[128 * ti:128 * ti + 128, :], xt[:], [xk], [('x', ti)], q='sp')
                S.barrier()

        with ExitStack() as ph:
            modrow = ph.enter_context(nc.sbuf_tensor("modrow", [128, 6 * D], F32))
            rowtmp = ph.enter_context(nc.sbuf_tensor("rowtmp", [128, D], F32))
            wr0 = Ring([ph.enter_context(nc.sbuf_tensor("wsl0_%d" % i, [128, 8, 512], F32)) for i in range(2)], "wsl0")
            for l in range(L):
                S.dma(modrow[:], ada_b[l].partition_broadcast(128), ['modrow'], ['modrow'])
                for oc in range(12):
                    wt, wk = load_w(wr0, ada_w[l], oc * 512, 512)
                    ps, pk = psr.next()
                    for kc in range(8):
                        mm(ps[:], cbc[:, kc, :], wt[:, kc, :], kc == 0, kc == 7, ['cbc', wk], [pk], inc=(kc == 7))
                    tt('dve', modrow[:, oc * 512:(oc + 1) * 512], ps[:], modrow[:, oc * 512:(oc + 1) * 512], ALU.add,
                       [pk, 'modrow'], ['modrow'])
                for (o_, ng) in ((1, norm1_g), (4, norm2_g)):
                    S.dma(rowtmp[:], ng[l].partition_broadcast(128), ['rowtmp'], ['rowtmp'])
                    stt(modrow[:, o_ * D:(o_ + 1) * D], modrow[:, o_ * D:(o_ + 1) * D], 1.0, rowtmp[:], ALU.add, ALU.mult,
                        ['modrow', 'rowtmp'], ['modrow'])
                S.dma(mod_d[l:l + 1, :], modrow[0:1, :], ['modrow'], [('mod', l)], q='pool')
            S.barrier()

        for l in range(L if 'stop0' not in dbg else 0):
            src_x = x_in if l == 0 else out
            with ExitStack() as phm:
                A1t = phm.enter_context(nc.sbuf_tensor("A1t_%d" % l, [128, D], F32))
                sh1t = phm.enter_context(nc.sbuf_tensor("sh1t_%d" % l, [128, D], F32))
                S.dma(sh1t[:], mod_d[l, 0:D].partition_broadcast(128), (), ['modrow'])
                S.dma(A1t[:], mod_d[l, D:2 * D].partition_broadcast(128), (), ['modrow'])
                A1row, sh1row = A1t[:], sh1t[:]
                phase_A(l, src_x, A1row, sh1row)
            if 'stopA' in dbg:
                break
            phase_B(l)
            if 'stopB' in dbg:
                break
            phase_C(l)
            if 'stopC' in dbg:
                break
            phase_D(l)
            if 'stopD' in dbg:
                break
            phase_E(l, src_x)
            if 'stopE' in dbg:
                break
            phase_F(l)
        S.barrier()
    return nc


def _host_tables(rel_bias, SEQ):
    FDW = NEGPAD + SEQ
    d = np.arange(SEQ)
    bk = rel_bucket_np(d)
    g = np.asarray(rel_bias, np.float32)[bk].T
    fdg = np.full((NH, FDW), NEGM, np.float32)
    fdg[:, NEGPAD:] = g
    fdw = np.full((NH, FDW), NEGM, np.float32)
    fdw[:, NEGPAD:NEGPAD + 512] = g[:, :512]
    return fdg, fdw


_CACHE = {}


def kernel(**inputs):
    x = np.asarray(inputs["x"], np.float32)
    B, SEQ, _ = x.shape
    L = int(np.asarray(inputs["ada_w"]).shape[0])
    key = (SEQ, L)
    if key not in _CACHE:
        _CACHE[key] = build_program(SEQ, L)
    nc = _CACHE[key]
    fdg, fdw = _host_tables(inputs["rel_bias"], SEQ)
    names = ["router_w", "router_b", "ada_w", "ada_b", "norm1_g", "norm2_g", "w_in", "qk_norm_g", "cmp_pos", "cmp_w1",
             "cmp_w2", "dn_conv_w", "dn_a_log", "dn_dt_bias", "dn_norm_g", "w_branch_a", "w_branch_b", "w_out",
             "moe_w_gate", "moe_w_up", "moe_w_down"]
    shared = {n: np.ascontiguousarray(np.asarray(inputs[n], np.float32)) for n in names}
    shared["fdg"] = fdg
    shared["fdw"] = fdw
    c = np.asarray(inputs["c"], np.float32)
    in_maps = []
    for b in range(B):
        m = dict(shared)
        m["x"] = np.ascontiguousarray(x[b])
        m["cT"] = np.ascontiguousarray(c[b].reshape(8, 128).T)
        in_maps.append(m)
    res = run_bass_kernel_spmd(nc, in_maps, core_ids=list(range(B)))
    return np.stack([np.asarray(r["out"], np.float32) for r in res.results], axis=0)
```

```python
import math
from contextlib import ExitStack
import numpy as np
import concourse.bass as bass
import concourse.mybir as mybir
from concourse.bass_utils import run_bass_kernel_spmd

F32 = mybir.dt.float32
I32 = mybir.dt.int32
AF = mybir.ActivationFunctionType
ALU = mybir.AluOpType
AX = mybir.AxisListType

D = 1024
HD = 64
NH = 8
DIN = 5416
NEGM = -30000.0
EPS = 1e-6
U0 = 384
OFFMAX = 1024
WGEN = U0 + OFFMAX + 512
WWIN = U0 + 512 + 512
NEGPAD = 1024


class Sched:
    def __init__(self, nc, es, ndma=14):
        self.nc = nc
        self.eng = {'pe': nc.tensor, 'act': nc.scalar, 'dve': nc.vector, 'pool': nc.gpsimd, 'sp': nc.sync}
        self.sem = {k: es.enter_context(nc.semaphore('s_' + k)) for k in self.eng}
        self.cnt = {k: 0 for k in self.eng}
        self.dsem = [es.enter_context(nc.semaphore('d%d' % i)) for i in range(ndma)]
        self.dcnt = [0] * ndma
        self.dnext = 0
        self.seen = {k: {} for k in self.eng}
        self.res = {}
        self.nops = 0

    def _deps(self, r, w):
        deps = {}

        def add(t):
            if t is not None and deps.get(t[0], 0) < t[1]:
                deps[t[0]] = t[1]
        for k in r:
            st = self.res.get(k)
            if st:
                add(st[0])
        for k in w:
            st = self.res.get(k)
            if st:
                add(st[0])
                for s, v in st[1].items():
                    add((s, v))
        return deps

    def _wait(self, e, deps):
        for s, v in deps.items():
            if s == 'pe' and e == 'pe':
                continue
            if self.seen[e].get(s, 0) < v:
                sem = self.sem[s] if isinstance(s, str) else self.dsem[s]
                self.eng[e].wait_ge(sem, v)
                self.seen[e][s] = v

    def _mark(self, tag, r, w):
        for k in r:
            st = self.res.setdefault(k, [None, {}])
            if st[1].get(tag[0], 0) < tag[1]:
                st[1][tag[0]] = tag[1]
        for k in w:
            self.res[k] = [tag, {}]

    def op(self, e, emit, r=(), w=(), inc=True):
        self._wait(e, self._deps(r, w))
        inst = emit(self.eng[e])
        self.nops += 1
        if inc:
            self.cnt[e] += 1
            inst.then_inc(self.sem[e], 1)
            tag = (e, self.cnt[e])
        else:
            tag = (e, self.cnt[e] + 1)
        self._mark(tag, r, w)

    def dma(self, out, in_, r=(), w=(), q='sp'):
        i = self.dnext
        self.dnext = (i + 1) % len(self.dsem)
        deps = self._deps(r, w)
        if self.dcnt[i]:
            deps[i] = max(deps.get(i, 0), self.dcnt[i])
        self._wait(q, deps)
        self.dcnt[i] += 16
        self.eng[q].dma_start(out=out, in_=in_).then_inc(self.dsem[i], 16)
        self.nops += 1
        self._mark((i, self.dcnt[i]), r, w)

    def idma(self, out, in_, out_off, in_off, r=(), w=()):
        i = self.dnext
        self.dnext = (i + 1) % len(self.dsem)
        deps = self._deps(r, w)
        if self.dcnt[i]:
            deps[i] = max(deps.get(i, 0), self.dcnt[i])
        self._wait('pool', deps)
        self.dcnt[i] += 16
        self.eng['pool'].indirect_dma_start(out=out, out_offset=out_off, in_=in_, in_offset=in_off).then_inc(self.dsem[i], 16)
        self.nops += 1
        self._mark((i, self.dcnt[i]), r, w)

    def barrier(self):
        deps = {s: c for s, c in self.cnt.items() if c}
        for i, v in enumerate(self.dcnt):
            if v:
                deps[i] = v
        for e in self.eng:
            d = dict(deps)
            self._wait(e, d)
        self.res = {}


class Ring:
    def __init__(self, tiles, name):
        self.tiles = tiles
        self.name = name
        self.i = 0

    def next(self):
        k = self.i % len(self.tiles)
        self.i += 1
        return self.tiles[k], (self.name, k)


def rel_bucket_np(dist):
    exact = 16
    dist = np.maximum(dist, 0)
    far = np.maximum(dist, exact).astype(np.float32)
    large = exact + (np.log(far / np.float32(exact)) / np.float32(math.log(1024 / exact)) * np.float32(32 - exact)).astype(np.int32)
    return np.where(dist < exact, dist, np.minimum(large, 31))


def build_program(SEQ, DEPTH, dbg=()):
    NT = SEQ // 128
    QT = SEQ // 512
    NCH = SEQ // 64
    NCMP = SEQ // 16 - 1
    NBLK = SEQ // 64
    NCT = (NCMP + 127) // 128
    FDW = NEGPAD + SEQ
    JB = NBLK
    nc = bass.Bass("TRN2", target_bir_lowering=False)

    def din(name, shape):
        return nc.dram_tensor(name, list(shape), F32, kind="ExternalInput").ap()

    def dscr(name, shape, kind="Internal"):
        if name in dbg:
            kind = "ExternalOutput"
        return nc.dram_tensor(name, list(shape), F32, kind=kind).ap()

    L = DEPTH
    x_in = din("x", (SEQ, D))
    cT_in = din("cT", (128, 8))
    fdg_in = din("fdg", (NH, FDW))
    fdw_in = din("fdw", (NH, FDW))
    router_w = din("router_w", (D, 16))
    router_b = din("router_b", (16,))
    ada_w = din("ada_w", (L, D, 6 * D))
    ada_b = din("ada_b", (L, 6 * D))
    norm1_g = din("norm1_g", (L, D))
    norm2_g = din("norm2_g", (L, D))
    w_in = din("w_in", (L, D, DIN))
    qk_norm_g = din("qk_norm_g", (L, 4, HD))
    cmp_pos = din("cmp_pos", (L, 2, 32, HD))
    cmp_w1 = din("cmp_w1", (L, 2, 2048, 256))
    cmp_w2 = din("cmp_w2", (L, 2, 256, HD))
    dn_conv_w = din("dn_conv_w", (L, 4, 1536))
    dn_a_log = din("dn_a_log", (L, 8))
    dn_dt_bias = din("dn_dt_bias", (L, 8))
    dn_norm_g = din("dn_norm_g", (L, HD))
    w_br_a = din("w_branch_a", (L, 512, D))
    w_br_b = din("w_branch_b", (L, 512, D))
    w_out = din("w_out", (L, D, D))
    moe_wg = din("moe_w_gate", (L, 16, D, 512))
    moe_wu = din("moe_w_up", (L, 16, D, 512))
    moe_wd = din("moe_w_down", (L, 16, 512, D))
    out = nc.dram_tensor("out", [SEQ, D], F32, kind="ExternalOutput").ap()

    qT_d = dscr("qT_d", (4, 128, SEQ))
    kcT_d = dscr("kcT_d", (2, 128, SEQ))
    kslcT_d = dscr("kslcT_d", (128, SEQ))
    kwinT_d = dscr("kwinT_d", (128, SEQ))
    vslc_d = dscr("vslc_d", (SEQ, 128))
    vwin_d = dscr("vwin_d", (SEQ, 128))
    gate_d = dscr("gate_d", (SEQ, 24))
    dnraw_d = dscr("dnraw_d", (12, 128, SEQ))
    dnc_d = dscr("dnc_d", (12, 128, SEQ))
    bg_d = dscr("bg_d", (SEQ, 16))
    zs_d = dscr("zs_d", (SEQ, 512))
    mergeT_d = dscr("mergeT_d", (16, 128, SEQ))
    obr_d = dscr("obr_d", (3, SEQ, 512))
    ybT_d = dscr("ybT_d", (4, 128, SEQ))
    bct_d = dscr("bct_d", (NH, NCT * 128, SEQ))
    bgen_d = dscr("bgen_d", (128, NH, WGEN))
    bwin_d = dscr("bwin_d", (128, NH, WWIN))
    selbT_d = dscr("selbT_d", (128, SEQ))
    MBLK = 256
    NBK = (2 * SEQ + 16 * MBLK) // MBLK
    h2_d = dscr("h2_d", (SEQ, D))
    Xs_d = dscr("Xs_d", (NBK * MBLK, D))
    Ys_d = dscr("Ys_d", (NBK * MBLK, D))
    mod_d = dscr("mod_d", (L, 6 * D))

    es = ExitStack()
    with es:
        S = Sched(nc, es)

        def sb(name, shape):
            return es.enter_context(nc.sbuf_tensor(name, list(shape), F32))

        PS = [es.enter_context(nc.psum_tensor("ps%d" % i, [128, 512], F32)) for i in range(8)]
        psr = Ring(PS, "ps")

        def tt(e, o, a, b, op, r, w):
            S.op(e, lambda g: g.tensor_tensor(out=o, in0=a, in1=b, op=op), r, w)

        def ts(e, o, a, s1, op0, r, w, s2=None, op1=None):
            if op1 is None:
                S.op(e, lambda g: g.tensor_scalar(out=o, in0=a, scalar1=s1, scalar2=None, op0=op0), r, w)
            else:
                S.op(e, lambda g: g.tensor_scalar(out=o, in0=a, scalar1=s1, scalar2=s2, op0=op0, op1=op1), r, w)

        def stt(o, a, sc, b, op0, op1, r, w):
            S.op('dve', lambda g: g.scalar_tensor_tensor(out=o, in0=a, scalar=sc, in1=b, op0=op0, op1=op1), r, w)

        def act(o, a, f, r, w, bias=None, scale=1.0, accum=None):
            kw = {}
            if bias is not None:
                kw['bias'] = bias
            if accum is not None:
                kw['accum_out'] = accum
            S.op('act', lambda g: g.activation(out=o, in_=a, func=f, scale=scale, **kw), r, w)

        def mm(o, lT, rh, st, sp, r, w, inc=True):
            S.op('pe', lambda g: g.matmul(o, lT, rh, start=st, stop=sp), r, w, inc=inc)

        def tr(o, a, idn, r, w, inc=True):
            S.op('pe', lambda g: g.transpose(o, a, idn), r, w, inc=inc)

        def cp(e, o, a, r, w):
            S.op(e, lambda g: g.tensor_copy(o, a), r, w)

        def recip(o, a, r, w):
            S.op('dve', lambda g: g.reciprocal(o, a), r, w)

        def memset(e, o, v, w):
            S.op(e, lambda g: g.memset(o, v), (), w)

        def asel(o, a, pattern, base, cm, fill, r, w, op=ALU.is_ge):
            S.op('pool', lambda g: g.affine_select(out=o, in_=a, pattern=pattern, compare_op=op, fill=fill,
                                                   base=base, channel_multiplier=cm), r, w)

        ident = sb("ident", (128, 128))
        ones = sb("ones", (128, 128))
        bdones = sb("bdones", (128, 128))
        UT = sb("UT", (128, 64))
        maskU = sb("maskU", (64, 64))
        maskL = sb("maskL", (64, 64))
        nsU = sb("nsU", (64, 64))
        nsL = sb("nsL", (64, 64))
        ovl = sb("ovl", (128, NCT, JB))
        memset('pool', ident[:], 0.0, ['ident'])
        asel(ident[:], ident[:], [[-1, 128]], 0, 1, 1.0, ['ident'], ['ident'], op=ALU.not_equal)
        memset('pool', ones[:], 1.0, ['ones'])
        memset('pool', bdones[:], 0.0, ['bdones'])
        memset('pool', bdones[0:64, 0:64], 1.0, ['bdones'])
        memset('pool', bdones[64:128, 64:128], 1.0, ['bdones'])
        for h0 in (0, 64):
            memset('pool', UT[h0:h0 + 64, :], 1.0, ['UT'])
            asel(UT[h0:h0 + 64, :], UT[h0:h0 + 64, :], [[1, 64]], 0, -1, 0.0, ['UT'], ['UT'])
        memset('pool', maskU[:], 0.0, ['maskU'])
        asel(maskU[:], maskU[:], [[1, 64]], 0, -1, NEGM, ['maskU'], ['maskU'])
        memset('pool', maskL[:], 0.0, ['maskL'])
        asel(maskL[:], maskL[:], [[-1, 64]], 0, 1, NEGM, ['maskL'], ['maskL'])
        memset('pool', nsU[:], -1.0, ['nsU'])
        asel(nsU[:], nsU[:], [[1, 64]], -1, -1, 0.0, ['nsU'], ['nsU'])
        memset('pool', nsL[:], -1.0, ['nsL'])
        asel(nsL[:], nsL[:], [[-1, 64]], -1, 1, 0.0, ['nsL'], ['nsL'])
        ovt = sb("ovt", (128, NCT, JB))
        memset('pool', ovl[:], 0.0, ['ovl'])
        for m in (0, 1):
            memset('pool', ovt[:], 1.0, ['ovt'])
            asel(ovt[:], ovt[:], [[128, NCT], [-4, JB]], m, 1, 0.0, ['ovt'], ['ovt'])
            asel(ovt[:], ovt[:], [[-128, NCT], [4, JB]], 3 - m, -1, 0.0, ['ovt'], ['ovt'])
            tt('pool', ovl[:], ovl[:], ovt[:], ALU.add, ['ovl', 'ovt'], ['ovl'])

        with nc.allow_non_contiguous_dma(reason="table build"):
            for p in range(128):
                o0 = NEGPAD - U0 - p
                S.dma(bgen_d[p, :, :], fdg_in[:, o0:o0 + WGEN], (), [('bgen', p)], q='sp')
                S.dma(bwin_d[p, :, :], fdw_in[:, o0:o0 + WWIN], (), [('bwin', p)], q='pool')
            for n in range(NCT * 128):
                o0 = NEGPAD - (16 * n + 31)
                if n >= NCMP:
                    o0 = 0
                q = 'sp' if n % 2 == 0 else 'pool'
                if n >= NCMP:
                    S.dma(bct_d[:, n, 0:NEGPAD], fdg_in[:, 0:NEGPAD], (), [('bct', n)], q=q)
                    for c0 in range(NEGPAD, SEQ, NEGPAD):
                        S.dma(bct_d[:, n, c0:c0 + NEGPAD], fdg_in[:, 0:NEGPAD], (), [('bct', n, c0)], q=q)
                elif o0 >= 0:
                    S.dma(bct_d[:, n, :], fdg_in[:, o0:o0 + SEQ], (), [('bct', n)], q=q)
                else:
                    nn = -o0
                    for c0 in range(0, nn, NEGPAD):
                        cw = min(NEGPAD, nn - c0)
                        S.dma(bct_d[:, n, c0:c0 + cw], fdg_in[:, 0:cw], (), [('bct', n, c0)], q=q)
                    S.dma(bct_d[:, n, nn:SEQ], fdg_in[:, 0:SEQ - nn], (), [('bct', n)], q=q)
        S.barrier()

        cact = sb("cact", (128, 8))
        S.dma(cact[:], cT_in[:, :], (), ['cact'])
        act(cact[:], cact[:], AF.Silu, ['cact'], ['cact'])
        cbc = sb("cbc", (128, 8, 128))
        for kc in range(8):
            ts('dve', cbc[:, kc, :], ones[:], cact[:, kc:kc + 1], ALU.mult, ['ones', 'cact'], ['cbc'])
        rtrb = sb("rtrb", (128, 16))
        S.dma(rtrb[:], router_b.partition_broadcast(128), (), ['rtrb'])
        rtrw = sb("rtrw", (128, 8, 16))
        with nc.allow_non_contiguous_dma(reason="router w"):
            S.dma(rtrw[:], router_w.rearrange("(kc p) e -> p kc e", p=128), (), ['rtrw'])

        def load_w(ring, src2d, c0, ncols, kch=8):
            t, k = ring.next()
            with nc.allow_non_contiguous_dma(reason="weight slab"):
                S.dma(t[:, 0:kch, 0:ncols], src2d.rearrange("(kc p) n -> p kc n", p=128)[:, :, c0:c0 + ncols], (), [k])
            return t, k

        def norm_tile(src, t0, Arow, shrow, xr, hr, sm):
            xt, xk = xr.next()
            S.dma(xt[:], src[t0:t0 + 128, :], [('x', t0 // 128)], [xk])
            ht, hk = hr.next()
            s, sk = sm.next()
            act(ht[:], xt[:], AF.Square, [xk], [hk, sk], accum=s[:, 0:1])
            act(s[:, 1:2], s[:, 0:1], AF.Sqrt, [sk], [sk], bias=EPS, scale=1.0 / D)
            recip(s[:, 2:3], s[:, 1:2], [sk], [sk])
            stt(ht[:], xt[:], s[:, 2:3], Arow, ALU.mult, ALU.mult, [xk, sk, 'modrow'], [hk])
            tt('pool', ht[:], ht[:], shrow, ALU.add, [hk, 'modrow'], [hk])
            return ht, hk, xt, xk

        def transpose_to(ht, hk, hT, hTk, sub, nkc=8):
            for k0 in range(0, nkc, 4):
                ps, pk = psr.next()
                for kc in range(k0, k0 + 4):
                    tr(ps[:, (kc - k0) * 128:(kc - k0 + 1) * 128], ht[:, kc * 128:(kc + 1) * 128], ident[:],
                       [hk, 'ident'], [pk], inc=(kc == k0 + 3))
                e = 'act' if (k0 // 4) % 2 == 0 else 'dve'
                dst = hT[:, k0:k0 + 4, sub * 128:(sub + 1) * 128]
                srcv = ps[:].rearrange("p (a b) -> p a b", a=4)
                if e == 'act':
                    act(dst, srcv, AF.Copy, [pk], [hTk])
                else:
                    cp('dve', dst, srcv, [pk], [hTk])

        def phase_A(l, src_x, A1row, sh1row):
            with ExitStack() as ph:
                def psb(name, shape):
                    return ph.enter_context(nc.sbuf_tensor("%s_A%d" % (name, l), list(shape), F32))
                wr = Ring([psb("wsl%d" % i, (128, 8, 512)) for i in range(3)], "wsl")
                xr = Ring([psb("xa%d" % i, (128, D)) for i in range(2)], "xa")
                hr = Ring([psb("ha%d" % i, (128, D)) for i in range(2)], "ha")
                hTr = Ring([psb("hT%d" % i, (128, 8, 512)) for i in range(2)], "hT")
                st = Ring([psb("st%d" % i, (128, 512)) for i in range(4)], "st")
                sq = Ring([psb("sq%d" % i, (128, 512)) for i in range(2)], "sq")
                sm = Ring([psb("sm%d" % i, (128, 8)) for i in range(4)], "sm")
                gains = psb("gains", (128, 4))
                dtb = psb("dtb", (128, 8))
                nea = psb("nea", (128, 8))
                with nc.allow_non_contiguous_dma(reason="small"):
                    for h0 in (0, 64):
                        S.dma(gains[h0:h0 + 64, :], qk_norm_g[l].rearrange("i d -> d i"), (), ['gains'])
                ts('dve', gains[:, 0:1], gains[:, 0:1], HD ** -0.5, ALU.mult, ['gains'], ['gains'])
                S.dma(dtb[:], dn_dt_bias[l].partition_broadcast(128), (), ['dtb'])
                S.dma(nea[:], dn_a_log[l].partition_broadcast(128), (), ['nea'])
                act(nea[:], nea[:], AF.Exp, ['nea'], ['nea'])
                ts('dve', nea[:], nea[:], -1.0, ALU.mult, ['nea'], ['nea'])

                def rms64_store(ps, pk, gcol, dst, dkey):
                    q1, qk1 = sq.next()
                    act(q1[:], ps[:], AF.Square, [pk], [qk1])
                    p2, pk2 = psr.next()
                    mm(p2[:], bdones[:], q1[:], True, True, ['bdones', qk1], [pk2])
                    act(q1[:], p2[:], AF.Sqrt, [pk2], [qk1], bias=EPS, scale=1.0 / 64)
                    recip(q1[:], q1[:], [qk1], [qk1])
                    o, ok = st.next()
                    stt(o[:], ps[:], gcol, q1[:], ALU.mult, ALU.mult, [pk, qk1, 'gains'], [ok])
                    S.dma(dst, o[:], [ok], [dkey], q='pool')

                def fm_store(ps, pk, dst, dkey, func=AF.Copy):
                    o, ok = st.next()
                    act(o[:], ps[:], func, [pk], [ok])
                    S.dma(dst, o[:], [ok], [dkey], q='pool')

                for j in range(QT):
                    t0 = 512 * j
                    hT, hTk = hTr.next()
                    for sub in range(4):
                        ht, hk, _, _ = norm_tile(src_x, t0 + 128 * sub, A1row, sh1row, xr, hr, sm)
                        transpose_to(ht, hk, hT, hTk, sub)

                    def fm(wt, wk, lsel):
                        ps, pk = psr.next()
                        for kc in range(8):
                            mm(ps[:], lsel(kc), hT[:, kc, :], kc == 0, kc == 7, [wk, hTk], [pk], inc=(kc == 7))
                        return ps, pk

                    def tm(wt, wk, c0, ncols, sub):
                        ps, pk = psr.next()
                        for kc in range(8):
                            mm(ps[:, 0:ncols], hT[:, kc, sub * 128:(sub + 1) * 128], wt[:, kc, c0:c0 + ncols],
                               kc == 0, kc == 7, [wk, hTk], [pk], inc=(kc == 7))
                        return ps, pk
                    cs = slice(t0, t0 + 512)
                    wt, wk = wr.next()
                    with nc.allow_non_contiguous_dma(reason="q slab"):
                        for a in range(2):
                            for c in range(4):
                                S.dma(wt[:, :, c * 128 + a * 64:c * 128 + a * 64 + 64],
                                      w_in[l].rearrange("(kc p) n -> p kc n", p=128)[:, :, a * 256 + c * 64:a * 256 + c * 64 + 64], (), [wk])
                    for c in range(4):
                        ps, pk = fm(wt, wk, lambda kc, c=c: wt[:, kc, c * 128:(c + 1) * 128])
                        rms64_store(ps, pk, gains[:, 0:1], qT_d[c, :, cs], ('qT', c, j))
                    wt, wk = load_w(wr, w_in[l], 512, 512)
                    for c in range(3):
                        ps, pk = fm(wt, wk, lambda kc, c=c: wt[:, kc, c * 128:(c + 1) * 128])
                        if c < 2:
                            fm_store(ps, pk, kcT_d[c, :, cs], ('kcT', c, j))
                        else:
                            rms64_store(ps, pk, gains[:, 2:3], kslcT_d[:, cs], ('kslcT', j))
                    for sub in range(4):
                        ps, pk = tm(wt, wk, 384, 128, sub)
                        o, ok = st.next()
                        act(o[:, 0:128], ps[:, 0:128], AF.Copy, [pk], [ok])
                        S.dma(vslc_d[t0 + 128 * sub:t0 + 128 * sub + 128, :], o[:, 0:128], [ok], [('vslc', j, sub)], q='pool')
                    wt, wk = load_w(wr, w_in[l], 1024, 280)
                    ps, pk = fm(wt, wk, lambda kc: wt[:, kc, 0:128])
                    rms64_store(ps, pk, gains[:, 3:4], kwinT_d[:, cs], ('kwinT', j))
                    for sub in range(4):
                        r0 = t0 + 128 * sub
                        ps, pk = tm(wt, wk, 128, 152, sub)
                        o, ok = st.next()
                        act(o[:, 0:128], ps[:, 0:128], AF.Copy, [pk], [ok])
                        act(o[:, 128:152], ps[:, 128:152], AF.Sigmoid, [pk], [ok])
                        S.dma(vwin_d[r0:r0 + 128, :], o[:, 0:128], [ok], [('vwin', j, sub)], q='pool')
                        S.dma(gate_d[r0:r0 + 128, :], o[:, 128:152], [ok], [('gate', j, sub)], q='pool')
                    for i in range(3):
                        wt, wk = load_w(wr, w_in[l], 1304 + 512 * i, 512)
                        for c in range(4):
                            ps, pk = fm(wt, wk, lambda kc, c=c: wt[:, kc, c * 128:(c + 1) * 128])
                            fm_store(ps, pk, dnraw_d[4 * i + c, :, cs], ('dnraw', 4 * i + c, j))
                    wt, wk = load_w(wr, w_in[l], 2840, 16)
                    for sub in range(4):
                        r0 = t0 + 128 * sub
                        ps, pk = tm(wt, wk, 0, 16, sub)
                        o, ok = st.next()
                        act(o[:, 0:8], ps[:, 0:8], AF.Sigmoid, [pk], [ok])
                        tt('dve', o[:, 8:16], ps[:, 8:16], dtb[:], ALU.add, [pk, 'dtb'], [ok])
                        act(o[:, 8:16], o[:, 8:16], AF.Exp, [ok], [ok])
                        act(o[:, 8:16], o[:, 8:16], AF.Ln, [ok], [ok], bias=1.0)
                        tt('dve', o[:, 8:16], o[:, 8:16], nea[:], ALU.mult, [ok, 'nea'], [ok])
                        S.dma(bg_d[r0:r0 + 128, :], o[:, 0:16], [ok], [('bg', j, sub)], q='pool')
                    wt, wk = load_w(wr, w_in[l], 2856, 512)
                    for sub in range(4):
                        r0 = t0 + 128 * sub
                        ps, pk = tm(wt, wk, 0, 512, sub)
                        fm_store(ps, pk, zs_d[r0:r0 + 128, :], ('zs', j, sub), func=AF.Silu)
                    for i in range(4):
                        wt, wk = load_w(wr, w_in[l], 3368 + 512 * i, 512)
                        for c in range(4):
                            ps, pk = fm(wt, wk, lambda kc, c=c: wt[:, kc, c * 128:(c + 1) * 128])
                            fm_store(ps, pk, mergeT_d[4 * i + c, :, cs], ('mergeT', 4 * i + c, j), func=AF.Sigmoid)
                S.barrier()
        def phase_B(l):
            with ExitStack() as ph:
                def psb(name, shape):
                    return ph.enter_context(nc.sbuf_tensor("%s_B%d" % (name, l), list(shape), F32))
                kst = [psb("kst%d" % g, (128, NCT * 128)) for g in range(2)]
                for g in range(2):
                    memset('pool', kst[g][:], 0.0, ['kcmpT'])
                vcmp = psb("vcmp", (128, NCT, 2, 65 + JB))
                memset('pool', vcmp[:], 0.0, ['vcmp'])
                memset('pool', vcmp[:, :, :, 64:65], 1.0, ['vcmp'])
                for g in range(2):
                    cp('pool', vcmp[:, :, g, 65:65 + JB], ovl[:], ['ovl', 'vcmp'], ['vcmp'])
                gains = psb("gains", (128, 4))
                with nc.allow_non_contiguous_dma(reason="small"):
                    for h0 in (0, 64):
                        S.dma(gains[h0:h0 + 64, :], qk_norm_g[l].rearrange("i d -> d i"), (), ['gains'])
                with ExitStack() as ph2:
                    def psb2(name, shape):
                        return ph2.enter_context(nc.sbuf_tensor("%s_B2%d" % (name, l), list(shape), F32))
                    kcT = psb2("kcT", (128, SEQ))
                    w1 = psb2("w1", (128, 32, 256))
                    posT = psb2("posT", (128, 32))
                    w2 = psb2("w2", (128, 2, 64))
                    pbias = psb2("pbias", (128, 2))
                    gx = psb2("gx", (128, 2, 2, 256))
                    gt = psb2("gt", (128, 256))
                    sqc = psb2("sqc", (128, 256))
                    for kvi in range(2):
                        S.dma(kcT[:], kcT_d[kvi, :, :], [('kcT', kvi, j) for j in range(QT)], ['kcT'])
                        with nc.allow_non_contiguous_dma(reason="cmp weights"):
                            for h0 in (0, 64):
                                S.dma(w1[h0:h0 + 64, :, :], cmp_w1[l, kvi].rearrange("(j d) f -> d j f", d=64), (), ['w1'])
                                S.dma(posT[h0:h0 + 64, :], cmp_pos[l, kvi].rearrange("j d -> d j"), (), ['posT'])
                            S.dma(w2[:], cmp_w2[l, kvi].rearrange("(fc p) d -> p fc d", p=128), (), ['w2'])
                        for fc in range(2):
                            ps, pk = psr.next()
                            for j in range(32):
                                mm(ps[:, 0:1], w1[0:64, j, fc * 128:(fc + 1) * 128], posT[0:64, j:j + 1], j == 0, j == 31,
                                   ['w1', 'posT'], [pk], inc=(j == 31))
                            cp('dve', pbias[:, fc:fc + 1], ps[:, 0:1], [pk], ['pbias'])
                        for g in range(2):
                            hs = slice(64 * g, 64 * g + 64)
                            for fc in range(2):
                                ps, pk = psr.next()
                                for j in range(32):
                                    mm(ps[:, 0:NCMP], w1[hs, j, fc * 128:(fc + 1) * 128], kcT[hs, j:j + 16 * (NCMP - 1) + 1:16],
                                       j == 0, j == 31, ['w1', 'kcT'], [pk], inc=(j == 31))
                                xs = gx[:, g, fc, 0:NCMP]
                                ts('dve', xs, ps[:, 0:NCMP], pbias[:, fc:fc + 1], ALU.add, [pk, 'pbias'], ['gx'])
                                tt('dve', gt[:, 0:NCMP], xs, xs, ALU.mult, ['gx'], ['gt'])
                                ts('dve', gt[:, 0:NCMP], gt[:, 0:NCMP], 0.044715, ALU.mult, ['gt'], ['gt'], s2=1.0, op1=ALU.add)
                                tt('dve', gt[:, 0:NCMP], gt[:, 0:NCMP], xs, ALU.mult, ['gt', 'gx'], ['gt'])
                                act(gt[:, 0:NCMP], gt[:, 0:NCMP], AF.Sigmoid, ['gt'], ['gt'], scale=1.5957691216057308)
                                tt('dve', xs, xs, gt[:, 0:NCMP], ALU.mult, ['gx', 'gt'], ['gx'])
                            if kvi == 0:
                                ps, pk = psr.next()
                                for fc in range(2):
                                    mm(ps[hs, 0:NCMP], w2[:, fc, :], gx[:, g, fc, 0:NCMP], fc == 0, fc == 1, ['w2', 'gx'], [pk], inc=(fc == 1))
                                act(sqc[hs, 0:NCMP], ps[hs, 0:NCMP], AF.Square, [pk], ['sqc'])
                                p2, pk2 = psr.next()
                                mm(p2[hs, 0:NCMP], ones[hs, 0:64], sqc[hs, 0:NCMP], True, True, ['ones', 'sqc'], [pk2])
                                act(sqc[hs, 0:NCMP], p2[hs, 0:NCMP], AF.Sqrt, [pk2], ['sqc'], bias=EPS, scale=1.0 / 64)
                                recip(sqc[hs, 0:NCMP], sqc[hs, 0:NCMP], ['sqc'], ['sqc'])
                                stt(kst[g][hs, 0:NCMP], ps[hs, 0:NCMP], gains[hs, 1:2], sqc[hs, 0:NCMP], ALU.mult, ALU.mult,
                                    [pk, 'sqc', 'gains'], ['kcmpT'])
                            else:
                                for nt in range(NCT):
                                    nn = min(128, NCMP - nt * 128)
                                    ps, pk = psr.next()
                                    for fc in range(2):
                                        mm(ps[0:nn, 0:64], gx[:, g, fc, nt * 128:nt * 128 + nn], w2[:, fc, :], fc == 0, fc == 1,
                                           ['w2', 'gx'], [pk], inc=(fc == 1))
                                    cp('dve', vcmp[0:nn, nt, g, 0:64], ps[0:nn, 0:64], [pk], ['vcmp'])
                    S.barrier()
                qr = Ring([psb("qTb%d" % i, (128, SEQ)) for i in range(2)], "qTb")
                btr = Ring([psb("bt%d" % i, (128, NCT, 512)) for i in range(2)], "bt")
                pcr = Ring([psb("pc%d" % i, (128, NCT, 512)) for i in range(2)], "pc")
                osr = Ring([psb("os%d" % i, (128, 64)) for i in range(4)], "os")
                rdr = Ring([psb("rd%d" % i, (128, 2)) for i in range(4)], "rd")
                gate_sb = psb("gate_sb", (128, NT, 24))
                impacc = psb("impacc", (128, NT, 2, JB))
                with nc.allow_non_contiguous_dma(reason="gate"):
                    S.dma(gate_sb[:], gate_d.rearrange("(t p) c -> p t c", p=128),
                          [('gate', j, s) for j in range(QT) for s in range(4)], ['gate_sb'])
                for c in range(4):
                    qT, qk = qr.next()
                    S.dma(qT[:], qT_d[c, :, :], [('qT', c, j) for j in range(QT)], [qk])
                    for half in range(2):
                        h = c + 4 * half
                        g = half
                        hs = slice(64 * half, 64 * half + 64)
                        for jq in range(QT):
                            tq0 = 512 * jq
                            nts = [nt for nt in range(NCT) if 16 * 128 * nt + 31 <= tq0 + 511]
                            bt, bk = btr.next()
                            pc, pck = pcr.next()
                            for nt in nts:
                                S.dma(bt[:, nt, :], bct_d[h, nt * 128:(nt + 1) * 128, tq0:tq0 + 512], (), [bk])
                            for nt in nts:
                                ps, pk = psr.next()
                                mm(ps[:], kst[g][:, nt * 128:(nt + 1) * 128], qT[:, tq0:tq0 + 512], True, True, ['kcmpT', qk], [pk])
                                tt('dve', pc[:, nt, :], ps[:], bt[:, nt, :], ALU.add, [pk, bk], [pck])
                                act(pc[:, nt, :], pc[:, nt, :], AF.Exp, [pck], [pck])
                            for sub in range(4):
                                tsi = 4 * jq + sub
                                po, pok = psr.next()
                                W = 65 + JB
                                for nt in nts:
                                    mm(po[:, 0:W], pc[:, nt, sub * 128:(sub + 1) * 128], vcmp[:, nt, g, :], nt == nts[0], nt == nts[-1],
                                       [pck, 'vcmp'], [pok], inc=(nt == nts[-1]))
                                rd, rk = rdr.next()
                                ts('dve', rd[:, 0:1], po[:, 64:65], 1e-30, ALU.add, [pok], [rk])
                                recip(rd[:, 1:2], rd[:, 0:1], [rk], [rk])
                                o, ok = osr.next()
                                ts('dve', o[:], po[:, 0:64], rd[:, 1:2], ALU.mult, [pok, rk, 'gate_sb'], [ok],
                                   s2=gate_sb[:, tsi, 3 * h:3 * h + 1], op1=ALU.mult)
                                S.dma(obr_d[0, tsi * 128:(tsi + 1) * 128, 64 * h:64 * h + 64], o[:], [ok], [('obr', 0, h, tsi)], q='pool')
                                if c == 0:
                                    ts('dve', impacc[:, tsi, g, :], po[:, 65:W], rd[:, 1:2], ALU.mult, [pok, rk], [('imp', tsi, g)])
                                else:
                                    stt(impacc[:, tsi, g, :], po[:, 65:W], rd[:, 1:2], impacc[:, tsi, g, :], ALU.mult, ALU.add,
                                        [pok, rk, ('imp', tsi, g)], [('imp', tsi, g)])
                selM = psb("selM", (128, NT, JB))
                selA = psb("selA", (128, NT, JB))
                memset('pool', selM[:], 1.0, ['selM'])
                asel(selM[:], selM[:], [[128, NT], [-64, JB]], -128, 1, 0.0, ['selM'], ['selM'])
                memset('pool', selM[:, :, 0:1], 0.0, ['selM'])
                memset('pool', selA[:], 0.0, ['selA'])
                asel(selA[:], selA[:], [[128, NT], [-64, JB]], -128, 1, 1e9, ['selA'], ['selA'])
                asel(selA[:], selA[:], [[128, NT], [-64, JB]], 0, 1, -1.0, ['selA'], ['selA'])
                memset('pool', selA[:, :, 0:1], 1e9, ['selA'])
                scr = Ring([psb("sc%d" % i, (128, 2, JB)) for i in range(2)], "sc")
                sc2r = Ring([psb("sd%d" % i, (128, JB)) for i in range(2)], "sd")
                m8r = Ring([psb("m8%d" % i, (128, 16)) for i in range(4)], "m8")
                sbr = Ring([psb("sbi%d" % i, (128, 128)) for i in range(2)], "sbi")
                sto = Ring([psb("sto%d" % i, (128, 128)) for i in range(2)], "sto")
                for tsi in range(NT):
                    sc, sck = scr.next()
                    sbi, sbk = sbr.next()
                    if JB < 64:
                        memset('pool', sbi[:], 0.0, [sbk])
                    for g in range(2):
                        tt('dve', sc[:, g, :], impacc[:, tsi, g, :], selM[:, tsi, :], ALU.mult, [('imp', tsi, g), 'selM'], [sck])
                        tt('dve', sc[:, g, :], sc[:, g, :], selA[:, tsi, :], ALU.add, [sck, 'selA'], [sck])
                        m8, mk = m8r.next()
                        sd, sdk = sc2r.next()
                        S.op('dve', lambda e, a=m8, b=sc, g=g: e.max(out=a[:, 0:8], in_=b[:, g, :]), [sck], [mk])
                        S.op('dve', lambda e, a=m8, b=sc, d=sd, g=g: e.match_replace(out=d[:], in_to_replace=a[:, 0:8], in_values=b[:, g, :],
                                                                                   imm_value=-3e38), [sck, mk], [sdk])
                        S.op('dve', lambda e, a=m8, d=sd: e.max(out=a[:, 8:16], in_=d[:]), [sdk], [mk])
                        ts('dve', sbi[:, 64 * g:64 * g + JB], sc[:, g, :], m8[:, 15:16], ALU.is_ge, [sck, mk], [sbk], s2=-NEGM, op1=ALU.mult)
                        ts('dve', sbi[:, 64 * g:64 * g + JB], sbi[:, 64 * g:64 * g + JB], NEGM, ALU.add, [sbk], [sbk])
                    ps, pk = psr.next()
                    tr(ps[:, 0:128], sbi[:], ident[:], [sbk, 'ident'], [pk])
                    so, sok = sto.next()
                    act(so[:], ps[:, 0:128], AF.Copy, [pk], [sok])
                    S.dma(selbT_d[:, tsi * 128:(tsi + 1) * 128], so[:], [sok], [('selbT', tsi)], q='pool')
                S.barrier()

        def phase_C(l):
            for br in (1, 2):
                with ExitStack() as ph:
                    def psb(name, shape):
                        return ph.enter_context(nc.sbuf_tensor("%s_C%d_%d" % (name, l, br), list(shape), F32))
                    Wt = WGEN if br == 1 else WWIN
                    tab_d = bgen_d if br == 1 else bwin_d
                    KT_d = kslcT_d if br == 1 else kwinT_d
                    V_d = vslc_d if br == 1 else vwin_d
                    tab = psb("tab", (128, NH, Wt))
                    for h in range(NH):
                        S.dma(tab[:, h, :], tab_d[:, h, :], (), ['tab'])
                    LS = [psb("LS%d" % g, (128, SEQ)) for g in range(2)]
                    for g in range(2):
                        if br == 1:
                            S.dma(LS[g][0:64, :], KT_d[64 * g:64 * g + 64, :], (), [('LS', g)])
                            v = LS[g][64:128, :]
                            memset('pool', v, 1.0, [('LS', g)])
                            asel(v, v, [[1, SEQ]], 0, -64, 0.0, [('LS', g)], [('LS', g)])
                            asel(v, v, [[-1, SEQ]], 63, 64, 0.0, [('LS', g)], [('LS', g)])
                        else:
                            memset('pool', LS[g][64 * (1 - g):64 * (1 - g) + 64, :], 0.0, [('LS', g)])
                            S.dma(LS[g][64 * g:64 * g + 64, :], KT_d[64 * g:64 * g + 64, :], (), [('LS', g)])
                    V = psb("V", (128, NT, 2, 65))
                    memset('pool', V[:, :, :, 64:65], 1.0, ['V'])
                    with nc.allow_non_contiguous_dma(reason="V"):
                        for g in range(2):
                            S.dma(V[:, :, g, 0:64], V_d.rearrange("(t p) c -> p t c", p=128)[:, :, 64 * g:64 * g + 64], (), ['V'])
                    gate_sb = psb("gate_sb", (128, NT, 24))
                    with nc.allow_non_contiguous_dma(reason="gate"):
                        S.dma(gate_sb[:], gate_d.rearrange("(t p) c -> p t c", p=128), (), ['gate_sb'])
                    qr = Ring([psb("qTc%d" % i, (128, SEQ)) for i in range(2)], "qTc")
                    ptr = Ring([psb("pt%d" % i, (128, 512)) for i in range(4)], "pt")
                    osr = Ring([psb("os%d" % i, (128, 64)) for i in range(4)], "os")
                    rdr = Ring([psb("rd%d" % i, (128, 2)) for i in range(4)], "rd")
                    b31 = psb("b31", (128, NH))
                    with nc.allow_non_contiguous_dma(reason="b31"):
                        S.dma(b31[:], fdg_in[:, NEGPAD + OFFMAX - 1].partition_broadcast(128), (), ['b31'])
                    psr4 = Ring(PS[0:4], "ps")
                    LAG = 2
                    for c in range(4):
                        if br == 2:
                            qT, qk = qr.next()
                            S.dma(qT[:], qT_d[c, :, :], (), [qk])
                        for half in range(2):
                            h = c + 4 * half
                            g = half
                            if br == 1:
                                qT, qk = qr.next()
                                S.dma(qT[0:64, :], qT_d[c, 64 * half:64 * half + 64, :], (), [qk])
                                S.dma(qT[64:128, :], selbT_d[64 * g:64 * g + 64, :], (), [qk])
                            pend = []

                            def stage3(item):
                                jq_, tk0_, pt_, ptk_, tks_, last_ = item
                                tq0_ = 512 * jq_
                                for sub in range(4):
                                    if tk0_ > tq0_ + 128 * sub + 127:
                                        continue
                                    mm(PS[4 + sub][:, 0:65], pt_[:, sub * 128:(sub + 1) * 128], V[:, tk0_ // 128, g, :],
                                       tk0_ == tks_[0], tk0_ == last_[sub], [ptk_, 'V'], [('ps', 4 + sub)], inc=(tk0_ == last_[sub]))
                                if tk0_ == tks_[-1]:
                                    for sub in range(4):
                                        tsi = 4 * jq_ + sub
                                        po = PS[4 + sub]
                                        pok = ('ps', 4 + sub)
                                        rd, rk = rdr.next()
                                        ts('dve', rd[:, 0:1], po[:, 64:65], 1e-30, ALU.add, [pok], [rk])
                                        recip(rd[:, 1:2], rd[:, 0:1], [rk], [rk])
                                        o, ok = osr.next()
                                        ts('dve', o[:], po[:, 0:64], rd[:, 1:2], ALU.mult, [pok, rk, 'gate_sb'], [ok],
                                           s2=gate_sb[:, tsi, 3 * h + br:3 * h + br + 1], op1=ALU.mult)
                                        S.dma(obr_d[br, tsi * 128:(tsi + 1) * 128, 64 * h:64 * h + 64], o[:], [ok], [('obr', br, h, tsi)], q='pool')

                            for jq in range(QT):
                                tq0 = 512 * jq
                                lo = 0 if br == 1 else max(0, tq0 - 512)
                                tks = list(range(lo, tq0 + 512, 128))
                                last = {sub: max(tk for tk in tks if tk <= tq0 + 128 * sub + 127) for sub in range(4)}
                                for tk0 in tks:
                                    ps, pk = psr4.next()
                                    mm(ps[:], LS[g][:, tk0:tk0 + 128], qT[:, tq0:tq0 + 512], True, True, [('LS', g), qk], [pk])
                                    pt, ptk = ptr.next()
                                    if tq0 - tk0 >= OFFMAX + 128:
                                        act(pt[:], ps[:], AF.Exp, [pk, 'b31'], [ptk], bias=b31[:, h:h + 1])
                                    else:
                                        off = min(tq0 - tk0, OFFMAX) + U0
                                        tt('dve', pt[:], ps[:], tab[:, h, off:off + 512], ALU.add, [pk, 'tab'], [ptk])
                                        act(pt[:], pt[:], AF.Exp, [ptk], [ptk])
                                    pend.append((jq, tk0, pt, ptk, tks, last))
                                    if len(pend) > LAG:
                                        stage3(pend.pop(0))
                            while pend:
                                stage3(pend.pop(0))
                    S.barrier()
        def bc(ap2, n, axis):
            P, A = ap2.shape
            if axis == 2:
                return ap2.unsqueeze(2).to_broadcast([P, A, n])
            return ap2.unsqueeze(1).to_broadcast([P, n, A])

        def phase_D(l):
            with ExitStack() as ph:
                def psb(name, shape):
                    return ph.enter_context(nc.sbuf_tensor("%s_D1%d" % (name, l), list(shape), F32))
                xr = Ring([psb("xin%d" % i, (128, SEQ + 3)) for i in range(2)], "xin")
                ar = Ring([psb("acc%d" % i, (128, SEQ)) for i in range(2)], "acc")
                sq = Ring([psb("sq%d" % i, (128, 512)) for i in range(2)], "sq")
                cw = psb("cw", (128, 4, 12))
                with nc.allow_non_contiguous_dma(reason="conv w"):
                    for i in range(4):
                        S.dma(cw[:, i, :], dn_conv_w[l, i].rearrange("(c p) -> p c", p=128), (), ['cw'])
                for ch in range(12):
                    xin, xk = xr.next()
                    acc, ak = ar.next()
                    memset('pool', xin[:, 0:3], 0.0, [xk])
                    S.dma(xin[:, 3:SEQ + 3], dnraw_d[ch, :, :], (), [xk])
                    ts('dve', acc[:], xin[:, 0:SEQ], cw[:, 0, ch:ch + 1], ALU.mult, [xk, 'cw'], [ak])
                    for i in range(1, 4):
                        stt(acc[:], xin[:, i:SEQ + i], cw[:, i, ch:ch + 1], acc[:], ALU.mult, ALU.add, [xk, 'cw', ak], [ak])
                    act(acc[:], acc[:], AF.Silu, [ak], [ak])
                    if ch < 8:
                        for j in range(QT):
                            cs = slice(512 * j, 512 * j + 512)
                            q1, qk1 = sq.next()
                            act(q1[:], acc[:, cs], AF.Square, [ak], [qk1])
                            p2, pk2 = psr.next()
                            mm(p2[:], bdones[:], q1[:], True, True, ['bdones', qk1], [pk2])
                            act(q1[:], p2[:], AF.Sqrt, [pk2], [qk1], bias=EPS, scale=1.0)
                            recip(q1[:], q1[:], [qk1], [qk1])
                            if ch < 4:
                                stt(acc[:, cs], acc[:, cs], HD ** -0.5, q1[:], ALU.mult, ALU.mult, [ak, qk1], [ak])
                            else:
                                tt('dve', acc[:, cs], acc[:, cs], q1[:], ALU.mult, [ak, qk1], [ak])
                    S.dma(dnc_d[ch, :, :], acc[:], [ak], [('dnc', ch)], q='pool')
                S.barrier()
            if 'stopD1' in dbg:
                return
            with ExitStack() as ph:
                def psb(name, shape):
                    return ph.enter_context(nc.sbuf_tensor("%s_D2%d" % (name, l), list(shape), F32))
                NG = NCH * 8
                g_tm = psb("g_tm", (64, NCH, 8))
                b_tm = psb("b_tm", (64, NCH, 8))
                gc_tm = psb("gc_tm", (64, NCH, 8))
                eg_tm = psb("eg_tm", (64, NCH, 8))
                bg_tm = psb("bg_tm", (64, NCH, 8))
                kds_tm = psb("kds_tm", (64, NCH, 8))
                egl = psb("egl", (64, NCH, 8))
                sel63 = psb("sel63", (64, 64))
                ngrow = psb("ngrow", (64, 64))
                with nc.allow_non_contiguous_dma(reason="bg"):
                    S.dma(b_tm[:], bg_d.rearrange("(n i) c -> i n c", i=64)[:, :, 0:8], (), ['b_tm'])
                    S.dma(g_tm[:], bg_d.rearrange("(n i) c -> i n c", i=64)[:, :, 8:16], (), ['g_tm'])
                S.dma(ngrow[:], dn_norm_g[l].partition_broadcast(64), (), ['ngrow'])
                ts('dve', sel63[:], ones[0:64, 0:64], ident[0:64, 63:64], ALU.mult, ['ones', 'ident'], ['sel63'])
                gflat = g_tm[:].rearrange("p n h -> p (n h)")
                gcflat = gc_tm[:].rearrange("p n h -> p (n h)")
                for c0 in range(0, NG, 512):
                    cw_ = min(512, NG - c0)
                    ps, pk = psr.next()
                    mm(ps[0:64, 0:cw_], UT[0:64, :], gflat[:, c0:c0 + cw_], True, True, ['UT', 'g_tm'], [pk])
                    cp('dve', gcflat[:, c0:c0 + cw_], ps[0:64, 0:cw_], [pk], ['gc_tm'])
                    ps2, pk2 = psr.next()
                    mm(ps2[0:64, 0:cw_], sel63[:], gcflat[:, c0:c0 + cw_], True, True, ['sel63', 'gc_tm'], [pk2])
                    act(egl[:].rearrange("p n h -> p (n h)")[:, c0:c0 + cw_], ps2[0:64, 0:cw_], AF.Exp, [pk2], ['egl'])
                    tt('dve', kds_tm[:].rearrange("p n h -> p (n h)")[:, c0:c0 + cw_], ps2[0:64, 0:cw_], gcflat[:, c0:c0 + cw_],
                       ALU.subtract, [pk2, 'gc_tm'], ['kds_tm'])
                act(kds_tm[:], kds_tm[:], AF.Exp, ['kds_tm'], ['kds_tm'])
                act(eg_tm[:], gc_tm[:], AF.Exp, ['gc_tm'], ['eg_tm'])
                tt('dve', bg_tm[:], b_tm[:], eg_tm[:], ALU.mult, ['b_tm', 'eg_tm'], ['bg_tm'])

                def t3(name):
                    return psb(name, (64, 8, 64))
                NPS, NOS = 2, 4
                Pt = [{nm: t3("%s_%d" % (nm, i)) for nm in ['ktm', 'vtm', 'qtm', 'rhsg', 'rhsb', 'DTr', 'decT', 'dec', 't1', 'Xa', 'Xb', 'Ya', 'Yb', 'TT', 'vb', 'kbg']} for i in range(NPS)]
                Ot = [{nm: t3("%s_%d" % (nm, i)) for nm in ['AT', 'kd', 'qdT', 'wT', 'u_sb']} for i in range(NOS)]
                Xs_ = [psb("X%d" % i, (64, 24, 64)) for i in range(NPS)]
                zs_ = [psb("z%d" % i, (64, 512)) for i in range(NOS)]
                vn, Sst, o_sb, osq = t3("vn"), t3("Sst"), t3("o_sb"), t3("osq")
                ssum = psb("ssum", (64, 16))
                ytm = psb("ytm", (64, 512))
                ybo = Ring([psb("ybo%d" % i, (128, 4, 64)) for i in range(2)], "ybo")
                memset('pool', Sst[:], 0.0, ['Sst'])
                I64 = ident[0:64, 0:64]

                def hmm(pst, pk, lhs, rhs, r, first=True, last=True):
                    for h in range(8):
                        mm(pst[0:64, 64 * h:64 * h + 64], lhs(h), rhs(h), first, last, r, [pk], inc=(h == 7))

                def v3(ps):
                    return ps[0:64, :].rearrange("p (h c) -> p h c", h=8)

                def pre_gen(n, sp_, so_):
                    ktm, vtm, qtm, rhsg, rhsb, DTr, decT, dec, t1, Xa, Xb, Ya, Yb, TT, vb, kbg = [Pt[sp_][k_] for k_ in ['ktm', 'vtm', 'qtm', 'rhsg', 'rhsb', 'DTr', 'decT', 'dec', 't1', 'Xa', 'Xb', 'Ya', 'Yb', 'TT', 'vb', 'kbg']]
                    AT, kd, qdT, wT, u_sb = [Ot[so_][k_] for k_ in ['AT', 'kd', 'qdT', 'wT', 'u_sb']]
                    X, Xk = Xs_[sp_], ('X', sp_)
                    with nc.allow_non_contiguous_dma(reason="chunk load"):
                        S.dma(X[:], dnc_d.rearrange("c (a p) t -> p (c a) t", a=2)[:, :, 64 * n:64 * n + 64], (), [Xk])
                        yield
                    z, zk = zs_[so_], ('z', so_)
                    S.dma(z[:], zs_d[64 * n:64 * n + 64, :], (), [zk])
                    yield
                    for (dst, dk_, c0) in ((qtm, ('qtm', sp_), 0), (ktm, ('ktm', sp_), 8), (vtm, ('vtm', sp_), 16)):
                        ps, pk = prg[sp_].next()
                        for kc in range(8):
                            tr(ps[0:64, 64 * kc:64 * kc + 64], X[:, c0 + kc, :], I64, [Xk, 'ident'], [pk], inc=(kc == 7))
                            yield
                        if dk_ == ('ktm', sp_):
                            cp('dve', dst[:], v3(ps), [pk], [dk_])
                            yield
                        else:
                            act(dst[:], v3(ps), AF.Copy, [pk], [dk_])
                            yield
                    tt('dve', rhsg[:], bc(g_tm[:, n, :], 64, 2), bc(UT[0:64, :], 8, 1), ALU.mult, ['g_tm', 'UT'], [('rhsg', sp_)])
                    yield
                    tt('dve', rhsb[:], bc(b_tm[:, n, :], 64, 2), bc(I64, 8, 1), ALU.mult, ['b_tm', 'ident'], [('rhsb', sp_)])
                    yield
                    pg, pgk = prg[sp_].next()
                    mm(pg[0:64, :], ones[0:64, 0:64], rhsg[:].rearrange("p h c -> p (h c)"), True, True, ['ones', ('rhsg', sp_)], [pgk])
                    yield
                    pb, pbk = prg[sp_].next()
                    mm(pb[0:64, :], ones[0:64, 0:64], rhsb[:].rearrange("p h c -> p (h c)"), True, True, ['ones', ('rhsb', sp_)], [pbk])
                    yield
                    tt('dve', DTr[:], v3(pg), bc(gc_tm[:, n, :], 64, 2), ALU.subtract, [pgk, 'gc_tm'], [('DTr', sp_)])
                    yield
                    tt('dve', decT[:], DTr[:], bc(maskU[:], 8, 1), ALU.add, [('DTr', sp_), 'maskU'], [('decT', sp_)])
                    yield
                    act(decT[:], decT[:], AF.Exp, [('decT', sp_)], [('decT', sp_)])
                    yield
                    ts('dve', dec[:], DTr[:], -1.0, ALU.mult, [('DTr', sp_)], [('dec', sp_)])
                    yield
                    tt('dve', dec[:], dec[:], bc(maskL[:], 8, 1), ALU.add, [('dec', sp_), 'maskL'], [('dec', sp_)])
                    yield
                    act(dec[:], dec[:], AF.Exp, [('dec', sp_)], [('dec', sp_)])
                    yield
                    pkk, pkkk = prg[sp_].next()
                    hmm(pkk, pkkk, lambda h: X[:, 8 + h, :], lambda h: X[:, 8 + h, :], [Xk])
                    yield
                    pqk, pqkk = prg[sp_].next()
                    hmm(pqk, pqkk, lambda h: X[:, 8 + h, :], lambda h: X[:, h, :], [Xk])
                    yield
                    tt('dve', AT[:], v3(pqk), decT[:], ALU.mult, [pqkk, ('decT', sp_)], [('AT', so_)])
                    yield
                    tt('dve', t1[:], v3(pkk), decT[:], ALU.mult, [pkkk, ('decT', sp_)], [('t1', sp_)])
                    yield
                    tt('dve', t1[:], v3(pb), t1[:], ALU.mult, [pbk, ('t1', sp_)], [('t1', sp_)])
                    yield
                    tt('dve', Ya[:], t1[:], bc(nsU[:], 8, 1), ALU.mult, [('t1', sp_), 'nsU'], [('Ya', sp_)])
                    yield
                    tt('dve', t1[:], v3(pkk), dec[:], ALU.mult, [pkkk, ('dec', sp_)], [('t1', sp_)])
                    yield
                    tt('dve', t1[:], t1[:], bc(b_tm[:, n, :], 64, 2), ALU.mult, [('t1', sp_), 'b_tm'], [('t1', sp_)])
                    yield
                    tt('dve', Xa[:], t1[:], bc(nsL[:], 8, 1), ALU.mult, [('t1', sp_), 'nsL'], [('Xa', sp_)])
                    yield
                    tt('dve', TT[:], Ya[:], bc(I64, 8, 1), ALU.add, [('Ya', sp_), 'ident'], [('TT', sp_)])
                    yield
                    Xc, Xn_, Yc, Yn_ = (Xa, ('Xa', sp_)), (Xb, ('Xb', sp_)), (Ya, ('Ya', sp_)), (Yb, ('Yb', sp_))
                    for lvl in range(1, 6):
                        p1, p1k = prg[sp_].next()
                        hmm(p1, p1k, lambda h: Yc[0][:, h, :], lambda h: Xc[0][:, h, :], [Yc[1], Xc[1]])
                        yield
                        if lvl < 5:
                            p2, p2k = prg[sp_].next()
                            hmm(p2, p2k, lambda h: Xc[0][:, h, :], lambda h: Yc[0][:, h, :], [Yc[1], Xc[1]])
                            yield
                        act(Xn_[0][:], v3(p1), AF.Copy, [p1k], [Xn_[1]])
                        yield
                        if lvl < 5:
                            cp('dve', Yn_[0][:], v3(p2), [p2k], [Yn_[1]])
                            yield
                        Xc, Xn_ = Xn_, Xc
                        if lvl < 5:
                            Yc, Yn_ = Yn_, Yc
                        p3, p3k = prg[sp_].next()
                        hmm(p3, p3k, lambda h: Xc[0][:, h, :], lambda h: TT[:, h, :], [Xc[1], ('TT', sp_)])
                        yield
                        tt('dve', TT[:], TT[:], v3(p3), ALU.add, [('TT', sp_), p3k], [('TT', sp_)])
                        yield
                    tt('dve', vb[:], vtm[:], bc(b_tm[:, n, :], 64, 2), ALU.mult, [('vtm', sp_), 'b_tm'], [('vb', sp_)])
                    yield
                    tt('dve', kbg[:], ktm[:], bc(bg_tm[:, n, :], 64, 2), ALU.mult, [('ktm', sp_), 'bg_tm'], [('kbg', sp_)])
                    yield
                    tt('dve', kd[:], ktm[:], bc(kds_tm[:, n, :], 64, 2), ALU.mult, [('ktm', sp_), 'kds_tm'], [('kd', so_)])
                    yield
                    tt('dve', qtm[:], qtm[:], bc(eg_tm[:, n, :], 64, 2), ALU.mult, [('qtm', sp_), 'eg_tm'], [('qtm', sp_)])
                    yield
                    pu, puk = prg[sp_].next()
                    hmm(pu, puk, lambda h: TT[:, h, :], lambda h: vb[:, h, :], [('TT', sp_), ('vb', sp_)])
                    yield
                    act(u_sb[:], v3(pu), AF.Copy, [puk], [('u_sb', so_)])
                    yield
                    pw, pwk = prg[sp_].next()
                    hmm(pw, pwk, lambda h: kbg[:, h, :], lambda h: TT[:, h, :], [('TT', sp_), ('kbg', sp_)])
                    yield
                    act(wT[:], v3(pw), AF.Copy, [pwk], [('wT', so_)])
                    yield
                    pq, pqk2 = prg[sp_].next()
                    for h in range(8):
                        tr(pq[0:64, 64 * h:64 * h + 64], qtm[:, h, :], I64, [('qtm', sp_), 'ident'], [pqk2], inc=(h == 7))
                        yield
                    cp('dve', qdT[:], v3(pq), [pqk2], [('qdT', so_)])
                    yield
                def scan_gen(n, so_):
                    AT, kd, qdT, wT, u_sb = [Ot[so_][k_] for k_ in ['AT', 'kd', 'qdT', 'wT', 'u_sb']]
                    z, zk = zs_[so_], ('z', so_)
                    pv, pvk = psc.next()
                    hmm(pv, pvk, lambda h: wT[:, h, :], lambda h: Sst[:, h, :], [('wT', so_), 'Sst'])
                    yield
                    tt('dve', vn[:], u_sb[:], v3(pv), ALU.subtract, [('u_sb', so_), pvk], ['vn'])
                    yield
                    po, pok = psc.next()
                    for h in range(8):
                        mm(po[0:64, 64 * h:64 * h + 64], qdT[:, h, :], Sst[:, h, :], True, False, [('qdT', so_), 'Sst'], [pok], inc=False)
                        yield
                        mm(po[0:64, 64 * h:64 * h + 64], AT[:, h, :], vn[:, h, :], False, True, [('AT', so_), 'vn'], [pok], inc=(h == 7))
                        yield
                    pS, pSk = psc.next()
                    hmm(pS, pSk, lambda h: kd[:, h, :], lambda h: vn[:, h, :], [('kd', so_), 'vn'])
                    yield
                    tt('dve', Sst[:], Sst[:], bc(egl[:, n, :], 64, 2), ALU.mult, ['Sst', 'egl'], ['Sst'])
                    yield
                    tt('dve', Sst[:], Sst[:], v3(pS), ALU.add, ['Sst', pSk], ['Sst'])
                    yield
                    act(o_sb[:], v3(po), AF.Copy, [pok], ['o_sb'])
                    yield
                    tt('dve', osq[:], o_sb[:], o_sb[:], ALU.mult, ['o_sb'], ['osq'])
                    yield
                    S.op('dve', lambda e: e.tensor_reduce(out=ssum[:, 0:8], in_=osq[:], op=ALU.add, axis=AX.X), ['osq'], ['ssum'])
                    yield
                    act(ssum[:, 8:16], ssum[:, 0:8], AF.Sqrt, ['ssum'], ['ssum'], bias=EPS, scale=1.0 / 64)
                    yield
                    recip(ssum[:, 8:16], ssum[:, 8:16], ['ssum'], ['ssum'])
                    yield
                    tt('dve', o_sb[:], o_sb[:], bc(ssum[:, 8:16], 64, 2), ALU.mult, ['o_sb', 'ssum'], ['o_sb'])
                    yield
                    tt('dve', o_sb[:], o_sb[:], bc(ngrow[:], 8, 1), ALU.mult, ['o_sb', 'ngrow'], ['o_sb'])
                    yield
                    tt('dve', ytm[:], o_sb[:].rearrange("p h c -> p (h c)"), z[:], ALU.mult, ['o_sb', zk], ['ytm'])
                    yield
                    py, pyk = psc.next()
                    for kc in range(4):
                        tr(py[:, 64 * kc:64 * kc + 64], ytm[:, 128 * kc:128 * kc + 128], I64, ['ytm', 'ident'], [pyk], inc=(kc == 3))
                        yield
                    yo, yok = ybo.next()
                    act(yo[:], py[:, 0:256].rearrange("p (a b) -> p a b", a=4), AF.Copy, [pyk], [yok])
                    yield
                    with nc.allow_non_contiguous_dma(reason="yb store"):
                        S.dma(ybT_d.rearrange("c p t -> p c t")[:, :, 64 * n:64 * n + 64], yo[:], [yok], [('ybT', n)], q='pool')
                        yield


                S.barrier()
                prg = [Ring(PS[0:3], "psA"), Ring(PS[3:6], "psB")]
                psc = Ring(PS[6:8], "psC")

                def run_rr(gens):
                    gens = list(gens)
                    while gens:
                        for g_ in list(gens):
                            try:
                                next(g_)
                            except StopIteration:
                                gens.remove(g_)

                def scan_pair(a_, b_):
                    yield from scan_gen(a_, a_ % NOS)
                    yield from scan_gen(b_, b_ % NOS)
                prev_ = None
                for grp in range(NCH // 2):
                    a_, b_ = 2 * grp, 2 * grp + 1
                    gl = [pre_gen(a_, 0, a_ % NOS), pre_gen(b_, 1, b_ % NOS)]
                    if prev_ is not None:
                        gl.append(scan_pair(*prev_))
                    run_rr(gl)
                    prev_ = (a_, b_)
                run_rr([scan_pair(*prev_)])
                S.barrier()

        def phase_E(l, src_x):
            with ExitStack() as ph:
                def psb(name, shape):
                    return ph.enter_context(nc.sbuf_tensor("%s_E%d" % (name, l), list(shape), F32))
                wa = psb("wa", (128, 4, D))
                wb = psb("wb", (128, 4, D))
                wo = psb("wo", (128, 8, D))
                g1r = psb("g1r", (128, D))
                S.dma(g1r[:], mod_d[l, 2 * D:3 * D].partition_broadcast(128), (), ['g1r'])
                S.dma(wa[:], w_br_a[l].rearrange("(kc p) n -> p kc n", p=128), (), ['wa'])
                S.dma(wb[:], w_br_b[l].rearrange("(kc p) n -> p kc n", p=128), (), ['wb'])
                S.dma(wo[:], w_out[l].rearrange("(kc p) n -> p kc n", p=128), (), ['wo'])
                obr = Ring([psb("ob%d" % i, (128, 3, 512)) for i in range(2)], "ob")
                yaT = psb("yaT", (128, 4, 512))
                ybT = psb("ybT", (128, 4, 512))
                mg = Ring([psb("mg%d" % i, (128, 2, 512)) for i in range(2)], "mg")
                yT = psb("yT", (128, 8, 512))
                tmp = Ring([psb("tmp%d" % i, (128, 512)) for i in range(2)], "tmp")
                xr = Ring([psb("xe%d" % i, (128, D)) for i in range(2)], "xe")
                for j in range(QT):
                    t0 = 512 * j
                    cs = slice(t0, t0 + 512)
                    for sub in range(4):
                        r0 = t0 + 128 * sub
                        ob, obk = obr.next()
                        S.dma(ob[:], obr_d[:, r0:r0 + 128, :].rearrange("b p c -> p b c"), (), [obk])
                        tt('dve', ob[:, 0, :], ob[:, 0, :], ob[:, 1, :], ALU.add, [obk], [obk])
                        tt('pool', ob[:, 0, :], ob[:, 0, :], ob[:, 2, :], ALU.add, [obk], [obk])
                        transpose_to(ob[:, 0, :], obk, yaT, 'yaT', sub, nkc=4)
                    S.dma(ybT[:], ybT_d.rearrange("c p t -> p c t")[:, :, cs], (), ['ybT'])
                    for dc in range(8):
                        m, mk = mg.next()
                        S.dma(m[:, 0, :], mergeT_d[dc, :, cs], (), [mk])
                        S.dma(m[:, 1, :], mergeT_d[8 + dc, :, cs], (), [mk])
                        pa, pak = psr.next()
                        for kc in range(4):
                            mm(pa[:], wa[:, kc, 128 * dc:128 * dc + 128], yaT[:, kc, :], kc == 0, kc == 3, ['wa', 'yaT'], [pak], inc=(kc == 3))
                        pb, pbk = psr.next()
                        for kc in range(4):
                            mm(pb[:], wb[:, kc, 128 * dc:128 * dc + 128], ybT[:, kc, :], kc == 0, kc == 3, ['wb', 'ybT'], [pbk], inc=(kc == 3))
                        t, tk = tmp.next()
                        tt('dve', t[:], pa[:], m[:, 0, :], ALU.mult, [pak, mk], [tk])
                        tt('dve', m[:, 1, :], pb[:], m[:, 1, :], ALU.mult, [pbk, mk], [mk])
                        tt('pool', yT[:, dc, :], t[:], m[:, 1, :], ALU.add, [tk, mk], ['yT'])
                    for sub in range(4):
                        r0 = t0 + 128 * sub
                        xt, xk = xr.next()
                        S.dma(xt[:], src_x[r0:r0 + 128, :], [('x', r0 // 128)], [xk])
                        for nh in range(2):
                            po, pok = psr.next()
                            for kc in range(8):
                                mm(po[:], yT[:, kc, 128 * sub:128 * sub + 128], wo[:, kc, 512 * nh:512 * nh + 512], kc == 0, kc == 7,
                                   ['yT', 'wo'], [pok], inc=(kc == 7))
                            t, tk = tmp.next()
                            tt('dve', t[:], po[:], g1r[:, 512 * nh:512 * nh + 512], ALU.mult, [pok, 'g1r'], [tk])
                            tt('pool', xt[:, 512 * nh:512 * nh + 512], xt[:, 512 * nh:512 * nh + 512], t[:], ALU.add, [xk, tk], [xk])
                        S.dma(out[r0:r0 + 128, :], xt[:], [xk], [('x', r0 // 128)], q='pool')
                S.barrier()

        def phase_F(l):
            NE = 16
            with ExitStack() as ph:
                def psb(name, shape, dt=F32):
                    return ph.enter_context(nc.sbuf_tensor("%s_F%d" % (name, l), list(shape), dt))
                A2r = psb("A2r", (128, D))
                sh2r = psb("sh2r", (128, D))
                g2r = psb("g2r", (128, D))
                S.dma(sh2r[:], mod_d[l, 3 * D:4 * D].partition_broadcast(128), (), ['modrow'])
                S.dma(A2r[:], mod_d[l, 4 * D:5 * D].partition_broadcast(128), (), ['modrow'])
                S.dma(g2r[:], mod_d[l, 5 * D:6 * D].partition_broadcast(128), (), ['g2r'])
                xr = Ring([psb("xf%d" % i, (128, D)) for i in range(2)], "xf")
                hr = Ring([psb("hf%d" % i, (128, D)) for i in range(2)], "hf")
                sm = Ring([psb("smf%d" % i, (128, 8)) for i in range(4)], "smf")
                hTr = Ring([psb("hTf%d" % i, (128, 8, 256)) for i in range(2)], "hTf")
                rt = psb("rt", (128, 8, 16))
                rs = psb("rs", (128, 16))
                M1a = psb("M1a", (128, NT, NE))
                M2a = psb("M2a", (128, NT, NE))
                wn = psb("wn", (128, NT, 2))
                SU = psb("SU", (128, 128))
                memset('pool', SU[:], 1.0, ['SU'])
                asel(SU[:], SU[:], [[1, 128]], -1, -1, 0.0, ['SU'], ['SU'])
                for ti in range(NT):
                    hT, hTk = hTr.next()
                    ht, hk, xt, xk = norm_tile(out, 128 * ti, A2r[:], sh2r[:], xr, hr, sm)
                    S.dma(h2_d[128 * ti:128 * ti + 128, :], ht[:], [hk], [('h2', ti)], q='pool')
                    transpose_to(ht, hk, hT, hTk, 0)
                    pr, prk = psr.next()
                    for kc in range(8):
                        mm(pr[:, 0:16], hT[:, kc, 0:128], rtrw[:, kc, :], kc == 0, kc == 7, [hTk, 'rtrw'], [prk], inc=(kc == 7))
                    sc = rt[:, 0, :]
                    sel = rt[:, 1, :]
                    w1_ = rt[:, 2, :]
                    w2_ = rt[:, 3, :]
                    act(sc, pr[:, 0:16], AF.Sigmoid, [prk], ['rt'])
                    tt('dve', sel, sc, rtrb[:], ALU.add, ['rt', 'rtrb'], ['rt'])
                    sel3 = sel.rearrange("p (g e) -> p g e", g=4)
                    S.op('dve', lambda e, s3=sel3: e.tensor_reduce(out=rs[:, 0:4], in_=s3, op=ALU.max, axis=AX.X), ['rt'], ['rs'])
                    tt('dve', w1_.rearrange("p (g e) -> p g e", g=4), sel3, bc(rs[:, 0:4], 4, 2), ALU.is_ge, ['rt', 'rs'], ['rt'])
                    stt(w2_, w1_, -1e9, sel, ALU.mult, ALU.add, ['rt'], ['rt'])
                    S.op('dve', lambda e, a=w2_: e.tensor_reduce(out=rs[:, 4:8], in_=a.rearrange("p (g e) -> p g e", g=4), op=ALU.max, axis=AX.X),
                         ['rt'], ['rs'])
                    tt('dve', rs[:, 8:12], rs[:, 0:4], rs[:, 4:8], ALU.add, ['rs'], ['rs'])
                    S.op('dve', lambda e: e.tensor_reduce(out=rs[:, 12:13], in_=rs[:, 8:12], op=ALU.max, axis=AX.X), ['rs'], ['rs'])
                    ts('dve', rs[:, 4:8], rs[:, 8:12], rs[:, 12:13], ALU.is_ge, ['rs'], ['rs'], s2=-1.0, op1=ALU.add)
                    ts('dve', rs[:, 4:8], rs[:, 4:8], 1e9, ALU.mult, ['rs'], ['rs'])
                    tt('dve', w1_.rearrange("p (g e) -> p g e", g=4), sel3, bc(rs[:, 4:8], 4, 2), ALU.add, ['rt', 'rs'], ['rt'])
                    S.op('dve', lambda e, a=w1_: e.max(out=rt[:, 4, 0:8], in_=a), ['rt'], ['rt'])
                    ts('dve', M1a[:, ti, :], w1_, rt[:, 4, 0:1], ALU.is_ge, ['rt'], ['M1a'])
                    ts('dve', w2_, w1_, rt[:, 4, 1:2], ALU.is_ge, ['rt'], ['rt'])
                    tt('dve', M2a[:, ti, :], w2_, M1a[:, ti, :], ALU.subtract, ['rt', 'M1a'], ['M2a'])
                    tt('dve', w2_, M1a[:, ti, :], sc, ALU.mult, ['rt', 'M1a'], ['rt'])
                    S.op('dve', lambda e, a=w2_: e.tensor_reduce(out=rs[:, 13:14], in_=a, op=ALU.add, axis=AX.X), ['rt'], ['rs'])
                    tt('dve', w2_, M2a[:, ti, :], sc, ALU.mult, ['rt', 'M2a'], ['rt'])
                    S.op('dve', lambda e, a=w2_: e.tensor_reduce(out=rs[:, 14:15], in_=a, op=ALU.add, axis=AX.X), ['rt'], ['rs'])
                    tt('dve', rs[:, 15:16], rs[:, 13:14], rs[:, 14:15], ALU.add, ['rs'], ['rs'])
                    recip(rs[:, 15:16], rs[:, 15:16], ['rs'], ['rs'])
                    ts('dve', wn[:, ti, :], rs[:, 13:15], rs[:, 15:16], ALU.mult, ['rs'], ['wn'])
                NG = NT * NE
                Mall = psb("Mall", (128, NT, NE))
                cnt = psb("cnt", (128, NT, NE))
                base = psb("base", (128, NT, NE))
                dest = psb("dest", (128, NT, NE))
                dtmp = psb("dtmp", (128, NT, NE))
                d12 = psb("d12", (128, 2, NT))
                idx12 = psb("idx12", (128, 2, NT), I32)
                ev = psb("ev", (128, 8, NE))
                ebf = psb("ebf", (128, NBK))
                bidx_i = psb("bidx_i", (128, NBK), I32)
                bidx = psb("bidx", (128, NBK))
                pcol_i = psb("pcol_i", (128, 1), I32)
                pcol = psb("pcol", (128, 1))
                widx = psb("widx", (128, NBK), I32)
                tt('dve', Mall[:], M1a[:], M2a[:], ALU.add, ['M1a', 'M2a'], ['Mall'])
                Mf = Mall[:].rearrange("p t e -> p (t e)")
                prk_, prkk = psr.next()
                pcn, pcnk = psr.next()
                for c0 in range(0, NG, 512):
                    cw_ = min(512, NG - c0)
                    assert NG <= 512
                    mm(prk_[:, 0:cw_], SU[:], Mf[:, c0:c0 + cw_], True, True, ['SU', 'Mall'], [prkk])
                    mm(pcn[:, 0:cw_], ones[:], Mf[:, c0:c0 + cw_], True, True, ['ones', 'Mall'], [pcnk])
                cp('dve', cnt[:].rearrange("p t e -> p (t e)"), pcn[:, 0:NG], [pcnk], ['cnt'])
                memset('dve', base[:, 0, :], 0.0, ['base'])
                for ti in range(1, NT):
                    tt('dve', base[:, ti, :], base[:, ti - 1, :], cnt[:, ti - 1, :], ALU.add, ['base', 'cnt'], ['base'])
                tt('dve', ev[:, 0, :], base[:, NT - 1, :], cnt[:, NT - 1, :], ALU.add, ['base', 'cnt'], ['ev'])
                memset('dve', ev[:, 1, :], 0.0, ['ev'])
                for m in range(SEQ // MBLK):
                    stt(ev[:, 1, :], ev[:, 0, :], float(MBLK * m), ev[:, 1, :], ALU.is_gt, ALU.add, ['ev'], ['ev'])
                ts('dve', ev[:, 2, :], ev[:, 1, :], float(MBLK), ALU.mult, ['ev'], ['ev'])
                cp('dve', ev[:, 3, 0:1], ev[:, 2, 0:1], ['ev'], ['ev'])
                for e_ in range(1, NE):
                    tt('dve', ev[:, 3, e_:e_ + 1], ev[:, 3, e_ - 1:e_], ev[:, 2, e_:e_ + 1], ALU.add, ['ev'], ['ev'])
                tt('dve', ev[:, 4, :], ev[:, 3, :], ev[:, 2, :], ALU.subtract, ['ev'], ['ev'])
                tt('dve', dest[:].rearrange("p t e -> p (t e)"), prk_[:, 0:NG], base[:].rearrange("p t e -> p (t e)"), ALU.add, [prkk, 'base'], ['dest'])
                tt('dve', dest[:], dest[:], bc(ev[:, 4, :], NT, 1), ALU.add, ['dest', 'ev'], ['dest'])
                for k_, Mk in ((0, M1a), (1, M2a)):
                    tt('dve', dtmp[:], dest[:], Mk[:], ALU.mult, ['dest', 'M1a', 'M2a'], ['dtmp'])
                    S.op('dve', lambda e, k_=k_: e.tensor_reduce(out=d12[:, k_, :], in_=dtmp[:], op=ALU.add, axis=AX.X), ['dtmp'], ['d12'])
                cp('dve', idx12[:], d12[:], ['d12'], ['idx12'])
                S.op('pool', lambda e: e.iota(bidx_i[:], pattern=[[MBLK, NBK]], base=0, channel_multiplier=0), (), ['bidx_i'])
                S.op('pool', lambda e: e.iota(pcol_i[:], pattern=[[0, 1]], base=0, channel_multiplier=1), (), ['pcol_i'])
                cp('dve', bidx[:], bidx_i[:], ['bidx_i'], ['bidx'])
                cp('dve', pcol[:], pcol_i[:], ['pcol_i'], ['pcol'])
                memset('dve', ebf[:], 0.0, ['ebf'])
                for e_ in range(NE):
                    stt(ebf[:], bidx[:], ev[:, 3, e_:e_ + 1], ebf[:], ALU.is_ge, ALU.add, ['bidx', 'ev', 'ebf'], ['ebf'])
                ts('dve', ebf[:], ebf[:], float(NE - 1), ALU.min, ['ebf'], ['ebf'], s2=128.0, op1=ALU.mult)
                ts('dve', ebf[:], ebf[:], pcol[:, 0:1], ALU.add, ['ebf', 'pcol'], ['ebf'], s2=float(l * 16 * 128), op1=ALU.add)
                cp('dve', widx[:], ebf[:], ['ebf'], ['widx'])
                for ti in range(NT):
                    ht, hk = hr.next()
                    S.dma(ht[:], h2_d[128 * ti:128 * ti + 128, :], [('h2', ti)], [hk], q='sp')
                    for k_ in range(2):
                        S.idma(Xs_d[:, :], ht[:, :], bass.IndirectOffsetOnAxis(ap=idx12[:, k_, ti:ti + 1], axis=0), None,
                               [hk, 'idx12'], [('Xs', ti, k_)])
                S.barrier()
                with ExitStack() as ph3:
                    def psb3(name, shape, dt=F32):
                        return ph3.enter_context(nc.sbuf_tensor("%s_F3%d" % (name, l), list(shape), dt))
                    wgr = Ring([psb3("wg%d" % i, (128, 8 * 512)) for i in range(2)], "wg")
                    wur = Ring([psb3("wu%d" % i, (128, 8 * 512)) for i in range(2)], "wu")
                    wdr = Ring([psb3("wd%d" % i, (128, 4 * D)) for i in range(2)], "wd")
                    xgr = Ring([psb3("xg%d" % i, (128, D)) for i in range(4)], "xg")
                    actT = psb3("actT", (128, 4, 256))
                    sg = Ring([psb3("sg%d" % i, (128, 256)) for i in range(2)], "sg")
                    yor = Ring([psb3("yo%d" % i, (128, D)) for i in range(2)], "yo")
                    wg_v = moe_wg.rearrange("l e (p kc) n -> (l e p) (kc n)", kc=8)
                    wu_v = moe_wu.rearrange("l e (p kc) n -> (l e p) (kc n)", kc=8)
                    wd_v = moe_wd.rearrange("l e (p fc) n -> (l e p) (fc n)", fc=4)
                    deferred = []
                    for b_ in range(NBK):
                        off = bass.IndirectOffsetOnAxis(ap=widx[:, b_:b_ + 1], axis=0)
                        wg, wgk = wgr.next()
                        wu, wuk = wur.next()
                        wd, wdk = wdr.next()
                        S.idma(wg[:, :], wg_v, None, off, ['widx'], [wgk])
                        S.idma(wu[:, :], wu_v, None, off, ['widx'], [wuk])
                        S.idma(wd[:, :], wd_v, None, off, ['widx'], [wdk])
                        hT, hTk = hTr.next()
                        xgs = []
                        for sub in range(2):
                            xg, xgk = xgr.next()
                            r0 = b_ * MBLK + 128 * sub
                            S.dma(xg[:], Xs_d[r0:r0 + 128, :], (), [xgk], q='sp')
                            xgs.append((xg, xgk))
                        for d_ in deferred:
                            S.dma(*d_[0], **d_[1])
                        deferred = []
                        for sub in range(2):
                            xg, xgk = xgs[sub]
                            for k0 in (0, 4):
                                ps, pk = psr.next()
                                for kc in range(k0, k0 + 4):
                                    tr(ps[:, (kc - k0) * 128:(kc - k0 + 1) * 128], xg[:, kc:D:8], ident[:], [xgk, 'ident'], [pk], inc=(kc == k0 + 3))
                                dst = hT[:, k0:k0 + 4, sub * 128:(sub + 1) * 128]
                                srcv = ps[:].rearrange("p (a b) -> p a b", a=4)
                                if k0 == 0:
                                    act(dst, srcv, AF.Copy, [pk], [hTk])
                                else:
                                    cp('dve', dst, srcv, [pk], [hTk])
                        for fc in range(4):
                            pg_, pgk_ = psr.next()
                            for kc in range(8):
                                mm(pg_[:, 0:256], wg[:, kc * 512 + fc:(kc + 1) * 512:4], hT[:, kc, :], kc == 0, kc == 7, [wgk, hTk], [pgk_], inc=(kc == 7))
                            pu_, puk_ = psr.next()
                            for kc in range(8):
                                mm(pu_[:, 0:256], wu[:, kc * 512 + fc:(kc + 1) * 512:4], hT[:, kc, :], kc == 0, kc == 7, [wuk, hTk], [puk_], inc=(kc == 7))
                            s_, sk_ = sg.next()
                            act(s_[:], pg_[:, 0:256], AF.Silu, [pgk_], [sk_])
                            tt('dve', actT[:, fc, :], pu_[:, 0:256], s_[:], ALU.mult, [puk_, sk_], [('actT', fc)])
                        for sub in range(2):
                            yo, yok = yor.next()
                            for nh in range(2):
                                pd, pdk = psr.next()
                                for fc in range(4):
                                    mm(pd[:], actT[:, fc, 128 * sub:128 * sub + 128], wd[:, fc * D + 512 * nh:fc * D + 512 * nh + 512], fc == 0, fc == 3,
                                       [('actT', fc), wdk], [pdk], inc=(fc == 3))
                                if nh == 0:
                                    act(yo[:, 0:512], pd[:], AF.Copy, [pdk], [yok])
                                else:
                                    cp('dve', yo[:, 512:1024], pd[:], [pdk], [yok])
                            r0 = b_ * MBLK + 128 * sub
                            deferred.append(((Ys_d[r0:r0 + 128, :], yo[:], [yok], [('Ys', b_, sub)]), dict(q='sp')))
                    for d_ in deferred:
                        S.dma(*d_[0], **d_[1])
                    S.barrier()
                y1r = Ring([psb("y1_%d" % i, (128, D)) for i in range(2)], "y1")
                y2r = Ring([psb("y2_%d" % i, (128, D)) for i in range(2)], "y2")
                for ti in range(NT):
                    y1, y1k = y1r.next()
                    y2, y2k = y2r.next()
                    S.idma(y1[:, :], Ys_d[:, :], None, bass.IndirectOffsetOnAxis(ap=idx12[:, 0, ti:ti + 1], axis=0), ['idx12'], [y1k])
                    S.idma(y2[:, :], Ys_d[:, :], None, bass.IndirectOffsetOnAxis(ap=idx12[:, 1, ti:ti + 1], axis=0), ['idx12'], [y2k])
                    xt, xk = xr.next()
                    S.dma(xt[:], out[128 * ti:128 * ti + 128, :], [('x', ti)], [xk], q='sp')
                    ts('dve', y1[:], y1[:], wn[:, ti, 0:1], ALU.mult, [y1k, 'wn'], [y1k])
                    stt(y1[:], y2[:], wn[:, ti, 1:2], y1[:], ALU.mult, ALU.add, [y2k, 'wn', y1k], [y1k])
                    tt('pool', y1[:], y1[:], g2r[:], ALU.mult, [y1k, 'g2r'], [y1k])
                    tt('pool', xt[:], xt[:], y1[:], ALU.add, [xk, y1k], [xk])
                    S.dma(out[128 * ti:128 * ti + 128, :], xt[:], [xk], [('x', ti)], q='sp')
                S.barrier()

        with ExitStack() as ph:
            modrow = ph.enter_context(nc.sbuf_tensor("modrow", [128, 6 * D], F32))
            rowtmp = ph.enter_context(nc.sbuf_tensor("rowtmp", [128, D], F32))
            wr0 = Ring([ph.enter_context(nc.sbuf_tensor("wsl0_%d" % i, [128, 8, 512], F32)) for i in range(2)], "wsl0")
            for l in range(L):
                S.dma(modrow[:], ada_b[l].partition_broadcast(128), ['modrow'], ['modrow'])
                for oc in range(12):
                    wt, wk = load_w(wr0, ada_w[l], oc * 512, 512)
                    ps, pk = psr.next()
                    for kc in range(8):
                        mm(ps[:], cbc[:, kc, :], wt[:, kc, :], kc == 0, kc == 7, ['cbc', wk], [pk], inc=(kc == 7))
                    tt('dve', modrow[:, oc * 512:(oc + 1) * 512], ps[:], modrow[:, oc * 512:(oc + 1) * 512], ALU.add,
                       [pk, 'modrow'], ['modrow'])
                for (o_, ng) in ((1, norm1_g), (4, norm2_g)):
                    S.dma(rowtmp[:], ng[l].partition_broadcast(128), ['rowtmp'], ['rowtmp'])
                    stt(modrow[:, o_ * D:(o_ + 1) * D], modrow[:, o_ * D:(o_ + 1) * D], 1.0, rowtmp[:], ALU.add, ALU.mult,
                        ['modrow', 'rowtmp'], ['modrow'])
                S.dma(mod_d[l:l + 1, :], modrow[0:1, :], ['modrow'], [('mod', l)], q='pool')
            S.barrier()

        for l in range(L if 'stop0' not in dbg else 0):
            src_x = x_in if l == 0 else out
            with ExitStack() as phm:
                A1t = phm.enter_context(nc.sbuf_tensor("A1t_%d" % l, [128, D], F32))
                sh1t = phm.enter_context(nc.sbuf_tensor("sh1t_%d" % l, [128, D], F32))
                S.dma(sh1t[:], mod_d[l, 0:D].partition_broadcast(128), (), ['modrow'])
                S.dma(A1t[:], mod_d[l, D:2 * D].partition_broadcast(128), (), ['modrow'])
                A1row, sh1row = A1t[:], sh1t[:]
                phase_A(l, src_x, A1row, sh1row)
            if 'stopA' in dbg:
                break
            phase_B(l)
            if 'stopB' in dbg:
                break
            phase_C(l)
            if 'stopC' in dbg:
                break
            phase_D(l)
            if 'stopD' in dbg:
                break
            phase_E(l, src_x)
            if 'stopE' in dbg:
                break
            phase_F(l)
        S.barrier()
    return nc


def _host_tables(rel_bias, SEQ):
    FDW = NEGPAD + SEQ
    d = np.arange(SEQ)
    bk = rel_bucket_np(d)
    g = np.asarray(rel_bias, np.float32)[bk].T
    fdg = np.full((NH, FDW), NEGM, np.float32)
    fdg[:, NEGPAD:] = g
    fdw = np.full((NH, FDW), NEGM, np.float32)
    fdw[:, NEGPAD:NEGPAD + 512] = g[:, :512]
    return fdg, fdw


_CACHE = {}


def kernel(**inputs):
    x = np.asarray(inputs["x"], np.float32)
    B, SEQ, _ = x.shape
    L = int(np.asarray(inputs["ada_w"]).shape[0])
    key = (SEQ, L)
    if key not in _CACHE:
        _CACHE[key] = build_program(SEQ, L)
    nc = _CACHE[key]
    fdg, fdw = _host_tables(inputs["rel_bias"], SEQ)
    names = ["router_w", "router_b", "ada_w", "ada_b", "norm1_g", "norm2_g", "w_in", "qk_norm_g", "cmp_pos", "cmp_w1",
             "cmp_w2", "dn_conv_w", "dn_a_log", "dn_dt_bias", "dn_norm_g", "w_branch_a", "w_branch_b", "w_out",
             "moe_w_gate", "moe_w_up", "moe_w_down"]
    shared = {n: np.ascontiguousarray(np.asarray(inputs[n], np.float32)) for n in names}
    shared["fdg"] = fdg
    shared["fdw"] = fdw
    c = np.asarray(inputs["c"], np.float32)
    in_maps = []
    for b in range(B):
        m = dict(shared)
        m["x"] = np.ascontiguousarray(x[b])
        m["cT"] = np.ascontiguousarray(c[b].reshape(8, 128).T)
        in_maps.append(m)
    res = run_bass_kernel_spmd(nc, in_maps, core_ids=list(range(B)))
    return np.stack([np.asarray(r["out"], np.float32) for r in res.results], axis=0)
```

```python
import math
from contextlib import ExitStack
import numpy as np
import concourse.bass as bass
import concourse.mybir as mybir
from concourse.bass_utils import run_bass_kernel_spmd

F32 = mybir.dt.float32
I32 = mybir.dt.int32
AF = mybir.ActivationFunctionType
ALU = mybir.AluOpType
AX = mybir.AxisListType

D = 1024
HD = 64
NH = 8
DIN = 5416
NEGM = -30000.0
EPS = 1e-6
U0 = 384
OFFMAX = 1024
WGEN = U0 + OFFMAX + 512
WWIN = U0 + 512 + 512
NEGPAD = 1024


class Sched:
    def __init__(self, nc, es, ndma=14):
        self.nc = nc
        self.eng = {'pe': nc.tensor, 'act': nc.scalar, 'dve': nc.vector, 'pool': nc.gpsimd, 'sp': nc.sync}
        self.sem = {k: es.enter_context(nc.semaphore('s_' + k)) for k in self.eng}
        self.cnt = {k: 0 for k in self.eng}
        self.dsem = [es.enter_context(nc.semaphore('d%d' % i)) for i in range(ndma)]
        self.dcnt = [0] * ndma
        self.dnext = 0
        self.seen = {k: {} for k in self.eng}
        self.res = {}
        self.nops = 0

    def _deps(self, r, w):
        deps = {}

        def add(t):
            if t is not None and deps.get(t[0], 0) < t[1]:
                deps[t[0]] = t[1]
        for k in r:
            st = self.res.get(k)
            if st:
                add(st[0])
        for k in w:
            st = self.res.get(k)
            if st:
                add(st[0])
                for s, v in st[1].items():
                    add((s, v))
        return deps

    def _wait(self, e, deps):
        for s, v in deps.items():
            if s == 'pe' and e == 'pe':
                continue
            if self.seen[e].get(s, 0) < v:
                sem = self.sem[s] if isinstance(s, str) else self.dsem[s]
                self.eng[e].wait_ge(sem, v)
                self.seen[e][s] = v

    def _mark(self, tag, r, w):
        for k in r:
            st = self.res.setdefault(k, [None, {}])
            if st[1].get(tag[0], 0) < tag[1]:
                st[1][tag[0]] = tag[1]
        for k in w:
            self.res[k] = [tag, {}]

    def op(self, e, emit, r=(), w=(), inc=True):
        self._wait(e, self._deps(r, w))
        inst = emit(self.eng[e])
        self.nops += 1
        if inc:
            self.cnt[e] += 1
            inst.then_inc(self.sem[e], 1)
            tag = (e, self.cnt[e])
        else:
            tag = (e, self.cnt[e] + 1)
        self._mark(tag, r, w)

    def dma(self, out, in_, r=(), w=(), q='sp'):
        i = self.dnext
        self.dnext = (i + 1) % len(self.dsem)
        deps = self._deps(r, w)
        if self.dcnt[i]:
            deps[i] = max(deps.get(i, 0), self.dcnt[i])
        self._wait(q, deps)
        self.dcnt[i] += 16
        self.eng[q].dma_start(out=out, in_=in_).then_inc(self.dsem[i], 16)
        self.nops += 1
        self._mark((i, self.dcnt[i]), r, w)

    def idma(self, out, in_, out_off, in_off, r=(), w=()):
        i = self.dnext
        self.dnext = (i + 1) % len(self.dsem)
        deps = self._deps(r, w)
        if self.dcnt[i]:
            deps[i] = max(deps.get(i, 0), self.dcnt[i])
        self._wait('pool', deps)
        self.dcnt[i] += 16
        self.eng['pool'].indirect_dma_start(out=out, out_offset=out_off, in_=in_, in_offset=in_off).then_inc(self.dsem[i], 16)
        self.nops += 1
        self._mark((i, self.dcnt[i]), r, w)

    def barrier(self):
        deps = {s: c for s, c in self.cnt.items() if c}
        for i, v in enumerate(self.dcnt):
            if v:
                deps[i] = v
        for e in self.eng:
            d = dict(deps)
            self._wait(e, d)
        self.res = {}


class Ring:
    def __init__(self, tiles, name):
        self.tiles = tiles
        self.name = name
        self.i = 0

    def next(self):
        k = self.i % len(self.tiles)
        self.i += 1
        return self.tiles[k], (self.name, k)


def rel_bucket_np(dist):
    exact = 16
    dist = np.maximum(dist, 0)
    far = np.maximum(dist, exact).astype(np.float32)
    large = exact + (np.log(far / np.float32(exact)) / np.float32(math.log(1024 / exact)) * np.float32(32 - exact)).astype(np.int32)
    return np.where(dist < exact, dist, np.minimum(large, 31))


def build_program(SEQ, DEPTH, dbg=()):
    NT = SEQ // 128
    QT = SEQ // 512
    NCH = SEQ // 64
    NCMP = SEQ // 16 - 1
    NBLK = SEQ // 64
    NCT = (NCMP + 127) // 128
    FDW = NEGPAD + SEQ
    JB = NBLK
    nc = bass.Bass("TRN2", target_bir_lowering=False)

    def din(name, shape):
        return nc.dram_tensor(name, list(shape), F32, kind="ExternalInput").ap()

    def dscr(name, shape, kind="Internal"):
        if name in dbg:
            kind = "ExternalOutput"
        return nc.dram_tensor(name, list(shape), F32, kind=kind).ap()

    L = DEPTH
    x_in = din("x", (SEQ, D))
    cT_in = din("cT", (128, 8))
    fdg_in = din("fdg", (NH, FDW))
    fdw_in = din("fdw", (NH, FDW))
    router_w = din("router_w", (D, 16))
    router_b = din("router_b", (16,))
    ada_w = din("ada_w", (L, D, 6 * D))
    ada_b = din("ada_b", (L, 6 * D))
    norm1_g = din("norm1_g", (L, D))
    norm2_g = din("norm2_g", (L, D))
    w_in = din("w_in", (L, D, DIN))
    qk_norm_g = din("qk_norm_g", (L, 4, HD))
    cmp_pos = din("cmp_pos", (L, 2, 32, HD))
    cmp_w1 = din("cmp_w1", (L, 2, 2048, 256))
    cmp_w2 = din("cmp_w2", (L, 2, 256, HD))
    dn_conv_w = din("dn_conv_w", (L, 4, 1536))
    dn_a_log = din("dn_a_log", (L, 8))
    dn_dt_bias = din("dn_dt_bias", (L, 8))
    dn_norm_g = din("dn_norm_g", (L, HD))
    w_br_a = din("w_branch_a", (L, 512, D))
    w_br_b = din("w_branch_b", (L, 512, D))
    w_out = din("w_out", (L, D, D))
    moe_wg = din("moe_w_gate", (L, 16, D, 512))
    moe_wu = din("moe_w_up", (L, 16, D, 512))
    moe_wd = din("moe_w_down", (L, 16, 512, D))
    out = nc.dram_tensor("out", [SEQ, D], F32, kind="ExternalOutput").ap()

    qT_d = dscr("qT_d", (4, 128, SEQ))
    kcT_d = dscr("kcT_d", (2, 128, SEQ))
    kslcT_d = dscr("kslcT_d", (128, SEQ))
    kwinT_d = dscr("kwinT_d", (128, SEQ))
    vslc_d = dscr("vslc_d", (SEQ, 128))
    vwin_d = dscr("vwin_d", (SEQ, 128))
    gate_d = dscr("gate_d", (SEQ, 24))
    dnraw_d = dscr("dnraw_d", (12, 128, SEQ))
    dnc_d = dscr("dnc_d", (12, 128, SEQ))
    bg_d = dscr("bg_d", (SEQ, 16))
    zs_d = dscr("zs_d", (SEQ, 512))
    mergeT_d = dscr("mergeT_d", (16, 128, SEQ))
    obr_d = dscr("obr_d", (3, SEQ, 512))
    ybT_d = dscr("ybT_d", (4, 128, SEQ))
    bct_d = dscr("bct_d", (NH, NCT * 128, SEQ))
    bgen_d = dscr("bgen_d", (128, NH, WGEN))
    bwin_d = dscr("bwin_d", (128, NH, WWIN))
    selbT_d = dscr("selbT_d", (128, SEQ))
    MBLK = 256
    NBK = (2 * SEQ + 16 * MBLK) // MBLK
    h2_d = dscr("h2_d", (SEQ, D))
    Xs_d = dscr("Xs_d", (NBK * MBLK, D))
    Ys_d = dscr("Ys_d", (NBK * MBLK, D))
    mod_d = dscr("mod_d", (L, 6 * D))

    es = ExitStack()
    with es:
        S = Sched(nc, es)

        def sb(name, shape):
            return es.enter_context(nc.sbuf_tensor(name, list(shape), F32))

        PS = [es.enter_context(nc.psum_tensor("ps%d" % i, [128, 512], F32)) for i in range(8)]
        psr = Ring(PS, "ps")

        def tt(e, o, a, b, op, r, w):
            S.op(e, lambda g: g.tensor_tensor(out=o, in0=a, in1=b, op=op), r, w)

        def ts(e, o, a, s1, op0, r, w, s2=None, op1=None):
            if op1 is None:
                S.op(e, lambda g: g.tensor_scalar(out=o, in0=a, scalar1=s1, scalar2=None, op0=op0), r, w)
            else:
                S.op(e, lambda g: g.tensor_scalar(out=o, in0=a, scalar1=s1, scalar2=s2, op0=op0, op1=op1), r, w)

        def stt(o, a, sc, b, op0, op1, r, w):
            S.op('dve', lambda g: g.scalar_tensor_tensor(out=o, in0=a, scalar=sc, in1=b, op0=op0, op1=op1), r, w)

        def act(o, a, f, r, w, bias=None, scale=1.0, accum=None):
            kw = {}
            if bias is not None:
                kw['bias'] = bias
            if accum is not None:
                kw['accum_out'] = accum
            S.op('act', lambda g: g.activation(out=o, in_=a, func=f, scale=scale, **kw), r, w)

        def mm(o, lT, rh, st, sp, r, w, inc=True):
            S.op('pe', lambda g: g.matmul(o, lT, rh, start=st, stop=sp), r, w, inc=inc)

        def tr(o, a, idn, r, w, inc=True):
            S.op('pe', lambda g: g.transpose(o, a, idn), r, w, inc=inc)

        def cp(e, o, a, r, w):
            S.op(e, lambda g: g.tensor_copy(o, a), r, w)

        def recip(o, a, r, w):
            S.op('dve', lambda g: g.reciprocal(o, a), r, w)

        def memset(e, o, v, w):
            S.op(e, lambda g: g.memset(o, v), (), w)

        def asel(o, a, pattern, base, cm, fill, r, w, op=ALU.is_ge):
            S.op('pool', lambda g: g.affine_select(out=o, in_=a, pattern=pattern, compare_op=op, fill=fill,
                                                   base=base, channel_multiplier=cm), r, w)

        ident = sb("ident", (128, 128))
        ones = sb("ones", (128, 128))
        bdones = sb("bdones", (128, 128))
        UT = sb("UT", (128, 64))
        maskU = sb("maskU", (64, 64))
        maskL = sb("maskL", (64, 64))
        nsU = sb("nsU", (64, 64))
        nsL = sb("nsL", (64, 64))
        ovl = sb("ovl", (128, NCT, JB))
        memset('pool', ident[:], 0.0, ['ident'])
        asel(ident[:], ident[:], [[-1, 128]], 0, 1, 1.0, ['ident'], ['ident'], op=ALU.not_equal)
        memset('pool', ones[:], 1.0, ['ones'])
        memset('pool', bdones[:], 0.0, ['bdones'])
        memset('pool', bdones[0:64, 0:64], 1.0, ['bdones'])
        memset('pool', bdones[64:128, 64:128], 1.0, ['bdones'])
        for h0 in (0, 64):
            memset('pool', UT[h0:h0 + 64, :], 1.0, ['UT'])
            asel(UT[h0:h0 + 64, :], UT[h0:h0 + 64, :], [[1, 64]], 0, -1, 0.0, ['UT'], ['UT'])
        memset('pool', maskU[:], 0.0, ['maskU'])
        asel(maskU[:], maskU[:], [[1, 64]], 0, -1, NEGM, ['maskU'], ['maskU'])
        memset('pool', maskL[:], 0.0, ['maskL'])
        asel(maskL[:], maskL[:], [[-1, 64]], 0, 1, NEGM, ['maskL'], ['maskL'])
        memset('pool', nsU[:], -1.0, ['nsU'])
        asel(nsU[:], nsU[:], [[1, 64]], -1, -1, 0.0, ['nsU'], ['nsU'])
        memset('pool', nsL[:], -1.0, ['nsL'])
        asel(nsL[:], nsL[:], [[-1, 64]], -1, 1, 0.0, ['nsL'], ['nsL'])
        ovt = sb("ovt", (128, NCT, JB))
        memset('pool', ovl[:], 0.0, ['ovl'])
        for m in (0, 1):
            memset('pool', ovt[:], 1.0, ['ovt'])
            asel(ovt[:], ovt[:], [[128, NCT], [-4, JB]], m, 1, 0.0, ['ovt'], ['ovt'])
            asel(ovt[:], ovt[:], [[-128, NCT], [4, JB]], 3 - m, -1, 0.0, ['ovt'], ['ovt'])
            tt('pool', ovl[:], ovl[:], ovt[:], ALU.add, ['ovl', 'ovt'], ['ovl'])

        with nc.allow_non_contiguous_dma(reason="table build"):
            for p in range(128):
                o0 = NEGPAD - U0 - p
                S.dma(bgen_d[p, :, :], fdg_in[:, o0:o0 + WGEN], (), [('bgen', p)], q='sp')
                S.dma(bwin_d[p, :, :], fdw_in[:, o0:o0 + WWIN], (), [('bwin', p)], q='pool')
            for n in range(NCT * 128):
                o0 = NEGPAD - (16 * n + 31)
                if n >= NCMP:
                    o0 = 0
                q = 'sp' if n % 2 == 0 else 'pool'
                if n >= NCMP:
                    S.dma(bct_d[:, n, 0:NEGPAD], fdg_in[:, 0:NEGPAD], (), [('bct', n)], q=q)
                    for c0 in range(NEGPAD, SEQ, NEGPAD):
                        S.dma(bct_d[:, n, c0:c0 + NEGPAD], fdg_in[:, 0:NEGPAD], (), [('bct', n, c0)], q=q)
                elif o0 >= 0:
                    S.dma(bct_d[:, n, :], fdg_in[:, o0:o0 + SEQ], (), [('bct', n)], q=q)
                else:
                    nn = -o0
                    for c0 in range(0, nn, NEGPAD):
                        cw = min(NEGPAD, nn - c0)
                        S.dma(bct_d[:, n, c0:c0 + cw], fdg_in[:, 0:cw], (), [('bct', n, c0)], q=q)
                    S.dma(bct_d[:, n, nn:SEQ], fdg_in[:, 0:SEQ - nn], (), [('bct', n)], q=q)
        S.barrier()

        cact = sb("cact", (128, 8))
        S.dma(cact[:], cT_in[:, :], (), ['cact'])
        act(cact[:], cact[:], AF.Silu, ['cact'], ['cact'])
        cbc = sb("cbc", (128, 8, 128))
        for kc in range(8):
            ts('dve', cbc[:, kc, :], ones[:], cact[:, kc:kc + 1], ALU.mult, ['ones', 'cact'], ['cbc'])
        rtrb = sb("rtrb", (128, 16))
        S.dma(rtrb[:], router_b.partition_broadcast(128), (), ['rtrb'])
        rtrw = sb("rtrw", (128, 8, 16))
        with nc.allow_non_contiguous_dma(reason="router w"):
            S.dma(rtrw[:], router_w.rearrange("(kc p) e -> p kc e", p=128), (), ['rtrw'])

        def load_w(ring, src2d, c0, ncols, kch=8):
            t, k = ring.next()
            with nc.allow_non_contiguous_dma(reason="weight slab"):
                S.dma(t[:, 0:kch, 0:ncols], src2d.rearrange("(kc p) n -> p kc n", p=128)[:, :, c0:c0 + ncols], (), [k])
            return t, k

        def norm_tile(src, t0, Arow, shrow, xr, hr, sm):
            xt, xk = xr.next()
            S.dma(xt[:], src[t0:t0 + 128, :], [('x', t0 // 128)], [xk])
            ht, hk = hr.next()
            s, sk = sm.next()
            act(ht[:], xt[:], AF.Square, [xk], [hk, sk], accum=s[:, 0:1])
            act(s[:, 1:2], s[:, 0:1], AF.Sqrt, [sk], [sk], bias=EPS, scale=1.0 / D)
            recip(s[:, 2:3], s[:, 1:2], [sk], [sk])
            stt(ht[:], xt[:], s[:, 2:3], Arow, ALU.mult, ALU.mult, [xk, sk, 'modrow'], [hk])
            tt('pool', ht[:], ht[:], shrow, ALU.add, [hk, 'modrow'], [hk])
            return ht, hk, xt, xk

        def transpose_to(ht, hk, hT, hTk, sub, nkc=8):
            for k0 in range(0, nkc, 4):
                ps, pk = psr.next()
                for kc in range(k0, k0 + 4):
                    tr(ps[:, (kc - k0) * 128:(kc - k0 + 1) * 128], ht[:, kc * 128:(kc + 1) * 128], ident[:],
                       [hk, 'ident'], [pk], inc=(kc == k0 + 3))
                e = 'act' if (k0 // 4) % 2 == 0 else 'dve'
                dst = hT[:, k0:k0 + 4, sub * 128:(sub + 1) * 128]
                srcv = ps[:].rearrange("p (a b) -> p a b", a=4)
                if e == 'act':
                    act(dst, srcv, AF.Copy, [pk], [hTk])
                else:
                    cp('dve', dst, srcv, [pk], [hTk])

        def phase_A(l, src_x, A1row, sh1row):
            with ExitStack() as ph:
                def psb(name, shape):
                    return ph.enter_context(nc.sbuf_tensor("%s_A%d" % (name, l), list(shape), F32))
                wr = Ring([psb("wsl%d" % i, (128, 8, 512)) for i in range(3)], "wsl")
                xr = Ring([psb("xa%d" % i, (128, D)) for i in range(2)], "xa")
                hr = Ring([psb("ha%d" % i, (128, D)) for i in range(2)], "ha")
                hTr = Ring([psb("hT%d" % i, (128, 8, 512)) for i in range(2)], "hT")
                st = Ring([psb("st%d" % i, (128, 512)) for i in range(4)], "st")
                sq = Ring([psb("sq%d" % i, (128, 512)) for i in range(2)], "sq")
                sm = Ring([psb("sm%d" % i, (128, 8)) for i in range(4)], "sm")
                gains = psb("gains", (128, 4))
                dtb = psb("dtb", (128, 8))
                nea = psb("nea", (128, 8))
                with nc.allow_non_contiguous_dma(reason="small"):
                    for h0 in (0, 64):
                        S.dma(gains[h0:h0 + 64, :], qk_norm_g[l].rearrange("i d -> d i"), (), ['gains'])
                ts('dve', gains[:, 0:1], gains[:, 0:1], HD ** -0.5, ALU.mult, ['gains'], ['gains'])
                S.dma(dtb[:], dn_dt_bias[l].partition_broadcast(128), (), ['dtb'])
                S.dma(nea[:], dn_a_log[l].partition_broadcast(128), (), ['nea'])
                act(nea[:], nea[:], AF.Exp, ['nea'], ['nea'])
                ts('dve', nea[:], nea[:], -1.0, ALU.mult, ['nea'], ['nea'])

                def rms64_store(ps, pk, gcol, dst, dkey):
                    q1, qk1 = sq.next()
                    act(q1[:], ps[:], AF.Square, [pk], [qk1])
                    p2, pk2 = psr.next()
                    mm(p2[:], bdones[:], q1[:], True, True, ['bdones', qk1], [pk2])
                    act(q1[:], p2[:], AF.Sqrt, [pk2], [qk1], bias=EPS, scale=1.0 / 64)
                    recip(q1[:], q1[:], [qk1], [qk1])
                    o, ok = st.next()
                    stt(o[:], ps[:], gcol, q1[:], ALU.mult, ALU.mult, [pk, qk1, 'gains'], [ok])
                    S.dma(dst, o[:], [ok], [dkey], q='pool')

                def fm_store(ps, pk, dst, dkey, func=AF.Copy):
                    o, ok = st.next()
                    act(o[:], ps[:], func, [pk], [ok])
                    S.dma(dst, o[:], [ok], [dkey], q='pool')

                for j in range(QT):
                    t0 = 512 * j
                    hT, hTk = hTr.next()
                    for sub in range(4):
                        ht, hk, _, _ = norm_tile(src_x, t0 + 128 * sub, A1row, sh1row, xr, hr, sm)
                        transpose_to(ht, hk, hT, hTk, sub)

                    def fm(wt, wk, lsel):
                        ps, pk = psr.next()
                        for kc in range(8):
                            mm(ps[:], lsel(kc), hT[:, kc, :], kc == 0, kc == 7, [wk, hTk], [pk], inc=(kc == 7))
                        return ps, pk

                    def tm(wt, wk, c0, ncols, sub):
                        ps, pk = psr.next()
                        for kc in range(8):
                            mm(ps[:, 0:ncols], hT[:, kc, sub * 128:(sub + 1) * 128], wt[:, kc, c0:c0 + ncols],
                               kc == 0, kc == 7, [wk, hTk], [pk], inc=(kc == 7))
                        return ps, pk
                    cs = slice(t0, t0 + 512)
                    wt, wk = wr.next()
                    with nc.allow_non_contiguous_dma(reason="q slab"):
                        for a in range(2):
                            for c in range(4):
                                S.dma(wt[:, :, c * 128 + a * 64:c * 128 + a * 64 + 64],
                                      w_in[l].rearrange("(kc p) n -> p kc n", p=128)[:, :, a * 256 + c * 64:a * 256 + c * 64 + 64], (), [wk])
                    for c in range(4):
                        ps, pk = fm(wt, wk, lambda kc, c=c: wt[:, kc, c * 128:(c + 1) * 128])
                        rms64_store(ps, pk, gains[:, 0:1], qT_d[c, :, cs], ('qT', c, j))
                    wt, wk = load_w(wr, w_in[l], 512, 512)
                    for c in range(3):
                        ps, pk = fm(wt, wk, lambda kc, c=c: wt[:, kc, c * 128:(c + 1) * 128])
                        if c < 2:
                            fm_store(ps, pk, kcT_d[c, :, cs], ('kcT', c, j))
                        else:
                            rms64_store(ps, pk, gains[:, 2:3], kslcT_d[:, cs], ('kslcT', j))
                    for sub in range(4):
                        ps, pk = tm(wt, wk, 384, 128, sub)
                        o, ok = st.next()
                        act(o[:, 0:128], ps[:, 0:128], AF.Copy, [pk], [ok])
                        S.dma(vslc_d[t0 + 128 * sub:t0 + 128 * sub + 128, :], o[:, 0:128], [ok], [('vslc', j, sub)], q='pool')
                    wt, wk = load_w(wr, w_in[l], 1024, 280)
                    ps, pk = fm(wt, wk, lambda kc: wt[:, kc, 0:128])
                    rms64_store(ps, pk, gains[:, 3:4], kwinT_d[:, cs], ('kwinT', j))
                    for sub in range(4):
                        r0 = t0 + 128 * sub
                        ps, pk = tm(wt, wk, 128, 152, sub)
                        o, ok = st.next()
                        act(o[:, 0:128], ps[:, 0:128], AF.Copy, [pk], [ok])
                        act(o[:, 128:152], ps[:, 128:152], AF.Sigmoid, [pk], [ok])
                        S.dma(vwin_d[r0:r0 + 128, :], o[:, 0:128], [ok], [('vwin', j, sub)], q='pool')
                        S.dma(gate_d[r0:r0 + 128, :], o[:, 128:152], [ok], [('gate', j, sub)], q='pool')
                    for i in range(3):
                        wt, wk = load_w(wr, w_in[l], 1304 + 512 * i, 512)
                        for c in range(4):
                            ps, pk = fm(wt, wk, lambda kc, c=c: wt[:, kc, c * 128:(c + 1) * 128])
                            fm_store(ps, pk, dnraw_d[4 * i + c, :, cs], ('dnraw', 4 * i + c, j))
                    wt, wk = load_w(wr, w_in[l], 2840, 16)
                    for sub in range(4):
                        r0 = t0 + 128 * sub
                        ps, pk = tm(wt, wk, 0, 16, sub)
                        o, ok = st.next()
                        act(o[:, 0:8], ps[:, 0:8], AF.Sigmoid, [pk], [ok])
                        tt('dve', o[:, 8:16], ps[:, 8:16], dtb[:], ALU.add, [pk, 'dtb'], [ok])
                        act(o[:, 8:16], o[:, 8:16], AF.Exp, [ok], [ok])
                        act(o[:, 8:16], o[:, 8:16], AF.Ln, [ok], [ok], bias=1.0)
                        tt('dve', o[:, 8:16], o[:, 8:16], nea[:], ALU.mult, [ok, 'nea'], [ok])
                        S.dma(bg_d[r0:r0 + 128, :], o[:, 0:16], [ok], [('bg', j, sub)], q='pool')
                    wt, wk = load_w(wr, w_in[l], 2856, 512)
                    for sub in range(4):
                        r0 = t0 + 128 * sub
                        ps, pk = tm(wt, wk, 0, 512, sub)
                        fm_store(ps, pk, zs_d[r0:r0 + 128, :], ('zs', j, sub), func=AF.Silu)
                    for i in range(4):
                        wt, wk = load_w(wr, w_in[l], 3368 + 512 * i, 512)
                        for c in range(4):
                            ps, pk = fm(wt, wk, lambda kc, c=c: wt[:, kc, c * 128:(c + 1) * 128])
                            fm_store(ps, pk, mergeT_d[4 * i + c, :, cs], ('mergeT', 4 * i + c, j), func=AF.Sigmoid)
                S.barrier()
        def phase_B(l):
            with ExitStack() as ph:
                def psb(name, shape):
                    return ph.enter_context(nc.sbuf_tensor("%s_B%d" % (name, l), list(shape), F32))
                kst = [psb("kst%d" % g, (128, NCT * 128)) for g in range(2)]
                for g in range(2):
                    memset('pool', kst[g][:], 0.0, ['kcmpT'])
                vcmp = psb("vcmp", (128, NCT, 2, 65 + JB))
                memset('pool', vcmp[:], 0.0, ['vcmp'])
                memset('pool', vcmp[:, :, :, 64:65], 1.0, ['vcmp'])
                for g in range(2):
                    cp('pool', vcmp[:, :, g, 65:65 + JB], ovl[:], ['ovl', 'vcmp'], ['vcmp'])
                gains = psb("gains", (128, 4))
                with nc.allow_non_contiguous_dma(reason="small"):
                    for h0 in (0, 64):
                        S.dma(gains[h0:h0 + 64, :], qk_norm_g[l].rearrange("i d -> d i"), (), ['gains'])
                with ExitStack() as ph2:
                    def psb2(name, shape):
                        return ph2.enter_context(nc.sbuf_tensor("%s_B2%d" % (name, l), list(shape), F32))
                    kcT = psb2("kcT", (128, SEQ))
                    w1 = psb2("w1", (128, 32, 256))
                    posT = psb2("posT", (128, 32))
                    w2 = psb2("w2", (128, 2, 64))
                    pbias = psb2("pbias", (128, 2))
                    gx = psb2("gx", (128, 2, 2, 256))
                    gt = psb2("gt", (128, 256))
                    sqc = psb2("sqc", (128, 256))
                    for kvi in range(2):
                        S.dma(kcT[:], kcT_d[kvi, :, :], [('kcT', kvi, j) for j in range(QT)], ['kcT'])
                        with nc.allow_non_contiguous_dma(reason="cmp weights"):
                            for h0 in (0, 64):
                                S.dma(w1[h0:h0 + 64, :, :], cmp_w1[l, kvi].rearrange("(j d) f -> d j f", d=64), (), ['w1'])
                                S.dma(posT[h0:h0 + 64, :], cmp_pos[l, kvi].rearrange("j d -> d j"), (), ['posT'])
                            S.dma(w2[:], cmp_w2[l, kvi].rearrange("(fc p) d -> p fc d", p=128), (), ['w2'])
                        for fc in range(2):
                            ps, pk = psr.next()
                            for j in range(32):
                                mm(ps[:, 0:1], w1[0:64, j, fc * 128:(fc + 1) * 128], posT[0:64, j:j + 1], j == 0, j == 31,
                                   ['w1', 'posT'], [pk], inc=(j == 31))
                            cp('dve', pbias[:, fc:fc + 1], ps[:, 0:1], [pk], ['pbias'])
                        for g in range(2):
                            hs = slice(64 * g, 64 * g + 64)
                            for fc in range(2):
                                ps, pk = psr.next()
                                for j in range(32):
                                    mm(ps[:, 0:NCMP], w1[hs, j, fc * 128:(fc + 1) * 128], kcT[hs, j:j + 16 * (NCMP - 1) + 1:16],
                                       j == 0, j == 31, ['w1', 'kcT'], [pk], inc=(j == 31))
                                xs = gx[:, g, fc, 0:NCMP]
                                ts('dve', xs, ps[:, 0:NCMP], pbias[:, fc:fc + 1], ALU.add, [pk, 'pbias'], ['gx'])
                                tt('dve', gt[:, 0:NCMP], xs, xs, ALU.mult, ['gx'], ['gt'])
                                ts('dve', gt[:, 0:NCMP], gt[:, 0:NCMP], 0.044715, ALU.mult, ['gt'], ['gt'], s2=1.0, op1=ALU.add)
                                tt('dve', gt[:, 0:NCMP], gt[:, 0:NCMP], xs, ALU.mult, ['gt', 'gx'], ['gt'])
                                act(gt[:, 0:NCMP], gt[:, 0:NCMP], AF.Sigmoid, ['gt'], ['gt'], scale=1.5957691216057308)
                                tt('dve', xs, xs, gt[:, 0:NCMP], ALU.mult, ['gx', 'gt'], ['gx'])
                            if kvi == 0:
                                ps, pk = psr.next()
                                for fc in range(2):
                                    mm(ps[hs, 0:NCMP], w2[:, fc, :], gx[:, g, fc, 0:NCMP], fc == 0, fc == 1, ['w2', 'gx'], [pk], inc=(fc == 1))
                                act(sqc[hs, 0:NCMP], ps[hs, 0:NCMP], AF.Square, [pk], ['sqc'])
                                p2, pk2 = psr.next()
                                mm(p2[hs, 0:NCMP], ones[hs, 0:64], sqc[hs, 0:NCMP], True, True, ['ones', 'sqc'], [pk2])
                                act(sqc[hs, 0:NCMP], p2[hs, 0:NCMP], AF.Sqrt, [pk2], ['sqc'], bias=EPS, scale=1.0 / 64)
                                recip(sqc[hs, 0:NCMP], sqc[hs, 0:NCMP], ['sqc'], ['sqc'])
                                stt(kst[g][hs, 0:NCMP], ps[hs, 0:NCMP], gains[hs, 1:2], sqc[hs, 0:NCMP], ALU.mult, ALU.mult,
                                    [pk, 'sqc', 'gains'], ['kcmpT'])
                            else:
                                for nt in range(NCT):
                                    nn = min(128, NCMP - nt * 128)
                                    ps, pk = psr.next()
                                    for fc in range(2):
                                        mm(ps[0:nn, 0:64], gx[:, g, fc, nt * 128:nt * 128 + nn], w2[:, fc, :], fc == 0, fc == 1,
                                           ['w2', 'gx'], [pk], inc=(fc == 1))
                                    cp('dve', vcmp[0:nn, nt, g, 0:64], ps[0:nn, 0:64], [pk], ['vcmp'])
                    S.barrier()
                qr = Ring([psb("qTb%d" % i, (128, SEQ)) for i in range(2)], "qTb")
                btr = Ring([psb("bt%d" % i, (128, NCT, 512)) for i in range(3)], "bt")
                pcr = Ring([psb("pc%d" % i, (128, NCT, 512)) for i in range(3)], "pc")
                osr = Ring([psb("os%d" % i, (128, 64)) for i in range(4)], "os")
                rdr = Ring([psb("rd%d" % i, (128, 2)) for i in range(4)], "rd")
                gate_sb = psb("gate_sb", (128, NT, 24))
                impacc = psb("impacc", (128, NT, 2, JB))
                with nc.allow_non_contiguous_dma(reason="gate"):
                    S.dma(gate_sb[:], gate_d.rearrange("(t p) c -> p t c", p=128),
                          [('gate', j, s) for j in range(QT) for s in range(4)], ['gate_sb'])
                W = 65 + JB
                qcur = {}

                def stage1(c, half, jq):
                    if c not in qcur:
                        qT, qk = qr.next()
                        S.dma(qT[:], qT_d[c, :, :], [('qT', c, j) for j in range(QT)], [qk])
                        qcur.clear()
                        qcur[c] = (qT, qk)
                    qT, qk = qcur[c]
                    h = c + 4 * half
                    g = half
                    tq0 = 512 * jq
                    nts = [nt for nt in range(NCT) if 16 * 128 * nt + 31 <= tq0 + 511]
                    bt, bk = btr.next()
                    pc, pck = pcr.next()
                    for nt in nts:
                        S.dma(bt[:, nt, :], bct_d[h, nt * 128:(nt + 1) * 128, tq0:tq0 + 512], (), [bk])
                    for nt in nts:
                        ps, pk = psr.next()
                        mm(ps[:], kst[g][:, nt * 128:(nt + 1) * 128], qT[:, tq0:tq0 + 512], True, True, ['kcmpT', qk], [pk])
                        tt('dve', pc[:, nt, :], ps[:], bt[:, nt, :], ALU.add, [pk, bk], [pck])
                        act(pc[:, nt, :], pc[:, nt, :], AF.Exp, [pck], [pck])
                    return (c, h, g, jq, nts, pc, pck)

                def stage2(item):
                    c, h, g, jq, nts, pc, pck = item
                    for sub in range(4):
                        tsi = 4 * jq + sub
                        po, pok = psr.next()
                        for nt in nts:
                            mm(po[:, 0:W], pc[:, nt, sub * 128:(sub + 1) * 128], vcmp[:, nt, g, :], nt == nts[0], nt == nts[-1],
                               [pck, 'vcmp'], [pok], inc=(nt == nts[-1]))
                        rd, rk = rdr.next()
                        ts('dve', rd[:, 0:1], po[:, 64:65], 1e-30, ALU.add, [pok], [rk])
                        recip(rd[:, 1:2], rd[:, 0:1], [rk], [rk])
                        o, ok = osr.next()
                        ts('dve', o[:], po[:, 0:64], rd[:, 1:2], ALU.mult, [pok, rk, 'gate_sb'], [ok],
                           s2=gate_sb[:, tsi, 3 * h:3 * h + 1], op1=ALU.mult)
                        S.dma(obr_d[0, tsi * 128:(tsi + 1) * 128, 64 * h:64 * h + 64], o[:], [ok], [('obr', 0, h, tsi)], q='pool')
                        if c == 0:
                            ts('dve', impacc[:, tsi, g, :], po[:, 65:W], rd[:, 1:2], ALU.mult, [pok, rk], [('imp', tsi, g)])
                        else:
                            stt(impacc[:, tsi, g, :], po[:, 65:W], rd[:, 1:2], impacc[:, tsi, g, :], ALU.mult, ALU.add,
                                [pok, rk, ('imp', tsi, g)], [('imp', tsi, g)])

                prev_it = None
                for c in range(4):
                    for half in range(2):
                        for jq in range(QT):
                            it_ = stage1(c, half, jq)
                            if prev_it is not None:
                                stage2(prev_it)
                            prev_it = it_
                stage2(prev_it)
                selM = psb("selM", (128, NT, JB))
                selA = psb("selA", (128, NT, JB))
                memset('pool', selM[:], 1.0, ['selM'])
                asel(selM[:], selM[:], [[128, NT], [-64, JB]], -128, 1, 0.0, ['selM'], ['selM'])
                memset('pool', selM[:, :, 0:1], 0.0, ['selM'])
                memset('pool', selA[:], 0.0, ['selA'])
                asel(selA[:], selA[:], [[128, NT], [-64, JB]], -128, 1, 1e9, ['selA'], ['selA'])
                asel(selA[:], selA[:], [[128, NT], [-64, JB]], 0, 1, -1.0, ['selA'], ['selA'])
                memset('pool', selA[:, :, 0:1], 1e9, ['selA'])
                scr = Ring([psb("sc%d" % i, (128, 2, JB)) for i in range(2)], "sc")
                sc2r = Ring([psb("sd%d" % i, (128, JB)) for i in range(2)], "sd")
                m8r = Ring([psb("m8%d" % i, (128, 16)) for i in range(4)], "m8")
                sbr = Ring([psb("sbi%d" % i, (128, 128)) for i in range(2)], "sbi")
                sto = Ring([psb("sto%d" % i, (128, 128)) for i in range(2)], "sto")
                for tsi in range(NT):
                    sc, sck = scr.next()
                    sbi, sbk = sbr.next()
                    if JB < 64:
                        memset('pool', sbi[:], 0.0, [sbk])
                    for g in range(2):
                        tt('dve', sc[:, g, :], impacc[:, tsi, g, :], selM[:, tsi, :], ALU.mult, [('imp', tsi, g), 'selM'], [sck])
                        tt('dve', sc[:, g, :], sc[:, g, :], selA[:, tsi, :], ALU.add, [sck, 'selA'], [sck])
                        m8, mk = m8r.next()
                        sd, sdk = sc2r.next()
                        S.op('dve', lambda e, a=m8, b=sc, g=g: e.max(out=a[:, 0:8], in_=b[:, g, :]), [sck], [mk])
                        S.op('dve', lambda e, a=m8, b=sc, d=sd, g=g: e.match_replace(out=d[:], in_to_replace=a[:, 0:8], in_values=b[:, g, :],
                                                                                   imm_value=-3e38), [sck, mk], [sdk])
                        S.op('dve', lambda e, a=m8, d=sd: e.max(out=a[:, 8:16], in_=d[:]), [sdk], [mk])
                        ts('dve', sbi[:, 64 * g:64 * g + JB], sc[:, g, :], m8[:, 15:16], ALU.is_ge, [sck, mk], [sbk], s2=-NEGM, op1=ALU.mult)
                        ts('dve', sbi[:, 64 * g:64 * g + JB], sbi[:, 64 * g:64 * g + JB], NEGM, ALU.add, [sbk], [sbk])
                    ps, pk = psr.next()
                    tr(ps[:, 0:128], sbi[:], ident[:], [sbk, 'ident'], [pk])
                    so, sok = sto.next()
                    act(so[:], ps[:, 0:128], AF.Copy, [pk], [sok])
                    S.dma(selbT_d[:, tsi * 128:(tsi + 1) * 128], so[:], [sok], [('selbT', tsi)], q='pool')
                S.barrier()

        def phase_C(l):
            for br in (1, 2):
                with ExitStack() as ph:
                    def psb(name, shape):
                        return ph.enter_context(nc.sbuf_tensor("%s_C%d_%d" % (name, l, br), list(shape), F32))
                    Wt = WGEN if br == 1 else WWIN
                    tab_d = bgen_d if br == 1 else bwin_d
                    KT_d = kslcT_d if br == 1 else kwinT_d
                    V_d = vslc_d if br == 1 else vwin_d
                    tab = psb("tab", (128, NH, Wt))
                    for h in range(NH):
                        S.dma(tab[:, h, :], tab_d[:, h, :], (), ['tab'])
                    LS = [psb("LS%d" % g, (128, SEQ)) for g in range(2)]
                    for g in range(2):
                        if br == 1:
                            S.dma(LS[g][0:64, :], KT_d[64 * g:64 * g + 64, :], (), [('LS', g)])
                            v = LS[g][64:128, :]
                            memset('pool', v, 1.0, [('LS', g)])
                            asel(v, v, [[1, SEQ]], 0, -64, 0.0, [('LS', g)], [('LS', g)])
                            asel(v, v, [[-1, SEQ]], 63, 64, 0.0, [('LS', g)], [('LS', g)])
                        else:
                            memset('pool', LS[g][64 * (1 - g):64 * (1 - g) + 64, :], 0.0, [('LS', g)])
                            S.dma(LS[g][64 * g:64 * g + 64, :], KT_d[64 * g:64 * g + 64, :], (), [('LS', g)])
                    V = psb("V", (128, NT, 2, 65))
                    memset('pool', V[:, :, :, 64:65], 1.0, ['V'])
                    with nc.allow_non_contiguous_dma(reason="V"):
                        for g in range(2):
                            S.dma(V[:, :, g, 0:64], V_d.rearrange("(t p) c -> p t c", p=128)[:, :, 64 * g:64 * g + 64], (), ['V'])
                    gate_sb = psb("gate_sb", (128, NT, 24))
                    with nc.allow_non_contiguous_dma(reason="gate"):
                        S.dma(gate_sb[:], gate_d.rearrange("(t p) c -> p t c", p=128), (), ['gate_sb'])
                    qr = Ring([psb("qTc%d" % i, (128, SEQ)) for i in range(2)], "qTc")
                    ptr = Ring([psb("pt%d" % i, (128, 512)) for i in range(4)], "pt")
                    osr = Ring([psb("os%d" % i, (128, 64)) for i in range(4)], "os")
                    rdr = Ring([psb("rd%d" % i, (128, 2)) for i in range(4)], "rd")
                    b31 = psb("b31", (128, NH))
                    with nc.allow_non_contiguous_dma(reason="b31"):
                        S.dma(b31[:], fdg_in[:, NEGPAD + OFFMAX - 1].partition_broadcast(128), (), ['b31'])
                    psr4 = Ring(PS[0:4], "ps")
                    LAG = 2
                    for c in range(4):
                        if br == 2:
                            qT, qk = qr.next()
                            S.dma(qT[:], qT_d[c, :, :], (), [qk])
                        for half in range(2):
                            h = c + 4 * half
                            g = half
                            if br == 1:
                                qT, qk = qr.next()
                                S.dma(qT[0:64, :], qT_d[c, 64 * half:64 * half + 64, :], (), [qk])
                                S.dma(qT[64:128, :], selbT_d[64 * g:64 * g + 64, :], (), [qk])
                            pend = []

                            def stage3(item):
                                jq_, tk0_, pt_, ptk_, tks_, last_ = item
                                tq0_ = 512 * jq_
                                for sub in range(4):
                                    if tk0_ > tq0_ + 128 * sub + 127:
                                        continue
                                    mm(PS[4 + sub][:, 0:65], pt_[:, sub * 128:(sub + 1) * 128], V[:, tk0_ // 128, g, :],
                                       tk0_ == tks_[0], tk0_ == last_[sub], [ptk_, 'V'], [('ps', 4 + sub)], inc=(tk0_ == last_[sub]))
                                if tk0_ == tks_[-1]:
                                    for sub in range(4):
                                        tsi = 4 * jq_ + sub
                                        po = PS[4 + sub]
                                        pok = ('ps', 4 + sub)
                                        rd, rk = rdr.next()
                                        ts('dve', rd[:, 0:1], po[:, 64:65], 1e-30, ALU.add, [pok], [rk])
                                        recip(rd[:, 1:2], rd[:, 0:1], [rk], [rk])
                                        o, ok = osr.next()
                                        ts('dve', o[:], po[:, 0:64], rd[:, 1:2], ALU.mult, [pok, rk, 'gate_sb'], [ok],
                                           s2=gate_sb[:, tsi, 3 * h + br:3 * h + br + 1], op1=ALU.mult)
                                        S.dma(obr_d[br, tsi * 128:(tsi + 1) * 128, 64 * h:64 * h + 64], o[:], [ok], [('obr', br, h, tsi)], q='pool')

                            for jq in range(QT):
                                tq0 = 512 * jq
                                lo = 0 if br == 1 else max(0, tq0 - 512)
                                tks = list(range(lo, tq0 + 512, 128))
                                last = {sub: max(tk for tk in tks if tk <= tq0 + 128 * sub + 127) for sub in range(4)}
                                for tk0 in tks:
                                    ps, pk = psr4.next()
                                    mm(ps[:], LS[g][:, tk0:tk0 + 128], qT[:, tq0:tq0 + 512], True, True, [('LS', g), qk], [pk])
                                    pt, ptk = ptr.next()
                                    if tq0 - tk0 >= OFFMAX + 128:
                                        act(pt[:], ps[:], AF.Exp, [pk, 'b31'], [ptk], bias=b31[:, h:h + 1])
                                    else:
                                        off = min(tq0 - tk0, OFFMAX) + U0
                                        tt('dve', pt[:], ps[:], tab[:, h, off:off + 512], ALU.add, [pk, 'tab'], [ptk])
                                        act(pt[:], pt[:], AF.Exp, [ptk], [ptk])
                                    pend.append((jq, tk0, pt, ptk, tks, last))
                                    if len(pend) > LAG:
                                        stage3(pend.pop(0))
                            while pend:
                                stage3(pend.pop(0))
                    S.barrier()
        def bc(ap2, n, axis):
            P, A = ap2.shape
            if axis == 2:
                return ap2.unsqueeze(2).to_broadcast([P, A, n])
            return ap2.unsqueeze(1).to_broadcast([P, n, A])

        def phase_D(l):
            with ExitStack() as ph:
                def psb(name, shape):
                    return ph.enter_context(nc.sbuf_tensor("%s_D1%d" % (name, l), list(shape), F32))
                xr = Ring([psb("xin%d" % i, (128, SEQ + 3)) for i in range(2)], "xin")
                ar = Ring([psb("acc%d" % i, (128, SEQ)) for i in range(2)], "acc")
                sq = Ring([psb("sq%d" % i, (128, 512)) for i in range(4)], "sq")
                cw = psb("cw", (128, 4, 12))
                with nc.allow_non_contiguous_dma(reason="conv w"):
                    for i in range(4):
                        S.dma(cw[:, i, :], dn_conv_w[l, i].rearrange("(c p) -> p c", p=128), (), ['cw'])
                for ch in range(12):
                    xin, xk = xr.next()
                    acc, ak = ar.next()
                    memset('pool', xin[:, 0:3], 0.0, [xk])
                    S.dma(xin[:, 3:SEQ + 3], dnraw_d[ch, :, :], (), [xk])
                    ts('dve', acc[:], xin[:, 0:SEQ], cw[:, 0, ch:ch + 1], ALU.mult, [xk, 'cw'], [ak])
                    for i in range(1, 4):
                        stt(acc[:], xin[:, i:SEQ + i], cw[:, i, ch:ch + 1], acc[:], ALU.mult, ALU.add, [xk, 'cw', ak], [ak])
                    act(acc[:], acc[:], AF.Silu, [ak], [ak])
                    if ch < 8:
                        GRP = 4
                        for j0 in range(0, QT, GRP):
                            js = list(range(j0, min(QT, j0 + GRP)))
                            tiles = []
                            for j in js:
                                cs = slice(512 * j, 512 * j + 512)
                                q1, qk1 = sq.next()
                                act(q1[:], acc[:, cs], AF.Square, [ak], [qk1])
                                tiles.append((cs, q1, qk1))
                            pss = []
                            for (cs, q1, qk1) in tiles:
                                p2, pk2 = psr.next()
                                mm(p2[:], bdones[:], q1[:], True, True, ['bdones', qk1], [pk2])
                                pss.append((p2, pk2))
                            for (cs, q1, qk1), (p2, pk2) in zip(tiles, pss):
                                act(q1[:], p2[:], AF.Sqrt, [pk2], [qk1], bias=EPS, scale=1.0)
                            for (cs, q1, qk1) in tiles:
                                recip(q1[:], q1[:], [qk1], [qk1])
                            for (cs, q1, qk1) in tiles:
                                if ch < 4:
                                    stt(acc[:, cs], acc[:, cs], HD ** -0.5, q1[:], ALU.mult, ALU.mult, [ak, qk1], [ak])
                                else:
                                    tt('dve', acc[:, cs], acc[:, cs], q1[:], ALU.mult, [ak, qk1], [ak])
                    S.dma(dnc_d[ch, :, :], acc[:], [ak], [('dnc', ch)], q='pool')
                S.barrier()
            if 'stopD1' in dbg:
                return
            with ExitStack() as ph:
                def psb(name, shape):
                    return ph.enter_context(nc.sbuf_tensor("%s_D2%d" % (name, l), list(shape), F32))
                NG = NCH * 8
                g_tm = psb("g_tm", (64, NCH, 8))
                b_tm = psb("b_tm", (64, NCH, 8))
                gc_tm = psb("gc_tm", (64, NCH, 8))
                eg_tm = psb("eg_tm", (64, NCH, 8))
                bg_tm = psb("bg_tm", (64, NCH, 8))
                kds_tm = psb("kds_tm", (64, NCH, 8))
                egl = psb("egl", (64, NCH, 8))
                sel63 = psb("sel63", (64, 64))
                ngrow = psb("ngrow", (64, 64))
                with nc.allow_non_contiguous_dma(reason="bg"):
                    S.dma(b_tm[:], bg_d.rearrange("(n i) c -> i n c", i=64)[:, :, 0:8], (), ['b_tm'])
                    S.dma(g_tm[:], bg_d.rearrange("(n i) c -> i n c", i=64)[:, :, 8:16], (), ['g_tm'])
                S.dma(ngrow[:], dn_norm_g[l].partition_broadcast(64), (), ['ngrow'])
                ts('dve', sel63[:], ones[0:64, 0:64], ident[0:64, 63:64], ALU.mult, ['ones', 'ident'], ['sel63'])
                gflat = g_tm[:].rearrange("p n h -> p (n h)")
                gcflat = gc_tm[:].rearrange("p n h -> p (n h)")
                for c0 in range(0, NG, 512):
                    cw_ = min(512, NG - c0)
                    ps, pk = psr.next()
                    mm(ps[0:64, 0:cw_], UT[0:64, :], gflat[:, c0:c0 + cw_], True, True, ['UT', 'g_tm'], [pk])
                    cp('dve', gcflat[:, c0:c0 + cw_], ps[0:64, 0:cw_], [pk], ['gc_tm'])
                    ps2, pk2 = psr.next()
                    mm(ps2[0:64, 0:cw_], sel63[:], gcflat[:, c0:c0 + cw_], True, True, ['sel63', 'gc_tm'], [pk2])
                    act(egl[:].rearrange("p n h -> p (n h)")[:, c0:c0 + cw_], ps2[0:64, 0:cw_], AF.Exp, [pk2], ['egl'])
                    tt('dve', kds_tm[:].rearrange("p n h -> p (n h)")[:, c0:c0 + cw_], ps2[0:64, 0:cw_], gcflat[:, c0:c0 + cw_],
                       ALU.subtract, [pk2, 'gc_tm'], ['kds_tm'])
                act(kds_tm[:], kds_tm[:], AF.Exp, ['kds_tm'], ['kds_tm'])
                act(eg_tm[:], gc_tm[:], AF.Exp, ['gc_tm'], ['eg_tm'])
                tt('dve', bg_tm[:], b_tm[:], eg_tm[:], ALU.mult, ['b_tm', 'eg_tm'], ['bg_tm'])

                def t3(name):
                    return psb(name, (64, 8, 64))
                NPS, NOS = 2, 4
                Pt = [{nm: t3("%s_%d" % (nm, i)) for nm in ['ktm', 'vtm', 'qtm', 'rhsg', 'rhsb', 'DTr', 'decT', 'dec', 't1', 'Xa', 'Xb', 'Ya', 'Yb', 'TT', 'vb', 'kbg']} for i in range(NPS)]
                Ot = [{nm: t3("%s_%d" % (nm, i)) for nm in ['AT', 'kd', 'qdT', 'wT', 'u_sb']} for i in range(NOS)]
                Xs_ = [psb("X%d" % i, (64, 24, 64)) for i in range(NPS)]
                zs_ = [psb("z%d" % i, (64, 512)) for i in range(NOS)]
                vn, Sst, o_sb, osq = t3("vn"), t3("Sst"), t3("o_sb"), t3("osq")
                ssum = psb("ssum", (64, 16))
                ytm = psb("ytm", (64, 512))
                ybo = Ring([psb("ybo%d" % i, (128, 4, 64)) for i in range(2)], "ybo")
                memset('pool', Sst[:], 0.0, ['Sst'])
                I64 = ident[0:64, 0:64]

                def hmm(pst, pk, lhs, rhs, r, first=True, last=True):
                    for h in range(8):
                        mm(pst[0:64, 64 * h:64 * h + 64], lhs(h), rhs(h), first, last, r, [pk], inc=(h == 7))

                def v3(ps):
                    return ps[0:64, :].rearrange("p (h c) -> p h c", h=8)

                def pre_gen(n, sp_, so_):
                    ktm, vtm, qtm, rhsg, rhsb, DTr, decT, dec, t1, Xa, Xb, Ya, Yb, TT, vb, kbg = [Pt[sp_][k_] for k_ in ['ktm', 'vtm', 'qtm', 'rhsg', 'rhsb', 'DTr', 'decT', 'dec', 't1', 'Xa', 'Xb', 'Ya', 'Yb', 'TT', 'vb', 'kbg']]
                    AT, kd, qdT, wT, u_sb = [Ot[so_][k_] for k_ in ['AT', 'kd', 'qdT', 'wT', 'u_sb']]
                    X, Xk = Xs_[sp_], ('X', sp_)
                    with nc.allow_non_contiguous_dma(reason="chunk load"):
                        S.dma(X[:], dnc_d.rearrange("c (a p) t -> p (c a) t", a=2)[:, :, 64 * n:64 * n + 64], (), [Xk])
                        yield
                    z, zk = zs_[so_], ('z', so_)
                    S.dma(z[:], zs_d[64 * n:64 * n + 64, :], (), [zk])
                    yield
                    for (dst, dk_, c0) in ((qtm, ('qtm', sp_), 0), (ktm, ('ktm', sp_), 8), (vtm, ('vtm', sp_), 16)):
                        ps, pk = prg[sp_].next()
                        for kc in range(8):
                            tr(ps[0:64, 64 * kc:64 * kc + 64], X[:, c0 + kc, :], I64, [Xk, 'ident'], [pk], inc=(kc == 7))
                            yield
                        if dk_ == ('ktm', sp_):
                            cp('dve', dst[:], v3(ps), [pk], [dk_])
                            yield
                        else:
                            act(dst[:], v3(ps), AF.Copy, [pk], [dk_])
                            yield
                    tt('dve', rhsg[:], bc(g_tm[:, n, :], 64, 2), bc(UT[0:64, :], 8, 1), ALU.mult, ['g_tm', 'UT'], [('rhsg', sp_)])
                    yield
                    tt('dve', rhsb[:], bc(b_tm[:, n, :], 64, 2), bc(I64, 8, 1), ALU.mult, ['b_tm', 'ident'], [('rhsb', sp_)])
                    yield
                    pg, pgk = prg[sp_].next()
                    mm(pg[0:64, :], ones[0:64, 0:64], rhsg[:].rearrange("p h c -> p (h c)"), True, True, ['ones', ('rhsg', sp_)], [pgk])
                    yield
                    pb, pbk = prg[sp_].next()
                    mm(pb[0:64, :], ones[0:64, 0:64], rhsb[:].rearrange("p h c -> p (h c)"), True, True, ['ones', ('rhsb', sp_)], [pbk])
                    yield
                    tt('dve', DTr[:], v3(pg), bc(gc_tm[:, n, :], 64, 2), ALU.subtract, [pgk, 'gc_tm'], [('DTr', sp_)])
                    yield
                    tt('dve', decT[:], DTr[:], bc(maskU[:], 8, 1), ALU.add, [('DTr', sp_), 'maskU'], [('decT', sp_)])
                    yield
                    act(decT[:], decT[:], AF.Exp, [('decT', sp_)], [('decT', sp_)])
                    yield
                    ts('dve', dec[:], DTr[:], -1.0, ALU.mult, [('DTr', sp_)], [('dec', sp_)])
                    yield
                    tt('dve', dec[:], dec[:], bc(maskL[:], 8, 1), ALU.add, [('dec', sp_), 'maskL'], [('dec', sp_)])
                    yield
                    act(dec[:], dec[:], AF.Exp, [('dec', sp_)], [('dec', sp_)])
                    yield
                    pkk, pkkk = prg[sp_].next()
                    hmm(pkk, pkkk, lambda h: X[:, 8 + h, :], lambda h: X[:, 8 + h, :], [Xk])
                    yield
                    pqk, pqkk = prg[sp_].next()
                    hmm(pqk, pqkk, lambda h: X[:, 8 + h, :], lambda h: X[:, h, :], [Xk])
                    yield
                    tt('dve', AT[:], v3(pqk), decT[:], ALU.mult, [pqkk, ('decT', sp_)], [('AT', so_)])
                    yield
                    tt('dve', t1[:], v3(pkk), decT[:], ALU.mult, [pkkk, ('decT', sp_)], [('t1', sp_)])
                    yield
                    tt('dve', t1[:], v3(pb), t1[:], ALU.mult, [pbk, ('t1', sp_)], [('t1', sp_)])
                    yield
                    tt('dve', Ya[:], t1[:], bc(nsU[:], 8, 1), ALU.mult, [('t1', sp_), 'nsU'], [('Ya', sp_)])
                    yield
                    tt('dve', t1[:], v3(pkk), dec[:], ALU.mult, [pkkk, ('dec', sp_)], [('t1', sp_)])
                    yield
                    tt('dve', t1[:], t1[:], bc(b_tm[:, n, :], 64, 2), ALU.mult, [('t1', sp_), 'b_tm'], [('t1', sp_)])
                    yield
                    tt('dve', Xa[:], t1[:], bc(nsL[:], 8, 1), ALU.mult, [('t1', sp_), 'nsL'], [('Xa', sp_)])
                    yield
                    tt('dve', TT[:], Ya[:], bc(I64, 8, 1), ALU.add, [('Ya', sp_), 'ident'], [('TT', sp_)])
                    yield
                    Xc, Xn_, Yc, Yn_ = (Xa, ('Xa', sp_)), (Xb, ('Xb', sp_)), (Ya, ('Ya', sp_)), (Yb, ('Yb', sp_))
                    for lvl in range(1, 6):
                        p1, p1k = prg[sp_].next()
                        hmm(p1, p1k, lambda h: Yc[0][:, h, :], lambda h: Xc[0][:, h, :], [Yc[1], Xc[1]])
                        yield
                        if lvl < 5:
                            p2, p2k = prg[sp_].next()
                            hmm(p2, p2k, lambda h: Xc[0][:, h, :], lambda h: Yc[0][:, h, :], [Yc[1], Xc[1]])
                            yield
                        act(Xn_[0][:], v3(p1), AF.Copy, [p1k], [Xn_[1]])
                        yield
                        if lvl < 5:
                            cp('dve', Yn_[0][:], v3(p2), [p2k], [Yn_[1]])
                            yield
                        Xc, Xn_ = Xn_, Xc
                        if lvl < 5:
                            Yc, Yn_ = Yn_, Yc
                        p3, p3k = prg[sp_].next()
                        hmm(p3, p3k, lambda h: Xc[0][:, h, :], lambda h: TT[:, h, :], [Xc[1], ('TT', sp_)])
                        yield
                        tt('dve', TT[:], TT[:], v3(p3), ALU.add, [('TT', sp_), p3k], [('TT', sp_)])
                        yield
                    tt('dve', vb[:], vtm[:], bc(b_tm[:, n, :], 64, 2), ALU.mult, [('vtm', sp_), 'b_tm'], [('vb', sp_)])
                    yield
                    tt('dve', kbg[:], ktm[:], bc(bg_tm[:, n, :], 64, 2), ALU.mult, [('ktm', sp_), 'bg_tm'], [('kbg', sp_)])
                    yield
                    tt('dve', kd[:], ktm[:], bc(kds_tm[:, n, :], 64, 2), ALU.mult, [('ktm', sp_), 'kds_tm'], [('kd', so_)])
                    yield
                    tt('dve', qtm[:], qtm[:], bc(eg_tm[:, n, :], 64, 2), ALU.mult, [('qtm', sp_), 'eg_tm'], [('qtm', sp_)])
                    yield
                    pu, puk = prg[sp_].next()
                    hmm(pu, puk, lambda h: TT[:, h, :], lambda h: vb[:, h, :], [('TT', sp_), ('vb', sp_)])
                    yield
                    act(u_sb[:], v3(pu), AF.Copy, [puk], [('u_sb', so_)])
                    yield
                    pw, pwk = prg[sp_].next()
                    hmm(pw, pwk, lambda h: kbg[:, h, :], lambda h: TT[:, h, :], [('TT', sp_), ('kbg', sp_)])
                    yield
                    act(wT[:], v3(pw), AF.Copy, [pwk], [('wT', so_)])
                    yield
                    pq, pqk2 = prg[sp_].next()
                    for h in range(8):
                        tr(pq[0:64, 64 * h:64 * h + 64], qtm[:, h, :], I64, [('qtm', sp_), 'ident'], [pqk2], inc=(h == 7))
                        yield
                    cp('dve', qdT[:], v3(pq), [pqk2], [('qdT', so_)])
                    yield
                def scan_gen(n, so_):
                    AT, kd, qdT, wT, u_sb = [Ot[so_][k_] for k_ in ['AT', 'kd', 'qdT', 'wT', 'u_sb']]
                    z, zk = zs_[so_], ('z', so_)
                    pv, pvk = psc.next()
                    hmm(pv, pvk, lambda h: wT[:, h, :], lambda h: Sst[:, h, :], [('wT', so_), 'Sst'])
                    yield
                    tt('dve', vn[:], u_sb[:], v3(pv), ALU.subtract, [('u_sb', so_), pvk], ['vn'])
                    yield
                    po, pok = psc.next()
                    for h in range(8):
                        mm(po[0:64, 64 * h:64 * h + 64], qdT[:, h, :], Sst[:, h, :], True, False, [('qdT', so_), 'Sst'], [pok], inc=False)
                        yield
                        mm(po[0:64, 64 * h:64 * h + 64], AT[:, h, :], vn[:, h, :], False, True, [('AT', so_), 'vn'], [pok], inc=(h == 7))
                        yield
                    pS, pSk = psc.next()
                    hmm(pS, pSk, lambda h: kd[:, h, :], lambda h: vn[:, h, :], [('kd', so_), 'vn'])
                    yield
                    tt('dve', Sst[:], Sst[:], bc(egl[:, n, :], 64, 2), ALU.mult, ['Sst', 'egl'], ['Sst'])
                    yield
                    tt('dve', Sst[:], Sst[:], v3(pS), ALU.add, ['Sst', pSk], ['Sst'])
                    yield
                    act(o_sb[:], v3(po), AF.Copy, [pok], ['o_sb'])
                    yield
                    tt('dve', osq[:], o_sb[:], o_sb[:], ALU.mult, ['o_sb'], ['osq'])
                    yield
                    S.op('dve', lambda e: e.tensor_reduce(out=ssum[:, 0:8], in_=osq[:], op=ALU.add, axis=AX.X), ['osq'], ['ssum'])
                    yield
                    act(ssum[:, 8:16], ssum[:, 0:8], AF.Sqrt, ['ssum'], ['ssum'], bias=EPS, scale=1.0 / 64)
                    yield
                    recip(ssum[:, 8:16], ssum[:, 8:16], ['ssum'], ['ssum'])
                    yield
                    tt('dve', o_sb[:], o_sb[:], bc(ssum[:, 8:16], 64, 2), ALU.mult, ['o_sb', 'ssum'], ['o_sb'])
                    yield
                    tt('dve', o_sb[:], o_sb[:], bc(ngrow[:], 8, 1), ALU.mult, ['o_sb', 'ngrow'], ['o_sb'])
                    yield
                    tt('dve', ytm[:], o_sb[:].rearrange("p h c -> p (h c)"), z[:], ALU.mult, ['o_sb', zk], ['ytm'])
                    yield
                    py, pyk = psc.next()
                    for kc in range(4):
                        tr(py[:, 64 * kc:64 * kc + 64], ytm[:, 128 * kc:128 * kc + 128], I64, ['ytm', 'ident'], [pyk], inc=(kc == 3))
                        yield
                    yo, yok = ybo.next()
                    act(yo[:], py[:, 0:256].rearrange("p (a b) -> p a b", a=4), AF.Copy, [pyk], [yok])
                    yield
                    with nc.allow_non_contiguous_dma(reason="yb store"):
                        S.dma(ybT_d.rearrange("c p t -> p c t")[:, :, 64 * n:64 * n + 64], yo[:], [yok], [('ybT', n)], q='pool')
                        yield


                S.barrier()
                prg = [Ring(PS[0:3], "psA"), Ring(PS[3:6], "psB")]
                psc = Ring(PS[6:8], "psC")

                def run_rr(gens):
                    gens = list(gens)
                    while gens:
                        for g_ in list(gens):
                            try:
                                next(g_)
                            except StopIteration:
                                gens.remove(g_)

                def scan_pair(a_, b_):
                    yield from scan_gen(a_, a_ % NOS)
                    yield from scan_gen(b_, b_ % NOS)
                prev_ = None
                for grp in range(NCH // 2):
                    a_, b_ = 2 * grp, 2 * grp + 1
                    gl = [pre_gen(a_, 0, a_ % NOS), pre_gen(b_, 1, b_ % NOS)]
                    if prev_ is not None:
                        gl.append(scan_pair(*prev_))
                    run_rr(gl)
                    prev_ = (a_, b_)
                run_rr([scan_pair(*prev_)])
                S.barrier()

        def phase_E(l, src_x):
            with ExitStack() as ph:
                def psb(name, shape):
                    return ph.enter_context(nc.sbuf_tensor("%s_E%d" % (name, l), list(shape), F32))
                wa = psb("wa", (128, 4, D))
                wb = psb("wb", (128, 4, D))
                wo = psb("wo", (128, 8, D))
                g1r = psb("g1r", (128, D))
                S.dma(g1r[:], mod_d[l, 2 * D:3 * D].partition_broadcast(128), (), ['g1r'])
                S.dma(wa[:], w_br_a[l].rearrange("(kc p) n -> p kc n", p=128), (), ['wa'])
                S.dma(wb[:], w_br_b[l].rearrange("(kc p) n -> p kc n", p=128), (), ['wb'])
                S.dma(wo[:], w_out[l].rearrange("(kc p) n -> p kc n", p=128), (), ['wo'])
                obr = Ring([psb("ob%d" % i, (128, 3, 512)) for i in range(2)], "ob")
                yaT = psb("yaT", (128, 4, 512))
                ybT = psb("ybT", (128, 4, 512))
                mg = Ring([psb("mg%d" % i, (128, 2, 512)) for i in range(2)], "mg")
                yT = psb("yT", (128, 8, 512))
                tmp = Ring([psb("tmp%d" % i, (128, 512)) for i in range(2)], "tmp")
                xr = Ring([psb("xe%d" % i, (128, D)) for i in range(2)], "xe")
                for j in range(QT):
                    t0 = 512 * j
                    cs = slice(t0, t0 + 512)
                    for sub in range(4):
                        r0 = t0 + 128 * sub
                        ob, obk = obr.next()
                        S.dma(ob[:], obr_d[:, r0:r0 + 128, :].rearrange("b p c -> p b c"), (), [obk])
                        tt('dve', ob[:, 0, :], ob[:, 0, :], ob[:, 1, :], ALU.add, [obk], [obk])
                        tt('pool', ob[:, 0, :], ob[:, 0, :], ob[:, 2, :], ALU.add, [obk], [obk])
                        transpose_to(ob[:, 0, :], obk, yaT, 'yaT', sub, nkc=4)
                    S.dma(ybT[:], ybT_d.rearrange("c p t -> p c t")[:, :, cs], (), ['ybT'])
                    for dc in range(8):
                        m, mk = mg.next()
                        S.dma(m[:, 0, :], mergeT_d[dc, :, cs], (), [mk])
                        S.dma(m[:, 1, :], mergeT_d[8 + dc, :, cs], (), [mk])
                        pa, pak = psr.next()
                        for kc in range(4):
                            mm(pa[:], wa[:, kc, 128 * dc:128 * dc + 128], yaT[:, kc, :], kc == 0, kc == 3, ['wa', 'yaT'], [pak], inc=(kc == 3))
                        pb, pbk = psr.next()
                        for kc in range(4):
                            mm(pb[:], wb[:, kc, 128 * dc:128 * dc + 128], ybT[:, kc, :], kc == 0, kc == 3, ['wb', 'ybT'], [pbk], inc=(kc == 3))
                        t, tk = tmp.next()
                        tt('dve', t[:], pa[:], m[:, 0, :], ALU.mult, [pak, mk], [tk])
                        tt('dve', m[:, 1, :], pb[:], m[:, 1, :], ALU.mult, [pbk, mk], [mk])
                        tt('pool', yT[:, dc, :], t[:], m[:, 1, :], ALU.add, [tk, mk], ['yT'])
                    for sub in range(4):
                        r0 = t0 + 128 * sub
                        xt, xk = xr.next()
                        S.dma(xt[:], src_x[r0:r0 + 128, :], [('x', r0 // 128)], [xk])
                        for nh in range(2):
                            po, pok = psr.next()
                            for kc in range(8):
                                mm(po[:], yT[:, kc, 128 * sub:128 * sub + 128], wo[:, kc, 512 * nh:512 * nh + 512], kc == 0, kc == 7,
                                   ['yT', 'wo'], [pok], inc=(kc == 7))
                            t, tk = tmp.next()
                            tt('dve', t[:], po[:], g1r[:, 512 * nh:512 * nh + 512], ALU.mult, [pok, 'g1r'], [tk])
                            tt('pool', xt[:, 512 * nh:512 * nh + 512], xt[:, 512 * nh:512 * nh + 512], t[:], ALU.add, [xk, tk], [xk])
                        S.dma(out[r0:r0 + 128, :], xt[:], [xk], [('x', r0 // 128)], q='pool')
                S.barrier()

        def phase_F(l):
            NE = 16
            with ExitStack() as ph:
                def psb(name, shape, dt=F32):
                    return ph.enter_context(nc.sbuf_tensor("%s_F%d" % (name, l), list(shape), dt))
                A2r = psb("A2r", (128, D))
                sh2r = psb("sh2r", (128, D))
                g2r = psb("g2r", (128, D))
                S.dma(sh2r[:], mod_d[l, 3 * D:4 * D].partition_broadcast(128), (), ['modrow'])
                S.dma(A2r[:], mod_d[l, 4 * D:5 * D].partition_broadcast(128), (), ['modrow'])
                S.dma(g2r[:], mod_d[l, 5 * D:6 * D].partition_broadcast(128), (), ['g2r'])
                xr = Ring([psb("xf%d" % i, (128, D)) for i in range(2)], "xf")
                hr = Ring([psb("hf%d" % i, (128, D)) for i in range(2)], "hf")
                sm = Ring([psb("smf%d" % i, (128, 8)) for i in range(4)], "smf")
                hTr = Ring([psb("hTf%d" % i, (128, 8, 256)) for i in range(2)], "hTf")
                rt = psb("rt", (128, 8, 16))
                rs = psb("rs", (128, 16))
                M1a = psb("M1a", (128, NT, NE))
                M2a = psb("M2a", (128, NT, NE))
                wn = psb("wn", (128, NT, 2))
                SU = psb("SU", (128, 128))
                memset('pool', SU[:], 1.0, ['SU'])
                asel(SU[:], SU[:], [[1, 128]], -1, -1, 0.0, ['SU'], ['SU'])
                rtr = [rt, psb("rt2", (128, 8, 16))]
                rsr = [rs, psb("rs2", (128, 16))]

                def p1_stage1(ti):
                    rt = rtr[ti % 2]
                    RT = ('rt', ti % 2)
                    hT, hTk = hTr.next()
                    ht, hk, xt, xk = norm_tile(out, 128 * ti, A2r[:], sh2r[:], xr, hr, sm)
                    S.dma(h2_d[128 * ti:128 * ti + 128, :], ht[:], [hk], [('h2', ti)], q='pool')
                    transpose_to(ht, hk, hT, hTk, 0)
                    pr, prk = psr.next()
                    for kc in range(8):
                        mm(pr[:, 0:16], hT[:, kc, 0:128], rtrw[:, kc, :], kc == 0, kc == 7, [hTk, 'rtrw'], [prk], inc=(kc == 7))
                    sc = rt[:, 0, :]
                    sel = rt[:, 1, :]
                    w1_ = rt[:, 2, :]
                    w2_ = rt[:, 3, :]
                    act(sc, pr[:, 0:16], AF.Sigmoid, [prk], [RT])
                    return ti

                def p1_stage2(ti):
                    rt = rtr[ti % 2]
                    rs = rsr[ti % 2]
                    RT = ('rt', ti % 2)
                    RS = ('rs', ti % 2)
                    sc = rt[:, 0, :]
                    sel = rt[:, 1, :]
                    w1_ = rt[:, 2, :]
                    w2_ = rt[:, 3, :]
                    tt('dve', sel, sc, rtrb[:], ALU.add, [RT, 'rtrb'], [RT])
                    sel3 = sel.rearrange("p (g e) -> p g e", g=4)
                    S.op('dve', lambda e, s3=sel3: e.tensor_reduce(out=rs[:, 0:4], in_=s3, op=ALU.max, axis=AX.X), [RT], [RS])
                    tt('dve', w1_.rearrange("p (g e) -> p g e", g=4), sel3, bc(rs[:, 0:4], 4, 2), ALU.is_ge, [RT, RS], [RT])
                    stt(w2_, w1_, -1e9, sel, ALU.mult, ALU.add, [RT], [RT])
                    S.op('dve', lambda e, a=w2_: e.tensor_reduce(out=rs[:, 4:8], in_=a.rearrange("p (g e) -> p g e", g=4), op=ALU.max, axis=AX.X),
                         [RT], [RS])
                    tt('dve', rs[:, 8:12], rs[:, 0:4], rs[:, 4:8], ALU.add, [RS], [RS])
                    S.op('dve', lambda e: e.tensor_reduce(out=rs[:, 12:13], in_=rs[:, 8:12], op=ALU.max, axis=AX.X), [RS], [RS])
                    ts('dve', rs[:, 4:8], rs[:, 8:12], rs[:, 12:13], ALU.is_ge, [RS], [RS], s2=-1.0, op1=ALU.add)
                    ts('dve', rs[:, 4:8], rs[:, 4:8], 1e9, ALU.mult, [RS], [RS])
                    tt('dve', w1_.rearrange("p (g e) -> p g e", g=4), sel3, bc(rs[:, 4:8], 4, 2), ALU.add, [RT, RS], [RT])
                    S.op('dve', lambda e, a=w1_: e.max(out=rt[:, 4, 0:8], in_=a), [RT], [RT])
                    ts('dve', M1a[:, ti, :], w1_, rt[:, 4, 0:1], ALU.is_ge, [RT], ['M1a'])
                    ts('dve', w2_, w1_, rt[:, 4, 1:2], ALU.is_ge, [RT], [RT])
                    tt('dve', M2a[:, ti, :], w2_, M1a[:, ti, :], ALU.subtract, [RT, 'M1a'], ['M2a'])
                    tt('dve', w2_, M1a[:, ti, :], sc, ALU.mult, [RT, 'M1a'], [RT])
                    S.op('dve', lambda e, a=w2_: e.tensor_reduce(out=rs[:, 13:14], in_=a, op=ALU.add, axis=AX.X), [RT], [RS])
                    tt('dve', w2_, M2a[:, ti, :], sc, ALU.mult, [RT, 'M2a'], [RT])
                    S.op('dve', lambda e, a=w2_: e.tensor_reduce(out=rs[:, 14:15], in_=a, op=ALU.add, axis=AX.X), [RT], [RS])
                    tt('dve', rs[:, 15:16], rs[:, 13:14], rs[:, 14:15], ALU.add, [RS], [RS])
                    recip(rs[:, 15:16], rs[:, 15:16], [RS], [RS])
                    ts('dve', wn[:, ti, :], rs[:, 13:15], rs[:, 15:16], ALU.mult, [RS], ['wn'])

                prev_t = None
                for ti in range(NT):
                    p1_stage1(ti)
                    if prev_t is not None:
                        p1_stage2(prev_t)
                    prev_t = ti
                p1_stage2(prev_t)

                NG = NT * NE
                Mall = psb("Mall", (128, NT, NE))
                cnt = psb("cnt", (128, NT, NE))
                base = psb("base", (128, NT, NE))
                dest = psb("dest", (128, NT, NE))
                dtmp = psb("dtmp", (128, NT, NE))
                d12 = psb("d12", (128, 2, NT))
                idx12 = psb("idx12", (128, 2, NT), I32)
                ev = psb("ev", (128, 8, NE))
                ebf = psb("ebf", (128, NBK))
                bidx_i = psb("bidx_i", (128, NBK), I32)
                bidx = psb("bidx", (128, NBK))
                pcol_i = psb("pcol_i", (128, 1), I32)
                pcol = psb("pcol", (128, 1))
                widx = psb("widx", (128, NBK), I32)
                tt('dve', Mall[:], M1a[:], M2a[:], ALU.add, ['M1a', 'M2a'], ['Mall'])
                Mf = Mall[:].rearrange("p t e -> p (t e)")
                prk_, prkk = psr.next()
                pcn, pcnk = psr.next()
                for c0 in range(0, NG, 512):
                    cw_ = min(512, NG - c0)
                    assert NG <= 512
                    mm(prk_[:, 0:cw_], SU[:], Mf[:, c0:c0 + cw_], True, True, ['SU', 'Mall'], [prkk])
                    mm(pcn[:, 0:cw_], ones[:], Mf[:, c0:c0 + cw_], True, True, ['ones', 'Mall'], [pcnk])
                cp('dve', cnt[:].rearrange("p t e -> p (t e)"), pcn[:, 0:NG], [pcnk], ['cnt'])
                memset('dve', base[:, 0, :], 0.0, ['base'])
                for ti in range(1, NT):
                    tt('dve', base[:, ti, :], base[:, ti - 1, :], cnt[:, ti - 1, :], ALU.add, ['base', 'cnt'], ['base'])
                tt('dve', ev[:, 0, :], base[:, NT - 1, :], cnt[:, NT - 1, :], ALU.add, ['base', 'cnt'], ['ev'])
                memset('dve', ev[:, 1, :], 0.0, ['ev'])
                for m in range(SEQ // MBLK):
                    stt(ev[:, 1, :], ev[:, 0, :], float(MBLK * m), ev[:, 1, :], ALU.is_gt, ALU.add, ['ev'], ['ev'])
                ts('dve', ev[:, 2, :], ev[:, 1, :], float(MBLK), ALU.mult, ['ev'], ['ev'])
                cp('dve', ev[:, 3, 0:1], ev[:, 2, 0:1], ['ev'], ['ev'])
                for e_ in range(1, NE):
                    tt('dve', ev[:, 3, e_:e_ + 1], ev[:, 3, e_ - 1:e_], ev[:, 2, e_:e_ + 1], ALU.add, ['ev'], ['ev'])
                tt('dve', ev[:, 4, :], ev[:, 3, :], ev[:, 2, :], ALU.subtract, ['ev'], ['ev'])
                tt('dve', dest[:].rearrange("p t e -> p (t e)"), prk_[:, 0:NG], base[:].rearrange("p t e -> p (t e)"), ALU.add, [prkk, 'base'], ['dest'])
                tt('dve', dest[:], dest[:], bc(ev[:, 4, :], NT, 1), ALU.add, ['dest', 'ev'], ['dest'])
                for k_, Mk in ((0, M1a), (1, M2a)):
                    tt('dve', dtmp[:], dest[:], Mk[:], ALU.mult, ['dest', 'M1a', 'M2a'], ['dtmp'])
                    S.op('dve', lambda e, k_=k_: e.tensor_reduce(out=d12[:, k_, :], in_=dtmp[:], op=ALU.add, axis=AX.X), ['dtmp'], ['d12'])
                cp('dve', idx12[:], d12[:], ['d12'], ['idx12'])
                S.op('pool', lambda e: e.iota(bidx_i[:], pattern=[[MBLK, NBK]], base=0, channel_multiplier=0), (), ['bidx_i'])
                S.op('pool', lambda e: e.iota(pcol_i[:], pattern=[[0, 1]], base=0, channel_multiplier=1), (), ['pcol_i'])
                cp('dve', bidx[:], bidx_i[:], ['bidx_i'], ['bidx'])
                cp('dve', pcol[:], pcol_i[:], ['pcol_i'], ['pcol'])
                memset('dve', ebf[:], 0.0, ['ebf'])
                for e_ in range(NE):
                    stt(ebf[:], bidx[:], ev[:, 3, e_:e_ + 1], ebf[:], ALU.is_ge, ALU.add, ['bidx', 'ev', 'ebf'], ['ebf'])
                ts('dve', ebf[:], ebf[:], float(NE - 1), ALU.min, ['ebf'], ['ebf'], s2=128.0, op1=ALU.mult)
                ts('dve', ebf[:], ebf[:], pcol[:, 0:1], ALU.add, ['ebf', 'pcol'], ['ebf'], s2=float(l * 16 * 128), op1=ALU.add)
                cp('dve', widx[:], ebf[:], ['ebf'], ['widx'])
                for ti in range(NT):
                    ht, hk = hr.next()
                    S.dma(ht[:], h2_d[128 * ti:128 * ti + 128, :], [('h2', ti)], [hk], q='sp')
                    for k_ in range(2):
                        S.idma(Xs_d[:, :], ht[:, :], bass.IndirectOffsetOnAxis(ap=idx12[:, k_, ti:ti + 1], axis=0), None,
                               [hk, 'idx12'], [('Xs', ti, k_)])
                S.barrier()
                with ExitStack() as ph3:
                    def psb3(name, shape, dt=F32):
                        return ph3.enter_context(nc.sbuf_tensor("%s_F3%d" % (name, l), list(shape), dt))
                    wgr = Ring([psb3("wg%d" % i, (128, 8 * 512)) for i in range(2)], "wg")
                    wur = Ring([psb3("wu%d" % i, (128, 8 * 512)) for i in range(2)], "wu")
                    wdr = Ring([psb3("wd%d" % i, (128, 4 * D)) for i in range(2)], "wd")
                    xgr = Ring([psb3("xg%d" % i, (128, D)) for i in range(4)], "xg")
                    actT = psb3("actT", (128, 4, 256))
                    sg = Ring([psb3("sg%d" % i, (128, 256)) for i in range(2)], "sg")
                    yor = Ring([psb3("yo%d" % i, (128, D)) for i in range(2)], "yo")
                    wg_v = moe_wg.rearrange("l e (p kc) n -> (l e p) (kc n)", kc=8)
                    wu_v = moe_wu.rearrange("l e (p kc) n -> (l e p) (kc n)", kc=8)
                    wd_v = moe_wd.rearrange("l e (p fc) n -> (l e p) (fc n)", fc=4)
                    deferred = []
                    for b_ in range(NBK):
                        off = bass.IndirectOffsetOnAxis(ap=widx[:, b_:b_ + 1], axis=0)
                        wg, wgk = wgr.next()
                        wu, wuk = wur.next()
                        wd, wdk = wdr.next()
                        S.idma(wg[:, :], wg_v, None, off, ['widx'], [wgk])
                        S.idma(wu[:, :], wu_v, None, off, ['widx'], [wuk])
                        S.idma(wd[:, :], wd_v, None, off, ['widx'], [wdk])
                        hT, hTk = hTr.next()
                        xgs = []
                        for sub in range(2):
                            xg, xgk = xgr.next()
                            r0 = b_ * MBLK + 128 * sub
                            S.dma(xg[:], Xs_d[r0:r0 + 128, :], (), [xgk], q='sp')
                            xgs.append((xg, xgk))
                        for d_ in deferred:
                            S.dma(*d_[0], **d_[1])
                        deferred = []
                        for sub in range(2):
                            xg, xgk = xgs[sub]
                            for k0 in (0, 4):
                                ps, pk = psr.next()
                                for kc in range(k0, k0 + 4):
                                    tr(ps[:, (kc - k0) * 128:(kc - k0 + 1) * 128], xg[:, kc:D:8], ident[:], [xgk, 'ident'], [pk], inc=(kc == k0 + 3))
                                dst = hT[:, k0:k0 + 4, sub * 128:(sub + 1) * 128]
                                srcv = ps[:].rearrange("p (a b) -> p a b", a=4)
                                if k0 == 0:
                                    act(dst, srcv, AF.Copy, [pk], [hTk])
                                else:
                                    cp('dve', dst, srcv, [pk], [hTk])
                        for fc in range(4):
                            pg_, pgk_ = psr.next()
                            for kc in range(8):
                                mm(pg_[:, 0:256], wg[:, kc * 512 + fc:(kc + 1) * 512:4], hT[:, kc, :], kc == 0, kc == 7, [wgk, hTk], [pgk_], inc=(kc == 7))
                            pu_, puk_ = psr.next()
                            for kc in range(8):
                                mm(pu_[:, 0:256], wu[:, kc * 512 + fc:(kc + 1) * 512:4], hT[:, kc, :], kc == 0, kc == 7, [wuk, hTk], [puk_], inc=(kc == 7))
                            s_, sk_ = sg.next()
                            act(s_[:], pg_[:, 0:256], AF.Silu, [pgk_], [sk_])
                            tt('dve', actT[:, fc, :], pu_[:, 0:256], s_[:], ALU.mult, [puk_, sk_], [('actT', fc)])
                        for sub in range(2):
                            yo, yok = yor.next()
                            for nh in range(2):
                                pd, pdk = psr.next()
                                for fc in range(4):
                                    mm(pd[:], actT[:, fc, 128 * sub:128 * sub + 128], wd[:, fc * D + 512 * nh:fc * D + 512 * nh + 512], fc == 0, fc == 3,
                                       [('actT', fc), wdk], [pdk], inc=(fc == 3))
                                if nh == 0:
                                    act(yo[:, 0:512], pd[:], AF.Copy, [pdk], [yok])
                                else:
                                    cp('dve', yo[:, 512:1024], pd[:], [pdk], [yok])
                            r0 = b_ * MBLK + 128 * sub
                            deferred.append(((Ys_d[r0:r0 + 128, :], yo[:], [yok], [('Ys', b_, sub)]), dict(q='sp')))
                    for d_ in deferred:
                        S.dma(*d_[0], **d_[1])
                    S.barrier()
                y1r = Ring([psb("y1_%d" % i, (128, D)) for i in range(2)], "y1")
                y2r = Ring([psb("y2_%d" % i, (128, D)) for i in range(2)], "y2")
                for ti in range(NT):
                    y1, y1k = y1r.next()
                    y2, y2k = y2r.next()
                    S.idma(y1[:, :], Ys_d[:, :], None, bass.IndirectOffsetOnAxis(ap=idx12[:, 0, ti:ti + 1], axis=0), ['idx12'], [y1k])
                    S.idma(y2[:, :], Ys_d[:, :], None, bass.IndirectOffsetOnAxis(ap=idx12[:, 1, ti:ti + 1], axis=0), ['idx12'], [y2k])
                    xt, xk = xr.next()
                    S.dma(xt[:], out[128 * ti:128 * ti + 128, :], [('x', ti)], [xk], q='sp')
                    ts('dve', y1[:], y1[:], wn[:, ti, 0:1], ALU.mult, [y1k, 'wn'], [y1k])
                    stt(y1[:], y2[:], wn[:, ti, 1:2], y1[:], ALU.mult, ALU.add, [y2k, 'wn', y1k], [y1k])
                    tt('pool', y1[:], y1[:], g2r[:], ALU.mult, [y1k, 'g2r'], [y1k])
                    tt('pool', xt[:], xt[:], y1[:], ALU.add, [xk, y1k], [xk])
                    S.dma(out[128 * ti:128 * ti + 128, :], xt[:], [xk], [('x', ti)], q='sp')
                S.barrier()

        with ExitStack() as ph:
            modrow = ph.enter_context(nc.sbuf_tensor("modrow", [128, 6 * D], F32))
            rowtmp = ph.enter_context(nc.sbuf_tensor("rowtmp", [128, D], F32))
            wr0 = Ring([ph.enter_context(nc.sbuf_tensor("wsl0_%d" % i, [128, 8, 512], F32)) for i in range(2)], "wsl0")
            for l in range(L):
                S.dma(modrow[:], ada_b[l].partition_broadcast(128), ['modrow'], ['modrow'])
                for oc in range(12):
                    wt, wk = load_w(wr0, ada_w[l], oc * 512, 512)
                    ps, pk = psr.next()
                    for kc in range(8):
                        mm(ps[:], cbc[:, kc, :], wt[:, kc, :], kc == 0, kc == 7, ['cbc', wk], [pk], inc=(kc == 7))
                    tt('dve', modrow[:, oc * 512:(oc + 1) * 512], ps[:], modrow[:, oc * 512:(oc + 1) * 512], ALU.add,
                       [pk, 'modrow'], ['modrow'])
                for (o_, ng) in ((1, norm1_g), (4, norm2_g)):
                    S.dma(rowtmp[:], ng[l].partition_broadcast(128), ['rowtmp'], ['rowtmp'])
                    stt(modrow[:, o_ * D:(o_ + 1) * D], modrow[:, o_ * D:(o_ + 1) * D], 1.0, rowtmp[:], ALU.add, ALU.mult,
                        ['modrow', 'rowtmp'], ['modrow'])
                S.dma(mod_d[l:l + 1, :], modrow[0:1, :], ['modrow'], [('mod', l)], q='pool')
            S.barrier()

        for l in range(L if 'stop0' not in dbg else 0):
            src_x = x_in if l == 0 else out
            with ExitStack() as phm:
                A1t = phm.enter_context(nc.sbuf_tensor("A1t_%d" % l, [128, D], F32))
                sh1t = phm.enter_context(nc.sbuf_tensor("sh1t_%d" % l, [128, D], F32))
                S.dma(sh1t[:], mod_d[l, 0:D].partition_broadcast(128), (), ['modrow'])
                S.dma(A1t[:], mod_d[l, D:2 * D].partition_broadcast(128), (), ['modrow'])
                A1row, sh1row = A1t[:], sh1t[:]
                phase_A(l, src_x, A1row, sh1row)
            if 'stopA' in dbg:
                break
            phase_B(l)
            if 'stopB' in dbg:
                break
            phase_C(l)
            if 'stopC' in dbg:
                break
            phase_D(l)
            if 'stopD' in dbg:
                break
            phase_E(l, src_x)
            if 'stopE' in dbg:
                break
            phase_F(l)
        S.barrier()
    return nc


def _host_tables(rel_bias, SEQ):
    FDW = NEGPAD + SEQ
    d = np.arange(SEQ)
    bk = rel_bucket_np(d)
    g = np.asarray(rel_bias, np.float32)[bk].T
    fdg = np.full((NH, FDW), NEGM, np.float32)
    fdg[:, NEGPAD:] = g
    fdw = np.full((NH, FDW), NEGM, np.float32)
    fdw[:, NEGPAD:NEGPAD + 512] = g[:, :512]
    return fdg, fdw


_CACHE = {}


def kernel(**inputs):
    x = np.asarray(inputs["x"], np.float32)
    B, SEQ, _ = x.shape
    L = int(np.asarray(inputs["ada_w"]).shape[0])
    key = (SEQ, L)
    if key not in _CACHE:
        _CACHE[key] = build_program(SEQ, L)
    nc = _CACHE[key]
    fdg, fdw = _host_tables(inputs["rel_bias"], SEQ)
    names = ["router_w", "router_b", "ada_w", "ada_b", "norm1_g", "norm2_g", "w_in", "qk_norm_g", "cmp_pos", "cmp_w1",
             "cmp_w2", "dn_conv_w", "dn_a_log", "dn_dt_bias", "dn_norm_g", "w_branch_a", "w_branch_b", "w_out",
             "moe_w_gate", "moe_w_up", "moe_w_down"]
    shared = {n: np.ascontiguousarray(np.asarray(inputs[n], np.float32)) for n in names}
    shared["fdg"] = fdg
    shared["fdw"] = fdw
    c = np.asarray(inputs["c"], np.float32)
    in_maps = []
    for b in range(B):
        m = dict(shared)
        m["x"] = np.ascontiguousarray(x[b])
        m["cT"] = np.ascontiguousarray(c[b].reshape(8, 128).T)
        in_maps.append(m)
    res = run_bass_kernel_spmd(nc, in_maps, core_ids=list(range(B)))
    return np.stack([np.asarray(r["out"], np.float32) for r in res.results], axis=0)
```

```python
import math
from contextlib import ExitStack
import numpy as np
import concourse.bass as bass
import concourse.mybir as mybir
from concourse.bass_utils import run_bass_kernel_spmd

F32 = mybir.dt.float32
I32 = mybir.dt.int32
AF = mybir.ActivationFunctionType
ALU = mybir.AluOpType
AX = mybir.AxisListType

D = 1024
HD = 64
NH = 8
DIN = 5416
NEGM = -30000.0
EPS = 1e-6
U0 = 384
OFFMAX = 1024
WGEN = U0 + OFFMAX + 512
WWIN = U0 + 512 + 512
NEGPAD = 1024


class Sched:
    def __init__(self, nc, es, ndma=14):
        self.nc = nc
        self.eng = {'pe': nc.tensor, 'act': nc.scalar, 'dve': nc.vector, 'pool': nc.gpsimd, 'sp': nc.sync}
        self.sem = {k: es.enter_context(nc.semaphore('s_' + k)) for k in self.eng}
        self.cnt = {k: 0 for k in self.eng}
        self.dsem = [es.enter_context(nc.semaphore('d%d' % i)) for i in range(ndma)]
        self.dcnt = [0] * ndma
        self.dnext = 0
        self.seen = {k: {} for k in self.eng}
        self.res = {}
        self.nops = 0

    def _deps(self, r, w):
        deps = {}

        def add(t):
            if t is not None and deps.get(t[0], 0) < t[1]:
                deps[t[0]] = t[1]
        for k in r:
            st = self.res.get(k)
            if st:
                add(st[0])
        for k in w:
            st = self.res.get(k)
            if st:
                add(st[0])
                for s, v in st[1].items():
                    add((s, v))
        return deps

    def _wait(self, e, deps):
        for s, v in deps.items():
            if s == 'pe' and e == 'pe':
                continue
            if self.seen[e].get(s, 0) < v:
                sem = self.sem[s] if isinstance(s, str) else self.dsem[s]
                self.eng[e].wait_ge(sem, v)
                self.seen[e][s] = v

    def _mark(self, tag, r, w):
        for k in r:
            st = self.res.setdefault(k, [None, {}])
            if st[1].get(tag[0], 0) < tag[1]:
                st[1][tag[0]] = tag[1]
        for k in w:
            self.res[k] = [tag, {}]

    def op(self, e, emit, r=(), w=(), inc=True):
        self._wait(e, self._deps(r, w))
        inst = emit(self.eng[e])
        self.nops += 1
        if inc:
            self.cnt[e] += 1
            inst.then_inc(self.sem[e], 1)
            tag = (e, self.cnt[e])
        else:
            tag = (e, self.cnt[e] + 1)
        self._mark(tag, r, w)

    def dma(self, out, in_, r=(), w=(), q='sp'):
        i = self.dnext
        self.dnext = (i + 1) % len(self.dsem)
        deps = self._deps(r, w)
        if self.dcnt[i]:
            deps[i] = max(deps.get(i, 0), self.dcnt[i])
        self._wait(q, deps)
        self.dcnt[i] += 16
        self.eng[q].dma_start(out=out, in_=in_).then_inc(self.dsem[i], 16)
        self.nops += 1
        self._mark((i, self.dcnt[i]), r, w)

    def idma(self, out, in_, out_off, in_off, r=(), w=()):
        i = self.dnext
        self.dnext = (i + 1) % len(self.dsem)
        deps = self._deps(r, w)
        if self.dcnt[i]:
            deps[i] = max(deps.get(i, 0), self.dcnt[i])
        self._wait('pool', deps)
        self.dcnt[i] += 16
        self.eng['pool'].indirect_dma_start(out=out, out_offset=out_off, in_=in_, in_offset=in_off).then_inc(self.dsem[i], 16)
        self.nops += 1
        self._mark((i, self.dcnt[i]), r, w)

    def barrier(self):
        deps = {s: c for s, c in self.cnt.items() if c}
        for i, v in enumerate(self.dcnt):
            if v:
                deps[i] = v
        for e in self.eng:
            d = dict(deps)
            self._wait(e, d)
        self.res = {}


class Ring:
    def __init__(self, tiles, name):
        self.tiles = tiles
        self.name = name
        self.i = 0

    def next(self):
        k = self.i % len(self.tiles)
        self.i += 1
        return self.tiles[k], (self.name, k)


def rel_bucket_np(dist):
    exact = 16
    dist = np.maximum(dist, 0)
    far = np.maximum(dist, exact).astype(np.float32)
    large = exact + (np.log(far / np.float32(exact)) / np.float32(math.log(1024 / exact)) * np.float32(32 - exact)).astype(np.int32)
    return np.where(dist < exact, dist, np.minimum(large, 31))


def build_program(SEQ, DEPTH, dbg=()):
    NT = SEQ // 128
    QT = SEQ // 512
    NCH = SEQ // 64
    NCMP = SEQ // 16 - 1
    NBLK = SEQ // 64
    NCT = (NCMP + 127) // 128
    FDW = NEGPAD + SEQ
    JB = NBLK
    nc = bass.Bass("TRN2", target_bir_lowering=False)

    def din(name, shape):
        return nc.dram_tensor(name, list(shape), F32, kind="ExternalInput").ap()

    def dscr(name, shape, kind="Internal"):
        if name in dbg:
            kind = "ExternalOutput"
        return nc.dram_tensor(name, list(shape), F32, kind=kind).ap()

    L = DEPTH
    x_in = din("x", (SEQ, D))
    cT_in = din("cT", (128, 8))
    fdg_in = din("fdg", (NH, FDW))
    fdw_in = din("fdw", (NH, FDW))
    router_w = din("router_w", (D, 16))
    router_b = din("router_b", (16,))
    ada_w = din("ada_w", (L, D, 6 * D))
    ada_b = din("ada_b", (L, 6 * D))
    norm1_g = din("norm1_g", (L, D))
    norm2_g = din("norm2_g", (L, D))
    w_in = din("w_in", (L, D, DIN))
    qk_norm_g = din("qk_norm_g", (L, 4, HD))
    cmp_pos = din("cmp_pos", (L, 2, 32, HD))
    cmp_w1 = din("cmp_w1", (L, 2, 2048, 256))
    cmp_w2 = din("cmp_w2", (L, 2, 256, HD))
    dn_conv_w = din("dn_conv_w", (L, 4, 1536))
    dn_a_log = din("dn_a_log", (L, 8))
    dn_dt_bias = din("dn_dt_bias", (L, 8))
    dn_norm_g = din("dn_norm_g", (L, HD))
    w_br_a = din("w_branch_a", (L, 512, D))
    w_br_b = din("w_branch_b", (L, 512, D))
    w_out = din("w_out", (L, D, D))
    moe_wg = din("moe_w_gate", (L, 16, D, 512))
    moe_wu = din("moe_w_up", (L, 16, D, 512))
    moe_wd = din("moe_w_down", (L, 16, 512, D))
    out = nc.dram_tensor("out", [SEQ, D], F32, kind="ExternalOutput").ap()

    qT_d = dscr("qT_d", (4, 128, SEQ))
    kcT_d = dscr("kcT_d", (2, 128, SEQ))
    kslcT_d = dscr("kslcT_d", (128, SEQ))
    kwinT_d = dscr("kwinT_d", (128, SEQ))
    vslc_d = dscr("vslc_d", (SEQ, 128))
    vwin_d = dscr("vwin_d", (SEQ, 128))
    gate_d = dscr("gate_d", (SEQ, 24))
    dnraw_d = dscr("dnraw_d", (12, 128, SEQ))
    dnc_d = dscr("dnc_d", (12, 128, SEQ))
    bg_d = dscr("bg_d", (SEQ, 16))
    zs_d = dscr("zs_d", (SEQ, 512))
    mergeT_d = dscr("mergeT_d", (16, 128, SEQ))
    obr_d = dscr("obr_d", (3, SEQ, 512))
    ybT_d = dscr("ybT_d", (4, 128, SEQ))
    bct_d = dscr("bct_d", (NH, NCT * 128, SEQ))
    bgen_d = dscr("bgen_d", (128, NH, WGEN))
    bwin_d = dscr("bwin_d", (128, NH, WWIN))
    selbT_d = dscr("selbT_d", (128, SEQ))
    MBLK = 256
    NBK = (2 * SEQ + 16 * MBLK) // MBLK
    h2_d = dscr("h2_d", (SEQ, D))
    Xs_d = dscr("Xs_d", (NBK * MBLK, D))
    Ys_d = dscr("Ys_d", (NBK * MBLK, D))
    mod_d = dscr("mod_d", (L, 6 * D))

    es = ExitStack()
    with es:
        S = Sched(nc, es)

        def sb(name, shape):
            return es.enter_context(nc.sbuf_tensor(name, list(shape), F32))

        PS = [es.enter_context(nc.psum_tensor("ps%d" % i, [128, 512], F32)) for i in range(8)]
        psr = Ring(PS, "ps")

        def tt(e, o, a, b, op, r, w):
            S.op(e, lambda g: g.tensor_tensor(out=o, in0=a, in1=b, op=op), r, w)

        def ts(e, o, a, s1, op0, r, w, s2=None, op1=None):
            if op1 is None:
                S.op(e, lambda g: g.tensor_scalar(out=o, in0=a, scalar1=s1, scalar2=None, op0=op0), r, w)
            else:
                S.op(e, lambda g: g.tensor_scalar(out=o, in0=a, scalar1=s1, scalar2=s2, op0=op0, op1=op1), r, w)

        def stt(o, a, sc, b, op0, op1, r, w):
            S.op('dve', lambda g: g.scalar_tensor_tensor(out=o, in0=a, scalar=sc, in1=b, op0=op0, op1=op1), r, w)

        def act(o, a, f, r, w, bias=None, scale=1.0, accum=None):
            kw = {}
            if bias is not None:
                kw['bias'] = bias
            if accum is not None:
                kw['accum_out'] = accum
            S.op('act', lambda g: g.activation(out=o, in_=a, func=f, scale=scale, **kw), r, w)

        def mm(o, lT, rh, st, sp, r, w, inc=True):
            S.op('pe', lambda g: g.matmul(o, lT, rh, start=st, stop=sp), r, w, inc=inc)

        def tr(o, a, idn, r, w, inc=True):
            S.op('pe', lambda g: g.transpose(o, a, idn), r, w, inc=inc)

        def cp(e, o, a, r, w):
            S.op(e, lambda g: g.tensor_copy(o, a), r, w)

        def recip(o, a, r, w):
            S.op('dve', lambda g: g.reciprocal(o, a), r, w)

        def memset(e, o, v, w):
            S.op(e, lambda g: g.memset(o, v), (), w)

        def asel(o, a, pattern, base, cm, fill, r, w, op=ALU.is_ge):
            S.op('pool', lambda g: g.affine_select(out=o, in_=a, pattern=pattern, compare_op=op, fill=fill,
                                                   base=base, channel_multiplier=cm), r, w)

        ident = sb("ident", (128, 128))
        ones = sb("ones", (128, 128))
        bdones = sb("bdones", (128, 128))
        UT = sb("UT", (128, 64))
        maskU = sb("maskU", (64, 64))
        maskL = sb("maskL", (64, 64))
        nsU = sb("nsU", (64, 64))
        nsL = sb("nsL", (64, 64))
        ovl = sb("ovl", (128, NCT, JB))
        memset('pool', ident[:], 0.0, ['ident'])
        asel(ident[:], ident[:], [[-1, 128]], 0, 1, 1.0, ['ident'], ['ident'], op=ALU.not_equal)
        memset('pool', ones[:], 1.0, ['ones'])
        memset('pool', bdones[:], 0.0, ['bdones'])
        memset('pool', bdones[0:64, 0:64], 1.0, ['bdones'])
        memset('pool', bdones[64:128, 64:128], 1.0, ['bdones'])
        for h0 in (0, 64):
            memset('pool', UT[h0:h0 + 64, :], 1.0, ['UT'])
            asel(UT[h0:h0 + 64, :], UT[h0:h0 + 64, :], [[1, 64]], 0, -1, 0.0, ['UT'], ['UT'])
        memset('pool', maskU[:], 0.0, ['maskU'])
        asel(maskU[:], maskU[:], [[1, 64]], 0, -1, NEGM, ['maskU'], ['maskU'])
        memset('pool', maskL[:], 0.0, ['maskL'])
        asel(maskL[:], maskL[:], [[-1, 64]], 0, 1, NEGM, ['maskL'], ['maskL'])
        memset('pool', nsU[:], -1.0, ['nsU'])
        asel(nsU[:], nsU[:], [[1, 64]], -1, -1, 0.0, ['nsU'], ['nsU'])
        memset('pool', nsL[:], -1.0, ['nsL'])
        asel(nsL[:], nsL[:], [[-1, 64]], -1, 1, 0.0, ['nsL'], ['nsL'])
        ovt = sb("ovt", (128, NCT, JB))
        memset('pool', ovl[:], 0.0, ['ovl'])
        for m in (0, 1):
            memset('pool', ovt[:], 1.0, ['ovt'])
            asel(ovt[:], ovt[:], [[128, NCT], [-4, JB]], m, 1, 0.0, ['ovt'], ['ovt'])
            asel(ovt[:], ovt[:], [[-128, NCT], [4, JB]], 3 - m, -1, 0.0, ['ovt'], ['ovt'])
            tt('pool', ovl[:], ovl[:], ovt[:], ALU.add, ['ovl', 'ovt'], ['ovl'])

        with nc.allow_non_contiguous_dma(reason="table build"):
            for p in range(128):
                o0 = NEGPAD - U0 - p
                S.dma(bgen_d[p, :, :], fdg_in[:, o0:o0 + WGEN], (), [('bgen', p)], q='sp')
                S.dma(bwin_d[p, :, :], fdw_in[:, o0:o0 + WWIN], (), [('bwin', p)], q='pool')
            for n in range(NCT * 128):
                o0 = NEGPAD - (16 * n + 31)
                if n >= NCMP:
                    o0 = 0
                q = 'sp' if n % 2 == 0 else 'pool'
                if n >= NCMP:
                    S.dma(bct_d[:, n, 0:NEGPAD], fdg_in[:, 0:NEGPAD], (), [('bct', n)], q=q)
                    for c0 in range(NEGPAD, SEQ, NEGPAD):
                        S.dma(bct_d[:, n, c0:c0 + NEGPAD], fdg_in[:, 0:NEGPAD], (), [('bct', n, c0)], q=q)
                elif o0 >= 0:
                    S.dma(bct_d[:, n, :], fdg_in[:, o0:o0 + SEQ], (), [('bct', n)], q=q)
                else:
                    nn = -o0
                    for c0 in range(0, nn, NEGPAD):
                        cw = min(NEGPAD, nn - c0)
                        S.dma(bct_d[:, n, c0:c0 + cw], fdg_in[:, 0:cw], (), [('bct', n, c0)], q=q)
                    S.dma(bct_d[:, n, nn:SEQ], fdg_in[:, 0:SEQ - nn], (), [('bct', n)], q=q)
        S.barrier()

        cact = sb("cact", (128, 8))
        S.dma(cact[:], cT_in[:, :], (), ['cact'])
        act(cact[:], cact[:], AF.Silu, ['cact'], ['cact'])
        cbc = sb("cbc", (128, 8, 128))
        for kc in range(8):
            ts('dve', cbc[:, kc, :], ones[:], cact[:, kc:kc + 1], ALU.mult, ['ones', 'cact'], ['cbc'])
        rtrb = sb("rtrb", (128, 16))
        S.dma(rtrb[:], router_b.partition_broadcast(128), (), ['rtrb'])
        rtrw = sb("rtrw", (128, 8, 16))
        with nc.allow_non_contiguous_dma(reason="router w"):
            S.dma(rtrw[:], router_w.rearrange("(kc p) e -> p kc e", p=128), (), ['rtrw'])

        def load_w(ring, src2d, c0, ncols, kch=8):
            t, k = ring.next()
            with nc.allow_non_contiguous_dma(reason="weight slab"):
                S.dma(t[:, 0:kch, 0:ncols], src2d.rearrange("(kc p) n -> p kc n", p=128)[:, :, c0:c0 + ncols], (), [k])
            return t, k

        def norm_tile(src, t0, Arow, shrow, xr, hr, sm):
            xt, xk = xr.next()
            S.dma(xt[:], src[t0:t0 + 128, :], [('x', t0 // 128)], [xk])
            ht, hk = hr.next()
            s, sk = sm.next()
            act(ht[:], xt[:], AF.Square, [xk], [hk, sk], accum=s[:, 0:1])
            act(s[:, 1:2], s[:, 0:1], AF.Sqrt, [sk], [sk], bias=EPS, scale=1.0 / D)
            recip(s[:, 2:3], s[:, 1:2], [sk], [sk])
            stt(ht[:], xt[:], s[:, 2:3], Arow, ALU.mult, ALU.mult, [xk, sk, 'modrow'], [hk])
            tt('pool', ht[:], ht[:], shrow, ALU.add, [hk, 'modrow'], [hk])
            return ht, hk, xt, xk

        def transpose_to(ht, hk, hT, hTk, sub, nkc=8):
            for k0 in range(0, nkc, 4):
                ps, pk = psr.next()
                for kc in range(k0, k0 + 4):
                    tr(ps[:, (kc - k0) * 128:(kc - k0 + 1) * 128], ht[:, kc * 128:(kc + 1) * 128], ident[:],
                       [hk, 'ident'], [pk], inc=(kc == k0 + 3))
                e = 'act' if (k0 // 4) % 2 == 0 else 'dve'
                dst = hT[:, k0:k0 + 4, sub * 128:(sub + 1) * 128]
                srcv = ps[:].rearrange("p (a b) -> p a b", a=4)
                if e == 'act':
                    act(dst, srcv, AF.Copy, [pk], [hTk])
                else:
                    cp('dve', dst, srcv, [pk], [hTk])

        def phase_A(l, src_x, A1row, sh1row):
            with ExitStack() as ph:
                def psb(name, shape):
                    return ph.enter_context(nc.sbuf_tensor("%s_A%d" % (name, l), list(shape), F32))
                wr = Ring([psb("wsl%d" % i, (128, 8, 512)) for i in range(3)], "wsl")
                xr = Ring([psb("xa%d" % i, (128, D)) for i in range(2)], "xa")
                hr = Ring([psb("ha%d" % i, (128, D)) for i in range(2)], "ha")
                hTr = Ring([psb("hT%d" % i, (128, 8, 512)) for i in range(2)], "hT")
                st = Ring([psb("st%d" % i, (128, 512)) for i in range(4)], "st")
                sq = Ring([psb("sq%d" % i, (128, 512)) for i in range(2)], "sq")
                sm = Ring([psb("sm%d" % i, (128, 8)) for i in range(4)], "sm")
                gains = psb("gains", (128, 4))
                dtb = psb("dtb", (128, 8))
                nea = psb("nea", (128, 8))
                with nc.allow_non_contiguous_dma(reason="small"):
                    for h0 in (0, 64):
                        S.dma(gains[h0:h0 + 64, :], qk_norm_g[l].rearrange("i d -> d i"), (), ['gains'])
                ts('dve', gains[:, 0:1], gains[:, 0:1], HD ** -0.5, ALU.mult, ['gains'], ['gains'])
                S.dma(dtb[:], dn_dt_bias[l].partition_broadcast(128), (), ['dtb'])
                S.dma(nea[:], dn_a_log[l].partition_broadcast(128), (), ['nea'])
                act(nea[:], nea[:], AF.Exp, ['nea'], ['nea'])
                ts('dve', nea[:], nea[:], -1.0, ALU.mult, ['nea'], ['nea'])

                def rms64_store(ps, pk, gcol, dst, dkey):
                    q1, qk1 = sq.next()
                    act(q1[:], ps[:], AF.Square, [pk], [qk1])
                    p2, pk2 = psr.next()
                    mm(p2[:], bdones[:], q1[:], True, True, ['bdones', qk1], [pk2])
                    act(q1[:], p2[:], AF.Sqrt, [pk2], [qk1], bias=EPS, scale=1.0 / 64)
                    recip(q1[:], q1[:], [qk1], [qk1])
                    o, ok = st.next()
                    stt(o[:], ps[:], gcol, q1[:], ALU.mult, ALU.mult, [pk, qk1, 'gains'], [ok])
                    S.dma(dst, o[:], [ok], [dkey], q='pool')

                def fm_store(ps, pk, dst, dkey, func=AF.Copy):
                    o, ok = st.next()
                    act(o[:], ps[:], func, [pk], [ok])
                    S.dma(dst, o[:], [ok], [dkey], q='pool')

                for j in range(QT):
                    t0 = 512 * j
                    hT, hTk = hTr.next()
                    for sub in range(4):
                        ht, hk, _, _ = norm_tile(src_x, t0 + 128 * sub, A1row, sh1row, xr, hr, sm)
                        transpose_to(ht, hk, hT, hTk, sub)

                    def fm(wt, wk, lsel):
                        ps, pk = psr.next()
                        for kc in range(8):
                            mm(ps[:], lsel(kc), hT[:, kc, :], kc == 0, kc == 7, [wk, hTk], [pk], inc=(kc == 7))
                        return ps, pk

                    def tm(wt, wk, c0, ncols, sub):
                        ps, pk = psr.next()
                        for kc in range(8):
                            mm(ps[:, 0:ncols], hT[:, kc, sub * 128:(sub + 1) * 128], wt[:, kc, c0:c0 + ncols],
                               kc == 0, kc == 7, [wk, hTk], [pk], inc=(kc == 7))
                        return ps, pk
                    cs = slice(t0, t0 + 512)
                    wt, wk = wr.next()
                    with nc.allow_non_contiguous_dma(reason="q slab"):
                        for a in range(2):
                            for c in range(4):
                                S.dma(wt[:, :, c * 128 + a * 64:c * 128 + a * 64 + 64],
                                      w_in[l].rearrange("(kc p) n -> p kc n", p=128)[:, :, a * 256 + c * 64:a * 256 + c * 64 + 64], (), [wk])
                    for c in range(4):
                        ps, pk = fm(wt, wk, lambda kc, c=c: wt[:, kc, c * 128:(c + 1) * 128])
                        rms64_store(ps, pk, gains[:, 0:1], qT_d[c, :, cs], ('qT', c, j))
                    wt, wk = load_w(wr, w_in[l], 512, 512)
                    for c in range(3):
                        ps, pk = fm(wt, wk, lambda kc, c=c: wt[:, kc, c * 128:(c + 1) * 128])
                        if c < 2:
                            fm_store(ps, pk, kcT_d[c, :, cs], ('kcT', c, j))
                        else:
                            rms64_store(ps, pk, gains[:, 2:3], kslcT_d[:, cs], ('kslcT', j))
                    for sub in range(4):
                        ps, pk = tm(wt, wk, 384, 128, sub)
                        o, ok = st.next()
                        act(o[:, 0:128], ps[:, 0:128], AF.Copy, [pk], [ok])
                        S.dma(vslc_d[t0 + 128 * sub:t0 + 128 * sub + 128, :], o[:, 0:128], [ok], [('vslc', j, sub)], q='pool')
                    wt, wk = load_w(wr, w_in[l], 1024, 280)
                    ps, pk = fm(wt, wk, lambda kc: wt[:, kc, 0:128])
                    rms64_store(ps, pk, gains[:, 3:4], kwinT_d[:, cs], ('kwinT', j))
                    for sub in range(4):
                        r0 = t0 + 128 * sub
                        ps, pk = tm(wt, wk, 128, 152, sub)
                        o, ok = st.next()
                        act(o[:, 0:128], ps[:, 0:128], AF.Copy, [pk], [ok])
                        act(o[:, 128:152], ps[:, 128:152], AF.Sigmoid, [pk], [ok])
                        S.dma(vwin_d[r0:r0 + 128, :], o[:, 0:128], [ok], [('vwin', j, sub)], q='pool')
                        S.dma(gate_d[r0:r0 + 128, :], o[:, 128:152], [ok], [('gate', j, sub)], q='pool')
                    for i in range(3):
                        wt, wk = load_w(wr, w_in[l], 1304 + 512 * i, 512)
                        for c in range(4):
                            ps, pk = fm(wt, wk, lambda kc, c=c: wt[:, kc, c * 128:(c + 1) * 128])
                            fm_store(ps, pk, dnraw_d[4 * i + c, :, cs], ('dnraw', 4 * i + c, j))
                    wt, wk = load_w(wr, w_in[l], 2840, 16)
                    for sub in range(4):
                        r0 = t0 + 128 * sub
                        ps, pk = tm(wt, wk, 0, 16, sub)
                        o, ok = st.next()
                        act(o[:, 0:8], ps[:, 0:8], AF.Sigmoid, [pk], [ok])
                        tt('dve', o[:, 8:16], ps[:, 8:16], dtb[:], ALU.add, [pk, 'dtb'], [ok])
                        act(o[:, 8:16], o[:, 8:16], AF.Exp, [ok], [ok])
                        act(o[:, 8:16], o[:, 8:16], AF.Ln, [ok], [ok], bias=1.0)
                        tt('dve', o[:, 8:16], o[:, 8:16], nea[:], ALU.mult, [ok, 'nea'], [ok])
                        S.dma(bg_d[r0:r0 + 128, :], o[:, 0:16], [ok], [('bg', j, sub)], q='pool')
                    wt, wk = load_w(wr, w_in[l], 2856, 512)
                    for sub in range(4):
                        r0 = t0 + 128 * sub
                        ps, pk = tm(wt, wk, 0, 512, sub)
                        fm_store(ps, pk, zs_d[r0:r0 + 128, :], ('zs', j, sub), func=AF.Silu)
                    for i in range(4):
                        wt, wk = load_w(wr, w_in[l], 3368 + 512 * i, 512)
                        for c in range(4):
                            ps, pk = fm(wt, wk, lambda kc, c=c: wt[:, kc, c * 128:(c + 1) * 128])
                            fm_store(ps, pk, mergeT_d[4 * i + c, :, cs], ('mergeT', 4 * i + c, j), func=AF.Sigmoid)
                S.barrier()
        def phase_B(l):
            with ExitStack() as ph:
                def psb(name, shape):
                    return ph.enter_context(nc.sbuf_tensor("%s_B%d" % (name, l), list(shape), F32))
                kst = [psb("kst%d" % g, (128, NCT * 128)) for g in range(2)]
                for g in range(2):
                    memset('pool', kst[g][:], 0.0, ['kcmpT'])
                vcmp = psb("vcmp", (128, NCT, 2, 65 + JB))
                memset('pool', vcmp[:], 0.0, ['vcmp'])
                memset('pool', vcmp[:, :, :, 64:65], 1.0, ['vcmp'])
                for g in range(2):
                    cp('pool', vcmp[:, :, g, 65:65 + JB], ovl[:], ['ovl', 'vcmp'], ['vcmp'])
                gains = psb("gains", (128, 4))
                with nc.allow_non_contiguous_dma(reason="small"):
                    for h0 in (0, 64):
                        S.dma(gains[h0:h0 + 64, :], qk_norm_g[l].rearrange("i d -> d i"), (), ['gains'])
                with ExitStack() as ph2:
                    def psb2(name, shape):
                        return ph2.enter_context(nc.sbuf_tensor("%s_B2%d" % (name, l), list(shape), F32))
                    kcT = psb2("kcT", (128, SEQ))
                    w1 = psb2("w1", (128, 32, 256))
                    posT = psb2("posT", (128, 32))
                    w2 = psb2("w2", (128, 2, 64))
                    pbias = psb2("pbias", (128, 2))
                    gx = psb2("gx", (128, 2, 2, 256))
                    gt = psb2("gt", (128, 256))
                    sqc = psb2("sqc", (128, 256))
                    for kvi in range(2):
                        S.dma(kcT[:], kcT_d[kvi, :, :], [('kcT', kvi, j) for j in range(QT)], ['kcT'])
                        with nc.allow_non_contiguous_dma(reason="cmp weights"):
                            for h0 in (0, 64):
                                for j0 in range(0, 32, 8):
                                    S.dma(w1[h0:h0 + 64, j0:j0 + 8, :], cmp_w1[l, kvi].rearrange("(j d) f -> d j f", d=64)[:, j0:j0 + 8, :], (), ['w1'])
                                for j0 in range(0, 32, 8):
                                    S.dma(posT[h0:h0 + 64, j0:j0 + 8], cmp_pos[l, kvi].rearrange("j d -> d j")[:, j0:j0 + 8], (), ['posT'])
                            S.dma(w2[:], cmp_w2[l, kvi].rearrange("(fc p) d -> p fc d", p=128), (), ['w2'])
                        for fc in range(2):
                            ps, pk = psr.next()
                            for j in range(32):
                                mm(ps[:, 0:1], w1[0:64, j, fc * 128:(fc + 1) * 128], posT[0:64, j:j + 1], j == 0, j == 31,
                                   ['w1', 'posT'], [pk], inc=(j == 31))
                            cp('dve', pbias[:, fc:fc + 1], ps[:, 0:1], [pk], ['pbias'])
                        for g in range(2):
                            hs = slice(64 * g, 64 * g + 64)
                            for fc in range(2):
                                ps, pk = psr.next()
                                for j in range(32):
                                    mm(ps[:, 0:NCMP], w1[hs, j, fc * 128:(fc + 1) * 128], kcT[hs, j:j + 16 * (NCMP - 1) + 1:16],
                                       j == 0, j == 31, ['w1', 'kcT'], [pk], inc=(j == 31))
                                xs = gx[:, g, fc, 0:NCMP]
                                ts('dve', xs, ps[:, 0:NCMP], pbias[:, fc:fc + 1], ALU.add, [pk, 'pbias'], ['gx'])
                                tt('dve', gt[:, 0:NCMP], xs, xs, ALU.mult, ['gx'], ['gt'])
                                ts('dve', gt[:, 0:NCMP], gt[:, 0:NCMP], 0.044715, ALU.mult, ['gt'], ['gt'], s2=1.0, op1=ALU.add)
                                tt('dve', gt[:, 0:NCMP], gt[:, 0:NCMP], xs, ALU.mult, ['gt', 'gx'], ['gt'])
                                act(gt[:, 0:NCMP], gt[:, 0:NCMP], AF.Sigmoid, ['gt'], ['gt'], scale=1.5957691216057308)
                                tt('dve', xs, xs, gt[:, 0:NCMP], ALU.mult, ['gx', 'gt'], ['gx'])
                            if kvi == 0:
                                ps, pk = psr.next()
                                for fc in range(2):
                                    mm(ps[hs, 0:NCMP], w2[:, fc, :], gx[:, g, fc, 0:NCMP], fc == 0, fc == 1, ['w2', 'gx'], [pk], inc=(fc == 1))
                                act(sqc[hs, 0:NCMP], ps[hs, 0:NCMP], AF.Square, [pk], ['sqc'])
                                p2, pk2 = psr.next()
                                mm(p2[hs, 0:NCMP], ones[hs, 0:64], sqc[hs, 0:NCMP], True, True, ['ones', 'sqc'], [pk2])
                                act(sqc[hs, 0:NCMP], p2[hs, 0:NCMP], AF.Sqrt, [pk2], ['sqc'], bias=EPS, scale=1.0 / 64)
                                recip(sqc[hs, 0:NCMP], sqc[hs, 0:NCMP], ['sqc'], ['sqc'])
                                stt(kst[g][hs, 0:NCMP], ps[hs, 0:NCMP], gains[hs, 1:2], sqc[hs, 0:NCMP], ALU.mult, ALU.mult,
                                    [pk, 'sqc', 'gains'], ['kcmpT'])
                            else:
                                for nt in range(NCT):
                                    nn = min(128, NCMP - nt * 128)
                                    ps, pk = psr.next()
                                    for fc in range(2):
                                        mm(ps[0:nn, 0:64], gx[:, g, fc, nt * 128:nt * 128 + nn], w2[:, fc, :], fc == 0, fc == 1,
                                           ['w2', 'gx'], [pk], inc=(fc == 1))
                                    cp('dve', vcmp[0:nn, nt, g, 0:64], ps[0:nn, 0:64], [pk], ['vcmp'])
                    S.barrier()
                qr = Ring([psb("qTb%d" % i, (128, SEQ)) for i in range(2)], "qTb")
                btr = Ring([psb("bt%d" % i, (128, NCT, 512)) for i in range(3)], "bt")
                pcr = Ring([psb("pc%d" % i, (128, NCT, 512)) for i in range(3)], "pc")
                osr = Ring([psb("os%d" % i, (128, 64)) for i in range(4)], "os")
                rdr = Ring([psb("rd%d" % i, (128, 2)) for i in range(4)], "rd")
                gate_sb = psb("gate_sb", (128, NT, 24))
                impacc = psb("impacc", (128, NT, 2, JB))
                with nc.allow_non_contiguous_dma(reason="gate"):
                    for t0_ in range(0, NT, 8):
                        S.dma(gate_sb[:, t0_:t0_ + 8, :], gate_d.rearrange("(t p) c -> p t c", p=128)[:, t0_:t0_ + 8, :],
                              [('gate', j, s) for j in range(QT) for s in range(4)], ['gate_sb'])
                W = 65 + JB
                qcur = {}

                def stage1(c, half, jq):
                    if c not in qcur:
                        qT, qk = qr.next()
                        S.dma(qT[:], qT_d[c, :, :], [('qT', c, j) for j in range(QT)], [qk])
                        qcur.clear()
                        qcur[c] = (qT, qk)
                    qT, qk = qcur[c]
                    h = c + 4 * half
                    g = half
                    tq0 = 512 * jq
                    nts = [nt for nt in range(NCT) if 16 * 128 * nt + 31 <= tq0 + 511]
                    bt, bk = btr.next()
                    pc, pck = pcr.next()
                    for nt in nts:
                        S.dma(bt[:, nt, :], bct_d[h, nt * 128:(nt + 1) * 128, tq0:tq0 + 512], (), [bk])
                    for nt in nts:
                        ps, pk = psr.next()
                        mm(ps[:], kst[g][:, nt * 128:(nt + 1) * 128], qT[:, tq0:tq0 + 512], True, True, ['kcmpT', qk], [pk])
                        tt('dve', pc[:, nt, :], ps[:], bt[:, nt, :], ALU.add, [pk, bk], [pck])
                        act(pc[:, nt, :], pc[:, nt, :], AF.Exp, [pck], [pck])
                    return (c, h, g, jq, nts, pc, pck)

                def stage2(item):
                    c, h, g, jq, nts, pc, pck = item
                    for sub in range(4):
                        tsi = 4 * jq + sub
                        po, pok = psr.next()
                        for nt in nts:
                            mm(po[:, 0:W], pc[:, nt, sub * 128:(sub + 1) * 128], vcmp[:, nt, g, :], nt == nts[0], nt == nts[-1],
                               [pck, 'vcmp'], [pok], inc=(nt == nts[-1]))
                        rd, rk = rdr.next()
                        ts('dve', rd[:, 0:1], po[:, 64:65], 1e-30, ALU.add, [pok], [rk])
                        recip(rd[:, 1:2], rd[:, 0:1], [rk], [rk])
                        o, ok = osr.next()
                        ts('dve', o[:], po[:, 0:64], rd[:, 1:2], ALU.mult, [pok, rk, 'gate_sb'], [ok],
                           s2=gate_sb[:, tsi, 3 * h:3 * h + 1], op1=ALU.mult)
                        S.dma(obr_d[0, tsi * 128:(tsi + 1) * 128, 64 * h:64 * h + 64], o[:], [ok], [('obr', 0, h, tsi)], q='pool')
                        if c == 0:
                            ts('dve', impacc[:, tsi, g, :], po[:, 65:W], rd[:, 1:2], ALU.mult, [pok, rk], [('imp', tsi, g)])
                        else:
                            stt(impacc[:, tsi, g, :], po[:, 65:W], rd[:, 1:2], impacc[:, tsi, g, :], ALU.mult, ALU.add,
                                [pok, rk, ('imp', tsi, g)], [('imp', tsi, g)])

                prev_it = None
                for c in range(4):
                    for half in range(2):
                        for jq in range(QT):
                            it_ = stage1(c, half, jq)
                            if prev_it is not None:
                                stage2(prev_it)
                            prev_it = it_
                stage2(prev_it)
                selM = psb("selM", (128, NT, JB))
                selA = psb("selA", (128, NT, JB))
                memset('pool', selM[:], 1.0, ['selM'])
                asel(selM[:], selM[:], [[128, NT], [-64, JB]], -128, 1, 0.0, ['selM'], ['selM'])
                memset('pool', selM[:, :, 0:1], 0.0, ['selM'])
                memset('pool', selA[:], 0.0, ['selA'])
                asel(selA[:], selA[:], [[128, NT], [-64, JB]], -128, 1, 1e9, ['selA'], ['selA'])
                asel(selA[:], selA[:], [[128, NT], [-64, JB]], 0, 1, -1.0, ['selA'], ['selA'])
                memset('pool', selA[:, :, 0:1], 1e9, ['selA'])
                scr = Ring([psb("sc%d" % i, (128, 2, JB)) for i in range(2)], "sc")
                sc2r = Ring([psb("sd%d" % i, (128, JB)) for i in range(2)], "sd")
                m8r = Ring([psb("m8%d" % i, (128, 16)) for i in range(4)], "m8")
                sbr = Ring([psb("sbi%d" % i, (128, 128)) for i in range(2)], "sbi")
                sto = Ring([psb("sto%d" % i, (128, 128)) for i in range(2)], "sto")
                for tsi in range(NT):
                    sc, sck = scr.next()
                    sbi, sbk = sbr.next()
                    if JB < 64:
                        memset('pool', sbi[:], 0.0, [sbk])
                    for g in range(2):
                        tt('dve', sc[:, g, :], impacc[:, tsi, g, :], selM[:, tsi, :], ALU.mult, [('imp', tsi, g), 'selM'], [sck])
                        tt('dve', sc[:, g, :], sc[:, g, :], selA[:, tsi, :], ALU.add, [sck, 'selA'], [sck])
                        m8, mk = m8r.next()
                        sd, sdk = sc2r.next()
                        S.op('dve', lambda e, a=m8, b=sc, g=g: e.max(out=a[:, 0:8], in_=b[:, g, :]), [sck], [mk])
                        S.op('dve', lambda e, a=m8, b=sc, d=sd, g=g: e.match_replace(out=d[:], in_to_replace=a[:, 0:8], in_values=b[:, g, :],
                                                                                   imm_value=-3e38), [sck, mk], [sdk])
                        S.op('dve', lambda e, a=m8, d=sd: e.max(out=a[:, 8:16], in_=d[:]), [sdk], [mk])
                        ts('dve', sbi[:, 64 * g:64 * g + JB], sc[:, g, :], m8[:, 15:16], ALU.is_ge, [sck, mk], [sbk], s2=-NEGM, op1=ALU.mult)
                        ts('dve', sbi[:, 64 * g:64 * g + JB], sbi[:, 64 * g:64 * g + JB], NEGM, ALU.add, [sbk], [sbk])
                    ps, pk = psr.next()
                    tr(ps[:, 0:128], sbi[:], ident[:], [sbk, 'ident'], [pk])
                    so, sok = sto.next()
                    act(so[:], ps[:, 0:128], AF.Copy, [pk], [sok])
                    S.dma(selbT_d[:, tsi * 128:(tsi + 1) * 128], so[:], [sok], [('selbT', tsi)], q='pool')
                S.barrier()

        def phase_C(l):
            for br in (1, 2):
                with ExitStack() as ph:
                    def psb(name, shape):
                        return ph.enter_context(nc.sbuf_tensor("%s_C%d_%d" % (name, l, br), list(shape), F32))
                    Wt = WGEN if br == 1 else WWIN
                    tab_d = bgen_d if br == 1 else bwin_d
                    KT_d = kslcT_d if br == 1 else kwinT_d
                    V_d = vslc_d if br == 1 else vwin_d
                    tab = psb("tab", (128, NH, Wt))
                    for h in range(NH):
                        S.dma(tab[:, h, :], tab_d[:, h, :], (), ['tab'])
                    LS = [psb("LS%d" % g, (128, SEQ)) for g in range(2)]
                    for g in range(2):
                        if br == 1:
                            S.dma(LS[g][0:64, :], KT_d[64 * g:64 * g + 64, :], (), [('LS', g)])
                            v = LS[g][64:128, :]
                            memset('pool', v, 1.0, [('LS', g)])
                            asel(v, v, [[1, SEQ]], 0, -64, 0.0, [('LS', g)], [('LS', g)])
                            asel(v, v, [[-1, SEQ]], 63, 64, 0.0, [('LS', g)], [('LS', g)])
                        else:
                            memset('pool', LS[g][64 * (1 - g):64 * (1 - g) + 64, :], 0.0, [('LS', g)])
                            S.dma(LS[g][64 * g:64 * g + 64, :], KT_d[64 * g:64 * g + 64, :], (), [('LS', g)])
                    V = psb("V", (128, NT, 2, 65))
                    memset('pool', V[:, :, :, 64:65], 1.0, ['V'])
                    with nc.allow_non_contiguous_dma(reason="V"):
                        for g in range(2):
                            for t0_ in range(0, NT, 8):
                                S.dma(V[:, t0_:t0_ + 8, g, 0:64], V_d.rearrange("(t p) c -> p t c", p=128)[:, t0_:t0_ + 8, 64 * g:64 * g + 64], (), ['V'])
                    gate_sb = psb("gate_sb", (128, NT, 24))
                    with nc.allow_non_contiguous_dma(reason="gate"):
                        for t0_ in range(0, NT, 8):
                            S.dma(gate_sb[:, t0_:t0_ + 8, :], gate_d.rearrange("(t p) c -> p t c", p=128)[:, t0_:t0_ + 8, :], (), ['gate_sb'])
                    qr = Ring([psb("qTc%d" % i, (128, SEQ)) for i in range(2)], "qTc")
                    ptr = Ring([psb("pt%d" % i, (128, 512)) for i in range(4)], "pt")
                    osr = Ring([psb("os%d" % i, (128, 64)) for i in range(4)], "os")
                    rdr = Ring([psb("rd%d" % i, (128, 2)) for i in range(4)], "rd")
                    b31 = psb("b31", (128, NH))
                    with nc.allow_non_contiguous_dma(reason="b31"):
                        S.dma(b31[:], fdg_in[:, NEGPAD + OFFMAX - 1].partition_broadcast(128), (), ['b31'])
                    psr4 = Ring(PS[0:4], "ps")
                    LAG = 2
                    for c in range(4):
                        if br == 2:
                            qT, qk = qr.next()
                            S.dma(qT[:], qT_d[c, :, :], (), [qk])
                        for half in range(2):
                            h = c + 4 * half
                            g = half
                            if br == 1:
                                qT, qk = qr.next()
                                S.dma(qT[0:64, :], qT_d[c, 64 * half:64 * half + 64, :], (), [qk])
                                S.dma(qT[64:128, :], selbT_d[64 * g:64 * g + 64, :], (), [qk])
                            pend = []

                            def stage3(item):
                                jq_, tk0_, pt_, ptk_, tks_, last_ = item
                                tq0_ = 512 * jq_
                                for sub in range(4):
                                    if tk0_ > tq0_ + 128 * sub + 127:
                                        continue
                                    mm(PS[4 + sub][:, 0:65], pt_[:, sub * 128:(sub + 1) * 128], V[:, tk0_ // 128, g, :],
                                       tk0_ == tks_[0], tk0_ == last_[sub], [ptk_, 'V'], [('ps', 4 + sub)], inc=(tk0_ == last_[sub]))
                                if tk0_ == tks_[-1]:
                                    for sub in range(4):
                                        tsi = 4 * jq_ + sub
                                        po = PS[4 + sub]
                                        pok = ('ps', 4 + sub)
                                        rd, rk = rdr.next()
                                        ts('dve', rd[:, 0:1], po[:, 64:65], 1e-30, ALU.add, [pok], [rk])
                                        recip(rd[:, 1:2], rd[:, 0:1], [rk], [rk])
                                        o, ok = osr.next()
                                        ts('dve', o[:], po[:, 0:64], rd[:, 1:2], ALU.mult, [pok, rk, 'gate_sb'], [ok],
                                           s2=gate_sb[:, tsi, 3 * h + br:3 * h + br + 1], op1=ALU.mult)
                                        S.dma(obr_d[br, tsi * 128:(tsi + 1) * 128, 64 * h:64 * h + 64], o[:], [ok], [('obr', br, h, tsi)], q='pool')

                            for jq in range(QT):
                                tq0 = 512 * jq
                                lo = 0 if br == 1 else max(0, tq0 - 512)
                                tks = list(range(lo, tq0 + 512, 128))
                                last = {sub: max(tk for tk in tks if tk <= tq0 + 128 * sub + 127) for sub in range(4)}
                                for tk0 in tks:
                                    ps, pk = psr4.next()
                                    mm(ps[:], LS[g][:, tk0:tk0 + 128], qT[:, tq0:tq0 + 512], True, True, [('LS', g), qk], [pk])
                                    pt, ptk = ptr.next()
                                    if tq0 - tk0 >= OFFMAX + 128:
                                        act(pt[:], ps[:], AF.Exp, [pk, 'b31'], [ptk], bias=b31[:, h:h + 1])
                                    else:
                                        off = min(tq0 - tk0, OFFMAX) + U0
                                        tt('dve', pt[:], ps[:], tab[:, h, off:off + 512], ALU.add, [pk, 'tab'], [ptk])
                                        act(pt[:], pt[:], AF.Exp, [ptk], [ptk])
                                    pend.append((jq, tk0, pt, ptk, tks, last))
                                    if len(pend) > LAG:
                                        stage3(pend.pop(0))
                            while pend:
                                stage3(pend.pop(0))
                    S.barrier()
        def bc(ap2, n, axis):
            P, A = ap2.shape
            if axis == 2:
                return ap2.unsqueeze(2).to_broadcast([P, A, n])
            return ap2.unsqueeze(1).to_broadcast([P, n, A])

        def phase_D(l):
            with ExitStack() as ph:
                def psb(name, shape):
                    return ph.enter_context(nc.sbuf_tensor("%s_D1%d" % (name, l), list(shape), F32))
                xr = Ring([psb("xin%d" % i, (128, SEQ + 3)) for i in range(2)], "xin")
                ar = Ring([psb("acc%d" % i, (128, SEQ)) for i in range(2)], "acc")
                sq = Ring([psb("sq%d" % i, (128, 512)) for i in range(4)], "sq")
                cw = psb("cw", (128, 4, 12))
                with nc.allow_non_contiguous_dma(reason="conv w"):
                    for i in range(4):
                        S.dma(cw[:, i, :], dn_conv_w[l, i].rearrange("(c p) -> p c", p=128), (), ['cw'])
                for ch in range(12):
                    xin, xk = xr.next()
                    acc, ak = ar.next()
                    memset('pool', xin[:, 0:3], 0.0, [xk])
                    S.dma(xin[:, 3:SEQ + 3], dnraw_d[ch, :, :], (), [xk])
                    ts('dve', acc[:], xin[:, 0:SEQ], cw[:, 0, ch:ch + 1], ALU.mult, [xk, 'cw'], [ak])
                    for i in range(1, 4):
                        stt(acc[:], xin[:, i:SEQ + i], cw[:, i, ch:ch + 1], acc[:], ALU.mult, ALU.add, [xk, 'cw', ak], [ak])
                    act(acc[:], acc[:], AF.Silu, [ak], [ak])
                    if ch < 8:
                        GRP = 4
                        for j0 in range(0, QT, GRP):
                            js = list(range(j0, min(QT, j0 + GRP)))
                            tiles = []
                            for j in js:
                                cs = slice(512 * j, 512 * j + 512)
                                q1, qk1 = sq.next()
                                act(q1[:], acc[:, cs], AF.Square, [ak], [qk1])
                                tiles.append((cs, q1, qk1))
                            pss = []
                            for (cs, q1, qk1) in tiles:
                                p2, pk2 = psr.next()
                                mm(p2[:], bdones[:], q1[:], True, True, ['bdones', qk1], [pk2])
                                pss.append((p2, pk2))
                            for (cs, q1, qk1), (p2, pk2) in zip(tiles, pss):
                                act(q1[:], p2[:], AF.Sqrt, [pk2], [qk1], bias=EPS, scale=1.0)
                            for (cs, q1, qk1) in tiles:
                                recip(q1[:], q1[:], [qk1], [qk1])
                            for (cs, q1, qk1) in tiles:
                                if ch < 4:
                                    stt(acc[:, cs], acc[:, cs], HD ** -0.5, q1[:], ALU.mult, ALU.mult, [ak, qk1], [ak])
                                else:
                                    tt('dve', acc[:, cs], acc[:, cs], q1[:], ALU.mult, [ak, qk1], [ak])
                    S.dma(dnc_d[ch, :, :], acc[:], [ak], [('dnc', ch)], q='pool')
                S.barrier()
            if 'stopD1' in dbg:
                return
            with ExitStack() as ph:
                def psb(name, shape):
                    return ph.enter_context(nc.sbuf_tensor("%s_D2%d" % (name, l), list(shape), F32))
                NG = NCH * 8
                g_tm = psb("g_tm", (64, NCH, 8))
                b_tm = psb("b_tm", (64, NCH, 8))
                gc_tm = psb("gc_tm", (64, NCH, 8))
                eg_tm = psb("eg_tm", (64, NCH, 8))
                bg_tm = psb("bg_tm", (64, NCH, 8))
                kds_tm = psb("kds_tm", (64, NCH, 8))
                egl = psb("egl", (64, NCH, 8))
                sel63 = psb("sel63", (64, 64))
                ngrow = psb("ngrow", (64, 64))
                with nc.allow_non_contiguous_dma(reason="bg"):
                    for n0_ in range(0, NCH, 16):
                        S.dma(b_tm[:, n0_:n0_ + 16, :], bg_d.rearrange("(n i) c -> i n c", i=64)[:, n0_:n0_ + 16, 0:8], (), ['b_tm'])
                        S.dma(g_tm[:, n0_:n0_ + 16, :], bg_d.rearrange("(n i) c -> i n c", i=64)[:, n0_:n0_ + 16, 8:16], (), ['g_tm'])
                S.dma(ngrow[:], dn_norm_g[l].partition_broadcast(64), (), ['ngrow'])
                ts('dve', sel63[:], ones[0:64, 0:64], ident[0:64, 63:64], ALU.mult, ['ones', 'ident'], ['sel63'])
                gflat = g_tm[:].rearrange("p n h -> p (n h)")
                gcflat = gc_tm[:].rearrange("p n h -> p (n h)")
                for c0 in range(0, NG, 512):
                    cw_ = min(512, NG - c0)
                    ps, pk = psr.next()
                    mm(ps[0:64, 0:cw_], UT[0:64, :], gflat[:, c0:c0 + cw_], True, True, ['UT', 'g_tm'], [pk])
                    cp('dve', gcflat[:, c0:c0 + cw_], ps[0:64, 0:cw_], [pk], ['gc_tm'])
                    ps2, pk2 = psr.next()
                    mm(ps2[0:64, 0:cw_], sel63[:], gcflat[:, c0:c0 + cw_], True, True, ['sel63', 'gc_tm'], [pk2])
                    act(egl[:].rearrange("p n h -> p (n h)")[:, c0:c0 + cw_], ps2[0:64, 0:cw_], AF.Exp, [pk2], ['egl'])
                    tt('dve', kds_tm[:].rearrange("p n h -> p (n h)")[:, c0:c0 + cw_], ps2[0:64, 0:cw_], gcflat[:, c0:c0 + cw_],
                       ALU.subtract, [pk2, 'gc_tm'], ['kds_tm'])
                act(kds_tm[:], kds_tm[:], AF.Exp, ['kds_tm'], ['kds_tm'])
                act(eg_tm[:], gc_tm[:], AF.Exp, ['gc_tm'], ['eg_tm'])
                tt('dve', bg_tm[:], b_tm[:], eg_tm[:], ALU.mult, ['b_tm', 'eg_tm'], ['bg_tm'])

                def t3(name):
                    return psb(name, (64, 8, 64))
                NPS, NOS = 2, 4
                Pt = [{nm: t3("%s_%d" % (nm, i)) for nm in ['ktm', 'vtm', 'qtm', 'rhsg', 'rhsb', 'DTr', 'decT', 'dec', 't1', 'Xa', 'Xb', 'Ya', 'Yb', 'TT', 'vb', 'kbg']} for i in range(NPS)]
                Ot = [{nm: t3("%s_%d" % (nm, i)) for nm in ['AT', 'kd', 'qdT', 'wT', 'u_sb']} for i in range(NOS)]
                Xs_ = [psb("X%d" % i, (64, 24, 64)) for i in range(NPS)]
                zs_ = [psb("z%d" % i, (64, 512)) for i in range(NOS)]
                vn, Sst, o_sb, osq = t3("vn"), t3("Sst"), t3("o_sb"), t3("osq")
                ssum = psb("ssum", (64, 16))
                ytm = psb("ytm", (64, 512))
                ybo = Ring([psb("ybo%d" % i, (128, 4, 64)) for i in range(2)], "ybo")
                memset('pool', Sst[:], 0.0, ['Sst'])
                I64 = ident[0:64, 0:64]

                def hmm(pst, pk, lhs, rhs, r, first=True, last=True):
                    for h in range(8):
                        mm(pst[0:64, 64 * h:64 * h + 64], lhs(h), rhs(h), first, last, r, [pk], inc=(h == 7))

                def v3(ps):
                    return ps[0:64, :].rearrange("p (h c) -> p h c", h=8)

                def pre_gen(n, sp_, so_):
                    ktm, vtm, qtm, rhsg, rhsb, DTr, decT, dec, t1, Xa, Xb, Ya, Yb, TT, vb, kbg = [Pt[sp_][k_] for k_ in ['ktm', 'vtm', 'qtm', 'rhsg', 'rhsb', 'DTr', 'decT', 'dec', 't1', 'Xa', 'Xb', 'Ya', 'Yb', 'TT', 'vb', 'kbg']]
                    AT, kd, qdT, wT, u_sb = [Ot[so_][k_] for k_ in ['AT', 'kd', 'qdT', 'wT', 'u_sb']]
                    X, Xk = Xs_[sp_], ('X', sp_)
                    with nc.allow_non_contiguous_dma(reason="chunk load"):
                        for c0_ in (0, 12):
                            S.dma(X[:, c0_:c0_ + 12, :], dnc_d.rearrange("c (a p) t -> p (c a) t", a=2)[:, c0_:c0_ + 12, 64 * n:64 * n + 64], (), [Xk])
                        yield
                    z, zk = zs_[so_], ('z', so_)
                    S.dma(z[:], zs_d[64 * n:64 * n + 64, :], (), [zk])
                    yield
                    for (dst, dk_, c0) in ((qtm, ('qtm', sp_), 0), (ktm, ('ktm', sp_), 8), (vtm, ('vtm', sp_), 16)):
                        ps, pk = prg[sp_].next()
                        for kc in range(8):
                            tr(ps[0:64, 64 * kc:64 * kc + 64], X[:, c0 + kc, :], I64, [Xk, 'ident'], [pk], inc=(kc == 7))
                            yield
                        if dk_ == ('ktm', sp_):
                            cp('dve', dst[:], v3(ps), [pk], [dk_])
                            yield
                        else:
                            act(dst[:], v3(ps), AF.Copy, [pk], [dk_])
                            yield
                    tt('dve', rhsg[:], bc(g_tm[:, n, :], 64, 2), bc(UT[0:64, :], 8, 1), ALU.mult, ['g_tm', 'UT'], [('rhsg', sp_)])
                    yield
                    tt('dve', rhsb[:], bc(b_tm[:, n, :], 64, 2), bc(I64, 8, 1), ALU.mult, ['b_tm', 'ident'], [('rhsb', sp_)])
                    yield
                    pg, pgk = prg[sp_].next()
                    mm(pg[0:64, :], ones[0:64, 0:64], rhsg[:].rearrange("p h c -> p (h c)"), True, True, ['ones', ('rhsg', sp_)], [pgk])
                    yield
                    pb, pbk = prg[sp_].next()
                    mm(pb[0:64, :], ones[0:64, 0:64], rhsb[:].rearrange("p h c -> p (h c)"), True, True, ['ones', ('rhsb', sp_)], [pbk])
                    yield
                    tt('dve', DTr[:], v3(pg), bc(gc_tm[:, n, :], 64, 2), ALU.subtract, [pgk, 'gc_tm'], [('DTr', sp_)])
                    yield
                    tt('dve', decT[:], DTr[:], bc(maskU[:], 8, 1), ALU.add, [('DTr', sp_), 'maskU'], [('decT', sp_)])
                    yield
                    act(decT[:], decT[:], AF.Exp, [('decT', sp_)], [('decT', sp_)])
                    yield
                    ts('dve', dec[:], DTr[:], -1.0, ALU.mult, [('DTr', sp_)], [('dec', sp_)])
                    yield
                    tt('dve', dec[:], dec[:], bc(maskL[:], 8, 1), ALU.add, [('dec', sp_), 'maskL'], [('dec', sp_)])
                    yield
                    act(dec[:], dec[:], AF.Exp, [('dec', sp_)], [('dec', sp_)])
                    yield
                    pkk, pkkk = prg[sp_].next()
                    hmm(pkk, pkkk, lambda h: X[:, 8 + h, :], lambda h: X[:, 8 + h, :], [Xk])
                    yield
                    pqk, pqkk = prg[sp_].next()
                    hmm(pqk, pqkk, lambda h: X[:, 8 + h, :], lambda h: X[:, h, :], [Xk])
                    yield
                    tt('dve', AT[:], v3(pqk), decT[:], ALU.mult, [pqkk, ('decT', sp_)], [('AT', so_)])
                    yield
                    tt('dve', t1[:], v3(pkk), decT[:], ALU.mult, [pkkk, ('decT', sp_)], [('t1', sp_)])
                    yield
                    tt('dve', t1[:], v3(pb), t1[:], ALU.mult, [pbk, ('t1', sp_)], [('t1', sp_)])
                    yield
                    tt('dve', Ya[:], t1[:], bc(nsU[:], 8, 1), ALU.mult, [('t1', sp_), 'nsU'], [('Ya', sp_)])
                    yield
                    tt('dve', t1[:], v3(pkk), dec[:], ALU.mult, [pkkk, ('dec', sp_)], [('t1', sp_)])
                    yield
                    tt('dve', t1[:], t1[:], bc(b_tm[:, n, :], 64, 2), ALU.mult, [('t1', sp_), 'b_tm'], [('t1', sp_)])
                    yield
                    tt('dve', Xa[:], t1[:], bc(nsL[:], 8, 1), ALU.mult, [('t1', sp_), 'nsL'], [('Xa', sp_)])
                    yield
                    tt('dve', TT[:], Ya[:], bc(I64, 8, 1), ALU.add, [('Ya', sp_), 'ident'], [('TT', sp_)])
                    yield
                    Xc, Xn_, Yc, Yn_ = (Xa, ('Xa', sp_)), (Xb, ('Xb', sp_)), (Ya, ('Ya', sp_)), (Yb, ('Yb', sp_))
                    for lvl in range(1, 6):
                        p1, p1k = prg[sp_].next()
                        hmm(p1, p1k, lambda h: Yc[0][:, h, :], lambda h: Xc[0][:, h, :], [Yc[1], Xc[1]])
                        yield
                        if lvl < 5:
                            p2, p2k = prg[sp_].next()
                            hmm(p2, p2k, lambda h: Xc[0][:, h, :], lambda h: Yc[0][:, h, :], [Yc[1], Xc[1]])
                            yield
                        act(Xn_[0][:], v3(p1), AF.Copy, [p1k], [Xn_[1]])
                        yield
                        if lvl < 5:
                            cp('dve', Yn_[0][:], v3(p2), [p2k], [Yn_[1]])
                            yield
                        Xc, Xn_ = Xn_, Xc
                        if lvl < 5:
                            Yc, Yn_ = Yn_, Yc
                        p3, p3k = prg[sp_].next()
                        hmm(p3, p3k, lambda h: Xc[0][:, h, :], lambda h: TT[:, h, :], [Xc[1], ('TT', sp_)])
                        yield
                        tt('dve', TT[:], TT[:], v3(p3), ALU.add, [('TT', sp_), p3k], [('TT', sp_)])
                        yield
                    tt('dve', vb[:], vtm[:], bc(b_tm[:, n, :], 64, 2), ALU.mult, [('vtm', sp_), 'b_tm'], [('vb', sp_)])
                    yield
                    tt('dve', kbg[:], ktm[:], bc(bg_tm[:, n, :], 64, 2), ALU.mult, [('ktm', sp_), 'bg_tm'], [('kbg', sp_)])
                    yield
                    tt('dve', kd[:], ktm[:], bc(kds_tm[:, n, :], 64, 2), ALU.mult, [('ktm', sp_), 'kds_tm'], [('kd', so_)])
                    yield
                    tt('dve', qtm[:], qtm[:], bc(eg_tm[:, n, :], 64, 2), ALU.mult, [('qtm', sp_), 'eg_tm'], [('qtm', sp_)])
                    yield
                    pu, puk = prg[sp_].next()
                    hmm(pu, puk, lambda h: TT[:, h, :], lambda h: vb[:, h, :], [('TT', sp_), ('vb', sp_)])
                    yield
                    act(u_sb[:], v3(pu), AF.Copy, [puk], [('u_sb', so_)])
                    yield
                    pw, pwk = prg[sp_].next()
                    hmm(pw, pwk, lambda h: kbg[:, h, :], lambda h: TT[:, h, :], [('TT', sp_), ('kbg', sp_)])
                    yield
                    act(wT[:], v3(pw), AF.Copy, [pwk], [('wT', so_)])
                    yield
                    pq, pqk2 = prg[sp_].next()
                    for h in range(8):
                        tr(pq[0:64, 64 * h:64 * h + 64], qtm[:, h, :], I64, [('qtm', sp_), 'ident'], [pqk2], inc=(h == 7))
                        yield
                    cp('dve', qdT[:], v3(pq), [pqk2], [('qdT', so_)])
                    yield
                def scan_gen(n, so_):
                    AT, kd, qdT, wT, u_sb = [Ot[so_][k_] for k_ in ['AT', 'kd', 'qdT', 'wT', 'u_sb']]
                    z, zk = zs_[so_], ('z', so_)
                    pv, pvk = psc.next()
                    hmm(pv, pvk, lambda h: wT[:, h, :], lambda h: Sst[:, h, :], [('wT', so_), 'Sst'])
                    yield
                    tt('dve', vn[:], u_sb[:], v3(pv), ALU.subtract, [('u_sb', so_), pvk], ['vn'])
                    yield
                    po, pok = psc.next()
                    for h in range(8):
                        mm(po[0:64, 64 * h:64 * h + 64], qdT[:, h, :], Sst[:, h, :], True, False, [('qdT', so_), 'Sst'], [pok], inc=False)
                        yield
                        mm(po[0:64, 64 * h:64 * h + 64], AT[:, h, :], vn[:, h, :], False, True, [('AT', so_), 'vn'], [pok], inc=(h == 7))
                        yield
                    pS, pSk = psc.next()
                    hmm(pS, pSk, lambda h: kd[:, h, :], lambda h: vn[:, h, :], [('kd', so_), 'vn'])
                    yield
                    tt('dve', Sst[:], Sst[:], bc(egl[:, n, :], 64, 2), ALU.mult, ['Sst', 'egl'], ['Sst'])
                    yield
                    tt('dve', Sst[:], Sst[:], v3(pS), ALU.add, ['Sst', pSk], ['Sst'])
                    yield
                    act(o_sb[:], v3(po), AF.Copy, [pok], ['o_sb'])
                    yield
                    tt('dve', osq[:], o_sb[:], o_sb[:], ALU.mult, ['o_sb'], ['osq'])
                    yield
                    S.op('dve', lambda e: e.tensor_reduce(out=ssum[:, 0:8], in_=osq[:], op=ALU.add, axis=AX.X), ['osq'], ['ssum'])
                    yield
                    act(ssum[:, 8:16], ssum[:, 0:8], AF.Sqrt, ['ssum'], ['ssum'], bias=EPS, scale=1.0 / 64)
                    yield
                    recip(ssum[:, 8:16], ssum[:, 8:16], ['ssum'], ['ssum'])
                    yield
                    tt('dve', o_sb[:], o_sb[:], bc(ssum[:, 8:16], 64, 2), ALU.mult, ['o_sb', 'ssum'], ['o_sb'])
                    yield
                    tt('dve', o_sb[:], o_sb[:], bc(ngrow[:], 8, 1), ALU.mult, ['o_sb', 'ngrow'], ['o_sb'])
                    yield
                    tt('dve', ytm[:], o_sb[:].rearrange("p h c -> p (h c)"), z[:], ALU.mult, ['o_sb', zk], ['ytm'])
                    yield
                    py, pyk = psc.next()
                    for kc in range(4):
                        tr(py[:, 64 * kc:64 * kc + 64], ytm[:, 128 * kc:128 * kc + 128], I64, ['ytm', 'ident'], [pyk], inc=(kc == 3))
                        yield
                    yo, yok = ybo.next()
                    act(yo[:], py[:, 0:256].rearrange("p (a b) -> p a b", a=4), AF.Copy, [pyk], [yok])
                    yield
                    with nc.allow_non_contiguous_dma(reason="yb store"):
                        S.dma(ybT_d.rearrange("c p t -> p c t")[:, :, 64 * n:64 * n + 64], yo[:], [yok], [('ybT', n)], q='pool')
                        yield


                S.barrier()
                prg = [Ring(PS[0:3], "psA"), Ring(PS[3:6], "psB")]
                psc = Ring(PS[6:8], "psC")

                def run_rr(gens):
                    gens = list(gens)
                    while gens:
                        for g_ in list(gens):
                            try:
                                next(g_)
                            except StopIteration:
                                gens.remove(g_)

                def scan_pair(a_, b_):
                    yield from scan_gen(a_, a_ % NOS)
                    yield from scan_gen(b_, b_ % NOS)
                prev_ = None
                for grp in range(NCH // 2):
                    a_, b_ = 2 * grp, 2 * grp + 1
                    gl = [pre_gen(a_, 0, a_ % NOS), pre_gen(b_, 1, b_ % NOS)]
                    if prev_ is not None:
                        gl.append(scan_pair(*prev_))
                    run_rr(gl)
                    prev_ = (a_, b_)
                run_rr([scan_pair(*prev_)])
                S.barrier()

        def phase_E(l, src_x):
            with ExitStack() as ph:
                def psb(name, shape):
                    return ph.enter_context(nc.sbuf_tensor("%s_E%d" % (name, l), list(shape), F32))
                wa = psb("wa", (128, 4, D))
                wb = psb("wb", (128, 4, D))
                wo = psb("wo", (128, 8, D))
                g1r = psb("g1r", (128, D))
                S.dma(g1r[:], mod_d[l, 2 * D:3 * D].partition_broadcast(128), (), ['g1r'])
                S.dma(wa[:], w_br_a[l].rearrange("(kc p) n -> p kc n", p=128), (), ['wa'])
                S.dma(wb[:], w_br_b[l].rearrange("(kc p) n -> p kc n", p=128), (), ['wb'])
                S.dma(wo[:], w_out[l].rearrange("(kc p) n -> p kc n", p=128), (), ['wo'])
                obr = Ring([psb("ob%d" % i, (128, 3, 512)) for i in range(2)], "ob")
                yaT = psb("yaT", (128, 4, 512))
                ybT = psb("ybT", (128, 4, 512))
                mg = Ring([psb("mg%d" % i, (128, 2, 512)) for i in range(2)], "mg")
                yT = psb("yT", (128, 8, 512))
                tmp = Ring([psb("tmp%d" % i, (128, 512)) for i in range(2)], "tmp")
                xr = Ring([psb("xe%d" % i, (128, D)) for i in range(2)], "xe")
                for j in range(QT):
                    t0 = 512 * j
                    cs = slice(t0, t0 + 512)
                    for sub in range(4):
                        r0 = t0 + 128 * sub
                        ob, obk = obr.next()
                        S.dma(ob[:], obr_d[:, r0:r0 + 128, :].rearrange("b p c -> p b c"), (), [obk])
                        tt('dve', ob[:, 0, :], ob[:, 0, :], ob[:, 1, :], ALU.add, [obk], [obk])
                        tt('pool', ob[:, 0, :], ob[:, 0, :], ob[:, 2, :], ALU.add, [obk], [obk])
                        transpose_to(ob[:, 0, :], obk, yaT, 'yaT', sub, nkc=4)
                    S.dma(ybT[:], ybT_d.rearrange("c p t -> p c t")[:, :, cs], (), ['ybT'])
                    for dc in range(8):
                        m, mk = mg.next()
                        S.dma(m[:, 0, :], mergeT_d[dc, :, cs], (), [mk])
                        S.dma(m[:, 1, :], mergeT_d[8 + dc, :, cs], (), [mk])
                        pa, pak = psr.next()
                        for kc in range(4):
                            mm(pa[:], wa[:, kc, 128 * dc:128 * dc + 128], yaT[:, kc, :], kc == 0, kc == 3, ['wa', 'yaT'], [pak], inc=(kc == 3))
                        pb, pbk = psr.next()
                        for kc in range(4):
                            mm(pb[:], wb[:, kc, 128 * dc:128 * dc + 128], ybT[:, kc, :], kc == 0, kc == 3, ['wb', 'ybT'], [pbk], inc=(kc == 3))
                        t, tk = tmp.next()
                        tt('dve', t[:], pa[:], m[:, 0, :], ALU.mult, [pak, mk], [tk])
                        tt('dve', m[:, 1, :], pb[:], m[:, 1, :], ALU.mult, [pbk, mk], [mk])
                        tt('pool', yT[:, dc, :], t[:], m[:, 1, :], ALU.add, [tk, mk], ['yT'])
                    for sub in range(4):
                        r0 = t0 + 128 * sub
                        xt, xk = xr.next()
                        S.dma(xt[:], src_x[r0:r0 + 128, :], [('x', r0 // 128)], [xk])
                        for nh in range(2):
                            po, pok = psr.next()
                            for kc in range(8):
                                mm(po[:], yT[:, kc, 128 * sub:128 * sub + 128], wo[:, kc, 512 * nh:512 * nh + 512], kc == 0, kc == 7,
                                   ['yT', 'wo'], [pok], inc=(kc == 7))
                            t, tk = tmp.next()
                            tt('dve', t[:], po[:], g1r[:, 512 * nh:512 * nh + 512], ALU.mult, [pok, 'g1r'], [tk])
                            tt('pool', xt[:, 512 * nh:512 * nh + 512], xt[:, 512 * nh:512 * nh + 512], t[:], ALU.add, [xk, tk], [xk])
                        S.dma(out[r0:r0 + 128, :], xt[:], [xk], [('x', r0 // 128)], q='pool')
                S.barrier()

        def phase_F(l):
            NE = 16
            with ExitStack() as ph:
                def psb(name, shape, dt=F32):
                    return ph.enter_context(nc.sbuf_tensor("%s_F%d" % (name, l), list(shape), dt))
                A2r = psb("A2r", (128, D))
                sh2r = psb("sh2r", (128, D))
                g2r = psb("g2r", (128, D))
                S.dma(sh2r[:], mod_d[l, 3 * D:4 * D].partition_broadcast(128), (), ['modrow'])
                S.dma(A2r[:], mod_d[l, 4 * D:5 * D].partition_broadcast(128), (), ['modrow'])
                S.dma(g2r[:], mod_d[l, 5 * D:6 * D].partition_broadcast(128), (), ['g2r'])
                xr = Ring([psb("xf%d" % i, (128, D)) for i in range(2)], "xf")
                hr = Ring([psb("hf%d" % i, (128, D)) for i in range(2)], "hf")
                sm = Ring([psb("smf%d" % i, (128, 8)) for i in range(4)], "smf")
                hTr = Ring([psb("hTf%d" % i, (128, 8, 256)) for i in range(2)], "hTf")
                rt = psb("rt", (128, 8, 16))
                rs = psb("rs", (128, 16))
                M1a = psb("M1a", (128, NT, NE))
                M2a = psb("M2a", (128, NT, NE))
                wn = psb("wn", (128, NT, 2))
                SU = psb("SU", (128, 128))
                memset('pool', SU[:], 1.0, ['SU'])
                asel(SU[:], SU[:], [[1, 128]], -1, -1, 0.0, ['SU'], ['SU'])
                rtr = [rt, psb("rt2", (128, 8, 16))]
                rsr = [rs, psb("rs2", (128, 16))]

                def p1_stage1(ti):
                    rt = rtr[ti % 2]
                    RT = ('rt', ti % 2)
                    hT, hTk = hTr.next()
                    ht, hk, xt, xk = norm_tile(out, 128 * ti, A2r[:], sh2r[:], xr, hr, sm)
                    S.dma(h2_d[128 * ti:128 * ti + 128, :], ht[:], [hk], [('h2', ti)], q='pool')
                    transpose_to(ht, hk, hT, hTk, 0)
                    pr, prk = psr.next()
                    for kc in range(8):
                        mm(pr[:, 0:16], hT[:, kc, 0:128], rtrw[:, kc, :], kc == 0, kc == 7, [hTk, 'rtrw'], [prk], inc=(kc == 7))
                    sc = rt[:, 0, :]
                    sel = rt[:, 1, :]
                    w1_ = rt[:, 2, :]
                    w2_ = rt[:, 3, :]
                    act(sc, pr[:, 0:16], AF.Sigmoid, [prk], [RT])
                    return ti

                def p1_stage2(ti):
                    rt = rtr[ti % 2]
                    rs = rsr[ti % 2]
                    RT = ('rt', ti % 2)
                    RS = ('rs', ti % 2)
                    sc = rt[:, 0, :]
                    sel = rt[:, 1, :]
                    w1_ = rt[:, 2, :]
                    w2_ = rt[:, 3, :]
                    tt('dve', sel, sc, rtrb[:], ALU.add, [RT, 'rtrb'], [RT])
                    sel3 = sel.rearrange("p (g e) -> p g e", g=4)
                    S.op('dve', lambda e, s3=sel3: e.tensor_reduce(out=rs[:, 0:4], in_=s3, op=ALU.max, axis=AX.X), [RT], [RS])
                    tt('dve', w1_.rearrange("p (g e) -> p g e", g=4), sel3, bc(rs[:, 0:4], 4, 2), ALU.is_ge, [RT, RS], [RT])
                    stt(w2_, w1_, -1e9, sel, ALU.mult, ALU.add, [RT], [RT])
                    S.op('dve', lambda e, a=w2_: e.tensor_reduce(out=rs[:, 4:8], in_=a.rearrange("p (g e) -> p g e", g=4), op=ALU.max, axis=AX.X),
                         [RT], [RS])
                    tt('dve', rs[:, 8:12], rs[:, 0:4], rs[:, 4:8], ALU.add, [RS], [RS])
                    S.op('dve', lambda e: e.tensor_reduce(out=rs[:, 12:13], in_=rs[:, 8:12], op=ALU.max, axis=AX.X), [RS], [RS])
                    ts('dve', rs[:, 4:8], rs[:, 8:12], rs[:, 12:13], ALU.is_ge, [RS], [RS], s2=-1.0, op1=ALU.add)
                    ts('dve', rs[:, 4:8], rs[:, 4:8], 1e9, ALU.mult, [RS], [RS])
                    tt('dve', w1_.rearrange("p (g e) -> p g e", g=4), sel3, bc(rs[:, 4:8], 4, 2), ALU.add, [RT, RS], [RT])
                    S.op('dve', lambda e, a=w1_: e.max(out=rt[:, 4, 0:8], in_=a), [RT], [RT])
                    ts('dve', M1a[:, ti, :], w1_, rt[:, 4, 0:1], ALU.is_ge, [RT], ['M1a'])
                    ts('dve', w2_, w1_, rt[:, 4, 1:2], ALU.is_ge, [RT], [RT])
                    tt('dve', M2a[:, ti, :], w2_, M1a[:, ti, :], ALU.subtract, [RT, 'M1a'], ['M2a'])
                    tt('dve', w2_, M1a[:, ti, :], sc, ALU.mult, [RT, 'M1a'], [RT])
                    S.op('dve', lambda e, a=w2_: e.tensor_reduce(out=rs[:, 13:14], in_=a, op=ALU.add, axis=AX.X), [RT], [RS])
                    tt('dve', w2_, M2a[:, ti, :], sc, ALU.mult, [RT, 'M2a'], [RT])
                    S.op('dve', lambda e, a=w2_: e.tensor_reduce(out=rs[:, 14:15], in_=a, op=ALU.add, axis=AX.X), [RT], [RS])
                    tt('dve', rs[:, 15:16], rs[:, 13:14], rs[:, 14:15], ALU.add, [RS], [RS])
                    recip(rs[:, 15:16], rs[:, 15:16], [RS], [RS])
                    ts('dve', wn[:, ti, :], rs[:, 13:15], rs[:, 15:16], ALU.mult, [RS], ['wn'])

                prev_t = None
                for ti in range(NT):
                    p1_stage1(ti)
                    if prev_t is not None:
                        p1_stage2(prev_t)
                    prev_t = ti
                p1_stage2(prev_t)

                NG = NT * NE
                Mall = psb("Mall", (128, NT, NE))
                cnt = psb("cnt", (128, NT, NE))
                base = psb("base", (128, NT, NE))
                dest = psb("dest", (128, NT, NE))
                dtmp = psb("dtmp", (128, NT, NE))
                d12 = psb("d12", (128, 2, NT))
                idx12 = psb("idx12", (128, 2, NT), I32)
                ev = psb("ev", (128, 8, NE))
                ebf = psb("ebf", (128, NBK))
                bidx_i = psb("bidx_i", (128, NBK), I32)
                bidx = psb("bidx", (128, NBK))
                pcol_i = psb("pcol_i", (128, 1), I32)
                pcol = psb("pcol", (128, 1))
                widx = psb("widx", (128, NBK), I32)
                tt('dve', Mall[:], M1a[:], M2a[:], ALU.add, ['M1a', 'M2a'], ['Mall'])
                Mf = Mall[:].rearrange("p t e -> p (t e)")
                prk_, prkk = psr.next()
                pcn, pcnk = psr.next()
                for c0 in range(0, NG, 512):
                    cw_ = min(512, NG - c0)
                    assert NG <= 512
                    mm(prk_[:, 0:cw_], SU[:], Mf[:, c0:c0 + cw_], True, True, ['SU', 'Mall'], [prkk])
                    mm(pcn[:, 0:cw_], ones[:], Mf[:, c0:c0 + cw_], True, True, ['ones', 'Mall'], [pcnk])
                cp('dve', cnt[:].rearrange("p t e -> p (t e)"), pcn[:, 0:NG], [pcnk], ['cnt'])
                memset('dve', base[:, 0, :], 0.0, ['base'])
                for ti in range(1, NT):
                    tt('dve', base[:, ti, :], base[:, ti - 1, :], cnt[:, ti - 1, :], ALU.add, ['base', 'cnt'], ['base'])
                tt('dve', ev[:, 0, :], base[:, NT - 1, :], cnt[:, NT - 1, :], ALU.add, ['base', 'cnt'], ['ev'])
                memset('dve', ev[:, 1, :], 0.0, ['ev'])
                for m in range(SEQ // MBLK):
                    stt(ev[:, 1, :], ev[:, 0, :], float(MBLK * m), ev[:, 1, :], ALU.is_gt, ALU.add, ['ev'], ['ev'])
                ts('dve', ev[:, 2, :], ev[:, 1, :], float(MBLK), ALU.mult, ['ev'], ['ev'])
                cp('dve', ev[:, 3, 0:1], ev[:, 2, 0:1], ['ev'], ['ev'])
                for e_ in range(1, NE):
                    tt('dve', ev[:, 3, e_:e_ + 1], ev[:, 3, e_ - 1:e_], ev[:, 2, e_:e_ + 1], ALU.add, ['ev'], ['ev'])
                tt('dve', ev[:, 4, :], ev[:, 3, :], ev[:, 2, :], ALU.subtract, ['ev'], ['ev'])
                tt('dve', dest[:].rearrange("p t e -> p (t e)"), prk_[:, 0:NG], base[:].rearrange("p t e -> p (t e)"), ALU.add, [prkk, 'base'], ['dest'])
                tt('dve', dest[:], dest[:], bc(ev[:, 4, :], NT, 1), ALU.add, ['dest', 'ev'], ['dest'])
                for k_, Mk in ((0, M1a), (1, M2a)):
                    tt('dve', dtmp[:], dest[:], Mk[:], ALU.mult, ['dest', 'M1a', 'M2a'], ['dtmp'])
                    S.op('dve', lambda e, k_=k_: e.tensor_reduce(out=d12[:, k_, :], in_=dtmp[:], op=ALU.add, axis=AX.X), ['dtmp'], ['d12'])
                cp('dve', idx12[:], d12[:], ['d12'], ['idx12'])
                S.op('pool', lambda e: e.iota(bidx_i[:], pattern=[[MBLK, NBK]], base=0, channel_multiplier=0), (), ['bidx_i'])
                S.op('pool', lambda e: e.iota(pcol_i[:], pattern=[[0, 1]], base=0, channel_multiplier=1), (), ['pcol_i'])
                cp('dve', bidx[:], bidx_i[:], ['bidx_i'], ['bidx'])
                cp('dve', pcol[:], pcol_i[:], ['pcol_i'], ['pcol'])
                memset('dve', ebf[:], 0.0, ['ebf'])
                for e_ in range(NE):
                    stt(ebf[:], bidx[:], ev[:, 3, e_:e_ + 1], ebf[:], ALU.is_ge, ALU.add, ['bidx', 'ev', 'ebf'], ['ebf'])
                ts('dve', ebf[:], ebf[:], float(NE - 1), ALU.min, ['ebf'], ['ebf'], s2=128.0, op1=ALU.mult)
                ts('dve', ebf[:], ebf[:], pcol[:, 0:1], ALU.add, ['ebf', 'pcol'], ['ebf'], s2=float(l * 16 * 128), op1=ALU.add)
                cp('dve', widx[:], ebf[:], ['ebf'], ['widx'])
                for ti in range(NT):
                    ht, hk = hr.next()
                    S.dma(ht[:], h2_d[128 * ti:128 * ti + 128, :], [('h2', ti)], [hk], q='sp')
                    for k_ in range(2):
                        S.idma(Xs_d[:, :], ht[:, :], bass.IndirectOffsetOnAxis(ap=idx12[:, k_, ti:ti + 1], axis=0), None,
                               [hk, 'idx12'], [('Xs', ti, k_)])
                S.barrier()
                with ExitStack() as ph3:
                    def psb3(name, shape, dt=F32):
                        return ph3.enter_context(nc.sbuf_tensor("%s_F3%d" % (name, l), list(shape), dt))
                    wgr = Ring([psb3("wg%d" % i, (128, 8 * 512)) for i in range(2)], "wg")
                    wur = Ring([psb3("wu%d" % i, (128, 8 * 512)) for i in range(2)], "wu")
                    wdr = Ring([psb3("wd%d" % i, (128, 4 * D)) for i in range(2)], "wd")
                    xgr = Ring([psb3("xg%d" % i, (128, D)) for i in range(4)], "xg")
                    actT = psb3("actT", (128, 4, 256))
                    sg = Ring([psb3("sg%d" % i, (128, 256)) for i in range(2)], "sg")
                    yor = Ring([psb3("yo%d" % i, (128, D)) for i in range(2)], "yo")
                    wg_v = moe_wg.rearrange("l e (p kc) n -> (l e p) (kc n)", kc=8)
                    wu_v = moe_wu.rearrange("l e (p kc) n -> (l e p) (kc n)", kc=8)
                    wd_v = moe_wd.rearrange("l e (p fc) n -> (l e p) (fc n)", fc=4)
                    deferred = []
                    for b_ in range(NBK):
                        off = bass.IndirectOffsetOnAxis(ap=widx[:, b_:b_ + 1], axis=0)
                        wg, wgk = wgr.next()
                        wu, wuk = wur.next()
                        wd, wdk = wdr.next()
                        S.idma(wg[:, :], wg_v, None, off, ['widx'], [wgk])
                        S.idma(wu[:, :], wu_v, None, off, ['widx'], [wuk])
                        S.idma(wd[:, :], wd_v, None, off, ['widx'], [wdk])
                        hT, hTk = hTr.next()
                        xgs = []
                        for sub in range(2):
                            xg, xgk = xgr.next()
                            r0 = b_ * MBLK + 128 * sub
                            S.dma(xg[:], Xs_d[r0:r0 + 128, :], (), [xgk], q='sp')
                            xgs.append((xg, xgk))
                        for d_ in deferred:
                            S.dma(*d_[0], **d_[1])
                        deferred = []
                        for sub in range(2):
                            xg, xgk = xgs[sub]
                            for k0 in (0, 4):
                                ps, pk = psr.next()
                                for kc in range(k0, k0 + 4):
                                    tr(ps[:, (kc - k0) * 128:(kc - k0 + 1) * 128], xg[:, kc:D:8], ident[:], [xgk, 'ident'], [pk], inc=(kc == k0 + 3))
                                dst = hT[:, k0:k0 + 4, sub * 128:(sub + 1) * 128]
                                srcv = ps[:].rearrange("p (a b) -> p a b", a=4)
                                if k0 == 0:
                                    act(dst, srcv, AF.Copy, [pk], [hTk])
                                else:
                                    cp('dve', dst, srcv, [pk], [hTk])
                        for fc in range(4):
                            pg_, pgk_ = psr.next()
                            for kc in range(8):
                                mm(pg_[:, 0:256], wg[:, kc * 512 + fc:(kc + 1) * 512:4], hT[:, kc, :], kc == 0, kc == 7, [wgk, hTk], [pgk_], inc=(kc == 7))
                            pu_, puk_ = psr.next()
                            for kc in range(8):
                                mm(pu_[:, 0:256], wu[:, kc * 512 + fc:(kc + 1) * 512:4], hT[:, kc, :], kc == 0, kc == 7, [wuk, hTk], [puk_], inc=(kc == 7))
                            s_, sk_ = sg.next()
                            act(s_[:], pg_[:, 0:256], AF.Silu, [pgk_], [sk_])
                            tt('dve', actT[:, fc, :], pu_[:, 0:256], s_[:], ALU.mult, [puk_, sk_], [('actT', fc)])
                        for sub in range(2):
                            yo, yok = yor.next()
                            for nh in range(2):
                                pd, pdk = psr.next()
                                for fc in range(4):
                                    mm(pd[:], actT[:, fc, 128 * sub:128 * sub + 128], wd[:, fc * D + 512 * nh:fc * D + 512 * nh + 512], fc == 0, fc == 3,
                                       [('actT', fc), wdk], [pdk], inc=(fc == 3))
                                if nh == 0:
                                    act(yo[:, 0:512], pd[:], AF.Copy, [pdk], [yok])
                                else:
                                    cp('dve', yo[:, 512:1024], pd[:], [pdk], [yok])
                            r0 = b_ * MBLK + 128 * sub
                            deferred.append(((Ys_d[r0:r0 + 128, :], yo[:], [yok], [('Ys', b_, sub)]), dict(q='sp')))
                    for d_ in deferred:
                        S.dma(*d_[0], **d_[1])
                    S.barrier()
                y1r = Ring([psb("y1_%d" % i, (128, D)) for i in range(2)], "y1")
                y2r = Ring([psb("y2_%d" % i, (128, D)) for i in range(2)], "y2")
                for ti in range(NT):
                    y1, y1k = y1r.next()
                    y2, y2k = y2r.next()
                    S.idma(y1[:, :], Ys_d[:, :], None, bass.IndirectOffsetOnAxis(ap=idx12[:, 0, ti:ti + 1], axis=0), ['idx12'], [y1k])
                    S.idma(y2[:, :], Ys_d[:, :], None, bass.IndirectOffsetOnAxis(ap=idx12[:, 1, ti:ti + 1], axis=0), ['idx12'], [y2k])
                    xt, xk = xr.next()
                    S.dma(xt[:], out[128 * ti:128 * ti + 128, :], [('x', ti)], [xk], q='sp')
                    ts('dve', y1[:], y1[:], wn[:, ti, 0:1], ALU.mult, [y1k, 'wn'], [y1k])
                    stt(y1[:], y2[:], wn[:, ti, 1:2], y1[:], ALU.mult, ALU.add, [y2k, 'wn', y1k], [y1k])
                    tt('pool', y1[:], y1[:], g2r[:], ALU.mult, [y1k, 'g2r'], [y1k])
                    tt('pool', xt[:], xt[:], y1[:], ALU.add, [xk, y1k], [xk])
                    S.dma(out[128 * ti:128 * ti + 128, :], xt[:], [xk], [('x', ti)], q='sp')
                S.barrier()

        with ExitStack() as ph:
            modrow = ph.enter_context(nc.sbuf_tensor("modrow", [128, 6 * D], F32))
            rowtmp = ph.enter_context(nc.sbuf_tensor("rowtmp", [128, D], F32))
            wr0 = Ring([ph.enter_context(nc.sbuf_tensor("wsl0_%d" % i, [128, 8, 512], F32)) for i in range(2)], "wsl0")
            for l in range(L):
                S.dma(modrow[:], ada_b[l].partition_broadcast(128), ['modrow'], ['modrow'])
                for oc in range(12):
                    wt, wk = load_w(wr0, ada_w[l], oc * 512, 512)
                    ps, pk = psr.next()
                    for kc in range(8):
                        mm(ps[:], cbc[:, kc, :], wt[:, kc, :], kc == 0, kc == 7, ['cbc', wk], [pk], inc=(kc == 7))
                    tt('dve', modrow[:, oc * 512:(oc + 1) * 512], ps[:], modrow[:, oc * 512:(oc + 1) * 512], ALU.add,
                       [pk, 'modrow'], ['modrow'])
                for (o_, ng) in ((1, norm1_g), (4, norm2_g)):
                    S.dma(rowtmp[:], ng[l].partition_broadcast(128), ['rowtmp'], ['rowtmp'])
                    stt(modrow[:, o_ * D:(o_ + 1) * D], modrow[:, o_ * D:(o_ + 1) * D], 1.0, rowtmp[:], ALU.add, ALU.mult,
                        ['modrow', 'rowtmp'], ['modrow'])
                S.dma(mod_d[l:l + 1, :], modrow[0:1, :], ['modrow'], [('mod', l)], q='pool')
            S.barrier()

        for l in range(L if 'stop0' not in dbg else 0):
            src_x = x_in if l == 0 else out
            with ExitStack() as phm:
                A1t = phm.enter_context(nc.sbuf_tensor("A1t_%d" % l, [128, D], F32))
                sh1t = phm.enter_context(nc.sbuf_tensor("sh1t_%d" % l, [128, D], F32))
                S.dma(sh1t[:], mod_d[l, 0:D].partition_broadcast(128), (), ['modrow'])
                S.dma(A1t[:], mod_d[l, D:2 * D].partition_broadcast(128), (), ['modrow'])
                A1row, sh1row = A1t[:], sh1t[:]
                phase_A(l, src_x, A1row, sh1row)
            if 'stopA' in dbg:
                break
            phase_B(l)
            if 'stopB' in dbg:
                break
            phase_C(l)
            if 'stopC' in dbg:
                break
            phase_D(l)
            if 'stopD' in dbg:
                break
            phase_E(l, src_x)
            if 'stopE' in dbg:
                break
            phase_F(l)
        S.barrier()
    return nc


def _host_tables(rel_bias, SEQ):
    FDW = NEGPAD + SEQ
    d = np.arange(SEQ)
    bk = rel_bucket_np(d)
    g = np.asarray(rel_bias, np.float32)[bk].T
    fdg = np.full((NH, FDW), NEGM, np.float32)
    fdg[:, NEGPAD:] = g
    fdw = np.full((NH, FDW), NEGM, np.float32)
    fdw[:, NEGPAD:NEGPAD + 512] = g[:, :512]
    return fdg, fdw


_CACHE = {}


def kernel(**inputs):
    x = np.asarray(inputs["x"], np.float32)
    B, SEQ, _ = x.shape
    L = int(np.asarray(inputs["ada_w"]).shape[0])
    key = (SEQ, L)
    if key not in _CACHE:
        _CACHE[key] = build_program(SEQ, L)
    nc = _CACHE[key]
    fdg, fdw = _host_tables(inputs["rel_bias"], SEQ)
    names = ["router_w", "router_b", "ada_w", "ada_b", "norm1_g", "norm2_g", "w_in", "qk_norm_g", "cmp_pos", "cmp_w1",
             "cmp_w2", "dn_conv_w", "dn_a_log", "dn_dt_bias", "dn_norm_g", "w_branch_a", "w_branch_b", "w_out",
             "moe_w_gate", "moe_w_up", "moe_w_down"]
    shared = {n: np.ascontiguousarray(np.asarray(inputs[n], np.float32)) for n in names}
    shared["fdg"] = fdg
    shared["fdw"] = fdw
    c = np.asarray(inputs["c"], np.float32)
    in_maps = []
    for b in range(B):
        m = dict(shared)
        m["x"] = np.ascontiguousarray(x[b])
        m["cT"] = np.ascontiguousarray(c[b].reshape(8, 128).T)
        in_maps.append(m)
    res = run_bass_kernel_spmd(nc, in_maps, core_ids=list(range(B)))
    return np.stack([np.asarray(r["out"], np.float32) for r in res.results], axis=0)
```

```python
import math
from contextlib import ExitStack
import numpy as np
import concourse.bass as bass
import concourse.mybir as mybir
from concourse.bass_utils import run_bass_kernel_spmd

F32 = mybir.dt.float32
I32 = mybir.dt.int32
AF = mybir.ActivationFunctionType
ALU = mybir.AluOpType
AX = mybir.AxisListType

D = 1024
HD = 64
NH = 8
DIN = 5416
NEGM = -30000.0
EPS = 1e-6
U0 = 384
OFFMAX = 1024
WGEN = U0 + OFFMAX + 512
WWIN = U0 + 512 + 512
NEGPAD = 1024


class Sched:
    def __init__(self, nc, es, ndma=14):
        self.nc = nc
        self.eng = {'pe': nc.tensor, 'act': nc.scalar, 'dve': nc.vector, 'pool': nc.gpsimd, 'sp': nc.sync}
        self.sem = {k: es.enter_context(nc.semaphore('s_' + k)) for k in self.eng}
        self.cnt = {k: 0 for k in self.eng}
        self.dsem = [es.enter_context(nc.semaphore('d%d' % i)) for i in range(ndma)]
        self.dcnt = [0] * ndma
        self.dnext = 0
        self.seen = {k: {} for k in self.eng}
        self.res = {}
        self.nops = 0

    def _deps(self, r, w):
        deps = {}

        def add(t):
            if t is not None and deps.get(t[0], 0) < t[1]:
                deps[t[0]] = t[1]
        for k in r:
            st = self.res.get(k)
            if st:
                add(st[0])
        for k in w:
            st = self.res.get(k)
            if st:
                add(st[0])
                for s, v in st[1].items():
                    add((s, v))
        return deps

    def _wait(self, e, deps):
        for s, v in deps.items():
            if s == 'pe' and e == 'pe':
                continue
            if self.seen[e].get(s, 0) < v:
                sem = self.sem[s] if isinstance(s, str) else self.dsem[s]
                self.eng[e].wait_ge(sem, v)
                self.seen[e][s] = v

    def _mark(self, tag, r, w):
        for k in r:
            st = self.res.setdefault(k, [None, {}])
            if st[1].get(tag[0], 0) < tag[1]:
                st[1][tag[0]] = tag[1]
        for k in w:
            self.res[k] = [tag, {}]

    def op(self, e, emit, r=(), w=(), inc=True):
        self._wait(e, self._deps(r, w))
        inst = emit(self.eng[e])
        self.nops += 1
        if inc:
            self.cnt[e] += 1
            inst.then_inc(self.sem[e], 1)
            tag = (e, self.cnt[e])
        else:
            tag = (e, self.cnt[e] + 1)
        self._mark(tag, r, w)

    def dma(self, out, in_, r=(), w=(), q='sp'):
        i = self.dnext
        self.dnext = (i + 1) % len(self.dsem)
        deps = self._deps(r, w)
        if self.dcnt[i]:
            deps[i] = max(deps.get(i, 0), self.dcnt[i])
        self._wait(q, deps)
        self.dcnt[i] += 16
        self.eng[q].dma_start(out=out, in_=in_).then_inc(self.dsem[i], 16)
        self.nops += 1
        self._mark((i, self.dcnt[i]), r, w)

    def idma(self, out, in_, out_off, in_off, r=(), w=()):
        i = self.dnext
        self.dnext = (i + 1) % len(self.dsem)
        deps = self._deps(r, w)
        if self.dcnt[i]:
            deps[i] = max(deps.get(i, 0), self.dcnt[i])
        self._wait('pool', deps)
        self.dcnt[i] += 16
        self.eng['pool'].indirect_dma_start(out=out, out_offset=out_off, in_=in_, in_offset=in_off).then_inc(self.dsem[i], 16)
        self.nops += 1
        self._mark((i, self.dcnt[i]), r, w)

    def barrier(self):
        deps = {s: c for s, c in self.cnt.items() if c}
        for i, v in enumerate(self.dcnt):
            if v:
                deps[i] = v
        for e in self.eng:
            d = dict(deps)
            self._wait(e, d)
        self.res = {}


class Ring:
    def __init__(self, tiles, name):
        self.tiles = tiles
        self.name = name
        self.i = 0

    def next(self):
        k = self.i % len(self.tiles)
        self.i += 1
        return self.tiles[k], (self.name, k)


def rel_bucket_np(dist):
    exact = 16
    dist = np.maximum(dist, 0)
    far = np.maximum(dist, exact).astype(np.float32)
    large = exact + (np.log(far / np.float32(exact)) / np.float32(math.log(1024 / exact)) * np.float32(32 - exact)).astype(np.int32)
    return np.where(dist < exact, dist, np.minimum(large, 31))


def build_program(SEQ, DEPTH, dbg=()):
    NT = SEQ // 128
    QT = SEQ // 512
    NCH = SEQ // 64
    NCMP = SEQ // 16 - 1
    NBLK = SEQ // 64
    NCT = (NCMP + 127) // 128
    FDW = NEGPAD + SEQ
    JB = NBLK
    nc = bass.Bass("TRN2", target_bir_lowering=False)

    def din(name, shape):
        return nc.dram_tensor(name, list(shape), F32, kind="ExternalInput").ap()

    def dscr(name, shape, kind="Internal"):
        if name in dbg:
            kind = "ExternalOutput"
        return nc.dram_tensor(name, list(shape), F32, kind=kind).ap()

    L = DEPTH
    x_in = din("x", (SEQ, D))
    cT_in = din("cT", (128, 8))
    fdg_in = din("fdg", (NH, FDW))
    fdw_in = din("fdw", (NH, FDW))
    router_w = din("router_w", (D, 16))
    router_b = din("router_b", (16,))
    ada_w = din("ada_w", (L, D, 6 * D))
    ada_b = din("ada_b", (L, 6 * D))
    norm1_g = din("norm1_g", (L, D))
    norm2_g = din("norm2_g", (L, D))
    w_in = din("w_in", (L, D, DIN))
    qk_norm_g = din("qk_norm_g", (L, 4, HD))
    cmp_pos = din("cmp_pos", (L, 2, 32, HD))
    cmp_w1 = din("cmp_w1", (L, 2, 2048, 256))
    cmp_w2 = din("cmp_w2", (L, 2, 256, HD))
    dn_conv_w = din("dn_conv_w", (L, 4, 1536))
    dn_a_log = din("dn_a_log", (L, 8))
    dn_dt_bias = din("dn_dt_bias", (L, 8))
    dn_norm_g = din("dn_norm_g", (L, HD))
    w_br_a = din("w_branch_a", (L, 512, D))
    w_br_b = din("w_branch_b", (L, 512, D))
    w_out = din("w_out", (L, D, D))
    moe_wg = din("moe_w_gate", (L, 16, D, 512))
    moe_wu = din("moe_w_up", (L, 16, D, 512))
    moe_wd = din("moe_w_down", (L, 16, 512, D))
    out = nc.dram_tensor("out", [SEQ, D], F32, kind="ExternalOutput").ap()

    qT_d = dscr("qT_d", (4, 128, SEQ))
    kcT_d = dscr("kcT_d", (2, 128, SEQ))
    kslcT_d = dscr("kslcT_d", (128, SEQ))
    kwinT_d = dscr("kwinT_d", (128, SEQ))
    vslc_d = dscr("vslc_d", (SEQ, 128))
    vwin_d = dscr("vwin_d", (SEQ, 128))
    gate_d = dscr("gate_d", (SEQ, 24))
    dnraw_d = dscr("dnraw_d", (12, 128, SEQ))
    dnc_d = dscr("dnc_d", (12, 128, SEQ))
    bg_d = dscr("bg_d", (SEQ, 16))
    zs_d = dscr("zs_d", (SEQ, 512))
    mergeT_d = dscr("mergeT_d", (16, 128, SEQ))
    obr_d = dscr("obr_d", (3, SEQ, 512))
    ybT_d = dscr("ybT_d", (4, 128, SEQ))
    bct_d = dscr("bct_d", (NH, NCT * 128, SEQ))
    bgen_d = dscr("bgen_d", (128, NH, WGEN))
    bwin_d = dscr("bwin_d", (128, NH, WWIN))
    selbT_d = dscr("selbT_d", (128, SEQ))
    MBLK = 256
    NBK = (2 * SEQ + 16 * MBLK) // MBLK
    h2_d = dscr("h2_d", (SEQ, D))
    Xs_d = dscr("Xs_d", (NBK * MBLK, D))
    Ys_d = dscr("Ys_d", (NBK * MBLK, D))
    mod_d = dscr("mod_d", (L, 6 * D))

    es = ExitStack()
    with es:
        S = Sched(nc, es)

        def sb(name, shape):
            return es.enter_context(nc.sbuf_tensor(name, list(shape), F32))

        PS = [es.enter_context(nc.psum_tensor("ps%d" % i, [128, 512], F32)) for i in range(8)]
        psr = Ring(PS, "ps")

        def tt(e, o, a, b, op, r, w):
            S.op(e, lambda g: g.tensor_tensor(out=o, in0=a, in1=b, op=op), r, w)

        def ts(e, o, a, s1, op0, r, w, s2=None, op1=None):
            if op1 is None:
                S.op(e, lambda g: g.tensor_scalar(out=o, in0=a, scalar1=s1, scalar2=None, op0=op0), r, w)
            else:
                S.op(e, lambda g: g.tensor_scalar(out=o, in0=a, scalar1=s1, scalar2=s2, op0=op0, op1=op1), r, w)

        def stt(o, a, sc, b, op0, op1, r, w):
            S.op('dve', lambda g: g.scalar_tensor_tensor(out=o, in0=a, scalar=sc, in1=b, op0=op0, op1=op1), r, w)

        def act(o, a, f, r, w, bias=None, scale=1.0, accum=None):
            kw = {}
            if bias is not None:
                kw['bias'] = bias
            if accum is not None:
                kw['accum_out'] = accum
            S.op('act', lambda g: g.activation(out=o, in_=a, func=f, scale=scale, **kw), r, w)

        def mm(o, lT, rh, st, sp, r, w, inc=True):
            S.op('pe', lambda g: g.matmul(o, lT, rh, start=st, stop=sp), r, w, inc=inc)

        def tr(o, a, idn, r, w, inc=True):
            S.op('pe', lambda g: g.transpose(o, a, idn), r, w, inc=inc)

        def cp(e, o, a, r, w):
            S.op(e, lambda g: g.tensor_copy(o, a), r, w)

        def recip(o, a, r, w):
            S.op('dve', lambda g: g.reciprocal(o, a), r, w)

        def memset(e, o, v, w):
            S.op(e, lambda g: g.memset(o, v), (), w)

        def asel(o, a, pattern, base, cm, fill, r, w, op=ALU.is_ge):
            S.op('pool', lambda g: g.affine_select(out=o, in_=a, pattern=pattern, compare_op=op, fill=fill,
                                                   base=base, channel_multiplier=cm), r, w)

        ident = sb("ident", (128, 128))
        ones = sb("ones", (128, 128))
        bdones = sb("bdones", (128, 128))
        UT = sb("UT", (128, 64))
        maskU = sb("maskU", (64, 64))
        maskL = sb("maskL", (64, 64))
        nsU = sb("nsU", (64, 64))
        nsL = sb("nsL", (64, 64))
        ovl = sb("ovl", (128, NCT, JB))
        memset('pool', ident[:], 0.0, ['ident'])
        asel(ident[:], ident[:], [[-1, 128]], 0, 1, 1.0, ['ident'], ['ident'], op=ALU.not_equal)
        memset('pool', ones[:], 1.0, ['ones'])
        memset('pool', bdones[:], 0.0, ['bdones'])
        memset('pool', bdones[0:64, 0:64], 1.0, ['bdones'])
        memset('pool', bdones[64:128, 64:128], 1.0, ['bdones'])
        for h0 in (0, 64):
            memset('pool', UT[h0:h0 + 64, :], 1.0, ['UT'])
            asel(UT[h0:h0 + 64, :], UT[h0:h0 + 64, :], [[1, 64]], 0, -1, 0.0, ['UT'], ['UT'])
        memset('pool', maskU[:], 0.0, ['maskU'])
        asel(maskU[:], maskU[:], [[1, 64]], 0, -1, NEGM, ['maskU'], ['maskU'])
        memset('pool', maskL[:], 0.0, ['maskL'])
        asel(maskL[:], maskL[:], [[-1, 64]], 0, 1, NEGM, ['maskL'], ['maskL'])
        memset('pool', nsU[:], -1.0, ['nsU'])
        asel(nsU[:], nsU[:], [[1, 64]], -1, -1, 0.0, ['nsU'], ['nsU'])
        memset('pool', nsL[:], -1.0, ['nsL'])
        asel(nsL[:], nsL[:], [[-1, 64]], -1, 1, 0.0, ['nsL'], ['nsL'])
        ovt = sb("ovt", (128, NCT, JB))
        memset('pool', ovl[:], 0.0, ['ovl'])
        for m in (0, 1):
            memset('pool', ovt[:], 1.0, ['ovt'])
            asel(ovt[:], ovt[:], [[128, NCT], [-4, JB]], m, 1, 0.0, ['ovt'], ['ovt'])
            asel(ovt[:], ovt[:], [[-128, NCT], [4, JB]], 3 - m, -1, 0.0, ['ovt'], ['ovt'])
            tt('pool', ovl[:], ovl[:], ovt[:], ALU.add, ['ovl', 'ovt'], ['ovl'])

        with nc.allow_non_contiguous_dma(reason="table build"):
            for p in range(128):
                o0 = NEGPAD - U0 - p
                S.dma(bgen_d[p, :, :], fdg_in[:, o0:o0 + WGEN], (), [('bgen', p)], q='sp')
                S.dma(bwin_d[p, :, :], fdw_in[:, o0:o0 + WWIN], (), [('bwin', p)], q='pool')
            for n in range(NCT * 128):
                o0 = NEGPAD - (16 * n + 31)
                if n >= NCMP:
                    o0 = 0
                q = 'sp' if n % 2 == 0 else 'pool'
                if n >= NCMP:
                    S.dma(bct_d[:, n, 0:NEGPAD], fdg_in[:, 0:NEGPAD], (), [('bct', n)], q=q)
                    for c0 in range(NEGPAD, SEQ, NEGPAD):
                        S.dma(bct_d[:, n, c0:c0 + NEGPAD], fdg_in[:, 0:NEGPAD], (), [('bct', n, c0)], q=q)
                elif o0 >= 0:
                    S.dma(bct_d[:, n, :], fdg_in[:, o0:o0 + SEQ], (), [('bct', n)], q=q)
                else:
                    nn = -o0
                    for c0 in range(0, nn, NEGPAD):
                        cw = min(NEGPAD, nn - c0)
                        S.dma(bct_d[:, n, c0:c0 + cw], fdg_in[:, 0:cw], (), [('bct', n, c0)], q=q)
                    S.dma(bct_d[:, n, nn:SEQ], fdg_in[:, 0:SEQ - nn], (), [('bct', n)], q=q)
        S.barrier()

        cact = sb("cact", (128, 8))
        S.dma(cact[:], cT_in[:, :], (), ['cact'])
        act(cact[:], cact[:], AF.Silu, ['cact'], ['cact'])
        cbc = sb("cbc", (128, 8, 128))
        for kc in range(8):
            ts('dve', cbc[:, kc, :], ones[:], cact[:, kc:kc + 1], ALU.mult, ['ones', 'cact'], ['cbc'])
        rtrb = sb("rtrb", (128, 16))
        S.dma(rtrb[:], router_b.partition_broadcast(128), (), ['rtrb'])
        rtrw = sb("rtrw", (128, 8, 16))
        with nc.allow_non_contiguous_dma(reason="router w"):
            S.dma(rtrw[:], router_w.rearrange("(kc p) e -> p kc e", p=128), (), ['rtrw'])

        def load_w(ring, src2d, c0, ncols, kch=8):
            t, k = ring.next()
            with nc.allow_non_contiguous_dma(reason="weight slab"):
                S.dma(t[:, 0:kch, 0:ncols], src2d.rearrange("(kc p) n -> p kc n", p=128)[:, :, c0:c0 + ncols], (), [k])
            return t, k

        def norm_tile(src, t0, Arow, shrow, xr, hr, sm):
            xt, xk = xr.next()
            S.dma(xt[:], src[t0:t0 + 128, :], [('x', t0 // 128)], [xk])
            ht, hk = hr.next()
            s, sk = sm.next()
            act(ht[:], xt[:], AF.Square, [xk], [hk, sk], accum=s[:, 0:1])
            act(s[:, 1:2], s[:, 0:1], AF.Sqrt, [sk], [sk], bias=EPS, scale=1.0 / D)
            recip(s[:, 2:3], s[:, 1:2], [sk], [sk])
            stt(ht[:], xt[:], s[:, 2:3], Arow, ALU.mult, ALU.mult, [xk, sk, 'modrow'], [hk])
            tt('pool', ht[:], ht[:], shrow, ALU.add, [hk, 'modrow'], [hk])
            return ht, hk, xt, xk

        def transpose_to(ht, hk, hT, hTk, sub, nkc=8):
            for k0 in range(0, nkc, 4):
                ps, pk = psr.next()
                for kc in range(k0, k0 + 4):
                    tr(ps[:, (kc - k0) * 128:(kc - k0 + 1) * 128], ht[:, kc * 128:(kc + 1) * 128], ident[:],
                       [hk, 'ident'], [pk], inc=(kc == k0 + 3))
                e = 'act' if (k0 // 4) % 2 == 0 else 'dve'
                dst = hT[:, k0:k0 + 4, sub * 128:(sub + 1) * 128]
                srcv = ps[:].rearrange("p (a b) -> p a b", a=4)
                if e == 'act':
                    act(dst, srcv, AF.Copy, [pk], [hTk])
                else:
                    cp('dve', dst, srcv, [pk], [hTk])

        def phase_A(l, src_x, A1row, sh1row):
            with ExitStack() as ph:
                def psb(name, shape):
                    return ph.enter_context(nc.sbuf_tensor("%s_A%d" % (name, l), list(shape), F32))
                wr = Ring([psb("wsl%d" % i, (128, 8, 512)) for i in range(3)], "wsl")
                xr = Ring([psb("xa%d" % i, (128, D)) for i in range(2)], "xa")
                hr = Ring([psb("ha%d" % i, (128, D)) for i in range(2)], "ha")
                hTr = Ring([psb("hT%d" % i, (128, 8, 512)) for i in range(2)], "hT")
                st = Ring([psb("st%d" % i, (128, 512)) for i in range(4)], "st")
                sq = Ring([psb("sq%d" % i, (128, 512)) for i in range(2)], "sq")
                sm = Ring([psb("sm%d" % i, (128, 8)) for i in range(4)], "sm")
                gains = psb("gains", (128, 4))
                dtb = psb("dtb", (128, 8))
                nea = psb("nea", (128, 8))
                with nc.allow_non_contiguous_dma(reason="small"):
                    for h0 in (0, 64):
                        S.dma(gains[h0:h0 + 64, :], qk_norm_g[l].rearrange("i d -> d i"), (), ['gains'])
                ts('dve', gains[:, 0:1], gains[:, 0:1], HD ** -0.5, ALU.mult, ['gains'], ['gains'])
                S.dma(dtb[:], dn_dt_bias[l].partition_broadcast(128), (), ['dtb'])
                S.dma(nea[:], dn_a_log[l].partition_broadcast(128), (), ['nea'])
                act(nea[:], nea[:], AF.Exp, ['nea'], ['nea'])
                ts('dve', nea[:], nea[:], -1.0, ALU.mult, ['nea'], ['nea'])

                def rms64_store(ps, pk, gcol, dst, dkey):
                    q1, qk1 = sq.next()
                    act(q1[:], ps[:], AF.Square, [pk], [qk1])
                    p2, pk2 = psr.next()
                    mm(p2[:], bdones[:], q1[:], True, True, ['bdones', qk1], [pk2])
                    act(q1[:], p2[:], AF.Sqrt, [pk2], [qk1], bias=EPS, scale=1.0 / 64)
                    recip(q1[:], q1[:], [qk1], [qk1])
                    o, ok = st.next()
                    stt(o[:], ps[:], gcol, q1[:], ALU.mult, ALU.mult, [pk, qk1, 'gains'], [ok])
                    S.dma(dst, o[:], [ok], [dkey], q='pool')

                def fm_store(ps, pk, dst, dkey, func=AF.Copy):
                    o, ok = st.next()
                    act(o[:], ps[:], func, [pk], [ok])
                    S.dma(dst, o[:], [ok], [dkey], q='pool')

                for j in range(QT):
                    t0 = 512 * j
                    hT, hTk = hTr.next()
                    for sub in range(4):
                        ht, hk, _, _ = norm_tile(src_x, t0 + 128 * sub, A1row, sh1row, xr, hr, sm)
                        transpose_to(ht, hk, hT, hTk, sub)

                    def fm(wt, wk, lsel):
                        ps, pk = psr.next()
                        for kc in range(8):
                            mm(ps[:], lsel(kc), hT[:, kc, :], kc == 0, kc == 7, [wk, hTk], [pk], inc=(kc == 7))
                        return ps, pk

                    def tm(wt, wk, c0, ncols, sub):
                        ps, pk = psr.next()
                        for kc in range(8):
                            mm(ps[:, 0:ncols], hT[:, kc, sub * 128:(sub + 1) * 128], wt[:, kc, c0:c0 + ncols],
                               kc == 0, kc == 7, [wk, hTk], [pk], inc=(kc == 7))
                        return ps, pk
                    cs = slice(t0, t0 + 512)
                    wt, wk = wr.next()
                    with nc.allow_non_contiguous_dma(reason="q slab"):
                        for a in range(2):
                            for c in range(4):
                                S.dma(wt[:, :, c * 128 + a * 64:c * 128 + a * 64 + 64],
                                      w_in[l].rearrange("(kc p) n -> p kc n", p=128)[:, :, a * 256 + c * 64:a * 256 + c * 64 + 64], (), [wk])
                    for c in range(4):
                        ps, pk = fm(wt, wk, lambda kc, c=c: wt[:, kc, c * 128:(c + 1) * 128])
                        rms64_store(ps, pk, gains[:, 0:1], qT_d[c, :, cs], ('qT', c, j))
                    wt, wk = load_w(wr, w_in[l], 512, 512)
                    for c in range(3):
                        ps, pk = fm(wt, wk, lambda kc, c=c: wt[:, kc, c * 128:(c + 1) * 128])
                        if c < 2:
                            fm_store(ps, pk, kcT_d[c, :, cs], ('kcT', c, j))
                        else:
                            rms64_store(ps, pk, gains[:, 2:3], kslcT_d[:, cs], ('kslcT', j))
                    for sub in range(4):
                        ps, pk = tm(wt, wk, 384, 128, sub)
                        o, ok = st.next()
                        act(o[:, 0:128], ps[:, 0:128], AF.Copy, [pk], [ok])
                        S.dma(vslc_d[t0 + 128 * sub:t0 + 128 * sub + 128, :], o[:, 0:128], [ok], [('vslc', j, sub)], q='pool')
                    wt, wk = load_w(wr, w_in[l], 1024, 280)
                    ps, pk = fm(wt, wk, lambda kc: wt[:, kc, 0:128])
                    rms64_store(ps, pk, gains[:, 3:4], kwinT_d[:, cs], ('kwinT', j))
                    for sub in range(4):
                        r0 = t0 + 128 * sub
                        ps, pk = tm(wt, wk, 128, 152, sub)
                        o, ok = st.next()
                        act(o[:, 0:128], ps[:, 0:128], AF.Copy, [pk], [ok])
                        act(o[:, 128:152], ps[:, 128:152], AF.Sigmoid, [pk], [ok])
                        S.dma(vwin_d[r0:r0 + 128, :], o[:, 0:128], [ok], [('vwin', j, sub)], q='pool')
                        S.dma(gate_d[r0:r0 + 128, :], o[:, 128:152], [ok], [('gate', j, sub)], q='pool')
                    for i in range(3):
                        wt, wk = load_w(wr, w_in[l], 1304 + 512 * i, 512)
                        for c in range(4):
                            ps, pk = fm(wt, wk, lambda kc, c=c: wt[:, kc, c * 128:(c + 1) * 128])
                            fm_store(ps, pk, dnraw_d[4 * i + c, :, cs], ('dnraw', 4 * i + c, j))
                    wt, wk = load_w(wr, w_in[l], 2840, 16)
                    for sub in range(4):
                        r0 = t0 + 128 * sub
                        ps, pk = tm(wt, wk, 0, 16, sub)
                        o, ok = st.next()
                        act(o[:, 0:8], ps[:, 0:8], AF.Sigmoid, [pk], [ok])
                        tt('dve', o[:, 8:16], ps[:, 8:16], dtb[:], ALU.add, [pk, 'dtb'], [ok])
                        act(o[:, 8:16], o[:, 8:16], AF.Exp, [ok], [ok])
                        act(o[:, 8:16], o[:, 8:16], AF.Ln, [ok], [ok], bias=1.0)
                        tt('dve', o[:, 8:16], o[:, 8:16], nea[:], ALU.mult, [ok, 'nea'], [ok])
                        S.dma(bg_d[r0:r0 + 128, :], o[:, 0:16], [ok], [('bg', j, sub)], q='pool')
                    wt, wk = load_w(wr, w_in[l], 2856, 512)
                    for sub in range(4):
                        r0 = t0 + 128 * sub
                        ps, pk = tm(wt, wk, 0, 512, sub)
                        fm_store(ps, pk, zs_d[r0:r0 + 128, :], ('zs', j, sub), func=AF.Silu)
                    for i in range(4):
                        wt, wk = load_w(wr, w_in[l], 3368 + 512 * i, 512)
                        for c in range(4):
                            ps, pk = fm(wt, wk, lambda kc, c=c: wt[:, kc, c * 128:(c + 1) * 128])
                            fm_store(ps, pk, mergeT_d[4 * i + c, :, cs], ('mergeT', 4 * i + c, j), func=AF.Sigmoid)
                S.barrier()
        def phase_B(l):
            with ExitStack() as ph:
                def psb(name, shape):
                    return ph.enter_context(nc.sbuf_tensor("%s_B%d" % (name, l), list(shape), F32))
                kst = [psb("kst%d" % g, (128, NCT * 128)) for g in range(2)]
                for g in range(2):
                    memset('pool', kst[g][:], 0.0, ['kcmpT'])
                vcmp = psb("vcmp", (128, NCT, 2, 65 + JB))
                memset('pool', vcmp[:], 0.0, ['vcmp'])
                memset('pool', vcmp[:, :, :, 64:65], 1.0, ['vcmp'])
                for g in range(2):
                    cp('pool', vcmp[:, :, g, 65:65 + JB], ovl[:], ['ovl', 'vcmp'], ['vcmp'])
                gains = psb("gains", (128, 4))
                with nc.allow_non_contiguous_dma(reason="small"):
                    for h0 in (0, 64):
                        S.dma(gains[h0:h0 + 64, :], qk_norm_g[l].rearrange("i d -> d i"), (), ['gains'])
                with ExitStack() as ph2:
                    def psb2(name, shape):
                        return ph2.enter_context(nc.sbuf_tensor("%s_B2%d" % (name, l), list(shape), F32))
                    kcT = psb2("kcT", (128, SEQ))
                    w1 = psb2("w1", (128, 32, 256))
                    posT = psb2("posT", (128, 32))
                    w2 = psb2("w2", (128, 2, 64))
                    pbias = psb2("pbias", (128, 2))
                    gx = psb2("gx", (128, 2, 2, 256))
                    gt = psb2("gt", (128, 256))
                    sqc = psb2("sqc", (128, 256))
                    for kvi in range(2):
                        S.dma(kcT[:], kcT_d[kvi, :, :], [('kcT', kvi, j) for j in range(QT)], ['kcT'])
                        with nc.allow_non_contiguous_dma(reason="cmp weights"):
                            for h0 in (0, 64):
                                for j0 in range(0, 32, 8):
                                    S.dma(w1[h0:h0 + 64, j0:j0 + 8, :], cmp_w1[l, kvi].rearrange("(j d) f -> d j f", d=64)[:, j0:j0 + 8, :], (), ['w1'])
                                for j0 in range(0, 32, 8):
                                    S.dma(posT[h0:h0 + 64, j0:j0 + 8], cmp_pos[l, kvi].rearrange("j d -> d j")[:, j0:j0 + 8], (), ['posT'])
                            S.dma(w2[:], cmp_w2[l, kvi].rearrange("(fc p) d -> p fc d", p=128), (), ['w2'])
                        for fc in range(2):
                            ps, pk = psr.next()
                            for j in range(32):
                                mm(ps[:, 0:1], w1[0:64, j, fc * 128:(fc + 1) * 128], posT[0:64, j:j + 1], j == 0, j == 31,
                                   ['w1', 'posT'], [pk], inc=(j == 31))
                            cp('dve', pbias[:, fc:fc + 1], ps[:, 0:1], [pk], ['pbias'])
                        for g in range(2):
                            hs = slice(64 * g, 64 * g + 64)
                            for fc in range(2):
                                ps, pk = psr.next()
                                for j in range(32):
                                    mm(ps[:, 0:NCMP], w1[hs, j, fc * 128:(fc + 1) * 128], kcT[hs, j:j + 16 * (NCMP - 1) + 1:16],
                                       j == 0, j == 31, ['w1', 'kcT'], [pk], inc=(j == 31))
                                xs = gx[:, g, fc, 0:NCMP]
                                ts('dve', xs, ps[:, 0:NCMP], pbias[:, fc:fc + 1], ALU.add, [pk, 'pbias'], ['gx'])
                                tt('dve', gt[:, 0:NCMP], xs, xs, ALU.mult, ['gx'], ['gt'])
                                ts('dve', gt[:, 0:NCMP], gt[:, 0:NCMP], 0.044715, ALU.mult, ['gt'], ['gt'], s2=1.0, op1=ALU.add)
                                tt('dve', gt[:, 0:NCMP], gt[:, 0:NCMP], xs, ALU.mult, ['gt', 'gx'], ['gt'])
                                act(gt[:, 0:NCMP], gt[:, 0:NCMP], AF.Sigmoid, ['gt'], ['gt'], scale=1.5957691216057308)
                                tt('dve', xs, xs, gt[:, 0:NCMP], ALU.mult, ['gx', 'gt'], ['gx'])
                            if kvi == 0:
                                ps, pk = psr.next()
                                for fc in range(2):
                                    mm(ps[hs, 0:NCMP], w2[:, fc, :], gx[:, g, fc, 0:NCMP], fc == 0, fc == 1, ['w2', 'gx'], [pk], inc=(fc == 1))
                                act(sqc[hs, 0:NCMP], ps[hs, 0:NCMP], AF.Square, [pk], ['sqc'])
                                p2, pk2 = psr.next()
                                mm(p2[hs, 0:NCMP], ones[hs, 0:64], sqc[hs, 0:NCMP], True, True, ['ones', 'sqc'], [pk2])
                                act(sqc[hs, 0:NCMP], p2[hs, 0:NCMP], AF.Sqrt, [pk2], ['sqc'], bias=EPS, scale=1.0 / 64)
                                recip(sqc[hs, 0:NCMP], sqc[hs, 0:NCMP], ['sqc'], ['sqc'])
                                stt(kst[g][hs, 0:NCMP], ps[hs, 0:NCMP], gains[hs, 1:2], sqc[hs, 0:NCMP], ALU.mult, ALU.mult,
                                    [pk, 'sqc', 'gains'], ['kcmpT'])
                            else:
                                for nt in range(NCT):
                                    nn = min(128, NCMP - nt * 128)
                                    ps, pk = psr.next()
                                    for fc in range(2):
                                        mm(ps[0:nn, 0:64], gx[:, g, fc, nt * 128:nt * 128 + nn], w2[:, fc, :], fc == 0, fc == 1,
                                           ['w2', 'gx'], [pk], inc=(fc == 1))
                                    cp('dve', vcmp[0:nn, nt, g, 0:64], ps[0:nn, 0:64], [pk], ['vcmp'])
                    S.barrier()
                qr = Ring([psb("qTb%d" % i, (128, SEQ)) for i in range(2)], "qTb")
                btr = Ring([psb("bt%d" % i, (128, NCT, 512)) for i in range(3)], "bt")
                pcr = Ring([psb("pc%d" % i, (128, NCT, 512)) for i in range(3)], "pc")
                osr = Ring([psb("os%d" % i, (128, 64)) for i in range(4)], "os")
                rdr = Ring([psb("rd%d" % i, (128, 2)) for i in range(4)], "rd")
                gate_sb = psb("gate_sb", (128, NT, 24))
                impacc = psb("impacc", (128, NT, 2, JB))
                with nc.allow_non_contiguous_dma(reason="gate"):
                    for t0_ in range(0, NT, 8):
                        S.dma(gate_sb[:, t0_:t0_ + 8, :], gate_d.rearrange("(t p) c -> p t c", p=128)[:, t0_:t0_ + 8, :],
                              [('gate', j, s) for j in range(QT) for s in range(4)], ['gate_sb'])
                W = 65 + JB
                qcur = {}

                def stage1(c, half, jq):
                    if c not in qcur:
                        qT, qk = qr.next()
                        S.dma(qT[:], qT_d[c, :, :], [('qT', c, j) for j in range(QT)], [qk])
                        qcur.clear()
                        qcur[c] = (qT, qk)
                    qT, qk = qcur[c]
                    h = c + 4 * half
                    g = half
                    tq0 = 512 * jq
                    nts = [nt for nt in range(NCT) if 16 * 128 * nt + 31 <= tq0 + 511]
                    bt, bk = btr.next()
                    pc, pck = pcr.next()
                    for nt in nts:
                        S.dma(bt[:, nt, :], bct_d[h, nt * 128:(nt + 1) * 128, tq0:tq0 + 512], (), [bk])
                    for nt in nts:
                        ps, pk = psr.next()
                        mm(ps[:], kst[g][:, nt * 128:(nt + 1) * 128], qT[:, tq0:tq0 + 512], True, True, ['kcmpT', qk], [pk])
                        tt('dve', pc[:, nt, :], ps[:], bt[:, nt, :], ALU.add, [pk, bk], [pck])
                        act(pc[:, nt, :], pc[:, nt, :], AF.Exp, [pck], [pck])
                    return (c, h, g, jq, nts, pc, pck)

                def stage2(item):
                    c, h, g, jq, nts, pc, pck = item
                    for sub in range(4):
                        tsi = 4 * jq + sub
                        po, pok = psr.next()
                        for nt in nts:
                            mm(po[:, 0:W], pc[:, nt, sub * 128:(sub + 1) * 128], vcmp[:, nt, g, :], nt == nts[0], nt == nts[-1],
                               [pck, 'vcmp'], [pok], inc=(nt == nts[-1]))
                        rd, rk = rdr.next()
                        ts('dve', rd[:, 0:1], po[:, 64:65], 1e-30, ALU.add, [pok], [rk])
                        recip(rd[:, 1:2], rd[:, 0:1], [rk], [rk])
                        o, ok = osr.next()
                        ts('dve', o[:], po[:, 0:64], rd[:, 1:2], ALU.mult, [pok, rk, 'gate_sb'], [ok],
                           s2=gate_sb[:, tsi, 3 * h:3 * h + 1], op1=ALU.mult)
                        S.dma(obr_d[0, tsi * 128:(tsi + 1) * 128, 64 * h:64 * h + 64], o[:], [ok], [('obr', 0, h, tsi)], q='pool')
                        if c == 0:
                            ts('dve', impacc[:, tsi, g, :], po[:, 65:W], rd[:, 1:2], ALU.mult, [pok, rk], [('imp', tsi, g)])
                        else:
                            stt(impacc[:, tsi, g, :], po[:, 65:W], rd[:, 1:2], impacc[:, tsi, g, :], ALU.mult, ALU.add,
                                [pok, rk, ('imp', tsi, g)], [('imp', tsi, g)])

                prev_it = None
                for c in range(4):
                    for half in range(2):
                        for jq in range(QT):
                            it_ = stage1(c, half, jq)
                            if prev_it is not None:
                                stage2(prev_it)
                            prev_it = it_
                stage2(prev_it)
                selM = psb("selM", (128, NT, JB))
                selA = psb("selA", (128, NT, JB))
                memset('pool', selM[:], 1.0, ['selM'])
                asel(selM[:], selM[:], [[128, NT], [-64, JB]], -128, 1, 0.0, ['selM'], ['selM'])
                memset('pool', selM[:, :, 0:1], 0.0, ['selM'])
                memset('pool', selA[:], 0.0, ['selA'])
                asel(selA[:], selA[:], [[128, NT], [-64, JB]], -128, 1, 1e9, ['selA'], ['selA'])
                asel(selA[:], selA[:], [[128, NT], [-64, JB]], 0, 1, -1.0, ['selA'], ['selA'])
                memset('pool', selA[:, :, 0:1], 1e9, ['selA'])
                scr = Ring([psb("sc%d" % i, (128, 2, JB)) for i in range(2)], "sc")
                sc2r = Ring([psb("sd%d" % i, (128, JB)) for i in range(2)], "sd")
                m8r = Ring([psb("m8%d" % i, (128, 16)) for i in range(4)], "m8")
                sbr = Ring([psb("sbi%d" % i, (128, 128)) for i in range(2)], "sbi")
                sto = Ring([psb("sto%d" % i, (128, 128)) for i in range(2)], "sto")
                for tsi in range(NT):
                    sc, sck = scr.next()
                    sbi, sbk = sbr.next()
                    if JB < 64:
                        memset('pool', sbi[:], 0.0, [sbk])
                    for g in range(2):
                        tt('dve', sc[:, g, :], impacc[:, tsi, g, :], selM[:, tsi, :], ALU.mult, [('imp', tsi, g), 'selM'], [sck])
                        tt('dve', sc[:, g, :], sc[:, g, :], selA[:, tsi, :], ALU.add, [sck, 'selA'], [sck])
                        m8, mk = m8r.next()
                        sd, sdk = sc2r.next()
                        S.op('dve', lambda e, a=m8, b=sc, g=g: e.max(out=a[:, 0:8], in_=b[:, g, :]), [sck], [mk])
                        S.op('dve', lambda e, a=m8, b=sc, d=sd, g=g: e.match_replace(out=d[:], in_to_replace=a[:, 0:8], in_values=b[:, g, :],
                                                                                   imm_value=-3e38), [sck, mk], [sdk])
                        S.op('dve', lambda e, a=m8, d=sd: e.max(out=a[:, 8:16], in_=d[:]), [sdk], [mk])
                        ts('dve', sbi[:, 64 * g:64 * g + JB], sc[:, g, :], m8[:, 15:16], ALU.is_ge, [sck, mk], [sbk], s2=-NEGM, op1=ALU.mult)
                        ts('dve', sbi[:, 64 * g:64 * g + JB], sbi[:, 64 * g:64 * g + JB], NEGM, ALU.add, [sbk], [sbk])
                    ps, pk = psr.next()
                    tr(ps[:, 0:128], sbi[:], ident[:], [sbk, 'ident'], [pk])
                    so, sok = sto.next()
                    act(so[:], ps[:, 0:128], AF.Copy, [pk], [sok])
                    S.dma(selbT_d[:, tsi * 128:(tsi + 1) * 128], so[:], [sok], [('selbT', tsi)], q='pool')
                S.barrier()

        def phase_C(l):
            for br in (1, 2):
                with ExitStack() as ph:
                    def psb(name, shape):
                        return ph.enter_context(nc.sbuf_tensor("%s_C%d_%d" % (name, l, br), list(shape), F32))
                    Wt = WGEN if br == 1 else WWIN
                    tab_d = bgen_d if br == 1 else bwin_d
                    KT_d = kslcT_d if br == 1 else kwinT_d
                    V_d = vslc_d if br == 1 else vwin_d
                    tab = psb("tab", (128, NH, Wt))
                    for h in range(NH):
                        S.dma(tab[:, h, :], tab_d[:, h, :], (), ['tab'])
                    LS = [psb("LS%d" % g, (128, SEQ)) for g in range(2)]
                    for g in range(2):
                        if br == 1:
                            S.dma(LS[g][0:64, :], KT_d[64 * g:64 * g + 64, :], (), [('LS', g)])
                            v = LS[g][64:128, :]
                            memset('pool', v, 1.0, [('LS', g)])
                            asel(v, v, [[1, SEQ]], 0, -64, 0.0, [('LS', g)], [('LS', g)])
                            asel(v, v, [[-1, SEQ]], 63, 64, 0.0, [('LS', g)], [('LS', g)])
                        else:
                            memset('pool', LS[g][64 * (1 - g):64 * (1 - g) + 64, :], 0.0, [('LS', g)])
                            S.dma(LS[g][64 * g:64 * g + 64, :], KT_d[64 * g:64 * g + 64, :], (), [('LS', g)])
                    V = psb("V", (128, NT, 2, 65))
                    memset('pool', V[:, :, :, 64:65], 1.0, ['V'])
                    with nc.allow_non_contiguous_dma(reason="V"):
                        for g in range(2):
                            for t0_ in range(0, NT, 8):
                                S.dma(V[:, t0_:t0_ + 8, g, 0:64], V_d.rearrange("(t p) c -> p t c", p=128)[:, t0_:t0_ + 8, 64 * g:64 * g + 64], (), ['V'])
                    gate_sb = psb("gate_sb", (128, NT, 24))
                    with nc.allow_non_contiguous_dma(reason="gate"):
                        for t0_ in range(0, NT, 8):
                            S.dma(gate_sb[:, t0_:t0_ + 8, :], gate_d.rearrange("(t p) c -> p t c", p=128)[:, t0_:t0_ + 8, :], (), ['gate_sb'])
                    qr = Ring([psb("qTc%d" % i, (128, SEQ)) for i in range(2)], "qTc")
                    ptr = Ring([psb("pt%d" % i, (128, 512)) for i in range(5)], "pt")
                    osr = Ring([psb("os%d" % i, (128, 64)) for i in range(4)], "os")
                    rdr = Ring([psb("rd%d" % i, (128, 2)) for i in range(4)], "rd")
                    b31 = psb("b31", (128, NH))
                    with nc.allow_non_contiguous_dma(reason="b31"):
                        S.dma(b31[:], fdg_in[:, NEGPAD + OFFMAX - 1].partition_broadcast(128), (), ['b31'])
                    psr4 = Ring(PS[0:4], "ps")
                    LAG = 3
                    for c in range(4):
                        if br == 2:
                            qT, qk = qr.next()
                            S.dma(qT[:], qT_d[c, :, :], (), [qk])
                        for half in range(2):
                            h = c + 4 * half
                            g = half
                            if br == 1:
                                qT, qk = qr.next()
                                S.dma(qT[0:64, :], qT_d[c, 64 * half:64 * half + 64, :], (), [qk])
                                S.dma(qT[64:128, :], selbT_d[64 * g:64 * g + 64, :], (), [qk])
                            pend = []

                            def stage3(item):
                                jq_, tk0_, pt_, ptk_, tks_, last_, first_ = item
                                tq0_ = 512 * jq_
                                for sub in range(4):
                                    if tk0_ > last_[sub] or tk0_ < first_[sub]:
                                        continue
                                    mm(PS[4 + sub][:, 0:65], pt_[:, sub * 128:(sub + 1) * 128], V[:, tk0_ // 128, g, :],
                                       tk0_ == first_[sub], tk0_ == last_[sub], [ptk_, 'V'], [('ps', 4 + sub)], inc=(tk0_ == last_[sub]))
                                if tk0_ == tks_[-1]:
                                    for sub in range(4):
                                        tsi = 4 * jq_ + sub
                                        po = PS[4 + sub]
                                        pok = ('ps', 4 + sub)
                                        rd, rk = rdr.next()
                                        ts('dve', rd[:, 0:1], po[:, 64:65], 1e-30, ALU.add, [pok], [rk])
                                        recip(rd[:, 1:2], rd[:, 0:1], [rk], [rk])
                                        o, ok = osr.next()
                                        ts('dve', o[:], po[:, 0:64], rd[:, 1:2], ALU.mult, [pok, rk, 'gate_sb'], [ok],
                                           s2=gate_sb[:, tsi, 3 * h + br:3 * h + br + 1], op1=ALU.mult)
                                        S.dma(obr_d[br, tsi * 128:(tsi + 1) * 128, 64 * h:64 * h + 64], o[:], [ok], [('obr', br, h, tsi)], q='pool')

                            for jq in range(QT):
                                tq0 = 512 * jq
                                lo = 0 if br == 1 else max(0, tq0 - 512)
                                tks = list(range(lo, tq0 + 512, 128))
                                last = {sub: max(tk for tk in tks if tk <= tq0 + 128 * sub + 127) for sub in range(4)}
                                if br == 2:
                                    first = {sub: min(tk for tk in tks if tk + 127 >= tq0 + 128 * sub - 511) for sub in range(4)}
                                else:
                                    first = {sub: tks[0] for sub in range(4)}
                                for tk0 in tks:
                                    ps, pk = psr4.next()
                                    mm(ps[:], LS[g][:, tk0:tk0 + 128], qT[:, tq0:tq0 + 512], True, True, [('LS', g), qk], [pk])
                                    pt, ptk = ptr.next()
                                    if tq0 - tk0 >= OFFMAX + 128:
                                        act(pt[:], ps[:], AF.Exp, [pk, 'b31'], [ptk], bias=b31[:, h:h + 1])
                                    else:
                                        off = min(tq0 - tk0, OFFMAX) + U0
                                        tt('dve', pt[:], ps[:], tab[:, h, off:off + 512], ALU.add, [pk, 'tab'], [ptk])
                                        act(pt[:], pt[:], AF.Exp, [ptk], [ptk])
                                    pend.append((jq, tk0, pt, ptk, tks, last, first))
                                    if len(pend) > LAG:
                                        stage3(pend.pop(0))
                            while pend:
                                stage3(pend.pop(0))
                    S.barrier()
        def bc(ap2, n, axis):
            P, A = ap2.shape
            if axis == 2:
                return ap2.unsqueeze(2).to_broadcast([P, A, n])
            return ap2.unsqueeze(1).to_broadcast([P, n, A])

        def phase_D(l):
            with ExitStack() as ph:
                def psb(name, shape):
                    return ph.enter_context(nc.sbuf_tensor("%s_D1%d" % (name, l), list(shape), F32))
                xr = Ring([psb("xin%d" % i, (128, SEQ + 3)) for i in range(2)], "xin")
                ar = Ring([psb("acc%d" % i, (128, SEQ)) for i in range(2)], "acc")
                sq = Ring([psb("sq%d" % i, (128, 512)) for i in range(4)], "sq")
                cw = psb("cw", (128, 4, 12))
                with nc.allow_non_contiguous_dma(reason="conv w"):
                    for i in range(4):
                        S.dma(cw[:, i, :], dn_conv_w[l, i].rearrange("(c p) -> p c", p=128), (), ['cw'])
                for ch in range(12):
                    xin, xk = xr.next()
                    acc, ak = ar.next()
                    memset('pool', xin[:, 0:3], 0.0, [xk])
                    S.dma(xin[:, 3:SEQ + 3], dnraw_d[ch, :, :], (), [xk])
                    ts('dve', acc[:], xin[:, 0:SEQ], cw[:, 0, ch:ch + 1], ALU.mult, [xk, 'cw'], [ak])
                    for i in range(1, 4):
                        stt(acc[:], xin[:, i:SEQ + i], cw[:, i, ch:ch + 1], acc[:], ALU.mult, ALU.add, [xk, 'cw', ak], [ak])
                    act(acc[:], acc[:], AF.Silu, [ak], [ak])
                    if ch < 8:
                        GRP = 4
                        for j0 in range(0, QT, GRP):
                            js = list(range(j0, min(QT, j0 + GRP)))
                            tiles = []
                            for j in js:
                                cs = slice(512 * j, 512 * j + 512)
                                q1, qk1 = sq.next()
                                act(q1[:], acc[:, cs], AF.Square, [ak], [qk1])
                                tiles.append((cs, q1, qk1))
                            pss = []
                            for (cs, q1, qk1) in tiles:
                                p2, pk2 = psr.next()
                                mm(p2[:], bdones[:], q1[:], True, True, ['bdones', qk1], [pk2])
                                pss.append((p2, pk2))
                            for (cs, q1, qk1), (p2, pk2) in zip(tiles, pss):
                                act(q1[:], p2[:], AF.Sqrt, [pk2], [qk1], bias=EPS, scale=1.0)
                            for (cs, q1, qk1) in tiles:
                                recip(q1[:], q1[:], [qk1], [qk1])
                            for (cs, q1, qk1) in tiles:
                                if ch < 4:
                                    stt(acc[:, cs], acc[:, cs], HD ** -0.5, q1[:], ALU.mult, ALU.mult, [ak, qk1], [ak])
                                else:
                                    tt('dve', acc[:, cs], acc[:, cs], q1[:], ALU.mult, [ak, qk1], [ak])
                    S.dma(dnc_d[ch, :, :], acc[:], [ak], [('dnc', ch)], q='pool')
                S.barrier()
            if 'stopD1' in dbg:
                return
            with ExitStack() as ph:
                def psb(name, shape):
                    return ph.enter_context(nc.sbuf_tensor("%s_D2%d" % (name, l), list(shape), F32))
                NG = NCH * 8
                g_tm = psb("g_tm", (64, NCH, 8))
                b_tm = psb("b_tm", (64, NCH, 8))
                gc_tm = psb("gc_tm", (64, NCH, 8))
                eg_tm = psb("eg_tm", (64, NCH, 8))
                bg_tm = psb("bg_tm", (64, NCH, 8))
                kds_tm = psb("kds_tm", (64, NCH, 8))
                egl = psb("egl", (64, NCH, 8))
                sel63 = psb("sel63", (64, 64))
                ngrow = psb("ngrow", (64, 64))
                with nc.allow_non_contiguous_dma(reason="bg"):
                    for n0_ in range(0, NCH, 16):
                        S.dma(b_tm[:, n0_:n0_ + 16, :], bg_d.rearrange("(n i) c -> i n c", i=64)[:, n0_:n0_ + 16, 0:8], (), ['b_tm'])
                        S.dma(g_tm[:, n0_:n0_ + 16, :], bg_d.rearrange("(n i) c -> i n c", i=64)[:, n0_:n0_ + 16, 8:16], (), ['g_tm'])
                S.dma(ngrow[:], dn_norm_g[l].partition_broadcast(64), (), ['ngrow'])
                ts('dve', sel63[:], ones[0:64, 0:64], ident[0:64, 63:64], ALU.mult, ['ones', 'ident'], ['sel63'])
                gflat = g_tm[:].rearrange("p n h -> p (n h)")
                gcflat = gc_tm[:].rearrange("p n h -> p (n h)")
                for c0 in range(0, NG, 512):
                    cw_ = min(512, NG - c0)
                    ps, pk = psr.next()
                    mm(ps[0:64, 0:cw_], UT[0:64, :], gflat[:, c0:c0 + cw_], True, True, ['UT', 'g_tm'], [pk])
                    cp('dve', gcflat[:, c0:c0 + cw_], ps[0:64, 0:cw_], [pk], ['gc_tm'])
                    ps2, pk2 = psr.next()
                    mm(ps2[0:64, 0:cw_], sel63[:], gcflat[:, c0:c0 + cw_], True, True, ['sel63', 'gc_tm'], [pk2])
                    act(egl[:].rearrange("p n h -> p (n h)")[:, c0:c0 + cw_], ps2[0:64, 0:cw_], AF.Exp, [pk2], ['egl'])
                    tt('dve', kds_tm[:].rearrange("p n h -> p (n h)")[:, c0:c0 + cw_], ps2[0:64, 0:cw_], gcflat[:, c0:c0 + cw_],
                       ALU.subtract, [pk2, 'gc_tm'], ['kds_tm'])
                act(kds_tm[:], kds_tm[:], AF.Exp, ['kds_tm'], ['kds_tm'])
                act(eg_tm[:], gc_tm[:], AF.Exp, ['gc_tm'], ['eg_tm'])
                tt('dve', bg_tm[:], b_tm[:], eg_tm[:], ALU.mult, ['b_tm', 'eg_tm'], ['bg_tm'])

                def t3(name):
                    return psb(name, (64, 8, 64))
                NPS, NOS = 2, 4
                Pt = [{nm: t3("%s_%d" % (nm, i)) for nm in ['ktm', 'vtm', 'qtm', 'rhsg', 'rhsb', 'DTr', 'decT', 'dec', 't1', 'Xa', 'Xb', 'Ya', 'Yb', 'TT', 'vb', 'kbg']} for i in range(NPS)]
                Ot = [{nm: t3("%s_%d" % (nm, i)) for nm in ['AT', 'kd', 'qdT', 'wT', 'u_sb']} for i in range(NOS)]
                Xs_ = [psb("X%d" % i, (64, 24, 64)) for i in range(NPS)]
                zs_ = [psb("z%d" % i, (64, 512)) for i in range(NOS)]
                vn, Sst, o_sb, osq = t3("vn"), t3("Sst"), t3("o_sb"), t3("osq")
                ssum = psb("ssum", (64, 16))
                ytm = psb("ytm", (64, 512))
                ybo = Ring([psb("ybo%d" % i, (128, 4, 64)) for i in range(2)], "ybo")
                memset('pool', Sst[:], 0.0, ['Sst'])
                I64 = ident[0:64, 0:64]

                def hmm(pst, pk, lhs, rhs, r, first=True, last=True):
                    for h in range(8):
                        mm(pst[0:64, 64 * h:64 * h + 64], lhs(h), rhs(h), first, last, r, [pk], inc=(h == 7))

                def v3(ps):
                    return ps[0:64, :].rearrange("p (h c) -> p h c", h=8)

                def pre_gen(n, sp_, so_):
                    ktm, vtm, qtm, rhsg, rhsb, DTr, decT, dec, t1, Xa, Xb, Ya, Yb, TT, vb, kbg = [Pt[sp_][k_] for k_ in ['ktm', 'vtm', 'qtm', 'rhsg', 'rhsb', 'DTr', 'decT', 'dec', 't1', 'Xa', 'Xb', 'Ya', 'Yb', 'TT', 'vb', 'kbg']]
                    AT, kd, qdT, wT, u_sb = [Ot[so_][k_] for k_ in ['AT', 'kd', 'qdT', 'wT', 'u_sb']]
                    X, Xk = Xs_[sp_], ('X', sp_)
                    with nc.allow_non_contiguous_dma(reason="chunk load"):
                        for c0_ in (0, 12):
                            S.dma(X[:, c0_:c0_ + 12, :], dnc_d.rearrange("c (a p) t -> p (c a) t", a=2)[:, c0_:c0_ + 12, 64 * n:64 * n + 64], (), [Xk])
                        yield
                    z, zk = zs_[so_], ('z', so_)
                    S.dma(z[:], zs_d[64 * n:64 * n + 64, :], (), [zk])
                    yield
                    for (dst, dk_, c0) in ((qtm, ('qtm', sp_), 0), (ktm, ('ktm', sp_), 8), (vtm, ('vtm', sp_), 16)):
                        ps, pk = prg[sp_].next()
                        for kc in range(8):
                            tr(ps[0:64, 64 * kc:64 * kc + 64], X[:, c0 + kc, :], I64, [Xk, 'ident'], [pk], inc=(kc == 7))
                            yield
                        if dk_ == ('ktm', sp_):
                            cp('dve', dst[:], v3(ps), [pk], [dk_])
                            yield
                        else:
                            act(dst[:], v3(ps), AF.Copy, [pk], [dk_])
                            yield
                    tt('dve', rhsg[:], bc(g_tm[:, n, :], 64, 2), bc(UT[0:64, :], 8, 1), ALU.mult, ['g_tm', 'UT'], [('rhsg', sp_)])
                    yield
                    tt('dve', rhsb[:], bc(b_tm[:, n, :], 64, 2), bc(I64, 8, 1), ALU.mult, ['b_tm', 'ident'], [('rhsb', sp_)])
                    yield
                    pg, pgk = prg[sp_].next()
                    mm(pg[0:64, :], ones[0:64, 0:64], rhsg[:].rearrange("p h c -> p (h c)"), True, True, ['ones', ('rhsg', sp_)], [pgk])
                    yield
                    pb, pbk = prg[sp_].next()
                    mm(pb[0:64, :], ones[0:64, 0:64], rhsb[:].rearrange("p h c -> p (h c)"), True, True, ['ones', ('rhsb', sp_)], [pbk])
                    yield
                    tt('dve', DTr[:], v3(pg), bc(gc_tm[:, n, :], 64, 2), ALU.subtract, [pgk, 'gc_tm'], [('DTr', sp_)])
                    yield
                    tt('dve', decT[:], DTr[:], bc(maskU[:], 8, 1), ALU.add, [('DTr', sp_), 'maskU'], [('decT', sp_)])
                    yield
                    act(decT[:], decT[:], AF.Exp, [('decT', sp_)], [('decT', sp_)])
                    yield
                    ts('dve', dec[:], DTr[:], -1.0, ALU.mult, [('DTr', sp_)], [('dec', sp_)])
                    yield
                    tt('dve', dec[:], dec[:], bc(maskL[:], 8, 1), ALU.add, [('dec', sp_), 'maskL'], [('dec', sp_)])
                    yield
                    act(dec[:], dec[:], AF.Exp, [('dec', sp_)], [('dec', sp_)])
                    yield
                    pkk, pkkk = prg[sp_].next()
                    hmm(pkk, pkkk, lambda h: X[:, 8 + h, :], lambda h: X[:, 8 + h, :], [Xk])
                    yield
                    pqk, pqkk = prg[sp_].next()
                    hmm(pqk, pqkk, lambda h: X[:, 8 + h, :], lambda h: X[:, h, :], [Xk])
                    yield
                    tt('dve', AT[:], v3(pqk), decT[:], ALU.mult, [pqkk, ('decT', sp_)], [('AT', so_)])
                    yield
                    tt('dve', t1[:], v3(pkk), decT[:], ALU.mult, [pkkk, ('decT', sp_)], [('t1', sp_)])
                    yield
                    tt('dve', t1[:], v3(pb), t1[:], ALU.mult, [pbk, ('t1', sp_)], [('t1', sp_)])
                    yield
                    tt('dve', Ya[:], t1[:], bc(nsU[:], 8, 1), ALU.mult, [('t1', sp_), 'nsU'], [('Ya', sp_)])
                    yield
                    tt('dve', t1[:], v3(pkk), dec[:], ALU.mult, [pkkk, ('dec', sp_)], [('t1', sp_)])
                    yield
                    tt('dve', t1[:], t1[:], bc(b_tm[:, n, :], 64, 2), ALU.mult, [('t1', sp_), 'b_tm'], [('t1', sp_)])
                    yield
                    tt('dve', Xa[:], t1[:], bc(nsL[:], 8, 1), ALU.mult, [('t1', sp_), 'nsL'], [('Xa', sp_)])
                    yield
                    tt('dve', TT[:], Ya[:], bc(I64, 8, 1), ALU.add, [('Ya', sp_), 'ident'], [('TT', sp_)])
                    yield
                    Xc, Xn_, Yc, Yn_ = (Xa, ('Xa', sp_)), (Xb, ('Xb', sp_)), (Ya, ('Ya', sp_)), (Yb, ('Yb', sp_))
                    for lvl in range(1, 6):
                        p1, p1k = prg[sp_].next()
                        hmm(p1, p1k, lambda h: Yc[0][:, h, :], lambda h: Xc[0][:, h, :], [Yc[1], Xc[1]])
                        yield
                        if lvl < 5:
                            p2, p2k = prg[sp_].next()
                            hmm(p2, p2k, lambda h: Xc[0][:, h, :], lambda h: Yc[0][:, h, :], [Yc[1], Xc[1]])
                            yield
                        act(Xn_[0][:], v3(p1), AF.Copy, [p1k], [Xn_[1]])
                        yield
                        if lvl < 5:
                            cp('dve', Yn_[0][:], v3(p2), [p2k], [Yn_[1]])
                            yield
                        Xc, Xn_ = Xn_, Xc
                        if lvl < 5:
                            Yc, Yn_ = Yn_, Yc
                        p3, p3k = prg[sp_].next()
                        hmm(p3, p3k, lambda h: Xc[0][:, h, :], lambda h: TT[:, h, :], [Xc[1], ('TT', sp_)])
                        yield
                        tt('dve', TT[:], TT[:], v3(p3), ALU.add, [('TT', sp_), p3k], [('TT', sp_)])
                        yield
                    tt('dve', vb[:], vtm[:], bc(b_tm[:, n, :], 64, 2), ALU.mult, [('vtm', sp_), 'b_tm'], [('vb', sp_)])
                    yield
                    tt('dve', kbg[:], ktm[:], bc(bg_tm[:, n, :], 64, 2), ALU.mult, [('ktm', sp_), 'bg_tm'], [('kbg', sp_)])
                    yield
                    tt('dve', kd[:], ktm[:], bc(kds_tm[:, n, :], 64, 2), ALU.mult, [('ktm', sp_), 'kds_tm'], [('kd', so_)])
                    yield
                    tt('dve', qtm[:], qtm[:], bc(eg_tm[:, n, :], 64, 2), ALU.mult, [('qtm', sp_), 'eg_tm'], [('qtm', sp_)])
                    yield
                    pu, puk = prg[sp_].next()
                    hmm(pu, puk, lambda h: TT[:, h, :], lambda h: vb[:, h, :], [('TT', sp_), ('vb', sp_)])
                    yield
                    act(u_sb[:], v3(pu), AF.Copy, [puk], [('u_sb', so_)])
                    yield
                    pw, pwk = prg[sp_].next()
                    hmm(pw, pwk, lambda h: kbg[:, h, :], lambda h: TT[:, h, :], [('TT', sp_), ('kbg', sp_)])
                    yield
                    act(wT[:], v3(pw), AF.Copy, [pwk], [('wT', so_)])
                    yield
                    pq, pqk2 = prg[sp_].next()
                    for h in range(8):
                        tr(pq[0:64, 64 * h:64 * h + 64], qtm[:, h, :], I64, [('qtm', sp_), 'ident'], [pqk2], inc=(h == 7))
                        yield
                    cp('dve', qdT[:], v3(pq), [pqk2], [('qdT', so_)])
                    yield
                def scan_gen(n, so_):
                    AT, kd, qdT, wT, u_sb = [Ot[so_][k_] for k_ in ['AT', 'kd', 'qdT', 'wT', 'u_sb']]
                    z, zk = zs_[so_], ('z', so_)
                    pv, pvk = psc.next()
                    hmm(pv, pvk, lambda h: wT[:, h, :], lambda h: Sst[:, h, :], [('wT', so_), 'Sst'])
                    yield
                    tt('dve', vn[:], u_sb[:], v3(pv), ALU.subtract, [('u_sb', so_), pvk], ['vn'])
                    yield
                    po, pok = psc.next()
                    for h in range(8):
                        mm(po[0:64, 64 * h:64 * h + 64], qdT[:, h, :], Sst[:, h, :], True, False, [('qdT', so_), 'Sst'], [pok], inc=False)
                        yield
                        mm(po[0:64, 64 * h:64 * h + 64], AT[:, h, :], vn[:, h, :], False, True, [('AT', so_), 'vn'], [pok], inc=(h == 7))
                        yield
                    pS, pSk = psc.next()
                    hmm(pS, pSk, lambda h: kd[:, h, :], lambda h: vn[:, h, :], [('kd', so_), 'vn'])
                    yield
                    tt('dve', Sst[:], Sst[:], bc(egl[:, n, :], 64, 2), ALU.mult, ['Sst', 'egl'], ['Sst'])
                    yield
                    tt('dve', Sst[:], Sst[:], v3(pS), ALU.add, ['Sst', pSk], ['Sst'])
                    yield
                    act(o_sb[:], v3(po), AF.Copy, [pok], ['o_sb'])
                    yield
                    tt('dve', osq[:], o_sb[:], o_sb[:], ALU.mult, ['o_sb'], ['osq'])
                    yield
                    S.op('dve', lambda e: e.tensor_reduce(out=ssum[:, 0:8], in_=osq[:], op=ALU.add, axis=AX.X), ['osq'], ['ssum'])
                    yield
                    act(ssum[:, 8:16], ssum[:, 0:8], AF.Sqrt, ['ssum'], ['ssum'], bias=EPS, scale=1.0 / 64)
                    yield
                    recip(ssum[:, 8:16], ssum[:, 8:16], ['ssum'], ['ssum'])
                    yield
                    tt('dve', o_sb[:], o_sb[:], bc(ssum[:, 8:16], 64, 2), ALU.mult, ['o_sb', 'ssum'], ['o_sb'])
                    yield
                    tt('dve', o_sb[:], o_sb[:], bc(ngrow[:], 8, 1), ALU.mult, ['o_sb', 'ngrow'], ['o_sb'])
                    yield
                    tt('dve', ytm[:], o_sb[:].rearrange("p h c -> p (h c)"), z[:], ALU.mult, ['o_sb', zk], ['ytm'])
                    yield
                    py, pyk = psc.next()
                    for kc in range(4):
                        tr(py[:, 64 * kc:64 * kc + 64], ytm[:, 128 * kc:128 * kc + 128], I64, ['ytm', 'ident'], [pyk], inc=(kc == 3))
                        yield
                    yo, yok = ybo.next()
                    act(yo[:], py[:, 0:256].rearrange("p (a b) -> p a b", a=4), AF.Copy, [pyk], [yok])
                    yield
                    with nc.allow_non_contiguous_dma(reason="yb store"):
                        S.dma(ybT_d.rearrange("c p t -> p c t")[:, :, 64 * n:64 * n + 64], yo[:], [yok], [('ybT', n)], q='pool')
                        yield


                S.barrier()
                prg = [Ring(PS[0:3], "psA"), Ring(PS[3:6], "psB")]
                psc = Ring(PS[6:8], "psC")

                def run_rr(gens):
                    gens = list(gens)
                    while gens:
                        for g_ in list(gens):
                            try:
                                next(g_)
                            except StopIteration:
                                gens.remove(g_)

                def scan_pair(a_, b_):
                    yield from scan_gen(a_, a_ % NOS)
                    yield from scan_gen(b_, b_ % NOS)
                prev_ = None
                for grp in range(NCH // 2):
                    a_, b_ = 2 * grp, 2 * grp + 1
                    gl = [pre_gen(a_, 0, a_ % NOS), pre_gen(b_, 1, b_ % NOS)]
                    if prev_ is not None:
                        gl.append(scan_pair(*prev_))
                    run_rr(gl)
                    prev_ = (a_, b_)
                run_rr([scan_pair(*prev_)])
                S.barrier()

        def phase_E(l, src_x):
            with ExitStack() as ph:
                def psb(name, shape):
                    return ph.enter_context(nc.sbuf_tensor("%s_E%d" % (name, l), list(shape), F32))
                wa = psb("wa", (128, 4, D))
                wb = psb("wb", (128, 4, D))
                wo = psb("wo", (128, 8, D))
                g1r = psb("g1r", (128, D))
                S.dma(g1r[:], mod_d[l, 2 * D:3 * D].partition_broadcast(128), (), ['g1r'])
                S.dma(wa[:], w_br_a[l].rearrange("(kc p) n -> p kc n", p=128), (), ['wa'])
                S.dma(wb[:], w_br_b[l].rearrange("(kc p) n -> p kc n", p=128), (), ['wb'])
                S.dma(wo[:], w_out[l].rearrange("(kc p) n -> p kc n", p=128), (), ['wo'])
                obr = Ring([psb("ob%d" % i, (128, 3, 512)) for i in range(2)], "ob")
                yaT = psb("yaT", (128, 4, 512))
                ybT = psb("ybT", (128, 4, 512))
                mg = Ring([psb("mg%d" % i, (128, 2, 512)) for i in range(2)], "mg")
                yT = psb("yT", (128, 8, 512))
                tmp = Ring([psb("tmp%d" % i, (128, 512)) for i in range(2)], "tmp")
                xr = Ring([psb("xe%d" % i, (128, D)) for i in range(2)], "xe")
                for j in range(QT):
                    t0 = 512 * j
                    cs = slice(t0, t0 + 512)
                    for sub in range(4):
                        r0 = t0 + 128 * sub
                        ob, obk = obr.next()
                        S.dma(ob[:], obr_d[:, r0:r0 + 128, :].rearrange("b p c -> p b c"), (), [obk])
                        tt('dve', ob[:, 0, :], ob[:, 0, :], ob[:, 1, :], ALU.add, [obk], [obk])
                        tt('pool', ob[:, 0, :], ob[:, 0, :], ob[:, 2, :], ALU.add, [obk], [obk])
                        transpose_to(ob[:, 0, :], obk, yaT, 'yaT', sub, nkc=4)
                    S.dma(ybT[:], ybT_d.rearrange("c p t -> p c t")[:, :, cs], (), ['ybT'])
                    for dc in range(8):
                        m, mk = mg.next()
                        S.dma(m[:, 0, :], mergeT_d[dc, :, cs], (), [mk])
                        S.dma(m[:, 1, :], mergeT_d[8 + dc, :, cs], (), [mk])
                        pa, pak = psr.next()
                        for kc in range(4):
                            mm(pa[:], wa[:, kc, 128 * dc:128 * dc + 128], yaT[:, kc, :], kc == 0, kc == 3, ['wa', 'yaT'], [pak], inc=(kc == 3))
                        pb, pbk = psr.next()
                        for kc in range(4):
                            mm(pb[:], wb[:, kc, 128 * dc:128 * dc + 128], ybT[:, kc, :], kc == 0, kc == 3, ['wb', 'ybT'], [pbk], inc=(kc == 3))
                        t, tk = tmp.next()
                        tt('dve', t[:], pa[:], m[:, 0, :], ALU.mult, [pak, mk], [tk])
                        tt('dve', m[:, 1, :], pb[:], m[:, 1, :], ALU.mult, [pbk, mk], [mk])
                        tt('pool', yT[:, dc, :], t[:], m[:, 1, :], ALU.add, [tk, mk], ['yT'])
                    for sub in range(4):
                        r0 = t0 + 128 * sub
                        xt, xk = xr.next()
                        S.dma(xt[:], src_x[r0:r0 + 128, :], [('x', r0 // 128)], [xk])
                        for nh in range(2):
                            po, pok = psr.next()
                            for kc in range(8):
                                mm(po[:], yT[:, kc, 128 * sub:128 * sub + 128], wo[:, kc, 512 * nh:512 * nh + 512], kc == 0, kc == 7,
                                   ['yT', 'wo'], [pok], inc=(kc == 7))
                            t, tk = tmp.next()
                            tt('dve', t[:], po[:], g1r[:, 512 * nh:512 * nh + 512], ALU.mult, [pok, 'g1r'], [tk])
                            tt('pool', xt[:, 512 * nh:512 * nh + 512], xt[:, 512 * nh:512 * nh + 512], t[:], ALU.add, [xk, tk], [xk])
                        S.dma(out[r0:r0 + 128, :], xt[:], [xk], [('x', r0 // 128)], q='pool')
                S.barrier()

        def phase_F(l):
            NE = 16
            with ExitStack() as ph:
                def psb(name, shape, dt=F32):
                    return ph.enter_context(nc.sbuf_tensor("%s_F%d" % (name, l), list(shape), dt))
                A2r = psb("A2r", (128, D))
                sh2r = psb("sh2r", (128, D))
                g2r = psb("g2r", (128, D))
                S.dma(sh2r[:], mod_d[l, 3 * D:4 * D].partition_broadcast(128), (), ['modrow'])
                S.dma(A2r[:], mod_d[l, 4 * D:5 * D].partition_broadcast(128), (), ['modrow'])
                S.dma(g2r[:], mod_d[l, 5 * D:6 * D].partition_broadcast(128), (), ['g2r'])
                xr = Ring([psb("xf%d" % i, (128, D)) for i in range(2)], "xf")
                hr = Ring([psb("hf%d" % i, (128, D)) for i in range(2)], "hf")
                sm = Ring([psb("smf%d" % i, (128, 8)) for i in range(4)], "smf")
                hTr = Ring([psb("hTf%d" % i, (128, 8, 256)) for i in range(2)], "hTf")
                rt = psb("rt", (128, 8, 16))
                rs = psb("rs", (128, 16))
                M1a = psb("M1a", (128, NT, NE))
                M2a = psb("M2a", (128, NT, NE))
                wn = psb("wn", (128, NT, 2))
                SU = psb("SU", (128, 128))
                memset('pool', SU[:], 1.0, ['SU'])
                asel(SU[:], SU[:], [[1, 128]], -1, -1, 0.0, ['SU'], ['SU'])
                rtr = [rt, psb("rt2", (128, 8, 16))]
                rsr = [rs, psb("rs2", (128, 16))]

                def p1_stage1(ti):
                    rt = rtr[ti % 2]
                    RT = ('rt', ti % 2)
                    hT, hTk = hTr.next()
                    ht, hk, xt, xk = norm_tile(out, 128 * ti, A2r[:], sh2r[:], xr, hr, sm)
                    S.dma(h2_d[128 * ti:128 * ti + 128, :], ht[:], [hk], [('h2', ti)], q='pool')
                    transpose_to(ht, hk, hT, hTk, 0)
                    pr, prk = psr.next()
                    for kc in range(8):
                        mm(pr[:, 0:16], hT[:, kc, 0:128], rtrw[:, kc, :], kc == 0, kc == 7, [hTk, 'rtrw'], [prk], inc=(kc == 7))
                    sc = rt[:, 0, :]
                    sel = rt[:, 1, :]
                    w1_ = rt[:, 2, :]
                    w2_ = rt[:, 3, :]
                    act(sc, pr[:, 0:16], AF.Sigmoid, [prk], [RT])
                    return ti

                def p1_stage2(ti):
                    rt = rtr[ti % 2]
                    rs = rsr[ti % 2]
                    RT = ('rt', ti % 2)
                    RS = ('rs', ti % 2)
                    sc = rt[:, 0, :]
                    sel = rt[:, 1, :]
                    w1_ = rt[:, 2, :]
                    w2_ = rt[:, 3, :]
                    tt('dve', sel, sc, rtrb[:], ALU.add, [RT, 'rtrb'], [RT])
                    sel3 = sel.rearrange("p (g e) -> p g e", g=4)
                    S.op('dve', lambda e, s3=sel3: e.tensor_reduce(out=rs[:, 0:4], in_=s3, op=ALU.max, axis=AX.X), [RT], [RS])
                    tt('dve', w1_.rearrange("p (g e) -> p g e", g=4), sel3, bc(rs[:, 0:4], 4, 2), ALU.is_ge, [RT, RS], [RT])
                    stt(w2_, w1_, -1e9, sel, ALU.mult, ALU.add, [RT], [RT])
                    S.op('dve', lambda e, a=w2_: e.tensor_reduce(out=rs[:, 4:8], in_=a.rearrange("p (g e) -> p g e", g=4), op=ALU.max, axis=AX.X),
                         [RT], [RS])
                    tt('dve', rs[:, 8:12], rs[:, 0:4], rs[:, 4:8], ALU.add, [RS], [RS])
                    S.op('dve', lambda e: e.tensor_reduce(out=rs[:, 12:13], in_=rs[:, 8:12], op=ALU.max, axis=AX.X), [RS], [RS])
                    ts('dve', rs[:, 4:8], rs[:, 8:12], rs[:, 12:13], ALU.is_ge, [RS], [RS], s2=-1.0, op1=ALU.add)
                    ts('dve', rs[:, 4:8], rs[:, 4:8], 1e9, ALU.mult, [RS], [RS])
                    tt('dve', w1_.rearrange("p (g e) -> p g e", g=4), sel3, bc(rs[:, 4:8], 4, 2), ALU.add, [RT, RS], [RT])
                    S.op('dve', lambda e, a=w1_: e.max(out=rt[:, 4, 0:8], in_=a), [RT], [RT])
                    ts('dve', M1a[:, ti, :], w1_, rt[:, 4, 0:1], ALU.is_ge, [RT], ['M1a'])
                    ts('dve', w2_, w1_, rt[:, 4, 1:2], ALU.is_ge, [RT], [RT])
                    tt('dve', M2a[:, ti, :], w2_, M1a[:, ti, :], ALU.subtract, [RT, 'M1a'], ['M2a'])
                    tt('dve', w2_, M1a[:, ti, :], sc, ALU.mult, [RT, 'M1a'], [RT])
                    S.op('dve', lambda e, a=w2_: e.tensor_reduce(out=rs[:, 13:14], in_=a, op=ALU.add, axis=AX.X), [RT], [RS])
                    tt('dve', w2_, M2a[:, ti, :], sc, ALU.mult, [RT, 'M2a'], [RT])
                    S.op('dve', lambda e, a=w2_: e.tensor_reduce(out=rs[:, 14:15], in_=a, op=ALU.add, axis=AX.X), [RT], [RS])
                    tt('dve', rs[:, 15:16], rs[:, 13:14], rs[:, 14:15], ALU.add, [RS], [RS])
                    recip(rs[:, 15:16], rs[:, 15:16], [RS], [RS])
                    ts('dve', wn[:, ti, :], rs[:, 13:15], rs[:, 15:16], ALU.mult, [RS], ['wn'])

                prev_t = None
                for ti in range(NT):
                    p1_stage1(ti)
                    if prev_t is not None:
                        p1_stage2(prev_t)
                    prev_t = ti
                p1_stage2(prev_t)

                NG = NT * NE
                Mall = psb("Mall", (128, NT, NE))
                cnt = psb("cnt", (128, NT, NE))
                base = psb("base", (128, NT, NE))
                dest = psb("dest", (128, NT, NE))
                dtmp = psb("dtmp", (128, NT, NE))
                d12 = psb("d12", (128, 2, NT))
                idx12 = psb("idx12", (128, 2, NT), I32)
                ev = psb("ev", (128, 8, NE))
                ebf = psb("ebf", (128, NBK))
                bidx_i = psb("bidx_i", (128, NBK), I32)
                bidx = psb("bidx", (128, NBK))
                pcol_i = psb("pcol_i", (128, 1), I32)
                pcol = psb("pcol", (128, 1))
                widx = psb("widx", (128, NBK), I32)
                tt('dve', Mall[:], M1a[:], M2a[:], ALU.add, ['M1a', 'M2a'], ['Mall'])
                Mf = Mall[:].rearrange("p t e -> p (t e)")
                prk_, prkk = psr.next()
                pcn, pcnk = psr.next()
                for c0 in range(0, NG, 512):
                    cw_ = min(512, NG - c0)
                    assert NG <= 512
                    mm(prk_[:, 0:cw_], SU[:], Mf[:, c0:c0 + cw_], True, True, ['SU', 'Mall'], [prkk])
                    mm(pcn[:, 0:cw_], ones[:], Mf[:, c0:c0 + cw_], True, True, ['ones', 'Mall'], [pcnk])
                cp('dve', cnt[:].rearrange("p t e -> p (t e)"), pcn[:, 0:NG], [pcnk], ['cnt'])
                memset('dve', base[:, 0, :], 0.0, ['base'])
                for ti in range(1, NT):
                    tt('dve', base[:, ti, :], base[:, ti - 1, :], cnt[:, ti - 1, :], ALU.add, ['base', 'cnt'], ['base'])
                tt('dve', ev[:, 0, :], base[:, NT - 1, :], cnt[:, NT - 1, :], ALU.add, ['base', 'cnt'], ['ev'])
                memset('dve', ev[:, 1, :], 0.0, ['ev'])
                for m in range(SEQ // MBLK):
                    stt(ev[:, 1, :], ev[:, 0, :], float(MBLK * m), ev[:, 1, :], ALU.is_gt, ALU.add, ['ev'], ['ev'])
                ts('dve', ev[:, 2, :], ev[:, 1, :], float(MBLK), ALU.mult, ['ev'], ['ev'])
                cp('dve', ev[:, 3, 0:1], ev[:, 2, 0:1], ['ev'], ['ev'])
                for e_ in range(1, NE):
                    tt('dve', ev[:, 3, e_:e_ + 1], ev[:, 3, e_ - 1:e_], ev[:, 2, e_:e_ + 1], ALU.add, ['ev'], ['ev'])
                tt('dve', ev[:, 4, :], ev[:, 3, :], ev[:, 2, :], ALU.subtract, ['ev'], ['ev'])
                tt('dve', dest[:].rearrange("p t e -> p (t e)"), prk_[:, 0:NG], base[:].rearrange("p t e -> p (t e)"), ALU.add, [prkk, 'base'], ['dest'])
                tt('dve', dest[:], dest[:], bc(ev[:, 4, :], NT, 1), ALU.add, ['dest', 'ev'], ['dest'])
                for k_, Mk in ((0, M1a), (1, M2a)):
                    tt('dve', dtmp[:], dest[:], Mk[:], ALU.mult, ['dest', 'M1a', 'M2a'], ['dtmp'])
                    S.op('dve', lambda e, k_=k_: e.tensor_reduce(out=d12[:, k_, :], in_=dtmp[:], op=ALU.add, axis=AX.X), ['dtmp'], ['d12'])
                cp('dve', idx12[:], d12[:], ['d12'], ['idx12'])
                S.op('pool', lambda e: e.iota(bidx_i[:], pattern=[[MBLK, NBK]], base=0, channel_multiplier=0), (), ['bidx_i'])
                S.op('pool', lambda e: e.iota(pcol_i[:], pattern=[[0, 1]], base=0, channel_multiplier=1), (), ['pcol_i'])
                cp('dve', bidx[:], bidx_i[:], ['bidx_i'], ['bidx'])
                cp('dve', pcol[:], pcol_i[:], ['pcol_i'], ['pcol'])
                memset('dve', ebf[:], 0.0, ['ebf'])
                for e_ in range(NE):
                    stt(ebf[:], bidx[:], ev[:, 3, e_:e_ + 1], ebf[:], ALU.is_ge, ALU.add, ['bidx', 'ev', 'ebf'], ['ebf'])
                ts('dve', ebf[:], ebf[:], float(NE - 1), ALU.min, ['ebf'], ['ebf'], s2=128.0, op1=ALU.mult)
                ts('dve', ebf[:], ebf[:], pcol[:, 0:1], ALU.add, ['ebf', 'pcol'], ['ebf'], s2=float(l * 16 * 128), op1=ALU.add)
                cp('dve', widx[:], ebf[:], ['ebf'], ['widx'])
                for ti in range(NT):
                    ht, hk = hr.next()
                    S.dma(ht[:], h2_d[128 * ti:128 * ti + 128, :], [('h2', ti)], [hk], q='sp')
                    for k_ in range(2):
                        S.idma(Xs_d[:, :], ht[:, :], bass.IndirectOffsetOnAxis(ap=idx12[:, k_, ti:ti + 1], axis=0), None,
                               [hk, 'idx12'], [('Xs', ti, k_)])
                S.barrier()
                with ExitStack() as ph3:
                    def psb3(name, shape, dt=F32):
                        return ph3.enter_context(nc.sbuf_tensor("%s_F3%d" % (name, l), list(shape), dt))
                    wgr = Ring([psb3("wg%d" % i, (128, 8 * 512)) for i in range(2)], "wg")
                    wur = Ring([psb3("wu%d" % i, (128, 8 * 512)) for i in range(2)], "wu")
                    wdr = Ring([psb3("wd%d" % i, (128, 4 * D)) for i in range(2)], "wd")
                    xgr = Ring([psb3("xg%d" % i, (128, D)) for i in range(4)], "xg")
                    actT = psb3("actT", (128, 4, 256))
                    sg = Ring([psb3("sg%d" % i, (128, 256)) for i in range(2)], "sg")
                    yor = Ring([psb3("yo%d" % i, (128, D)) for i in range(2)], "yo")
                    wg_v = moe_wg.rearrange("l e (p kc) n -> (l e p) (kc n)", kc=8)
                    wu_v = moe_wu.rearrange("l e (p kc) n -> (l e p) (kc n)", kc=8)
                    wd_v = moe_wd.rearrange("l e (p fc) n -> (l e p) (fc n)", fc=4)
                    deferred = []
                    for b_ in range(NBK):
                        off = bass.IndirectOffsetOnAxis(ap=widx[:, b_:b_ + 1], axis=0)
                        wg, wgk = wgr.next()
                        wu, wuk = wur.next()
                        wd, wdk = wdr.next()
                        S.idma(wg[:, :], wg_v, None, off, ['widx'], [wgk])
                        S.idma(wu[:, :], wu_v, None, off, ['widx'], [wuk])
                        S.idma(wd[:, :], wd_v, None, off, ['widx'], [wdk])
                        hT, hTk = hTr.next()
                        xgs = []
                        for sub in range(2):
                            xg, xgk = xgr.next()
                            r0 = b_ * MBLK + 128 * sub
                            S.dma(xg[:], Xs_d[r0:r0 + 128, :], (), [xgk], q='sp')
                            xgs.append((xg, xgk))
                        for d_ in deferred:
                            S.dma(*d_[0], **d_[1])
                        deferred = []
                        for sub in range(2):
                            xg, xgk = xgs[sub]
                            for k0 in (0, 4):
                                ps, pk = psr.next()
                                for kc in range(k0, k0 + 4):
                                    tr(ps[:, (kc - k0) * 128:(kc - k0 + 1) * 128], xg[:, kc:D:8], ident[:], [xgk, 'ident'], [pk], inc=(kc == k0 + 3))
                                dst = hT[:, k0:k0 + 4, sub * 128:(sub + 1) * 128]
                                srcv = ps[:].rearrange("p (a b) -> p a b", a=4)
                                if k0 == 0:
                                    act(dst, srcv, AF.Copy, [pk], [hTk])
                                else:
                                    cp('dve', dst, srcv, [pk], [hTk])
                        for fc in range(4):
                            pg_, pgk_ = psr.next()
                            for kc in range(8):
                                mm(pg_[:, 0:256], wg[:, kc * 512 + fc:(kc + 1) * 512:4], hT[:, kc, :], kc == 0, kc == 7, [wgk, hTk], [pgk_], inc=(kc == 7))
                            pu_, puk_ = psr.next()
                            for kc in range(8):
                                mm(pu_[:, 0:256], wu[:, kc * 512 + fc:(kc + 1) * 512:4], hT[:, kc, :], kc == 0, kc == 7, [wuk, hTk], [puk_], inc=(kc == 7))
                            s_, sk_ = sg.next()
                            act(s_[:], pg_[:, 0:256], AF.Silu, [pgk_], [sk_])
                            tt('dve', actT[:, fc, :], pu_[:, 0:256], s_[:], ALU.mult, [puk_, sk_], [('actT', fc)])
                        for sub in range(2):
                            yo, yok = yor.next()
                            for nh in range(2):
                                pd, pdk = psr.next()
                                for fc in range(4):
                                    mm(pd[:], actT[:, fc, 128 * sub:128 * sub + 128], wd[:, fc * D + 512 * nh:fc * D + 512 * nh + 512], fc == 0, fc == 3,
                                       [('actT', fc), wdk], [pdk], inc=(fc == 3))
                                if nh == 0:
                                    act(yo[:, 0:512], pd[:], AF.Copy, [pdk], [yok])
                                else:
                                    cp('dve', yo[:, 512:1024], pd[:], [pdk], [yok])
                            r0 = b_ * MBLK + 128 * sub
                            deferred.append(((Ys_d[r0:r0 + 128, :], yo[:], [yok], [('Ys', b_, sub)]), dict(q='sp')))
                    for d_ in deferred:
                        S.dma(*d_[0], **d_[1])
                    S.barrier()
                y1r = Ring([psb("y1_%d" % i, (128, D)) for i in range(2)], "y1")
                y2r = Ring([psb("y2_%d" % i, (128, D)) for i in range(2)], "y2")
                for ti in range(NT):
                    y1, y1k = y1r.next()
                    y2, y2k = y2r.next()
                    S.idma(y1[:, :], Ys_d[:, :], None, bass.IndirectOffsetOnAxis(ap=idx12[:, 0, ti:ti + 1], axis=0), ['idx12'], [y1k])
                    S.idma(y2[:, :], Ys_d[:, :], None, bass.IndirectOffsetOnAxis(ap=idx12[:, 1, ti:ti + 1], axis=0), ['idx12'], [y2k])
                    xt, xk = xr.next()
                    S.dma(xt[:], out[128 * ti:128 * ti + 128, :], [('x', ti)], [xk], q='sp')
                    ts('dve', y1[:], y1[:], wn[:, ti, 0:1], ALU.mult, [y1k, 'wn'], [y1k])
                    stt(y1[:], y2[:], wn[:, ti, 1:2], y1[:], ALU.mult, ALU.add, [y2k, 'wn', y1k], [y1k])
                    tt('pool', y1[:], y1[:], g2r[:], ALU.mult, [y1k, 'g2r'], [y1k])
                    tt('pool', xt[:], xt[:], y1[:], ALU.add, [xk, y1k], [xk])
                    S.dma(out[128 * ti:128 * ti + 128, :], xt[:], [xk], [('x', ti)], q='sp')
                S.barrier()

        with ExitStack() as ph:
            modrow = ph.enter_context(nc.sbuf_tensor("modrow", [128, 6 * D], F32))
            rowtmp = ph.enter_context(nc.sbuf_tensor("rowtmp", [128, D], F32))
            wr0 = Ring([ph.enter_context(nc.sbuf_tensor("wsl0_%d" % i, [128, 8, 512], F32)) for i in range(2)], "wsl0")
            for l in range(L):
                S.dma(modrow[:], ada_b[l].partition_broadcast(128), ['modrow'], ['modrow'])
                for oc in range(12):
                    wt, wk = load_w(wr0, ada_w[l], oc * 512, 512)
                    ps, pk = psr.next()
                    for kc in range(8):
                        mm(ps[:], cbc[:, kc, :], wt[:, kc, :], kc == 0, kc == 7, ['cbc', wk], [pk], inc=(kc == 7))
                    tt('dve', modrow[:, oc * 512:(oc + 1) * 512], ps[:], modrow[:, oc * 512:(oc + 1) * 512], ALU.add,
                       [pk, 'modrow'], ['modrow'])
                for (o_, ng) in ((1, norm1_g), (4, norm2_g)):
                    S.dma(rowtmp[:], ng[l].partition_broadcast(128), ['rowtmp'], ['rowtmp'])
                    stt(modrow[:, o_ * D:(o_ + 1) * D], modrow[:, o_ * D:(o_ + 1) * D], 1.0, rowtmp[:], ALU.add, ALU.mult,
                        ['modrow', 'rowtmp'], ['modrow'])
                S.dma(mod_d[l:l + 1, :], modrow[0:1, :], ['modrow'], [('mod', l)], q='pool')
            S.barrier()

        for l in range(L if 'stop0' not in dbg else 0):
            src_x = x_in if l == 0 else out
            with ExitStack() as phm:
                A1t = phm.enter_context(nc.sbuf_tensor("A1t_%d" % l, [128, D], F32))
                sh1t = phm.enter_context(nc.sbuf_tensor("sh1t_%d" % l, [128, D], F32))
                S.dma(sh1t[:], mod_d[l, 0:D].partition_broadcast(128), (), ['modrow'])
                S.dma(A1t[:], mod_d[l, D:2 * D].partition_broadcast(128), (), ['modrow'])
                A1row, sh1row = A1t[:], sh1t[:]
                phase_A(l, src_x, A1row, sh1row)
            if 'stopA' in dbg:
                break
            phase_B(l)
            if 'stopB' in dbg:
                break
            phase_C(l)
            if 'stopC' in dbg:
                break
            phase_D(l)
            if 'stopD' in dbg:
                break
            phase_E(l, src_x)
            if 'stopE' in dbg:
                break
            phase_F(l)
        S.barrier()
    return nc


def _host_tables(rel_bias, SEQ):
    FDW = NEGPAD + SEQ
    d = np.arange(SEQ)
    bk = rel_bucket_np(d)
    g = np.asarray(rel_bias, np.float32)[bk].T
    fdg = np.full((NH, FDW), NEGM, np.float32)
    fdg[:, NEGPAD:] = g
    fdw = np.full((NH, FDW), NEGM, np.float32)
    fdw[:, NEGPAD:NEGPAD + 512] = g[:, :512]
    return fdg, fdw


_CACHE = {}


def kernel(**inputs):
    x = np.asarray(inputs["x"], np.float32)
    B, SEQ, _ = x.shape
    L = int(np.asarray(inputs["ada_w"]).shape[0])
    key = (SEQ, L)
    if key not in _CACHE:
        _CACHE[key] = build_program(SEQ, L)
    nc = _CACHE[key]
    fdg, fdw = _host_tables(inputs["rel_bias"], SEQ)
    names = ["router_w", "router_b", "ada_w", "ada_b", "norm1_g", "norm2_g", "w_in", "qk_norm_g", "cmp_pos", "cmp_w1",
             "cmp_w2", "dn_conv_w", "dn_a_log", "dn_dt_bias", "dn_norm_g", "w_branch_a", "w_branch_b", "w_out",
             "moe_w_gate", "moe_w_up", "moe_w_down"]
    shared = {n: np.ascontiguousarray(np.asarray(inputs[n], np.float32)) for n in names}
    shared["fdg"] = fdg
    shared["fdw"] = fdw
    c = np.asarray(inputs["c"], np.float32)
    in_maps = []
    for b in range(B):
        m = dict(shared)
        m["x"] = np.ascontiguousarray(x[b])
        m["cT"] = np.ascontiguousarray(c[b].reshape(8, 128).T)
        in_maps.append(m)
    res = run_bass_kernel_spmd(nc, in_maps, core_ids=list(range(B)))
    return np.stack([np.asarray(r["out"], np.float32) for r in res.results], axis=0)
```

```python
import math
from contextlib import ExitStack
import numpy as np
import concourse.bass as bass
import concourse.mybir as mybir
from concourse.bass_utils import run_bass_kernel_spmd

F32 = mybir.dt.float32
I32 = mybir.dt.int32
AF = mybir.ActivationFunctionType
ALU = mybir.AluOpType
AX = mybir.AxisListType

D = 1024
HD = 64
NH = 8
DIN = 5416
NEGM = -30000.0
EPS = 1e-6
U0 = 384
OFFMAX = 1024
WGEN = U0 + OFFMAX + 512
WWIN = U0 + 512 + 512
NEGPAD = 1024


class Sched:
    def __init__(self, nc, es, ndma=14):
        self.nc = nc
        self.eng = {'pe': nc.tensor, 'act': nc.scalar, 'dve': nc.vector, 'pool': nc.gpsimd, 'sp': nc.sync}
        self.sem = {k: es.enter_context(nc.semaphore('s_' + k)) for k in self.eng}
        self.cnt = {k: 0 for k in self.eng}
        self.dsem = [es.enter_context(nc.semaphore('d%d' % i)) for i in range(ndma)]
        self.dcnt = [0] * ndma
        self.dnext = 0
        self.seen = {k: {} for k in self.eng}
        self.res = {}
        self.nops = 0

    def _deps(self, r, w):
        deps = {}

        def add(t):
            if t is not None and deps.get(t[0], 0) < t[1]:
                deps[t[0]] = t[1]
        for k in r:
            st = self.res.get(k)
            if st:
                add(st[0])
        for k in w:
            st = self.res.get(k)
            if st:
                add(st[0])
                for s, v in st[1].items():
                    add((s, v))
        return deps

    def _wait(self, e, deps):
        for s, v in deps.items():
            if s == 'pe' and e == 'pe':
                continue
            if self.seen[e].get(s, 0) < v:
                sem = self.sem[s] if isinstance(s, str) else self.dsem[s]
                self.eng[e].wait_ge(sem, v)
                self.seen[e][s] = v

    def _mark(self, tag, r, w):
        for k in r:
            st = self.res.setdefault(k, [None, {}])
            if st[1].get(tag[0], 0) < tag[1]:
                st[1][tag[0]] = tag[1]
        for k in w:
            self.res[k] = [tag, {}]

    def op(self, e, emit, r=(), w=(), inc=True):
        self._wait(e, self._deps(r, w))
        inst = emit(self.eng[e])
        self.nops += 1
        if inc:
            self.cnt[e] += 1
            inst.then_inc(self.sem[e], 1)
            tag = (e, self.cnt[e])
        else:
            tag = (e, self.cnt[e] + 1)
        self._mark(tag, r, w)

    def dma(self, out, in_, r=(), w=(), q='sp'):
        i = self.dnext
        self.dnext = (i + 1) % len(self.dsem)
        deps = self._deps(r, w)
        if self.dcnt[i]:
            deps[i] = max(deps.get(i, 0), self.dcnt[i])
        self._wait(q, deps)
        self.dcnt[i] += 16
        self.eng[q].dma_start(out=out, in_=in_).then_inc(self.dsem[i], 16)
        self.nops += 1
        self._mark((i, self.dcnt[i]), r, w)

    def idma(self, out, in_, out_off, in_off, r=(), w=()):
        i = self.dnext
        self.dnext = (i + 1) % len(self.dsem)
        deps = self._deps(r, w)
        if self.dcnt[i]:
            deps[i] = max(deps.get(i, 0), self.dcnt[i])
        self._wait('pool', deps)
        self.dcnt[i] += 16
        self.eng['pool'].indirect_dma_start(out=out, out_offset=out_off, in_=in_, in_offset=in_off).then_inc(self.dsem[i], 16)
        self.nops += 1
        self._mark((i, self.dcnt[i]), r, w)

    def barrier(self):
        deps = {s: c for s, c in self.cnt.items() if c}
        for i, v in enumerate(self.dcnt):
            if v:
                deps[i] = v
        for e in self.eng:
            d = dict(deps)
            self._wait(e, d)
        self.res = {}


class Ring:
    def __init__(self, tiles, name):
        self.tiles = tiles
        self.name = name
        self.i = 0

    def next(self):
        k = self.i % len(self.tiles)
        self.i += 1
        return self.tiles[k], (self.name, k)


def rel_bucket_np(dist):
    exact = 16
    dist = np.maximum(dist, 0)
    far = np.maximum(dist, exact).astype(np.float32)
    large = exact + (np.log(far / np.float32(exact)) / np.float32(math.log(1024 / exact)) * np.float32(32 - exact)).astype(np.int32)
    return np.where(dist < exact, dist, np.minimum(large, 31))


def build_program(SEQ, DEPTH, dbg=()):
    NT = SEQ // 128
    QT = SEQ // 512
    NCH = SEQ // 64
    NCMP = SEQ // 16 - 1
    NBLK = SEQ // 64
    NCT = (NCMP + 127) // 128
    FDW = NEGPAD + SEQ
    JB = NBLK
    nc = bass.Bass("TRN2", target_bir_lowering=False)

    def din(name, shape):
        return nc.dram_tensor(name, list(shape), F32, kind="ExternalInput").ap()

    def dscr(name, shape, kind="Internal"):
        if name in dbg:
            kind = "ExternalOutput"
        return nc.dram_tensor(name, list(shape), F32, kind=kind).ap()

    L = DEPTH
    x_in = din("x", (SEQ, D))
    cT_in = din("cT", (128, 8))
    fdg_in = din("fdg", (NH, FDW))
    fdw_in = din("fdw", (NH, FDW))
    router_w = din("router_w", (D, 16))
    router_b = din("router_b", (16,))
    ada_w = din("ada_w", (L, D, 6 * D))
    ada_b = din("ada_b", (L, 6 * D))
    norm1_g = din("norm1_g", (L, D))
    norm2_g = din("norm2_g", (L, D))
    w_in = din("w_in", (L, D, DIN))
    qk_norm_g = din("qk_norm_g", (L, 4, HD))
    cmp_pos = din("cmp_pos", (L, 2, 32, HD))
    cmp_w1 = din("cmp_w1", (L, 2, 2048, 256))
    cmp_w2 = din("cmp_w2", (L, 2, 256, HD))
    dn_conv_w = din("dn_conv_w", (L, 4, 1536))
    dn_a_log = din("dn_a_log", (L, 8))
    dn_dt_bias = din("dn_dt_bias", (L, 8))
    dn_norm_g = din("dn_norm_g", (L, HD))
    w_br_a = din("w_branch_a", (L, 512, D))
    w_br_b = din("w_branch_b", (L, 512, D))
    w_out = din("w_out", (L, D, D))
    moe_wg = din("moe_w_gate", (L, 16, D, 512))
    moe_wu = din("moe_w_up", (L, 16, D, 512))
    moe_wd = din("moe_w_down", (L, 16, 512, D))
    out = nc.dram_tensor("out", [SEQ, D], F32, kind="ExternalOutput").ap()

    qT_d = dscr("qT_d", (4, 128, SEQ))
    kcT_d = dscr("kcT_d", (2, 128, SEQ))
    kslcT_d = dscr("kslcT_d", (128, SEQ))
    kwinT_d = dscr("kwinT_d", (128, SEQ))
    vslc_d = dscr("vslc_d", (SEQ, 128))
    vwin_d = dscr("vwin_d", (SEQ, 128))
    gate_d = dscr("gate_d", (SEQ, 24))
    dnraw_d = dscr("dnraw_d", (12, 128, SEQ))
    dnc_d = dscr("dnc_d", (12, 128, SEQ))
    bg_d = dscr("bg_d", (SEQ, 16))
    zs_d = dscr("zs_d", (SEQ, 512))
    mergeT_d = dscr("mergeT_d", (16, 128, SEQ))
    obr_d = dscr("obr_d", (3, SEQ, 512))
    ybT_d = dscr("ybT_d", (4, 128, SEQ))
    bct_d = dscr("bct_d", (NH, NCT * 128, SEQ))
    bgen_d = dscr("bgen_d", (128, NH, WGEN))
    bwin_d = dscr("bwin_d", (128, NH, WWIN))
    selbT_d = dscr("selbT_d", (128, SEQ))
    MBLK = 256
    NBK = (2 * SEQ + 16 * MBLK) // MBLK
    h2_d = dscr("h2_d", (SEQ, D))
    Xs_d = dscr("Xs_d", (NBK * MBLK, D))
    Ys_d = dscr("Ys_d", (NBK * MBLK, D))
    mod_d = dscr("mod_d", (L, 6 * D))

    es = ExitStack()
    with es:
        S = Sched(nc, es)

        def sb(name, shape):
            return es.enter_context(nc.sbuf_tensor(name, list(shape), F32))

        PS = [es.enter_context(nc.psum_tensor("ps%d" % i, [128, 512], F32)) for i in range(8)]
        psr = Ring(PS, "ps")

        def tt(e, o, a, b, op, r, w):
            S.op(e, lambda g: g.tensor_tensor(out=o, in0=a, in1=b, op=op), r, w)

        def ts(e, o, a, s1, op0, r, w, s2=None, op1=None):
            if op1 is None:
                S.op(e, lambda g: g.tensor_scalar(out=o, in0=a, scalar1=s1, scalar2=None, op0=op0), r, w)
            else:
                S.op(e, lambda g: g.tensor_scalar(out=o, in0=a, scalar1=s1, scalar2=s2, op0=op0, op1=op1), r, w)

        def stt(o, a, sc, b, op0, op1, r, w):
            S.op('dve', lambda g: g.scalar_tensor_tensor(out=o, in0=a, scalar=sc, in1=b, op0=op0, op1=op1), r, w)

        def act(o, a, f, r, w, bias=None, scale=1.0, accum=None):
            kw = {}
            if bias is not None:
                kw['bias'] = bias
            if accum is not None:
                kw['accum_out'] = accum
            S.op('act', lambda g: g.activation(out=o, in_=a, func=f, scale=scale, **kw), r, w)

        def mm(o, lT, rh, st, sp, r, w, inc=True):
            S.op('pe', lambda g: g.matmul(o, lT, rh, start=st, stop=sp), r, w, inc=inc)

        def tr(o, a, idn, r, w, inc=True):
            S.op('pe', lambda g: g.transpose(o, a, idn), r, w, inc=inc)

        def cp(e, o, a, r, w):
            S.op(e, lambda g: g.tensor_copy(o, a), r, w)

        def recip(o, a, r, w):
            S.op('dve', lambda g: g.reciprocal(o, a), r, w)

        def memset(e, o, v, w):
            S.op(e, lambda g: g.memset(o, v), (), w)

        def asel(o, a, pattern, base, cm, fill, r, w, op=ALU.is_ge):
            S.op('pool', lambda g: g.affine_select(out=o, in_=a, pattern=pattern, compare_op=op, fill=fill,
                                                   base=base, channel_multiplier=cm), r, w)

        ident = sb("ident", (128, 128))
        ones = sb("ones", (128, 128))
        bdones = sb("bdones", (128, 128))
        UT = sb("UT", (128, 64))
        maskU = sb("maskU", (64, 64))
        maskL = sb("maskL", (64, 64))
        nsU = sb("nsU", (64, 64))
        nsL = sb("nsL", (64, 64))
        ovl = sb("ovl", (128, NCT, JB))
        memset('pool', ident[:], 0.0, ['ident'])
        asel(ident[:], ident[:], [[-1, 128]], 0, 1, 1.0, ['ident'], ['ident'], op=ALU.not_equal)
        memset('pool', ones[:], 1.0, ['ones'])
        memset('pool', bdones[:], 0.0, ['bdones'])
        memset('pool', bdones[0:64, 0:64], 1.0, ['bdones'])
        memset('pool', bdones[64:128, 64:128], 1.0, ['bdones'])
        for h0 in (0, 64):
            memset('pool', UT[h0:h0 + 64, :], 1.0, ['UT'])
            asel(UT[h0:h0 + 64, :], UT[h0:h0 + 64, :], [[1, 64]], 0, -1, 0.0, ['UT'], ['UT'])
        memset('pool', maskU[:], 0.0, ['maskU'])
        asel(maskU[:], maskU[:], [[1, 64]], 0, -1, NEGM, ['maskU'], ['maskU'])
        memset('pool', maskL[:], 0.0, ['maskL'])
        asel(maskL[:], maskL[:], [[-1, 64]], 0, 1, NEGM, ['maskL'], ['maskL'])
        memset('pool', nsU[:], -1.0, ['nsU'])
        asel(nsU[:], nsU[:], [[1, 64]], -1, -1, 0.0, ['nsU'], ['nsU'])
        memset('pool', nsL[:], -1.0, ['nsL'])
        asel(nsL[:], nsL[:], [[-1, 64]], -1, 1, 0.0, ['nsL'], ['nsL'])
        ovt = sb("ovt", (128, NCT, JB))
        memset('pool', ovl[:], 0.0, ['ovl'])
        for m in (0, 1):
            memset('pool', ovt[:], 1.0, ['ovt'])
            asel(ovt[:], ovt[:], [[128, NCT], [-4, JB]], m, 1, 0.0, ['ovt'], ['ovt'])
            asel(ovt[:], ovt[:], [[-128, NCT], [4, JB]], 3 - m, -1, 0.0, ['ovt'], ['ovt'])
            tt('pool', ovl[:], ovl[:], ovt[:], ALU.add, ['ovl', 'ovt'], ['ovl'])

        with nc.allow_non_contiguous_dma(reason="table build"):
            for p in range(128):
                o0 = NEGPAD - U0 - p
                S.dma(bgen_d[p, :, :], fdg_in[:, o0:o0 + WGEN], (), [('bgen', p)], q='sp')
                S.dma(bwin_d[p, :, :], fdw_in[:, o0:o0 + WWIN], (), [('bwin', p)], q='pool')
            for n in range(NCT * 128):
                o0 = NEGPAD - (16 * n + 31)
                if n >= NCMP:
                    o0 = 0
                q = 'sp' if n % 2 == 0 else 'pool'
                if n >= NCMP:
                    S.dma(bct_d[:, n, 0:NEGPAD], fdg_in[:, 0:NEGPAD], (), [('bct', n)], q=q)
                    for c0 in range(NEGPAD, SEQ, NEGPAD):
                        S.dma(bct_d[:, n, c0:c0 + NEGPAD], fdg_in[:, 0:NEGPAD], (), [('bct', n, c0)], q=q)
                elif o0 >= 0:
                    S.dma(bct_d[:, n, :], fdg_in[:, o0:o0 + SEQ], (), [('bct', n)], q=q)
                else:
                    nn = -o0
                    for c0 in range(0, nn, NEGPAD):
                        cw = min(NEGPAD, nn - c0)
                        S.dma(bct_d[:, n, c0:c0 + cw], fdg_in[:, 0:cw], (), [('bct', n, c0)], q=q)
                    S.dma(bct_d[:, n, nn:SEQ], fdg_in[:, 0:SEQ - nn], (), [('bct', n)], q=q)
        S.barrier()

        cact = sb("cact", (128, 8))
        S.dma(cact[:], cT_in[:, :], (), ['cact'])
        act(cact[:], cact[:], AF.Silu, ['cact'], ['cact'])
        cbc = sb("cbc", (128, 8, 128))
        for kc in range(8):
            ts('dve', cbc[:, kc, :], ones[:], cact[:, kc:kc + 1], ALU.mult, ['ones', 'cact'], ['cbc'])
        rtrb = sb("rtrb", (128, 16))
        S.dma(rtrb[:], router_b.partition_broadcast(128), (), ['rtrb'])
        rtrw = sb("rtrw", (128, 8, 16))
        with nc.allow_non_contiguous_dma(reason="router w"):
            S.dma(rtrw[:], router_w.rearrange("(kc p) e -> p kc e", p=128), (), ['rtrw'])

        def load_w(ring, src2d, c0, ncols, kch=8):
            t, k = ring.next()
            with nc.allow_non_contiguous_dma(reason="weight slab"):
                S.dma(t[:, 0:kch, 0:ncols], src2d.rearrange("(kc p) n -> p kc n", p=128)[:, :, c0:c0 + ncols], (), [k])
            return t, k

        def norm_tile(src, t0, Arow, shrow, xr, hr, sm):
            xt, xk = xr.next()
            S.dma(xt[:], src[t0:t0 + 128, :], [('x', t0 // 128)], [xk])
            ht, hk = hr.next()
            s, sk = sm.next()
            act(ht[:], xt[:], AF.Square, [xk], [hk, sk], accum=s[:, 0:1])
            act(s[:, 1:2], s[:, 0:1], AF.Sqrt, [sk], [sk], bias=EPS, scale=1.0 / D)
            recip(s[:, 2:3], s[:, 1:2], [sk], [sk])
            stt(ht[:], xt[:], s[:, 2:3], Arow, ALU.mult, ALU.mult, [xk, sk, 'modrow'], [hk])
            tt('pool', ht[:], ht[:], shrow, ALU.add, [hk, 'modrow'], [hk])
            return ht, hk, xt, xk

        def transpose_to(ht, hk, hT, hTk, sub, nkc=8):
            for k0 in range(0, nkc, 4):
                ps, pk = psr.next()
                for kc in range(k0, k0 + 4):
                    tr(ps[:, (kc - k0) * 128:(kc - k0 + 1) * 128], ht[:, kc * 128:(kc + 1) * 128], ident[:],
                       [hk, 'ident'], [pk], inc=(kc == k0 + 3))
                e = 'act' if (k0 // 4) % 2 == 0 else 'dve'
                dst = hT[:, k0:k0 + 4, sub * 128:(sub + 1) * 128]
                srcv = ps[:].rearrange("p (a b) -> p a b", a=4)
                if e == 'act':
                    act(dst, srcv, AF.Copy, [pk], [hTk])
                else:
                    cp('dve', dst, srcv, [pk], [hTk])

        def phase_A(l, src_x, A1row, sh1row):
            with ExitStack() as ph:
                def psb(name, shape):
                    return ph.enter_context(nc.sbuf_tensor("%s_A%d" % (name, l), list(shape), F32))
                wr = Ring([psb("wsl%d" % i, (128, 8, 512)) for i in range(3)], "wsl")
                xr = Ring([psb("xa%d" % i, (128, D)) for i in range(2)], "xa")
                hr = Ring([psb("ha%d" % i, (128, D)) for i in range(2)], "ha")
                hTr = Ring([psb("hT%d" % i, (128, 8, 512)) for i in range(2)], "hT")
                st = Ring([psb("st%d" % i, (128, 512)) for i in range(4)], "st")
                sq = Ring([psb("sq%d" % i, (128, 512)) for i in range(2)], "sq")
                sm = Ring([psb("sm%d" % i, (128, 8)) for i in range(4)], "sm")
                gains = psb("gains", (128, 4))
                dtb = psb("dtb", (128, 8))
                nea = psb("nea", (128, 8))
                with nc.allow_non_contiguous_dma(reason="small"):
                    for h0 in (0, 64):
                        S.dma(gains[h0:h0 + 64, :], qk_norm_g[l].rearrange("i d -> d i"), (), ['gains'])
                ts('dve', gains[:, 0:1], gains[:, 0:1], HD ** -0.5, ALU.mult, ['gains'], ['gains'])
                S.dma(dtb[:], dn_dt_bias[l].partition_broadcast(128), (), ['dtb'])
                S.dma(nea[:], dn_a_log[l].partition_broadcast(128), (), ['nea'])
                act(nea[:], nea[:], AF.Exp, ['nea'], ['nea'])
                ts('dve', nea[:], nea[:], -1.0, ALU.mult, ['nea'], ['nea'])

                def rms64_store(ps, pk, gcol, dst, dkey):
                    q1, qk1 = sq.next()
                    act(q1[:], ps[:], AF.Square, [pk], [qk1])
                    p2, pk2 = psr.next()
                    mm(p2[:], bdones[:], q1[:], True, True, ['bdones', qk1], [pk2])
                    act(q1[:], p2[:], AF.Sqrt, [pk2], [qk1], bias=EPS, scale=1.0 / 64)
                    recip(q1[:], q1[:], [qk1], [qk1])
                    o, ok = st.next()
                    stt(o[:], ps[:], gcol, q1[:], ALU.mult, ALU.mult, [pk, qk1, 'gains'], [ok])
                    S.dma(dst, o[:], [ok], [dkey], q='pool')

                def fm_store(ps, pk, dst, dkey, func=AF.Copy):
                    o, ok = st.next()
                    act(o[:], ps[:], func, [pk], [ok])
                    S.dma(dst, o[:], [ok], [dkey], q='pool')

                for j in range(QT):
                    t0 = 512 * j
                    hT, hTk = hTr.next()
                    for sub in range(4):
                        ht, hk, _, _ = norm_tile(src_x, t0 + 128 * sub, A1row, sh1row, xr, hr, sm)
                        transpose_to(ht, hk, hT, hTk, sub)

                    def fm(wt, wk, lsel):
                        ps, pk = psr.next()
                        for kc in range(8):
                            mm(ps[:], lsel(kc), hT[:, kc, :], kc == 0, kc == 7, [wk, hTk], [pk], inc=(kc == 7))
                        return ps, pk

                    def tm(wt, wk, c0, ncols, sub):
                        ps, pk = psr.next()
                        for kc in range(8):
                            mm(ps[:, 0:ncols], hT[:, kc, sub * 128:(sub + 1) * 128], wt[:, kc, c0:c0 + ncols],
                               kc == 0, kc == 7, [wk, hTk], [pk], inc=(kc == 7))
                        return ps, pk
                    cs = slice(t0, t0 + 512)
                    wt, wk = wr.next()
                    with nc.allow_non_contiguous_dma(reason="q slab"):
                        for a in range(2):
                            for c in range(4):
                                S.dma(wt[:, :, c * 128 + a * 64:c * 128 + a * 64 + 64],
                                      w_in[l].rearrange("(kc p) n -> p kc n", p=128)[:, :, a * 256 + c * 64:a * 256 + c * 64 + 64], (), [wk])
                    for c in range(4):
                        ps, pk = fm(wt, wk, lambda kc, c=c: wt[:, kc, c * 128:(c + 1) * 128])
                        rms64_store(ps, pk, gains[:, 0:1], qT_d[c, :, cs], ('qT', c, j))
                    wt, wk = load_w(wr, w_in[l], 512, 512)
                    for c in range(3):
                        ps, pk = fm(wt, wk, lambda kc, c=c: wt[:, kc, c * 128:(c + 1) * 128])
                        if c < 2:
                            fm_store(ps, pk, kcT_d[c, :, cs], ('kcT', c, j))
                        else:
                            rms64_store(ps, pk, gains[:, 2:3], kslcT_d[:, cs], ('kslcT', j))
                    for sub in range(4):
                        ps, pk = tm(wt, wk, 384, 128, sub)
                        o, ok = st.next()
                        act(o[:, 0:128], ps[:, 0:128], AF.Copy, [pk], [ok])
                        S.dma(vslc_d[t0 + 128 * sub:t0 + 128 * sub + 128, :], o[:, 0:128], [ok], [('vslc', j, sub)], q='pool')
                    wt, wk = load_w(wr, w_in[l], 1024, 280)
                    ps, pk = fm(wt, wk, lambda kc: wt[:, kc, 0:128])
                    rms64_store(ps, pk, gains[:, 3:4], kwinT_d[:, cs], ('kwinT', j))
                    for sub in range(4):
                        r0 = t0 + 128 * sub
                        ps, pk = tm(wt, wk, 128, 152, sub)
                        o, ok = st.next()
                        act(o[:, 0:128], ps[:, 0:128], AF.Copy, [pk], [ok])
                        act(o[:, 128:152], ps[:, 128:152], AF.Sigmoid, [pk], [ok])
                        S.dma(vwin_d[r0:r0 + 128, :], o[:, 0:128], [ok], [('vwin', j, sub)], q='pool')
                        S.dma(gate_d[r0:r0 + 128, :], o[:, 128:152], [ok], [('gate', j, sub)], q='pool')
                    for i in range(3):
                        wt, wk = load_w(wr, w_in[l], 1304 + 512 * i, 512)
                        for c in range(4):
                            ps, pk = fm(wt, wk, lambda kc, c=c: wt[:, kc, c * 128:(c + 1) * 128])
                            fm_store(ps, pk, dnraw_d[4 * i + c, :, cs], ('dnraw', 4 * i + c, j))
                    wt, wk = load_w(wr, w_in[l], 2840, 16)
                    for sub in range(4):
                        r0 = t0 + 128 * sub
                        ps, pk = tm(wt, wk, 0, 16, sub)
                        o, ok = st.next()
                        act(o[:, 0:8], ps[:, 0:8], AF.Sigmoid, [pk], [ok])
                        tt('dve', o[:, 8:16], ps[:, 8:16], dtb[:], ALU.add, [pk, 'dtb'], [ok])
                        act(o[:, 8:16], o[:, 8:16], AF.Exp, [ok], [ok])
                        act(o[:, 8:16], o[:, 8:16], AF.Ln, [ok], [ok], bias=1.0)
                        tt('dve', o[:, 8:16], o[:, 8:16], nea[:], ALU.mult, [ok, 'nea'], [ok])
                        S.dma(bg_d[r0:r0 + 128, :], o[:, 0:16], [ok], [('bg', j, sub)], q='pool')
                    wt, wk = load_w(wr, w_in[l], 2856, 512)
                    for sub in range(4):
                        r0 = t0 + 128 * sub
                        ps, pk = tm(wt, wk, 0, 512, sub)
                        fm_store(ps, pk, zs_d[r0:r0 + 128, :], ('zs', j, sub), func=AF.Silu)
                    for i in range(4):
                        wt, wk = load_w(wr, w_in[l], 3368 + 512 * i, 512)
                        for c in range(4):
                            ps, pk = fm(wt, wk, lambda kc, c=c: wt[:, kc, c * 128:(c + 1) * 128])
                            fm_store(ps, pk, mergeT_d[4 * i + c, :, cs], ('mergeT', 4 * i + c, j), func=AF.Sigmoid)
                S.barrier()
        def phase_B(l):
            with ExitStack() as ph:
                def psb(name, shape):
                    return ph.enter_context(nc.sbuf_tensor("%s_B%d" % (name, l), list(shape), F32))
                kst = [psb("kst%d" % g, (128, NCT * 128)) for g in range(2)]
                for g in range(2):
                    memset('pool', kst[g][:], 0.0, ['kcmpT'])
                vcmp = psb("vcmp", (128, NCT, 2, 65 + JB))
                memset('pool', vcmp[:], 0.0, ['vcmp'])
                memset('pool', vcmp[:, :, :, 64:65], 1.0, ['vcmp'])
                for g in range(2):
                    cp('pool', vcmp[:, :, g, 65:65 + JB], ovl[:], ['ovl', 'vcmp'], ['vcmp'])
                gains = psb("gains", (128, 4))
                with nc.allow_non_contiguous_dma(reason="small"):
                    for h0 in (0, 64):
                        S.dma(gains[h0:h0 + 64, :], qk_norm_g[l].rearrange("i d -> d i"), (), ['gains'])
                with ExitStack() as ph2:
                    def psb2(name, shape):
                        return ph2.enter_context(nc.sbuf_tensor("%s_B2%d" % (name, l), list(shape), F32))
                    kcT = psb2("kcT", (128, SEQ))
                    w1 = psb2("w1", (128, 32, 256))
                    posT = psb2("posT", (128, 32))
                    w2 = psb2("w2", (128, 2, 64))
                    pbias = psb2("pbias", (128, 2))
                    gx = psb2("gx", (128, 2, 2, 256))
                    gt = psb2("gt", (128, 256))
                    sqc = psb2("sqc", (128, 256))
                    for kvi in range(2):
                        S.dma(kcT[:], kcT_d[kvi, :, :], [('kcT', kvi, j) for j in range(QT)], ['kcT'])
                        with nc.allow_non_contiguous_dma(reason="cmp weights"):
                            for h0 in (0, 64):
                                for j0 in range(0, 32, 8):
                                    S.dma(w1[h0:h0 + 64, j0:j0 + 8, :], cmp_w1[l, kvi].rearrange("(j d) f -> d j f", d=64)[:, j0:j0 + 8, :], (), ['w1'])
                                for j0 in range(0, 32, 8):
                                    S.dma(posT[h0:h0 + 64, j0:j0 + 8], cmp_pos[l, kvi].rearrange("j d -> d j")[:, j0:j0 + 8], (), ['posT'])
                            S.dma(w2[:], cmp_w2[l, kvi].rearrange("(fc p) d -> p fc d", p=128), (), ['w2'])
                        for fc in range(2):
                            ps, pk = psr.next()
                            for j in range(32):
                                mm(ps[:, 0:1], w1[0:64, j, fc * 128:(fc + 1) * 128], posT[0:64, j:j + 1], j == 0, j == 31,
                                   ['w1', 'posT'], [pk], inc=(j == 31))
                            cp('dve', pbias[:, fc:fc + 1], ps[:, 0:1], [pk], ['pbias'])
                        for g in range(2):
                            hs = slice(64 * g, 64 * g + 64)
                            for fc in range(2):
                                ps, pk = psr.next()
                                for j in range(32):
                                    mm(ps[:, 0:NCMP], w1[hs, j, fc * 128:(fc + 1) * 128], kcT[hs, j:j + 16 * (NCMP - 1) + 1:16],
                                       j == 0, j == 31, ['w1', 'kcT'], [pk], inc=(j == 31))
                                xs = gx[:, g, fc, 0:NCMP]
                                ts('dve', xs, ps[:, 0:NCMP], pbias[:, fc:fc + 1], ALU.add, [pk, 'pbias'], ['gx'])
                                tt('dve', gt[:, 0:NCMP], xs, xs, ALU.mult, ['gx'], ['gt'])
                                ts('dve', gt[:, 0:NCMP], gt[:, 0:NCMP], 0.044715, ALU.mult, ['gt'], ['gt'], s2=1.0, op1=ALU.add)
                                tt('dve', gt[:, 0:NCMP], gt[:, 0:NCMP], xs, ALU.mult, ['gt', 'gx'], ['gt'])
                                act(gt[:, 0:NCMP], gt[:, 0:NCMP], AF.Sigmoid, ['gt'], ['gt'], scale=1.5957691216057308)
                                tt('dve', xs, xs, gt[:, 0:NCMP], ALU.mult, ['gx', 'gt'], ['gx'])
                            if kvi == 0:
                                ps, pk = psr.next()
                                for fc in range(2):
                                    mm(ps[hs, 0:NCMP], w2[:, fc, :], gx[:, g, fc, 0:NCMP], fc == 0, fc == 1, ['w2', 'gx'], [pk], inc=(fc == 1))
                                act(sqc[hs, 0:NCMP], ps[hs, 0:NCMP], AF.Square, [pk], ['sqc'])
                                p2, pk2 = psr.next()
                                mm(p2[hs, 0:NCMP], ones[hs, 0:64], sqc[hs, 0:NCMP], True, True, ['ones', 'sqc'], [pk2])
                                act(sqc[hs, 0:NCMP], p2[hs, 0:NCMP], AF.Sqrt, [pk2], ['sqc'], bias=EPS, scale=1.0 / 64)
                                recip(sqc[hs, 0:NCMP], sqc[hs, 0:NCMP], ['sqc'], ['sqc'])
                                stt(kst[g][hs, 0:NCMP], ps[hs, 0:NCMP], gains[hs, 1:2], sqc[hs, 0:NCMP], ALU.mult, ALU.mult,
                                    [pk, 'sqc', 'gains'], ['kcmpT'])
                            else:
                                for nt in range(NCT):
                                    nn = min(128, NCMP - nt * 128)
                                    ps, pk = psr.next()
                                    for fc in range(2):
                                        mm(ps[0:nn, 0:64], gx[:, g, fc, nt * 128:nt * 128 + nn], w2[:, fc, :], fc == 0, fc == 1,
                                           ['w2', 'gx'], [pk], inc=(fc == 1))
                                    cp('dve', vcmp[0:nn, nt, g, 0:64], ps[0:nn, 0:64], [pk], ['vcmp'])
                    S.barrier()
                qr = Ring([psb("qTb%d" % i, (128, SEQ)) for i in range(2)], "qTb")
                btr = Ring([psb("bt%d" % i, (128, NCT, 512)) for i in range(3)], "bt")
                pcr = Ring([psb("pc%d" % i, (128, NCT, 512)) for i in range(3)], "pc")
                osr = Ring([psb("os%d" % i, (128, 64)) for i in range(4)], "os")
                rdr = Ring([psb("rd%d" % i, (128, 2)) for i in range(4)], "rd")
                gate_sb = psb("gate_sb", (128, NT, 24))
                impacc = psb("impacc", (128, NT, 2, JB))
                with nc.allow_non_contiguous_dma(reason="gate"):
                    for t0_ in range(0, NT, 8):
                        S.dma(gate_sb[:, t0_:t0_ + 8, :], gate_d.rearrange("(t p) c -> p t c", p=128)[:, t0_:t0_ + 8, :],
                              [('gate', j, s) for j in range(QT) for s in range(4)], ['gate_sb'])
                W = 65 + JB
                qcur = {}

                def stage1(c, half, jq):
                    if c not in qcur:
                        qT, qk = qr.next()
                        S.dma(qT[:], qT_d[c, :, :], [('qT', c, j) for j in range(QT)], [qk])
                        qcur.clear()
                        qcur[c] = (qT, qk)
                    qT, qk = qcur[c]
                    h = c + 4 * half
                    g = half
                    tq0 = 512 * jq
                    nts = [nt for nt in range(NCT) if 16 * 128 * nt + 31 <= tq0 + 511]
                    bt, bk = btr.next()
                    pc, pck = pcr.next()
                    for nt in nts:
                        S.dma(bt[:, nt, :], bct_d[h, nt * 128:(nt + 1) * 128, tq0:tq0 + 512], (), [bk])
                    for nt in nts:
                        ps, pk = psr.next()
                        mm(ps[:], kst[g][:, nt * 128:(nt + 1) * 128], qT[:, tq0:tq0 + 512], True, True, ['kcmpT', qk], [pk])
                        tt('dve', pc[:, nt, :], ps[:], bt[:, nt, :], ALU.add, [pk, bk], [pck])
                        act(pc[:, nt, :], pc[:, nt, :], AF.Exp, [pck], [pck])
                    return (c, h, g, jq, nts, pc, pck)

                def stage2(item):
                    c, h, g, jq, nts, pc, pck = item
                    for sub in range(4):
                        tsi = 4 * jq + sub
                        po, pok = psr.next()
                        for nt in nts:
                            mm(po[:, 0:W], pc[:, nt, sub * 128:(sub + 1) * 128], vcmp[:, nt, g, :], nt == nts[0], nt == nts[-1],
                               [pck, 'vcmp'], [pok], inc=(nt == nts[-1]))
                        rd, rk = rdr.next()
                        ts('dve', rd[:, 0:1], po[:, 64:65], 1e-30, ALU.add, [pok], [rk])
                        recip(rd[:, 1:2], rd[:, 0:1], [rk], [rk])
                        o, ok = osr.next()
                        ts('dve', o[:], po[:, 0:64], rd[:, 1:2], ALU.mult, [pok, rk, 'gate_sb'], [ok],
                           s2=gate_sb[:, tsi, 3 * h:3 * h + 1], op1=ALU.mult)
                        S.dma(obr_d[0, tsi * 128:(tsi + 1) * 128, 64 * h:64 * h + 64], o[:], [ok], [('obr', 0, h, tsi)], q='pool')
                        if c == 0:
                            ts('dve', impacc[:, tsi, g, :], po[:, 65:W], rd[:, 1:2], ALU.mult, [pok, rk], [('imp', tsi, g)])
                        else:
                            stt(impacc[:, tsi, g, :], po[:, 65:W], rd[:, 1:2], impacc[:, tsi, g, :], ALU.mult, ALU.add,
                                [pok, rk, ('imp', tsi, g)], [('imp', tsi, g)])

                prev_it = None
                for c in range(4):
                    for half in range(2):
                        for jq in range(QT):
                            it_ = stage1(c, half, jq)
                            if prev_it is not None:
                                stage2(prev_it)
                            prev_it = it_
                stage2(prev_it)
                selM = psb("selM", (128, NT, JB))
                selA = psb("selA", (128, NT, JB))
                memset('pool', selM[:], 1.0, ['selM'])
                asel(selM[:], selM[:], [[128, NT], [-64, JB]], -128, 1, 0.0, ['selM'], ['selM'])
                memset('pool', selM[:, :, 0:1], 0.0, ['selM'])
                memset('pool', selA[:], 0.0, ['selA'])
                asel(selA[:], selA[:], [[128, NT], [-64, JB]], -128, 1, 1e9, ['selA'], ['selA'])
                asel(selA[:], selA[:], [[128, NT], [-64, JB]], 0, 1, -1.0, ['selA'], ['selA'])
                memset('pool', selA[:, :, 0:1], 1e9, ['selA'])
                scr = Ring([psb("sc%d" % i, (128, 2, JB)) for i in range(2)], "sc")
                sc2r = Ring([psb("sd%d" % i, (128, JB)) for i in range(2)], "sd")
                m8r = Ring([psb("m8%d" % i, (128, 16)) for i in range(4)], "m8")
                sbr = Ring([psb("sbi%d" % i, (128, 128)) for i in range(2)], "sbi")
                sto = Ring([psb("sto%d" % i, (128, 128)) for i in range(2)], "sto")
                for tsi in range(NT):
                    sc, sck = scr.next()
                    sbi, sbk = sbr.next()
                    if JB < 64:
                        memset('pool', sbi[:], 0.0, [sbk])
                    for g in range(2):
                        tt('dve', sc[:, g, :], impacc[:, tsi, g, :], selM[:, tsi, :], ALU.mult, [('imp', tsi, g), 'selM'], [sck])
                        tt('dve', sc[:, g, :], sc[:, g, :], selA[:, tsi, :], ALU.add, [sck, 'selA'], [sck])
                        m8, mk = m8r.next()
                        sd, sdk = sc2r.next()
                        S.op('dve', lambda e, a=m8, b=sc, g=g: e.max(out=a[:, 0:8], in_=b[:, g, :]), [sck], [mk])
                        S.op('dve', lambda e, a=m8, b=sc, d=sd, g=g: e.match_replace(out=d[:], in_to_replace=a[:, 0:8], in_values=b[:, g, :],
                                                                                   imm_value=-3e38), [sck, mk], [sdk])
                        S.op('dve', lambda e, a=m8, d=sd: e.max(out=a[:, 8:16], in_=d[:]), [sdk], [mk])
                        ts('dve', sbi[:, 64 * g:64 * g + JB], sc[:, g, :], m8[:, 15:16], ALU.is_ge, [sck, mk], [sbk], s2=-NEGM, op1=ALU.mult)
                        ts('dve', sbi[:, 64 * g:64 * g + JB], sbi[:, 64 * g:64 * g + JB], NEGM, ALU.add, [sbk], [sbk])
                    ps, pk = psr.next()
                    tr(ps[:, 0:128], sbi[:], ident[:], [sbk, 'ident'], [pk])
                    so, sok = sto.next()
                    act(so[:], ps[:, 0:128], AF.Copy, [pk], [sok])
                    S.dma(selbT_d[:, tsi * 128:(tsi + 1) * 128], so[:], [sok], [('selbT', tsi)], q='pool')
                S.barrier()

        def phase_C(l):
            for br in (1, 2):
                with ExitStack() as ph:
                    def psb(name, shape):
                        return ph.enter_context(nc.sbuf_tensor("%s_C%d_%d" % (name, l, br), list(shape), F32))
                    Wt = WGEN if br == 1 else WWIN
                    tab_d = bgen_d if br == 1 else bwin_d
                    KT_d = kslcT_d if br == 1 else kwinT_d
                    V_d = vslc_d if br == 1 else vwin_d
                    tab = psb("tab", (128, NH, Wt))
                    for h in range(NH):
                        S.dma(tab[:, h, :], tab_d[:, h, :], (), ['tab'])
                    LS = [psb("LS%d" % g, (128, SEQ)) for g in range(2)]
                    for g in range(2):
                        if br == 1:
                            S.dma(LS[g][0:64, :], KT_d[64 * g:64 * g + 64, :], (), [('LS', g)])
                            v = LS[g][64:128, :]
                            memset('pool', v, 1.0, [('LS', g)])
                            asel(v, v, [[1, SEQ]], 0, -64, 0.0, [('LS', g)], [('LS', g)])
                            asel(v, v, [[-1, SEQ]], 63, 64, 0.0, [('LS', g)], [('LS', g)])
                        else:
                            memset('pool', LS[g][64 * (1 - g):64 * (1 - g) + 64, :], 0.0, [('LS', g)])
                            S.dma(LS[g][64 * g:64 * g + 64, :], KT_d[64 * g:64 * g + 64, :], (), [('LS', g)])
                    V = psb("V", (128, NT, 2, 65))
                    memset('pool', V[:, :, :, 64:65], 1.0, ['V'])
                    with nc.allow_non_contiguous_dma(reason="V"):
                        for g in range(2):
                            for t0_ in range(0, NT, 8):
                                S.dma(V[:, t0_:t0_ + 8, g, 0:64], V_d.rearrange("(t p) c -> p t c", p=128)[:, t0_:t0_ + 8, 64 * g:64 * g + 64], (), ['V'])
                    gate_sb = psb("gate_sb", (128, NT, 24))
                    with nc.allow_non_contiguous_dma(reason="gate"):
                        for t0_ in range(0, NT, 8):
                            S.dma(gate_sb[:, t0_:t0_ + 8, :], gate_d.rearrange("(t p) c -> p t c", p=128)[:, t0_:t0_ + 8, :], (), ['gate_sb'])
                    qr = Ring([psb("qTc%d" % i, (128, SEQ)) for i in range(2)], "qTc")
                    ptr = Ring([psb("pt%d" % i, (128, 512)) for i in range(5)], "pt")
                    osr = Ring([psb("os%d" % i, (128, 64)) for i in range(4)], "os")
                    rdr = Ring([psb("rd%d" % i, (128, 2)) for i in range(4)], "rd")
                    b31 = psb("b31", (128, NH))
                    with nc.allow_non_contiguous_dma(reason="b31"):
                        S.dma(b31[:], fdg_in[:, NEGPAD + OFFMAX - 1].partition_broadcast(128), (), ['b31'])
                    psr4 = Ring(PS[0:4], "ps")
                    LAG = 3
                    for c in range(4):
                        if br == 2:
                            qT, qk = qr.next()
                            S.dma(qT[:], qT_d[c, :, :], (), [qk])
                        for half in range(2):
                            h = c + 4 * half
                            g = half
                            if br == 1:
                                qT, qk = qr.next()
                                S.dma(qT[0:64, :], qT_d[c, 64 * half:64 * half + 64, :], (), [qk])
                                S.dma(qT[64:128, :], selbT_d[64 * g:64 * g + 64, :], (), [qk])
                            pend = []

                            def stage3(item):
                                jq_, tk0_, pt_, ptk_, tks_, last_, first_ = item
                                tq0_ = 512 * jq_
                                for sub in range(4):
                                    if tk0_ > last_[sub] or tk0_ < first_[sub]:
                                        continue
                                    mm(PS[4 + sub][:, 0:65], pt_[:, sub * 128:(sub + 1) * 128], V[:, tk0_ // 128, g, :],
                                       tk0_ == first_[sub], tk0_ == last_[sub], [ptk_, 'V'], [('ps', 4 + sub)], inc=(tk0_ == last_[sub]))
                                if tk0_ == tks_[-1]:
                                    for sub in range(4):
                                        tsi = 4 * jq_ + sub
                                        po = PS[4 + sub]
                                        pok = ('ps', 4 + sub)
                                        rd, rk = rdr.next()
                                        ts('dve', rd[:, 0:1], po[:, 64:65], 1e-30, ALU.add, [pok], [rk])
                                        recip(rd[:, 1:2], rd[:, 0:1], [rk], [rk])
                                        o, ok = osr.next()
                                        ts('dve', o[:], po[:, 0:64], rd[:, 1:2], ALU.mult, [pok, rk, 'gate_sb'], [ok],
                                           s2=gate_sb[:, tsi, 3 * h + br:3 * h + br + 1], op1=ALU.mult)
                                        S.dma(obr_d[br, tsi * 128:(tsi + 1) * 128, 64 * h:64 * h + 64], o[:], [ok], [('obr', br, h, tsi)], q='pool')

                            for jq in range(QT):
                                tq0 = 512 * jq
                                lo = 0 if br == 1 else max(0, tq0 - 512)
                                tks = list(range(lo, tq0 + 512, 128))
                                last = {sub: max(tk for tk in tks if tk <= tq0 + 128 * sub + 127) for sub in range(4)}
                                if br == 2:
                                    first = {sub: min(tk for tk in tks if tk + 127 >= tq0 + 128 * sub - 511) for sub in range(4)}
                                else:
                                    first = {sub: tks[0] for sub in range(4)}
                                for tk0 in tks:
                                    ps, pk = psr4.next()
                                    subs_ = [sb_ for sb_ in range(4) if first[sb_] <= tk0 <= last[sb_]]
                                    c_lo, c_hi = 128 * subs_[0], 128 * (subs_[-1] + 1)
                                    mm(ps[:, c_lo:c_hi], LS[g][:, tk0:tk0 + 128], qT[:, tq0 + c_lo:tq0 + c_hi], True, True, [('LS', g), qk], [pk])
                                    pt, ptk = ptr.next()
                                    if tq0 - tk0 >= OFFMAX + 128:
                                        act(pt[:, c_lo:c_hi], ps[:, c_lo:c_hi], AF.Exp, [pk, 'b31'], [ptk], bias=b31[:, h:h + 1])
                                    else:
                                        off = min(tq0 - tk0, OFFMAX) + U0
                                        tt('dve', pt[:, c_lo:c_hi], ps[:, c_lo:c_hi], tab[:, h, off + c_lo:off + c_hi], ALU.add, [pk, 'tab'], [ptk])
                                        act(pt[:, c_lo:c_hi], pt[:, c_lo:c_hi], AF.Exp, [ptk], [ptk])
                                    pend.append((jq, tk0, pt, ptk, tks, last, first))
                                    if len(pend) > LAG:
                                        stage3(pend.pop(0))
                            while pend:
                                stage3(pend.pop(0))
                    S.barrier()
        def bc(ap2, n, axis):
            P, A = ap2.shape
            if axis == 2:
                return ap2.unsqueeze(2).to_broadcast([P, A, n])
            return ap2.unsqueeze(1).to_broadcast([P, n, A])

        def phase_D(l):
            with ExitStack() as ph:
                def psb(name, shape):
                    return ph.enter_context(nc.sbuf_tensor("%s_D1%d" % (name, l), list(shape), F32))
                xr = Ring([psb("xin%d" % i, (128, SEQ + 3)) for i in range(2)], "xin")
                ar = Ring([psb("acc%d" % i, (128, SEQ)) for i in range(2)], "acc")
                sq = Ring([psb("sq%d" % i, (128, 512)) for i in range(4)], "sq")
                cw = psb("cw", (128, 4, 12))
                with nc.allow_non_contiguous_dma(reason="conv w"):
                    for i in range(4):
                        S.dma(cw[:, i, :], dn_conv_w[l, i].rearrange("(c p) -> p c", p=128), (), ['cw'])
                for ch in range(12):
                    xin, xk = xr.next()
                    acc, ak = ar.next()
                    memset('pool', xin[:, 0:3], 0.0, [xk])
                    S.dma(xin[:, 3:SEQ + 3], dnraw_d[ch, :, :], (), [xk])
                    ts('dve', acc[:], xin[:, 0:SEQ], cw[:, 0, ch:ch + 1], ALU.mult, [xk, 'cw'], [ak])
                    for i in range(1, 4):
                        stt(acc[:], xin[:, i:SEQ + i], cw[:, i, ch:ch + 1], acc[:], ALU.mult, ALU.add, [xk, 'cw', ak], [ak])
                    act(acc[:], acc[:], AF.Silu, [ak], [ak])
                    if ch < 8:
                        GRP = 4
                        for j0 in range(0, QT, GRP):
                            js = list(range(j0, min(QT, j0 + GRP)))
                            tiles = []
                            for j in js:
                                cs = slice(512 * j, 512 * j + 512)
                                q1, qk1 = sq.next()
                                act(q1[:], acc[:, cs], AF.Square, [ak], [qk1])
                                tiles.append((cs, q1, qk1))
                            pss = []
                            for (cs, q1, qk1) in tiles:
                                p2, pk2 = psr.next()
                                mm(p2[:], bdones[:], q1[:], True, True, ['bdones', qk1], [pk2])
                                pss.append((p2, pk2))
                            for (cs, q1, qk1), (p2, pk2) in zip(tiles, pss):
                                act(q1[:], p2[:], AF.Sqrt, [pk2], [qk1], bias=EPS, scale=1.0)
                            for (cs, q1, qk1) in tiles:
                                recip(q1[:], q1[:], [qk1], [qk1])
                            for (cs, q1, qk1) in tiles:
                                if ch < 4:
                                    stt(acc[:, cs], acc[:, cs], HD ** -0.5, q1[:], ALU.mult, ALU.mult, [ak, qk1], [ak])
                                else:
                                    tt('dve', acc[:, cs], acc[:, cs], q1[:], ALU.mult, [ak, qk1], [ak])
                    S.dma(dnc_d[ch, :, :], acc[:], [ak], [('dnc', ch)], q='pool')
                S.barrier()
            if 'stopD1' in dbg:
                return
            with ExitStack() as ph:
                def psb(name, shape):
                    return ph.enter_context(nc.sbuf_tensor("%s_D2%d" % (name, l), list(shape), F32))
                NG = NCH * 8
                g_tm = psb("g_tm", (64, NCH, 8))
                b_tm = psb("b_tm", (64, NCH, 8))
                gc_tm = psb("gc_tm", (64, NCH, 8))
                eg_tm = psb("eg_tm", (64, NCH, 8))
                bg_tm = psb("bg_tm", (64, NCH, 8))
                kds_tm = psb("kds_tm", (64, NCH, 8))
                egl = psb("egl", (64, NCH, 8))
                sel63 = psb("sel63", (64, 64))
                ngrow = psb("ngrow", (64, 64))
                with nc.allow_non_contiguous_dma(reason="bg"):
                    for n0_ in range(0, NCH, 16):
                        S.dma(b_tm[:, n0_:n0_ + 16, :], bg_d.rearrange("(n i) c -> i n c", i=64)[:, n0_:n0_ + 16, 0:8], (), ['b_tm'])
                        S.dma(g_tm[:, n0_:n0_ + 16, :], bg_d.rearrange("(n i) c -> i n c", i=64)[:, n0_:n0_ + 16, 8:16], (), ['g_tm'])
                S.dma(ngrow[:], dn_norm_g[l].partition_broadcast(64), (), ['ngrow'])
                ts('dve', sel63[:], ones[0:64, 0:64], ident[0:64, 63:64], ALU.mult, ['ones', 'ident'], ['sel63'])
                gflat = g_tm[:].rearrange("p n h -> p (n h)")
                gcflat = gc_tm[:].rearrange("p n h -> p (n h)")
                for c0 in range(0, NG, 512):
                    cw_ = min(512, NG - c0)
                    ps, pk = psr.next()
                    mm(ps[0:64, 0:cw_], UT[0:64, :], gflat[:, c0:c0 + cw_], True, True, ['UT', 'g_tm'], [pk])
                    cp('dve', gcflat[:, c0:c0 + cw_], ps[0:64, 0:cw_], [pk], ['gc_tm'])
                    ps2, pk2 = psr.next()
                    mm(ps2[0:64, 0:cw_], sel63[:], gcflat[:, c0:c0 + cw_], True, True, ['sel63', 'gc_tm'], [pk2])
                    act(egl[:].rearrange("p n h -> p (n h)")[:, c0:c0 + cw_], ps2[0:64, 0:cw_], AF.Exp, [pk2], ['egl'])
                    tt('dve', kds_tm[:].rearrange("p n h -> p (n h)")[:, c0:c0 + cw_], ps2[0:64, 0:cw_], gcflat[:, c0:c0 + cw_],
                       ALU.subtract, [pk2, 'gc_tm'], ['kds_tm'])
                act(kds_tm[:], kds_tm[:], AF.Exp, ['kds_tm'], ['kds_tm'])
                act(eg_tm[:], gc_tm[:], AF.Exp, ['gc_tm'], ['eg_tm'])
                tt('dve', bg_tm[:], b_tm[:], eg_tm[:], ALU.mult, ['b_tm', 'eg_tm'], ['bg_tm'])

                def t3(name):
                    return psb(name, (64, 8, 64))
                NPS, NOS = 2, 4
                Pt = [{nm: t3("%s_%d" % (nm, i)) for nm in ['ktm', 'vtm', 'qtm', 'rhsg', 'rhsb', 'DTr', 'decT', 'dec', 't1', 'Xa', 'Xb', 'Ya', 'Yb', 'TT', 'vb', 'kbg']} for i in range(NPS)]
                Ot = [{nm: t3("%s_%d" % (nm, i)) for nm in ['AT', 'kd', 'qdT', 'wT', 'u_sb']} for i in range(NOS)]
                Xs_ = [psb("X%d" % i, (64, 24, 64)) for i in range(NPS)]
                zs_ = [psb("z%d" % i, (64, 512)) for i in range(NOS)]
                vn, Sst, o_sb, osq = t3("vn"), t3("Sst"), t3("o_sb"), t3("osq")
                ssum = psb("ssum", (64, 16))
                ytm = psb("ytm", (64, 512))
                ybo = Ring([psb("ybo%d" % i, (128, 4, 64)) for i in range(2)], "ybo")
                memset('pool', Sst[:], 0.0, ['Sst'])
                I64 = ident[0:64, 0:64]

                def hmm(pst, pk, lhs, rhs, r, first=True, last=True):
                    for h in range(8):
                        mm(pst[0:64, 64 * h:64 * h + 64], lhs(h), rhs(h), first, last, r, [pk], inc=(h == 7))

                def v3(ps):
                    return ps[0:64, :].rearrange("p (h c) -> p h c", h=8)

                def pre_gen(n, sp_, so_):
                    ktm, vtm, qtm, rhsg, rhsb, DTr, decT, dec, t1, Xa, Xb, Ya, Yb, TT, vb, kbg = [Pt[sp_][k_] for k_ in ['ktm', 'vtm', 'qtm', 'rhsg', 'rhsb', 'DTr', 'decT', 'dec', 't1', 'Xa', 'Xb', 'Ya', 'Yb', 'TT', 'vb', 'kbg']]
                    AT, kd, qdT, wT, u_sb = [Ot[so_][k_] for k_ in ['AT', 'kd', 'qdT', 'wT', 'u_sb']]
                    X, Xk = Xs_[sp_], ('X', sp_)
                    with nc.allow_non_contiguous_dma(reason="chunk load"):
                        for c0_ in (0, 12):
                            S.dma(X[:, c0_:c0_ + 12, :], dnc_d.rearrange("c (a p) t -> p (c a) t", a=2)[:, c0_:c0_ + 12, 64 * n:64 * n + 64], (), [Xk])
                        yield
                    z, zk = zs_[so_], ('z', so_)
                    S.dma(z[:], zs_d[64 * n:64 * n + 64, :], (), [zk])
                    yield
                    for (dst, dk_, c0) in ((qtm, ('qtm', sp_), 0), (ktm, ('ktm', sp_), 8), (vtm, ('vtm', sp_), 16)):
                        ps, pk = prg[sp_].next()
                        for kc in range(8):
                            tr(ps[0:64, 64 * kc:64 * kc + 64], X[:, c0 + kc, :], I64, [Xk, 'ident'], [pk], inc=(kc == 7))
                            yield
                        if dk_ == ('ktm', sp_):
                            cp('dve', dst[:], v3(ps), [pk], [dk_])
                            yield
                        else:
                            act(dst[:], v3(ps), AF.Copy, [pk], [dk_])
                            yield
                    tt('dve', rhsg[:], bc(g_tm[:, n, :], 64, 2), bc(UT[0:64, :], 8, 1), ALU.mult, ['g_tm', 'UT'], [('rhsg', sp_)])
                    yield
                    tt('dve', rhsb[:], bc(b_tm[:, n, :], 64, 2), bc(I64, 8, 1), ALU.mult, ['b_tm', 'ident'], [('rhsb', sp_)])
                    yield
                    pg, pgk = prg[sp_].next()
                    mm(pg[0:64, :], ones[0:64, 0:64], rhsg[:].rearrange("p h c -> p (h c)"), True, True, ['ones', ('rhsg', sp_)], [pgk])
                    yield
                    pb, pbk = prg[sp_].next()
                    mm(pb[0:64, :], ones[0:64, 0:64], rhsb[:].rearrange("p h c -> p (h c)"), True, True, ['ones', ('rhsb', sp_)], [pbk])
                    yield
                    tt('dve', DTr[:], v3(pg), bc(gc_tm[:, n, :], 64, 2), ALU.subtract, [pgk, 'gc_tm'], [('DTr', sp_)])
                    yield
                    tt('dve', decT[:], DTr[:], bc(maskU[:], 8, 1), ALU.add, [('DTr', sp_), 'maskU'], [('decT', sp_)])
                    yield
                    act(decT[:], decT[:], AF.Exp, [('decT', sp_)], [('decT', sp_)])
                    yield
                    ts('dve', dec[:], DTr[:], -1.0, ALU.mult, [('DTr', sp_)], [('dec', sp_)])
                    yield
                    tt('dve', dec[:], dec[:], bc(maskL[:], 8, 1), ALU.add, [('dec', sp_), 'maskL'], [('dec', sp_)])
                    yield
                    act(dec[:], dec[:], AF.Exp, [('dec', sp_)], [('dec', sp_)])
                    yield
                    pkk, pkkk = prg[sp_].next()
                    hmm(pkk, pkkk, lambda h: X[:, 8 + h, :], lambda h: X[:, 8 + h, :], [Xk])
                    yield
                    pqk, pqkk = prg[sp_].next()
                    hmm(pqk, pqkk, lambda h: X[:, 8 + h, :], lambda h: X[:, h, :], [Xk])
                    yield
                    tt('dve', AT[:], v3(pqk), decT[:], ALU.mult, [pqkk, ('decT', sp_)], [('AT', so_)])
                    yield
                    tt('dve', t1[:], v3(pkk), decT[:], ALU.mult, [pkkk, ('decT', sp_)], [('t1', sp_)])
                    yield
                    tt('dve', t1[:], v3(pb), t1[:], ALU.mult, [pbk, ('t1', sp_)], [('t1', sp_)])
                    yield
                    tt('dve', Ya[:], t1[:], bc(nsU[:], 8, 1), ALU.mult, [('t1', sp_), 'nsU'], [('Ya', sp_)])
                    yield
                    tt('dve', t1[:], v3(pkk), dec[:], ALU.mult, [pkkk, ('dec', sp_)], [('t1', sp_)])
                    yield
                    tt('dve', t1[:], t1[:], bc(b_tm[:, n, :], 64, 2), ALU.mult, [('t1', sp_), 'b_tm'], [('t1', sp_)])
                    yield
                    tt('dve', Xa[:], t1[:], bc(nsL[:], 8, 1), ALU.mult, [('t1', sp_), 'nsL'], [('Xa', sp_)])
                    yield
                    tt('dve', TT[:], Ya[:], bc(I64, 8, 1), ALU.add, [('Ya', sp_), 'ident'], [('TT', sp_)])
                    yield
                    Xc, Xn_, Yc, Yn_ = (Xa, ('Xa', sp_)), (Xb, ('Xb', sp_)), (Ya, ('Ya', sp_)), (Yb, ('Yb', sp_))
                    for lvl in range(1, 6):
                        p1, p1k = prg[sp_].next()
                        hmm(p1, p1k, lambda h: Yc[0][:, h, :], lambda h: Xc[0][:, h, :], [Yc[1], Xc[1]])
                        yield
                        if lvl < 5:
                            p2, p2k = prg[sp_].next()
                            hmm(p2, p2k, lambda h: Xc[0][:, h, :], lambda h: Yc[0][:, h, :], [Yc[1], Xc[1]])
                            yield
                        act(Xn_[0][:], v3(p1), AF.Copy, [p1k], [Xn_[1]])
                        yield
                        if lvl < 5:
                            cp('dve', Yn_[0][:], v3(p2), [p2k], [Yn_[1]])
                            yield
                        Xc, Xn_ = Xn_, Xc
                        if lvl < 5:
                            Yc, Yn_ = Yn_, Yc
                        p3, p3k = prg[sp_].next()
                        hmm(p3, p3k, lambda h: Xc[0][:, h, :], lambda h: TT[:, h, :], [Xc[1], ('TT', sp_)])
                        yield
                        tt('dve', TT[:], TT[:], v3(p3), ALU.add, [('TT', sp_), p3k], [('TT', sp_)])
                        yield
                    tt('dve', vb[:], vtm[:], bc(b_tm[:, n, :], 64, 2), ALU.mult, [('vtm', sp_), 'b_tm'], [('vb', sp_)])
                    yield
                    tt('dve', kbg[:], ktm[:], bc(bg_tm[:, n, :], 64, 2), ALU.mult, [('ktm', sp_), 'bg_tm'], [('kbg', sp_)])
                    yield
                    tt('dve', kd[:], ktm[:], bc(kds_tm[:, n, :], 64, 2), ALU.mult, [('ktm', sp_), 'kds_tm'], [('kd', so_)])
                    yield
                    tt('dve', qtm[:], qtm[:], bc(eg_tm[:, n, :], 64, 2), ALU.mult, [('qtm', sp_), 'eg_tm'], [('qtm', sp_)])
                    yield
                    pu, puk = prg[sp_].next()
                    hmm(pu, puk, lambda h: TT[:, h, :], lambda h: vb[:, h, :], [('TT', sp_), ('vb', sp_)])
                    yield
                    act(u_sb[:], v3(pu), AF.Copy, [puk], [('u_sb', so_)])
                    yield
                    pw, pwk = prg[sp_].next()
                    hmm(pw, pwk, lambda h: kbg[:, h, :], lambda h: TT[:, h, :], [('TT', sp_), ('kbg', sp_)])
                    yield
                    act(wT[:], v3(pw), AF.Copy, [pwk], [('wT', so_)])
                    yield
                    pq, pqk2 = prg[sp_].next()
                    for h in range(8):
                        tr(pq[0:64, 64 * h:64 * h + 64], qtm[:, h, :], I64, [('qtm', sp_), 'ident'], [pqk2], inc=(h == 7))
                        yield
                    cp('dve', qdT[:], v3(pq), [pqk2], [('qdT', so_)])
                    yield
                def scan_gen(n, so_):
                    AT, kd, qdT, wT, u_sb = [Ot[so_][k_] for k_ in ['AT', 'kd', 'qdT', 'wT', 'u_sb']]
                    z, zk = zs_[so_], ('z', so_)
                    pv, pvk = psc.next()
                    hmm(pv, pvk, lambda h: wT[:, h, :], lambda h: Sst[:, h, :], [('wT', so_), 'Sst'])
                    yield
                    tt('dve', vn[:], u_sb[:], v3(pv), ALU.subtract, [('u_sb', so_), pvk], ['vn'])
                    yield
                    po, pok = psc.next()
                    for h in range(8):
                        mm(po[0:64, 64 * h:64 * h + 64], qdT[:, h, :], Sst[:, h, :], True, False, [('qdT', so_), 'Sst'], [pok], inc=False)
                        yield
                        mm(po[0:64, 64 * h:64 * h + 64], AT[:, h, :], vn[:, h, :], False, True, [('AT', so_), 'vn'], [pok], inc=(h == 7))
                        yield
                    pS, pSk = psc.next()
                    hmm(pS, pSk, lambda h: kd[:, h, :], lambda h: vn[:, h, :], [('kd', so_), 'vn'])
                    yield
                    tt('dve', Sst[:], Sst[:], bc(egl[:, n, :], 64, 2), ALU.mult, ['Sst', 'egl'], ['Sst'])
                    yield
                    tt('dve', Sst[:], Sst[:], v3(pS), ALU.add, ['Sst', pSk], ['Sst'])
                    yield
                    act(o_sb[:], v3(po), AF.Copy, [pok], ['o_sb'])
                    yield
                    tt('dve', osq[:], o_sb[:], o_sb[:], ALU.mult, ['o_sb'], ['osq'])
                    yield
                    S.op('dve', lambda e: e.tensor_reduce(out=ssum[:, 0:8], in_=osq[:], op=ALU.add, axis=AX.X), ['osq'], ['ssum'])
                    yield
                    act(ssum[:, 8:16], ssum[:, 0:8], AF.Sqrt, ['ssum'], ['ssum'], bias=EPS, scale=1.0 / 64)
                    yield
                    recip(ssum[:, 8:16], ssum[:, 8:16], ['ssum'], ['ssum'])
                    yield
                    tt('dve', o_sb[:], o_sb[:], bc(ssum[:, 8:16], 64, 2), ALU.mult, ['o_sb', 'ssum'], ['o_sb'])
                    yield
                    tt('dve', o_sb[:], o_sb[:], bc(ngrow[:], 8, 1), ALU.mult, ['o_sb', 'ngrow'], ['o_sb'])
                    yield
                    tt('dve', ytm[:], o_sb[:].rearrange("p h c -> p (h c)"), z[:], ALU.mult, ['o_sb', zk], ['ytm'])
                    yield
                    py, pyk = psc.next()
                    for kc in range(4):
                        tr(py[:, 64 * kc:64 * kc + 64], ytm[:, 128 * kc:128 * kc + 128], I64, ['ytm', 'ident'], [pyk], inc=(kc == 3))
                        yield
                    yo, yok = ybo.next()
                    act(yo[:], py[:, 0:256].rearrange("p (a b) -> p a b", a=4), AF.Copy, [pyk], [yok])
                    yield
                    with nc.allow_non_contiguous_dma(reason="yb store"):
                        S.dma(ybT_d.rearrange("c p t -> p c t")[:, :, 64 * n:64 * n + 64], yo[:], [yok], [('ybT', n)], q='pool')
                        yield


                S.barrier()
                prg = [Ring(PS[0:3], "psA"), Ring(PS[3:6], "psB")]
                psc = Ring(PS[6:8], "psC")

                def run_rr(gens):
                    gens = list(gens)
                    while gens:
                        for g_ in list(gens):
                            try:
                                next(g_)
                            except StopIteration:
                                gens.remove(g_)

                def scan_pair(a_, b_):
                    yield from scan_gen(a_, a_ % NOS)
                    yield from scan_gen(b_, b_ % NOS)
                prev_ = None
                for grp in range(NCH // 2):
                    a_, b_ = 2 * grp, 2 * grp + 1
                    gl = [pre_gen(a_, 0, a_ % NOS), pre_gen(b_, 1, b_ % NOS)]
                    if prev_ is not None:
                        gl.append(scan_pair(*prev_))
                    run_rr(gl)
                    prev_ = (a_, b_)
                run_rr([scan_pair(*prev_)])
                S.barrier()

        def phase_E(l, src_x):
            with ExitStack() as ph:
                def psb(name, shape):
                    return ph.enter_context(nc.sbuf_tensor("%s_E%d" % (name, l), list(shape), F32))
                wa = psb("wa", (128, 4, D))
                wb = psb("wb", (128, 4, D))
                wo = psb("wo", (128, 8, D))
                g1r = psb("g1r", (128, D))
                S.dma(g1r[:], mod_d[l, 2 * D:3 * D].partition_broadcast(128), (), ['g1r'])
                S.dma(wa[:], w_br_a[l].rearrange("(kc p) n -> p kc n", p=128), (), ['wa'])
                S.dma(wb[:], w_br_b[l].rearrange("(kc p) n -> p kc n", p=128), (), ['wb'])
                S.dma(wo[:], w_out[l].rearrange("(kc p) n -> p kc n", p=128), (), ['wo'])
                obr = Ring([psb("ob%d" % i, (128, 3, 512)) for i in range(2)], "ob")
                yaT = psb("yaT", (128, 4, 512))
                ybT = psb("ybT", (128, 4, 512))
                mg = Ring([psb("mg%d" % i, (128, 2, 512)) for i in range(2)], "mg")
                yT = psb("yT", (128, 8, 512))
                tmp = Ring([psb("tmp%d" % i, (128, 512)) for i in range(2)], "tmp")
                xr = Ring([psb("xe%d" % i, (128, D)) for i in range(2)], "xe")
                for j in range(QT):
                    t0 = 512 * j
                    cs = slice(t0, t0 + 512)
                    for sub in range(4):
                        r0 = t0 + 128 * sub
                        ob, obk = obr.next()
                        S.dma(ob[:], obr_d[:, r0:r0 + 128, :].rearrange("b p c -> p b c"), (), [obk])
                        tt('dve', ob[:, 0, :], ob[:, 0, :], ob[:, 1, :], ALU.add, [obk], [obk])
                        tt('pool', ob[:, 0, :], ob[:, 0, :], ob[:, 2, :], ALU.add, [obk], [obk])
                        transpose_to(ob[:, 0, :], obk, yaT, 'yaT', sub, nkc=4)
                    S.dma(ybT[:], ybT_d.rearrange("c p t -> p c t")[:, :, cs], (), ['ybT'])
                    for dc in range(8):
                        m, mk = mg.next()
                        S.dma(m[:, 0, :], mergeT_d[dc, :, cs], (), [mk])
                        S.dma(m[:, 1, :], mergeT_d[8 + dc, :, cs], (), [mk])
                        pa, pak = psr.next()
                        for kc in range(4):
                            mm(pa[:], wa[:, kc, 128 * dc:128 * dc + 128], yaT[:, kc, :], kc == 0, kc == 3, ['wa', 'yaT'], [pak], inc=(kc == 3))
                        pb, pbk = psr.next()
                        for kc in range(4):
                            mm(pb[:], wb[:, kc, 128 * dc:128 * dc + 128], ybT[:, kc, :], kc == 0, kc == 3, ['wb', 'ybT'], [pbk], inc=(kc == 3))
                        t, tk = tmp.next()
                        tt('dve', t[:], pa[:], m[:, 0, :], ALU.mult, [pak, mk], [tk])
                        tt('dve', m[:, 1, :], pb[:], m[:, 1, :], ALU.mult, [pbk, mk], [mk])
                        tt('pool', yT[:, dc, :], t[:], m[:, 1, :], ALU.add, [tk, mk], ['yT'])
                    for sub in range(4):
                        r0 = t0 + 128 * sub
                        xt, xk = xr.next()
                        S.dma(xt[:], src_x[r0:r0 + 128, :], [('x', r0 // 128)], [xk])
                        for nh in range(2):
                            po, pok = psr.next()
                            for kc in range(8):
                                mm(po[:], yT[:, kc, 128 * sub:128 * sub + 128], wo[:, kc, 512 * nh:512 * nh + 512], kc == 0, kc == 7,
                                   ['yT', 'wo'], [pok], inc=(kc == 7))
                            t, tk = tmp.next()
                            tt('dve', t[:], po[:], g1r[:, 512 * nh:512 * nh + 512], ALU.mult, [pok, 'g1r'], [tk])
                            tt('pool', xt[:, 512 * nh:512 * nh + 512], xt[:, 512 * nh:512 * nh + 512], t[:], ALU.add, [xk, tk], [xk])
                        S.dma(out[r0:r0 + 128, :], xt[:], [xk], [('x', r0 // 128)], q='pool')
                S.barrier()

        def phase_F(l):
            NE = 16
            with ExitStack() as ph:
                def psb(name, shape, dt=F32):
                    return ph.enter_context(nc.sbuf_tensor("%s_F%d" % (name, l), list(shape), dt))
                A2r = psb("A2r", (128, D))
                sh2r = psb("sh2r", (128, D))
                g2r = psb("g2r", (128, D))
                S.dma(sh2r[:], mod_d[l, 3 * D:4 * D].partition_broadcast(128), (), ['modrow'])
                S.dma(A2r[:], mod_d[l, 4 * D:5 * D].partition_broadcast(128), (), ['modrow'])
                S.dma(g2r[:], mod_d[l, 5 * D:6 * D].partition_broadcast(128), (), ['g2r'])
                xr = Ring([psb("xf%d" % i, (128, D)) for i in range(2)], "xf")
                hr = Ring([psb("hf%d" % i, (128, D)) for i in range(2)], "hf")
                sm = Ring([psb("smf%d" % i, (128, 8)) for i in range(4)], "smf")
                hTr = Ring([psb("hTf%d" % i, (128, 8, 256)) for i in range(2)], "hTf")
                rt = psb("rt", (128, 8, 16))
                rs = psb("rs", (128, 16))
                M1a = psb("M1a", (128, NT, NE))
                M2a = psb("M2a", (128, NT, NE))
                wn = psb("wn", (128, NT, 2))
                SU = psb("SU", (128, 128))
                memset('pool', SU[:], 1.0, ['SU'])
                asel(SU[:], SU[:], [[1, 128]], -1, -1, 0.0, ['SU'], ['SU'])
                rtr = [rt, psb("rt2", (128, 8, 16))]
                rsr = [rs, psb("rs2", (128, 16))]

                def p1_stage1(ti):
                    rt = rtr[ti % 2]
                    RT = ('rt', ti % 2)
                    hT, hTk = hTr.next()
                    ht, hk, xt, xk = norm_tile(out, 128 * ti, A2r[:], sh2r[:], xr, hr, sm)
                    S.dma(h2_d[128 * ti:128 * ti + 128, :], ht[:], [hk], [('h2', ti)], q='pool')
                    transpose_to(ht, hk, hT, hTk, 0)
                    pr, prk = psr.next()
                    for kc in range(8):
                        mm(pr[:, 0:16], hT[:, kc, 0:128], rtrw[:, kc, :], kc == 0, kc == 7, [hTk, 'rtrw'], [prk], inc=(kc == 7))
                    sc = rt[:, 0, :]
                    sel = rt[:, 1, :]
                    w1_ = rt[:, 2, :]
                    w2_ = rt[:, 3, :]
                    act(sc, pr[:, 0:16], AF.Sigmoid, [prk], [RT])
                    return ti

                def p1_stage2(ti):
                    rt = rtr[ti % 2]
                    rs = rsr[ti % 2]
                    RT = ('rt', ti % 2)
                    RS = ('rs', ti % 2)
                    sc = rt[:, 0, :]
                    sel = rt[:, 1, :]
                    w1_ = rt[:, 2, :]
                    w2_ = rt[:, 3, :]
                    tt('dve', sel, sc, rtrb[:], ALU.add, [RT, 'rtrb'], [RT])
                    sel3 = sel.rearrange("p (g e) -> p g e", g=4)
                    S.op('dve', lambda e, s3=sel3: e.tensor_reduce(out=rs[:, 0:4], in_=s3, op=ALU.max, axis=AX.X), [RT], [RS])
                    tt('dve', w1_.rearrange("p (g e) -> p g e", g=4), sel3, bc(rs[:, 0:4], 4, 2), ALU.is_ge, [RT, RS], [RT])
                    stt(w2_, w1_, -1e9, sel, ALU.mult, ALU.add, [RT], [RT])
                    S.op('dve', lambda e, a=w2_: e.tensor_reduce(out=rs[:, 4:8], in_=a.rearrange("p (g e) -> p g e", g=4), op=ALU.max, axis=AX.X),
                         [RT], [RS])
                    tt('dve', rs[:, 8:12], rs[:, 0:4], rs[:, 4:8], ALU.add, [RS], [RS])
                    S.op('dve', lambda e: e.tensor_reduce(out=rs[:, 12:13], in_=rs[:, 8:12], op=ALU.max, axis=AX.X), [RS], [RS])
                    ts('dve', rs[:, 4:8], rs[:, 8:12], rs[:, 12:13], ALU.is_ge, [RS], [RS], s2=-1.0, op1=ALU.add)
                    ts('dve', rs[:, 4:8], rs[:, 4:8], 1e9, ALU.mult, [RS], [RS])
                    tt('dve', w1_.rearrange("p (g e) -> p g e", g=4), sel3, bc(rs[:, 4:8], 4, 2), ALU.add, [RT, RS], [RT])
                    S.op('dve', lambda e, a=w1_: e.max(out=rt[:, 4, 0:8], in_=a), [RT], [RT])
                    ts('dve', M1a[:, ti, :], w1_, rt[:, 4, 0:1], ALU.is_ge, [RT], ['M1a'])
                    ts('dve', w2_, w1_, rt[:, 4, 1:2], ALU.is_ge, [RT], [RT])
                    tt('dve', M2a[:, ti, :], w2_, M1a[:, ti, :], ALU.subtract, [RT, 'M1a'], ['M2a'])
                    tt('dve', w2_, M1a[:, ti, :], sc, ALU.mult, [RT, 'M1a'], [RT])
                    S.op('dve', lambda e, a=w2_: e.tensor_reduce(out=rs[:, 13:14], in_=a, op=ALU.add, axis=AX.X), [RT], [RS])
                    tt('dve', w2_, M2a[:, ti, :], sc, ALU.mult, [RT, 'M2a'], [RT])
                    S.op('dve', lambda e, a=w2_: e.tensor_reduce(out=rs[:, 14:15], in_=a, op=ALU.add, axis=AX.X), [RT], [RS])
                    tt('dve', rs[:, 15:16], rs[:, 13:14], rs[:, 14:15], ALU.add, [RS], [RS])
                    recip(rs[:, 15:16], rs[:, 15:16], [RS], [RS])
                    ts('dve', wn[:, ti, :], rs[:, 13:15], rs[:, 15:16], ALU.mult, [RS], ['wn'])

                prev_t = None
                for ti in range(NT):
                    p1_stage1(ti)
                    if prev_t is not None:
                        p1_stage2(prev_t)
                    prev_t = ti
                p1_stage2(prev_t)

                NG = NT * NE
                Mall = psb("Mall", (128, NT, NE))
                cnt = psb("cnt", (128, NT, NE))
                base = psb("base", (128, NT, NE))
                dest = psb("dest", (128, NT, NE))
                dtmp = psb("dtmp", (128, NT, NE))
                d12 = psb("d12", (128, 2, NT))
                idx12 = psb("idx12", (128, 2, NT), I32)
                ev = psb("ev", (128, 8, NE))
                ebf = psb("ebf", (128, NBK))
                bidx_i = psb("bidx_i", (128, NBK), I32)
                bidx = psb("bidx", (128, NBK))
                pcol_i = psb("pcol_i", (128, 1), I32)
                pcol = psb("pcol", (128, 1))
                widx = psb("widx", (128, NBK), I32)
                tt('dve', Mall[:], M1a[:], M2a[:], ALU.add, ['M1a', 'M2a'], ['Mall'])
                Mf = Mall[:].rearrange("p t e -> p (t e)")
                prk_, prkk = psr.next()
                pcn, pcnk = psr.next()
                for c0 in range(0, NG, 512):
                    cw_ = min(512, NG - c0)
                    assert NG <= 512
                    mm(prk_[:, 0:cw_], SU[:], Mf[:, c0:c0 + cw_], True, True, ['SU', 'Mall'], [prkk])
                    mm(pcn[:, 0:cw_], ones[:], Mf[:, c0:c0 + cw_], True, True, ['ones', 'Mall'], [pcnk])
                cp('dve', cnt[:].rearrange("p t e -> p (t e)"), pcn[:, 0:NG], [pcnk], ['cnt'])
                memset('dve', base[:, 0, :], 0.0, ['base'])
                for ti in range(1, NT):
                    tt('dve', base[:, ti, :], base[:, ti - 1, :], cnt[:, ti - 1, :], ALU.add, ['base', 'cnt'], ['base'])
                tt('dve', ev[:, 0, :], base[:, NT - 1, :], cnt[:, NT - 1, :], ALU.add, ['base', 'cnt'], ['ev'])
                memset('dve', ev[:, 1, :], 0.0, ['ev'])
                for m in range(SEQ // MBLK):
                    stt(ev[:, 1, :], ev[:, 0, :], float(MBLK * m), ev[:, 1, :], ALU.is_gt, ALU.add, ['ev'], ['ev'])
                ts('dve', ev[:, 2, :], ev[:, 1, :], float(MBLK), ALU.mult, ['ev'], ['ev'])
                cp('dve', ev[:, 3, 0:1], ev[:, 2, 0:1], ['ev'], ['ev'])
                for e_ in range(1, NE):
                    tt('dve', ev[:, 3, e_:e_ + 1], ev[:, 3, e_ - 1:e_], ev[:, 2, e_:e_ + 1], ALU.add, ['ev'], ['ev'])
                tt('dve', ev[:, 4, :], ev[:, 3, :], ev[:, 2, :], ALU.subtract, ['ev'], ['ev'])
                tt('dve', dest[:].rearrange("p t e -> p (t e)"), prk_[:, 0:NG], base[:].rearrange("p t e -> p (t e)"), ALU.add, [prkk, 'base'], ['dest'])
                tt('dve', dest[:], dest[:], bc(ev[:, 4, :], NT, 1), ALU.add, ['dest', 'ev'], ['dest'])
                for k_, Mk in ((0, M1a), (1, M2a)):
                    tt('dve', dtmp[:], dest[:], Mk[:], ALU.mult, ['dest', 'M1a', 'M2a'], ['dtmp'])
                    S.op('dve', lambda e, k_=k_: e.tensor_reduce(out=d12[:, k_, :], in_=dtmp[:], op=ALU.add, axis=AX.X), ['dtmp'], ['d12'])
                cp('dve', idx12[:], d12[:], ['d12'], ['idx12'])
                S.op('pool', lambda e: e.iota(bidx_i[:], pattern=[[MBLK, NBK]], base=0, channel_multiplier=0), (), ['bidx_i'])
                S.op('pool', lambda e: e.iota(pcol_i[:], pattern=[[0, 1]], base=0, channel_multiplier=1), (), ['pcol_i'])
                cp('dve', bidx[:], bidx_i[:], ['bidx_i'], ['bidx'])
                cp('dve', pcol[:], pcol_i[:], ['pcol_i'], ['pcol'])
                memset('dve', ebf[:], 0.0, ['ebf'])
                for e_ in range(NE):
                    stt(ebf[:], bidx[:], ev[:, 3, e_:e_ + 1], ebf[:], ALU.is_ge, ALU.add, ['bidx', 'ev', 'ebf'], ['ebf'])
                ts('dve', ebf[:], ebf[:], float(NE - 1), ALU.min, ['ebf'], ['ebf'], s2=128.0, op1=ALU.mult)
                ts('dve', ebf[:], ebf[:], pcol[:, 0:1], ALU.add, ['ebf', 'pcol'], ['ebf'], s2=float(l * 16 * 128), op1=ALU.add)
                cp('dve', widx[:], ebf[:], ['ebf'], ['widx'])
                for ti in range(NT):
                    ht, hk = hr.next()
                    S.dma(ht[:], h2_d[128 * ti:128 * ti + 128, :], [('h2', ti)], [hk], q='sp')
                    for k_ in range(2):
                        S.idma(Xs_d[:, :], ht[:, :], bass.IndirectOffsetOnAxis(ap=idx12[:, k_, ti:ti + 1], axis=0), None,
                               [hk, 'idx12'], [('Xs', ti, k_)])
                S.barrier()
                with ExitStack() as ph3:
                    def psb3(name, shape, dt=F32):
                        return ph3.enter_context(nc.sbuf_tensor("%s_F3%d" % (name, l), list(shape), dt))
                    wgr = Ring([psb3("wg%d" % i, (128, 8 * 512)) for i in range(2)], "wg")
                    wur = Ring([psb3("wu%d" % i, (128, 8 * 512)) for i in range(2)], "wu")
                    wdr = Ring([psb3("wd%d" % i, (128, 4 * D)) for i in range(2)], "wd")
                    xgr = Ring([psb3("xg%d" % i, (128, D)) for i in range(4)], "xg")
                    actT = psb3("actT", (128, 4, 256))
                    sg = Ring([psb3("sg%d" % i, (128, 256)) for i in range(2)], "sg")
                    yor = Ring([psb3("yo%d" % i, (128, D)) for i in range(2)], "yo")
                    wg_v = moe_wg.rearrange("l e (p kc) n -> (l e p) (kc n)", kc=8)
                    wu_v = moe_wu.rearrange("l e (p kc) n -> (l e p) (kc n)", kc=8)
                    wd_v = moe_wd.rearrange("l e (p fc) n -> (l e p) (fc n)", fc=4)
                    deferred = []
                    for b_ in range(NBK):
                        off = bass.IndirectOffsetOnAxis(ap=widx[:, b_:b_ + 1], axis=0)
                        wg, wgk = wgr.next()
                        wu, wuk = wur.next()
                        wd, wdk = wdr.next()
                        S.idma(wg[:, :], wg_v, None, off, ['widx'], [wgk])
                        S.idma(wu[:, :], wu_v, None, off, ['widx'], [wuk])
                        S.idma(wd[:, :], wd_v, None, off, ['widx'], [wdk])
                        hT, hTk = hTr.next()
                        xgs = []
                        for sub in range(2):
                            xg, xgk = xgr.next()
                            r0 = b_ * MBLK + 128 * sub
                            S.dma(xg[:], Xs_d[r0:r0 + 128, :], (), [xgk], q='sp')
                            xgs.append((xg, xgk))
                        for d_ in deferred:
                            S.dma(*d_[0], **d_[1])
                        deferred = []
                        for sub in range(2):
                            xg, xgk = xgs[sub]
                            for k0 in (0, 4):
                                ps, pk = psr.next()
                                for kc in range(k0, k0 + 4):
                                    tr(ps[:, (kc - k0) * 128:(kc - k0 + 1) * 128], xg[:, kc:D:8], ident[:], [xgk, 'ident'], [pk], inc=(kc == k0 + 3))
                                dst = hT[:, k0:k0 + 4, sub * 128:(sub + 1) * 128]
                                srcv = ps[:].rearrange("p (a b) -> p a b", a=4)
                                if k0 == 0:
                                    act(dst, srcv, AF.Copy, [pk], [hTk])
                                else:
                                    cp('dve', dst, srcv, [pk], [hTk])
                        for fc in range(4):
                            pg_, pgk_ = psr.next()
                            for kc in range(8):
                                mm(pg_[:, 0:256], wg[:, kc * 512 + fc:(kc + 1) * 512:4], hT[:, kc, :], kc == 0, kc == 7, [wgk, hTk], [pgk_], inc=(kc == 7))
                            pu_, puk_ = psr.next()
                            for kc in range(8):
                                mm(pu_[:, 0:256], wu[:, kc * 512 + fc:(kc + 1) * 512:4], hT[:, kc, :], kc == 0, kc == 7, [wuk, hTk], [puk_], inc=(kc == 7))
                            s_, sk_ = sg.next()
                            act(s_[:], pg_[:, 0:256], AF.Silu, [pgk_], [sk_])
                            tt('dve', actT[:, fc, :], pu_[:, 0:256], s_[:], ALU.mult, [puk_, sk_], [('actT', fc)])
                        for sub in range(2):
                            yo, yok = yor.next()
                            for nh in range(2):
                                pd, pdk = psr.next()
                                for fc in range(4):
                                    mm(pd[:], actT[:, fc, 128 * sub:128 * sub + 128], wd[:, fc * D + 512 * nh:fc * D + 512 * nh + 512], fc == 0, fc == 3,
                                       [('actT', fc), wdk], [pdk], inc=(fc == 3))
                                if nh == 0:
                                    act(yo[:, 0:512], pd[:], AF.Copy, [pdk], [yok])
                                else:
                                    cp('dve', yo[:, 512:1024], pd[:], [pdk], [yok])
                            r0 = b_ * MBLK + 128 * sub
                            deferred.append(((Ys_d[r0:r0 + 128, :], yo[:], [yok], [('Ys', b_, sub)]), dict(q='sp')))
                    for d_ in deferred:
                        S.dma(*d_[0], **d_[1])
                    S.barrier()
                y1r = Ring([psb("y1_%d" % i, (128, D)) for i in range(2)], "y1")
                y2r = Ring([psb("y2_%d" % i, (128, D)) for i in range(2)], "y2")
                for ti in range(NT):
                    y1, y1k = y1r.next()
                    y2, y2k = y2r.next()
                    S.idma(y1[:, :], Ys_d[:, :], None, bass.IndirectOffsetOnAxis(ap=idx12[:, 0, ti:ti + 1], axis=0), ['idx12'], [y1k])
                    S.idma(y2[:, :], Ys_d[:, :], None, bass.IndirectOffsetOnAxis(ap=idx12[:, 1, ti:ti + 1], axis=0), ['idx12'], [y2k])
                    xt, xk = xr.next()
                    S.dma(xt[:], out[128 * ti:128 * ti + 128, :], [('x', ti)], [xk], q='sp')
                    ts('dve', y1[:], y1[:], wn[:, ti, 0:1], ALU.mult, [y1k, 'wn'], [y1k])
                    stt(y1[:], y2[:], wn[:, ti, 1:2], y1[:], ALU.mult, ALU.add, [y2k, 'wn', y1k], [y1k])
                    tt('pool', y1[:], y1[:], g2r[:], ALU.mult, [y1k, 'g2r'], [y1k])
                    tt('pool', xt[:], xt[:], y1[:], ALU.add, [xk, y1k], [xk])
                    S.dma(out[128 * ti:128 * ti + 128, :], xt[:], [xk], [('x', ti)], q='sp')
                S.barrier()

        with ExitStack() as ph:
            modrow = ph.enter_context(nc.sbuf_tensor("modrow", [128, 6 * D], F32))
            rowtmp = ph.enter_context(nc.sbuf_tensor("rowtmp", [128, D], F32))
            wr0 = Ring([ph.enter_context(nc.sbuf_tensor("wsl0_%d" % i, [128, 8, 512], F32)) for i in range(2)], "wsl0")
            for l in range(L):
                S.dma(modrow[:], ada_b[l].partition_broadcast(128), ['modrow'], ['modrow'])
                for oc in range(12):
                    wt, wk = load_w(wr0, ada_w[l], oc * 512, 512)
                    ps, pk = psr.next()
                    for kc in range(8):
                        mm(ps[:], cbc[:, kc, :], wt[:, kc, :], kc == 0, kc == 7, ['cbc', wk], [pk], inc=(kc == 7))
                    tt('dve', modrow[:, oc * 512:(oc + 1) * 512], ps[:], modrow[:, oc * 512:(oc + 1) * 512], ALU.add,
                       [pk, 'modrow'], ['modrow'])
                for (o_, ng) in ((1, norm1_g), (4, norm2_g)):
                    S.dma(rowtmp[:], ng[l].partition_broadcast(128), ['rowtmp'], ['rowtmp'])
                    stt(modrow[:, o_ * D:(o_ + 1) * D], modrow[:, o_ * D:(o_ + 1) * D], 1.0, rowtmp[:], ALU.add, ALU.mult,
                        ['modrow', 'rowtmp'], ['modrow'])
                S.dma(mod_d[l:l + 1, :], modrow[0:1, :], ['modrow'], [('mod', l)], q='pool')
            S.barrier()

        for l in range(L if 'stop0' not in dbg else 0):
            src_x = x_in if l == 0 else out
            with ExitStack() as phm:
                A1t = phm.enter_context(nc.sbuf_tensor("A1t_%d" % l, [128, D], F32))
                sh1t = phm.enter_context(nc.sbuf_tensor("sh1t_%d" % l, [128, D], F32))
                S.dma(sh1t[:], mod_d[l, 0:D].partition_broadcast(128), (), ['modrow'])
                S.dma(A1t[:], mod_d[l, D:2 * D].partition_broadcast(128), (), ['modrow'])
                A1row, sh1row = A1t[:], sh1t[:]
                phase_A(l, src_x, A1row, sh1row)
            if 'stopA' in dbg:
                break
            phase_B(l)
            if 'stopB' in dbg:
                break
            phase_C(l)
            if 'stopC' in dbg:
                break
            phase_D(l)
            if 'stopD' in dbg:
                break
            phase_E(l, src_x)
            if 'stopE' in dbg:
                break
            phase_F(l)
        S.barrier()
    return nc


def _host_tables(rel_bias, SEQ):
    FDW = NEGPAD + SEQ
    d = np.arange(SEQ)
    bk = rel_bucket_np(d)
    g = np.asarray(rel_bias, np.float32)[bk].T
    fdg = np.full((NH, FDW), NEGM, np.float32)
    fdg[:, NEGPAD:] = g
    fdw = np.full((NH, FDW), NEGM, np.float32)
    fdw[:, NEGPAD:NEGPAD + 512] = g[:, :512]
    return fdg, fdw


_CACHE = {}


def kernel(**inputs):
    x = np.asarray(inputs["x"], np.float32)
    B, SEQ, _ = x.shape
    L = int(np.asarray(inputs["ada_w"]).shape[0])
    key = (SEQ, L)
    if key not in _CACHE:
        _CACHE[key] = build_program(SEQ, L)
    nc = _CACHE[key]
    fdg, fdw = _host_tables(inputs["rel_bias"], SEQ)
    names = ["router_w", "router_b", "ada_w", "ada_b", "norm1_g", "norm2_g", "w_in", "qk_norm_g", "cmp_pos", "cmp_w1",
             "cmp_w2", "dn_conv_w", "dn_a_log", "dn_dt_bias", "dn_norm_g", "w_branch_a", "w_branch_b", "w_out",
             "moe_w_gate", "moe_w_up", "moe_w_down"]
    shared = {n: np.ascontiguousarray(np.asarray(inputs[n], np.float32)) for n in names}
    shared["fdg"] = fdg
    shared["fdw"] = fdw
    c = np.asarray(inputs["c"], np.float32)
    in_maps = []
    for b in range(B):
        m = dict(shared)
        m["x"] = np.ascontiguousarray(x[b])
        m["cT"] = np.ascontiguousarray(c[b].reshape(8, 128).T)
        in_maps.append(m)
    res = run_bass_kernel_spmd(nc, in_maps, core_ids=list(range(B)))
    return np.stack([np.asarray(r["out"], np.float32) for r in res.results], axis=0)
```
